# Optimizing a Trainium2 kernel written in Bass

```python
import jax, jax.numpy as jnp
from jax import lax
import numpy as np

D_MODEL = 2048
BATCH = 4
SEQ = 4096
DEPTH = 1

CHUNK = 64
LEFT_CHUNKS = 8
BAND = (LEFT_CHUNKS + 1) * CHUNK
HEAD_DIM = 128
N_HEADS_A = 8
N_HEADS_B = 8
WIDTH_A = N_HEADS_A * HEAD_DIM
WIDTH_B = N_HEADS_B * HEAD_DIM
REL_CLIP = 256
REL_TABLE = REL_CLIP + CHUNK
Q_BLOCK = 128
N_EXPERTS = 32
TOP_K = 4
D_EXPERT = D_MODEL
SWIGLU_LIMIT = 7.0
SWIGLU_ALPHA = 1.702
MOE_BLOCK = 128
NORM_EPS = 1e-5
NEG_INF = -1e30
IN_COLS = 3 * WIDTH_A + 3 * WIDTH_B + N_HEADS_B + 2 * D_MODEL

kernel_name = 'hybrid_chunked_relpos_fox_moe_block'


def rmsnorm(x, w):
    xf = x.astype(jnp.float32)
    y = xf * lax.rsqrt(jnp.mean(xf * xf, axis=-1, keepdims=True) + NORM_EPS)
    return (y * w.astype(jnp.float32)).astype(x.dtype)


def chunked_relpos_attention(q, k, v, rel_bias):
    B, S, H, dh = q.shape
    nc = S // CHUNK
    pad = LEFT_CHUNKS * CHUNK
    k_pad = jnp.pad(k, ((0, 0), (pad, 0), (0, 0), (0, 0)))
    v_pad = jnp.pad(v, ((0, 0), (pad, 0), (0, 0), (0, 0)))
    qi = jnp.arange(CHUNK)[:, None]
    kj = jnp.arange(BAND)[None, :]
    rel = qi - kj + pad
    rel_idx = jnp.clip(rel, -(CHUNK - 1), REL_CLIP) + (CHUNK - 1)
    bias = rel_bias[:, rel_idx].astype(jnp.float32)
    scale = HEAD_DIM ** -0.5

    def one_chunk(c):
        start = c * CHUNK
        qc = lax.dynamic_slice_in_dim(q, start, CHUNK, axis=1)
        kb = lax.dynamic_slice_in_dim(k_pad, start, BAND, axis=1)
        vb = lax.dynamic_slice_in_dim(v_pad, start, BAND, axis=1)
        s = jnp.einsum('bqhd,bkhd->bhqk', qc, kb, preferred_element_type=jnp.float32) * scale + bias[None]
        valid = (start - pad + jnp.arange(BAND)) >= 0
        s = jnp.where(valid[None, None, None, :], s, NEG_INF)
        p = jax.nn.softmax(s, axis=-1)
        return jnp.einsum('bhqk,bkhd->bqhd', p.astype(vb.dtype), vb)

    out = lax.map(one_chunk, jnp.arange(nc))
    return out.transpose(1, 0, 2, 3, 4).reshape(B, S, H * dh)


def forgetting_attention(q, k, v, forget_logit):
    B, S, H, dh = q.shape
    nb = S // Q_BLOCK
    log_f = jax.nn.log_sigmoid(forget_logit.astype(jnp.float32))
    cum = jnp.cumsum(log_f, axis=1).transpose(0, 2, 1)
    kpos = jnp.arange(S)
    scale = HEAD_DIM ** -0.5

    def one_block(blk):
        start = blk * Q_BLOCK
        qb = lax.dynamic_slice_in_dim(q, start, Q_BLOCK, axis=1)
        cq = lax.dynamic_slice_in_dim(cum, start, Q_BLOCK, axis=2)
        s = jnp.einsum('bqhd,bkhd->bhqk', qb, k, preferred_element_type=jnp.float32) * scale
        s = s + cq[..., :, None] - cum[..., None, :]
        qpos = start + jnp.arange(Q_BLOCK)
        s = jnp.where((kpos[None, :] <= qpos[:, None])[None, None], s, NEG_INF)
        p = jax.nn.softmax(s, axis=-1)
        return jnp.einsum('bhqk,bkhd->bqhd', p.astype(v.dtype), v)

    out = lax.map(one_block, jnp.arange(nb))
    return out.transpose(1, 0, 2, 3, 4).reshape(B, S, H * dh)


def moe_ffn(xn, w_router, b_router, w_gate_up, b_gate_up, w_down, b_down):
    B, S, D = xn.shape
    N = B * S
    A = N * TOP_K
    xf = xn.reshape(N, D)
    logits = (xf @ w_router + b_router).astype(jnp.float32)
    top_val, top_idx = lax.top_k(logits, TOP_K)
    gates = jax.nn.softmax(top_val, axis=-1)
    exp_flat = top_idx.reshape(A).astype(jnp.int32)
    tok_flat = jnp.repeat(jnp.arange(N, dtype=jnp.int32), TOP_K)
    gate_flat = gates.reshape(A)
    order = jnp.argsort(exp_flat)
    exp_sorted = exp_flat[order]
    counts = jnp.bincount(exp_flat, length=N_EXPERTS)
    starts = jnp.cumsum(counts) - counts
    padded = (counts + MOE_BLOCK - 1) // MOE_BLOCK * MOE_BLOCK
    pends = jnp.cumsum(padded)
    pstarts = pends - padded
    dest = pstarts[exp_sorted] + (jnp.arange(A) - starts[exp_sorted])
    P = A + N_EXPERTS * MOE_BLOCK
    nblk = P // MOE_BLOCK
    buf_tok = jnp.full((P,), N, jnp.int32).at[dest].set(tok_flat[order])
    buf_gate = jnp.zeros((P,), jnp.float32).at[dest].set(gate_flat[order])
    blk_expert = jnp.minimum(jnp.searchsorted(pends, jnp.arange(nblk) * MOE_BLOCK, side='right'), N_EXPERTS - 1).astype(jnp.int32)
    x_pad = jnp.concatenate([xf, jnp.zeros((1, D), xf.dtype)], axis=0)

    def one_block(args):
        tok, e = args
        xb = x_pad[tok]
        gu = xb @ w_gate_up[e] + b_gate_up[e]
        g, u = jnp.split(gu, 2, axis=-1)
        g = jnp.minimum(g, SWIGLU_LIMIT)
        u = jnp.clip(u, -SWIGLU_LIMIT, SWIGLU_LIMIT)
        hb = (u + 1.0) * (g * jax.nn.sigmoid(SWIGLU_ALPHA * g))
        return hb @ w_down[e] + b_down[e]

    y = lax.map(one_block, (buf_tok.reshape(nblk, MOE_BLOCK), blk_expert))
    y = y.reshape(P, D) * buf_gate[:, None].astype(y.dtype)
    out = jax.ops.segment_sum(y, buf_tok, num_segments=N + 1)[:N]
    return out.reshape(B, S, D)


def setup_inputs(seed: int = 0) -> dict:
    key = jax.random.key(seed)
    ks = jax.random.split(key, 24)
    f32 = jnp.float32
    nrm = lambda k, shape, s: (jax.random.normal(k, shape, f32) * s)
    return {
        'x': nrm(ks[0], (BATCH, SEQ, D_MODEL), 1.0),
        'attn_norm_w': 1.0 + nrm(ks[1], (D_MODEL,), 0.1),
        'w_in': nrm(ks[2], (D_MODEL, IN_COLS), D_MODEL ** -0.5),
        'b_forget': jax.random.uniform(ks[3], (N_HEADS_B,), f32, 1.0, 6.0),
        'qn_a': 1.0 + nrm(ks[4], (HEAD_DIM,), 0.1),
        'kn_a': 1.0 + nrm(ks[5], (HEAD_DIM,), 0.1),
        'qn_b': 1.0 + nrm(ks[6], (HEAD_DIM,), 0.1),
        'kn_b': 1.0 + nrm(ks[7], (HEAD_DIM,), 0.1),
        'rel_bias': nrm(ks[8], (N_HEADS_A, REL_TABLE), 0.5),
        'w_branch_a': nrm(ks[9], (WIDTH_A, D_MODEL), WIDTH_A ** -0.5),
        'w_branch_b': nrm(ks[10], (WIDTH_B, D_MODEL), WIDTH_B ** -0.5),
        'w_out': nrm(ks[11], (D_MODEL, D_MODEL), D_MODEL ** -0.5),
        'ffn_norm_w': 1.0 + nrm(ks[12], (D_MODEL,), 0.1),
        'w_router': nrm(ks[13], (D_MODEL, N_EXPERTS), D_MODEL ** -0.5),
        'b_router': nrm(ks[14], (N_EXPERTS,), 0.01),
        'w_gate_up': nrm(ks[15], (N_EXPERTS, D_MODEL, 2 * D_EXPERT), D_MODEL ** -0.5),
        'b_gate_up': nrm(ks[16], (N_EXPERTS, 2 * D_EXPERT), 0.02),
        'w_down': nrm(ks[17], (N_EXPERTS, D_EXPERT, D_MODEL), D_EXPERT ** -0.5),
        'b_down': nrm(ks[18], (N_EXPERTS, D_MODEL), 0.02),
    }


def reference(x, attn_norm_w, w_in, b_forget, qn_a, kn_a, qn_b, kn_b, rel_bias,
              w_branch_a, w_branch_b, w_out, ffn_norm_w, w_router, b_router,
              w_gate_up, b_gate_up, w_down, b_down):
    B, S, D = x.shape
    h = x
    for _ in range(DEPTH):
        xn = rmsnorm(h, attn_norm_w)
        proj = jnp.einsum('bsd,dc->bsc', xn, w_in)
        cuts = np.cumsum([WIDTH_A, WIDTH_A, WIDTH_A, WIDTH_B, WIDTH_B, WIDTH_B, N_HEADS_B, D_MODEL])
        qa, ka, va, qb, kb, vb, fb, ga, gb = jnp.split(proj, [int(c) for c in cuts], axis=-1)
        heads = lambda t, H: t.reshape(B, S, H, HEAD_DIM)
        qa = rmsnorm(heads(qa, N_HEADS_A), qn_a)
        ka = rmsnorm(heads(ka, N_HEADS_A), kn_a)
        qb = rmsnorm(heads(qb, N_HEADS_B), qn_b)
        kb = rmsnorm(heads(kb, N_HEADS_B), kn_b)
        y_a = chunked_relpos_attention(qa, ka, heads(va, N_HEADS_A), rel_bias)
        y_b = forgetting_attention(qb, kb, heads(vb, N_HEADS_B), fb + b_forget)
        z = jax.nn.sigmoid(ga) * (y_a @ w_branch_a) + jax.nn.sigmoid(gb) * (y_b @ w_branch_b)
        h = h + z @ w_out
        h = h + moe_ffn(rmsnorm(h, ffn_norm_w), w_router, b_router, w_gate_up, b_gate_up, w_down, b_down)
    return h
```

```python
import numpy as np
from contextlib import ExitStack
import concourse.bass as bass
import concourse.mybir as mybir
from concourse.alu_op_type import AluOpType as ALU
from concourse.bass_utils import run_bass_kernel_spmd

F32 = mybir.dt.float32
BF16 = mybir.dt.bfloat16
I32 = mybir.dt.int32
AF = mybir.ActivationFunctionType
AX = mybir.AxisListType

NORM_EPS = 1e-5
SWIGLU_LIMIT = 7.0
SWIGLU_ALPHA = 1.702
NEG = -30000.0


class Cfg:
    def __init__(self, D=2048, HA=8, HB=8, NS=32, E=32, C=320, G=16, n_cores=8, NP=0):
        self.D = D
        self.KC = D // 128
        self.HA = HA
        self.HB = HB
        self.H = HA + HB
        self.NS = NS
        self.NQ = NS // 2
        self.E = E
        self.C = C
        self.CT = (C + 127) // 128
        self.NP = NP
        self.G = G
        self.WA = HA * 128
        self.WB = HB * 128
        self.BW = min(512, self.WA, self.WB, D)
        self.HPB = self.BW // 128
        self.NB = D // self.BW
        self.INC = 3 * self.WA + 3 * self.WB + HB + 2 * D
        self.n_cores = n_cores


class Tok:
    __slots__ = ("w", "r", "name")

    def __init__(self, name=""):
        self.w = None
        self.r = []
        self.name = name


ENGS = ("pe", "act", "dve", "pool", "sp")
NDMA = 8


class Sched:
    def __init__(self, nc, st):
        self.nc = nc
        self.ops = {e: [] for e in ENGS}
        self.seq = {e: 0 for e in ENGS}
        self.sem = {e: st.enter_context(nc.semaphore("s_" + e)) for e in ENGS}
        self.semid = {}
        self.waited = {e: {} for e in ENGS}
        self.dsem = {}
        for q in ("sp", "act", "pool", "poolpc"):
            self.dsem[q] = [[st.enter_context(nc.semaphore("d_%s%d" % (q, i))), 0] for i in range(NDMA)]
        self.drr = {q: 0 for q in self.dsem}
        self.unsig = {e: False for e in ENGS}
        self.toks = []

    def tok(self, name=""):
        t = Tok(name)
        self.toks.append(t)
        return t

    def toks_n(self, n, name=""):
        return [self.tok(name + str(i)) for i in range(n)]

    def _key(self, sem):
        return id(sem)

    def _need(self, eng, deps):
        for (sem, val, src) in deps:
            if src == "pe" and eng == "pe":
                continue
            k = self._key(sem)
            if self.waited[eng].get(k, 0) < val:
                self.waited[eng][k] = val
                self.ops[eng].append(lambda e, sem=sem, val=val: e.wait_ge(sem, val))

    def _deps(self, reads, writes):
        deps = []
        for b in reads:
            if b.w is not None:
                deps.append(b.w)
        for b in writes:
            if b.w is not None:
                deps.append(b.w)
            deps.extend(b.r)
        return deps

    def _mark(self, reads, writes, t):
        for b in reads:
            b.r.append(t)
        for b in writes:
            b.w = t
            b.r = []

    def op(self, eng, fn, reads=(), writes=(), signal=True):
        self._need(eng, self._deps(reads, writes))
        sem = self.sem[eng]
        if signal:
            self.seq[eng] += 1
            t = (sem, self.seq[eng], eng)
            self.ops[eng].append(lambda e, fn=fn, sem=sem: fn(e).then_inc(sem, 1))
            self.unsig[eng] = False
        else:
            t = (sem, self.seq[eng] + 1, eng)
            self.ops[eng].append(lambda e, fn=fn: fn(e))
            self.unsig[eng] = True
        self._mark(reads, writes, t)

    def dma(self, q, fn, reads=(), writes=(), key=None):
        key = key or q
        self._need(q, self._deps(reads, writes))
        slot = self.dsem[key][self.drr[key] % NDMA]
        self.drr[key] += 1
        sem, uses = slot
        if uses > 0:
            self._need(q, [(sem, 16 * uses, "dma")])
        slot[1] = uses + 1
        t = (sem, 16 * (uses + 1), "dma")
        self.ops[q].append(lambda e, fn=fn, sem=sem: fn(e).then_inc(sem, 16))
        self._mark(reads, writes, t)

    def barrier(self):
        for e in ENGS:
            assert not self.unsig[e], e
        deps = []
        for e in ENGS:
            if self.seq[e] > 0:
                deps.append((self.sem[e], self.seq[e], "x"))
        for q in self.dsem:
            for sem, uses in self.dsem[q]:
                if uses > 0:
                    deps.append((sem, 16 * uses, "dma"))
        for e in ENGS:
            self._need(e, deps)
        for t in self.toks:
            t.w = None
            t.r = []
        self.toks = []


class Arena:
    def __init__(self, ap, nfloats):
        self.ap = ap
        self.n = nfloats
        self.off = 0

    def mark(self):
        return self.off

    def release(self, m):
        self.off = m

    def f32(self, n):
        n = (n + 1) // 2 * 2
        assert self.off + n <= self.n, ("arena overflow", self.off, n, self.n)
        a = self.ap[:, self.off:self.off + n]
        self.off += n
        return a

    def bf16(self, n):
        n = (n + 3) // 4 * 4
        return self.f32(n // 2).bitcast(BF16)

    def i32(self, n):
        return self.f32(n).bitcast(I32)


def r3(ap, b):
    return ap.rearrange("p (a b) -> p a b", b=b)


def build(cfg, debug=False):
    D, KC, HA, HB, H, NS, NQ, E, C, CT, G = (cfg.D, cfg.KC, cfg.HA, cfg.HB, cfg.H, cfg.NS, cfg.NQ,
                                             cfg.E, cfg.C, cfg.CT, cfg.G)
    WA, WB, BW, HPB, NB, INC = cfg.WA, cfg.WB, cfg.BW, cfg.HPB, cfg.NB, cfg.INC
    DE = D
    KCE = DE // 128
    NGU = 2 * DE // 128
    SCALE = 128.0 ** -0.5
    nc = bass.Bass("TRN2", target_bir_lowering=False)

    def din(name, shape, dt=F32):
        return nc.dram_tensor(name, list(shape), dt, kind="ExternalInput").ap()

    def dscr(name, shape, dt):
        kind = "ExternalOutput" if debug else "Internal"
        return nc.dram_tensor(name, list(shape), dt, kind=kind).ap()

    xkv = din("xkv", [NS * 128, D])
    valid_d = din("valid", [128, NS])
    w_in = din("w_in", [D, INC])
    anw = din("attn_norm_w", [D])
    fnw = din("ffn_norm_w", [D])
    qkn = din("qkn", [4 * BW])
    b_forget = din("b_forget", [HB])
    btab = din("btab", [128, HA, 5 * 128])
    w_ba = din("w_branch_a", [WA, D])
    w_bb = din("w_branch_b", [WB, D])
    w_out = din("w_out", [D, D])
    w_router = din("w_router", [D, E])
    b_router = din("b_router", [E])
    w_gu = din("w_gate_up", [E, D, 2 * DE])
    bgu_t = din("bgu_t", [128, E * NGU])
    w_dn = din("w_down", [E, DE, D])
    b_dn = din("b_down", [E, D])
    cst = din("cst", [128, 4 * 128])
    ebase_d = din("ebase", [128, E])
    out = nc.dram_tensor("out", [NQ * 128, D], F32, kind="ExternalOutput").ap()

    kT_d = dscr("kT_d", [H, 128, NS * 128], BF16)
    qT_d = dscr("qT_d", [H, 128, NQ * 128], BF16)
    V_d = dscr("V_d", [NS * 128, WA + WB], BF16)
    sg_d = dscr("sg_d", [NQ * 128, 2 * D], BF16)
    hbuf = dscr("hbuf", [E * C + 128, D], BF16)
    ybuf = dscr("ybuf", [E * C + 128, D], F32)
    NP = cfg.NP
    wgu_bf = nc.dram_tensor("wgu_bf", [max(NP, 1), D, 2 * DE], BF16, kind="Internal").ap()
    wdn_bf = nc.dram_tensor("wdn_bf", [max(NP, 1), DE, D], BF16, kind="Internal").ap()
    if debug:
        yT_dbg = dscr("yT_dbg", [128, H * NQ * 128], BF16)
        h_dbg = dscr("h_dbg", [NQ * 128, D], F32)
        gate_dbg = dscr("gate_dbg", [128, NQ * 4], F32)
        dest_dbg = dscr("dest_dbg", [128, NQ * 4], I32)
        cum_dbg = dscr("cum_dbg", [128, HB * NS], F32)
        xnT_dbg = dscr("xnT_dbg", [128, KC * G * 128], BF16)

    st = ExitStack()
    ARENA_F = 47000
    arena_t = st.enter_context(nc.sbuf_tensor("arena", [128, ARENA_F], F32))
    ps_t = st.enter_context(nc.psum_tensor("ps", [128, 4096], F32))
    S = Sched(nc, st)
    A = Arena(arena_t, ARENA_F)

    def bank(i, n=512, off=0):
        return ps_t[:, i * 512 + off:i * 512 + off + n]

    def bank_bf(i, n=1024, off=0):
        return ps_t[:, i * 512:(i + 1) * 512].bitcast(BF16)[:, off:off + n]

    psk = S.toks_n(8, "ps")

    pc_list = []
    for ep in range(NP):
        e_ = E - NP + ep
        cw = min(2048, 2 * DE)
        for q4 in range(4):
            r0, r1 = q4 * D // 4, (q4 + 1) * D // 4
            pc_list.append((wgu_bf[ep, r0:r1, :].rearrange("r (a b) -> r a b", b=cw),
                            w_gu[e_, r0:r1, :].rearrange("r (a b) -> r a b", b=cw)))
        cw = min(2048, D)
        for q2 in range(2):
            r0, r1 = q2 * DE // 2, (q2 + 1) * DE // 2
            pc_list.append((wdn_bf[ep, r0:r1, :].rearrange("r (a b) -> r a b", b=cw),
                            w_dn[e_, r0:r1, :].rearrange("r (a b) -> r a b", b=cw)))
    pc_pos = [0]

    def precast(n):
        for _ in range(n):
            if pc_pos[0] >= len(pc_list):
                return
            o_, i_ = pc_list[pc_pos[0]]
            pc_pos[0] += 1
            S.dma("pool", lambda e, o_=o_, i_=i_: e.dma_start(out=o_, in_=i_), key="poolpc")

    def new_ps():
        return S.toks_n(8, "ps")

    ident_f = A.f32(128)
    tri_incl_f = A.f32(128)
    tri_strict_f = A.f32(128)
    ones_f = A.f32(128)
    ident_b = A.bf16(128)
    tri_incl_b = A.bf16(128)
    valid_s = A.f32(NS)
    lf = A.f32(NS * HB)
    cum = A.f32(HB * NS)
    base = A.f32((NS + 1) * HB)
    gate_all = A.f32(NQ * 4)
    dest_i = A.i32(NQ * 4)
    Gd_all = A.f32(NQ * E)
    Gd3 = r3(Gd_all, E)
    tk_const = S.tok("const")

    cst_tmp = [ident_f, tri_incl_f, tri_strict_f, ones_f]
    for i, a in enumerate(cst_tmp):
        S.dma("sp", lambda e, a=a, i=i: e.dma_start(out=a, in_=cst[:, i * 128:(i + 1) * 128]), writes=[tk_const])
    S.dma("sp", lambda e: e.dma_start(out=valid_s, in_=valid_d), writes=[tk_const])
    S.op("dve", lambda e: e.tensor_copy(out=ident_b, in_=ident_f), reads=[tk_const], writes=[tk_const])
    S.op("dve", lambda e: e.tensor_copy(out=tri_incl_b, in_=tri_incl_f), reads=[tk_const], writes=[tk_const])
    S.barrier()
    psk = new_ps()

    m1 = A.mark()
    anw_b = A.f32(D)
    qkn_b = A.f32(4 * BW)
    bf_b = A.f32(HB)
    wf = A.bf16(KC * HB)
    xnT = A.bf16(KC * G * 128)
    xnT3 = r3(xnT, G * 128)
    xt = [A.f32(D) for _ in range(2)]
    xn = [A.bf16(D) for _ in range(2)]
    wblk = [A.bf16(KC * BW) for _ in range(2)]
    qf = [A.f32(BW) for _ in range(2)]
    qn = [A.bf16(BW) for _ in range(2)]
    stageT = A.bf16(HPB * G * 128)
    stageT3 = r3(stageT, G * 128)
    stageV = [A.bf16(BW) for _ in range(2)]
    ss = [A.f32(2) for _ in range(2)]
    rstd = [A.f32(2) for _ in range(2)]
    ssq = [A.f32(HPB) for _ in range(2)]
    rq = [A.f32(HPB) for _ in range(2)]
    f_all = A.f32(NS * HB)
    f_all3 = r3(f_all, HB)

    t_c1 = S.tok("c1")
    S.dma("sp", lambda e: e.dma_start(out=anw_b, in_=anw.partition_broadcast(128)), writes=[t_c1])
    S.dma("sp", lambda e: e.dma_start(out=qkn_b, in_=qkn.partition_broadcast(128)), writes=[t_c1])
    S.dma("sp", lambda e: e.dma_start(out=bf_b, in_=b_forget.partition_broadcast(128)), writes=[t_c1])
    fcol = 3 * WA + 3 * WB
    S.dma("pool", lambda e: e.dma_start(out=r3(wf, HB),
                                        in_=w_in[:, fcol:fcol + HB].rearrange("(kc p) c -> p kc c", p=128)),
          writes=[t_c1])

    t_xt = S.toks_n(2, "xt")
    t_xn = S.toks_n(2, "xn")
    t_ss = S.toks_n(2, "ss")
    t_xnT = S.toks_n(G, "xnT")
    t_wblk = S.toks_n(2, "wblk")
    t_qf = S.toks_n(2, "qf")
    t_qn = S.toks_n(2, "qn")
    t_sq = S.toks_n(2, "sq")
    t_stT = S.tok("stT")
    t_stV = S.toks_n(2, "stV")
    t_fall = S.tok("fall")
    t_dram1 = S.tok("dram1")

    blocks = []
    for hb_ in range(WA // BW):
        blocks.append(("q", 0 * WA + hb_ * BW, (0, hb_ * HPB)))
    for hb_ in range(WA // BW):
        blocks.append(("k", 1 * WA + hb_ * BW, (1, hb_ * HPB)))
    for hb_ in range(WA // BW):
        blocks.append(("v", 2 * WA + hb_ * BW, hb_ * BW))
    for hb_ in range(WB // BW):
        blocks.append(("q", 3 * WA + hb_ * BW, (2, HA + hb_ * HPB)))
    for hb_ in range(WB // BW):
        blocks.append(("k", 3 * WA + WB + hb_ * BW, (3, HA + hb_ * HPB)))
    for hb_ in range(WB // BW):
        blocks.append(("v", 3 * WA + 2 * WB + hb_ * BW, WA + hb_ * BW))
    gcol = 3 * WA + 3 * WB + HB
    for nb_ in range(2 * D // BW):
        blocks.append(("g", gcol + nb_ * BW, nb_ * BW))

    cnt = {"x": 0, "w": 0, "ps": 0, "q": 0, "sv": 0}
    PROJ_BANKS = (2, 3, 4)

    def rms_rstd(eng_src_ap, ss_ap, rstd_ap, n, toks_r, tok_ss, junk_ap, tok_junk):
        S.op("dve", lambda e: e.scalar_tensor_tensor(out=junk_ap, in0=eng_src_ap, scalar=1.0, in1=eng_src_ap, op0=ALU.mult, op1=ALU.mult, accum_out=ss_ap[:, 0:1]),
             reads=toks_r, writes=[tok_ss, tok_junk])
        S.op("dve", lambda e: e.tensor_scalar(out=ss_ap[:, 0:1], in0=ss_ap[:, 0:1], scalar1=1.0 / n,
                                              scalar2=NORM_EPS, op0=ALU.mult, op1=ALU.add),
             reads=[tok_ss], writes=[tok_ss])
        S.op("act", lambda e: e.activation(out=ss_ap[:, 0:1], in_=ss_ap[:, 0:1], func=AF.Sqrt),
             reads=[tok_ss], writes=[tok_ss])
        S.op("dve", lambda e: e.reciprocal(out=rstd_ap[:, 0:1], in_=ss_ap[:, 0:1]),
             reads=[tok_ss], writes=[tok_ss])

    def transposes_to(src_ap, src_tok, ncol_chunks, dst_fn, dst_tok, banks, dt_bf=True, ident=None, npart=128):
        per = 8 if dt_bf else 4
        c = 0
        bi = 0
        while c < ncol_chunks:
            n = min(per, ncol_chunks - c)
            bk = banks[bi % len(banks)]
            bi += 1
            for k in range(n):
                if dt_bf:
                    o = bank_bf(bk, 128, k * 128)
                else:
                    o = bank(bk, 128, k * 128)
                S.op("pe", lambda e, o=o, k=k, c=c: e.transpose(
                    o[:, 0:npart], src_ap[0:npart, (c + k) * 128:(c + k + 1) * 128], ident[0:npart, 0:npart]),
                     reads=[src_tok], writes=[psk[bk]], signal=(k == n - 1))
            if dt_bf:
                srcp = r3(bank_bf(bk, n * 128), 128)[:, :, 0:npart]
            else:
                srcp = r3(bank(bk, n * 128), 128)[:, :, 0:npart]
            S.op("act", lambda e, srcp=srcp, c=c, n=n: e.activation(out=dst_fn(c, n), in_=srcp, func=AF.Copy),
                 reads=[psk[bk]], writes=[dst_tok])
            c += n

    ngroups = NS // G
    pend1 = [None]
    npc = len(pc_list)
    nblk1 = len(blocks) * ngroups
    PC1 = max(1, (npc * 5 // 9 + nblk1 - 1) // nblk1) if npc else 0
    for g in range(ngroups):
        for tt in range(G):
            t = g * G + tt
            b = cnt["x"] % 2
            cnt["x"] += 1
            S.dma("sp", lambda e, b=b, t=t: e.dma_start(out=xt[b], in_=xkv[t * 128:(t + 1) * 128, :]),
                  writes=[t_xt[b]])
            rms_rstd(xt[b], ss[b], rstd[b], D, [t_xt[b]], t_ss[b], xn[b], t_xn[b])
            S.op("dve", lambda e, b=b: e.scalar_tensor_tensor(out=xn[b], in0=xt[b], scalar=rstd[b][:, 0:1],
                                                             in1=anw_b, op0=ALU.mult, op1=ALU.mult),
                 reads=[t_xt[b], t_ss[b], t_c1], writes=[t_xn[b]])
            transposes_to(xn[b], t_xn[b], KC,
                          lambda c, n, tt=tt: xnT3[:, c:c + n, tt * 128:(tt + 1) * 128],
                          t_xnT[tt], (0, 1), True, ident_b)
            for kc in range(KC):
                S.op("pe", lambda e, kc=kc, tt=tt: e.matmul(bank(7, HB), xnT3[:, kc, tt * 128:(tt + 1) * 128],
                                                            r3(wf, HB)[:, kc, :], start=(kc == 0), stop=(kc == KC - 1)),
                     reads=[t_xnT[tt], t_c1], writes=[psk[7]], signal=(kc == KC - 1))
            S.op("dve", lambda e, t=t: e.tensor_tensor(out=f_all3[:, t, :], in0=bank(7, HB), in1=bf_b, op=ALU.add),
                 reads=[psk[7], t_c1], writes=[t_fall])
        if debug and g == 0:
            S.dma("sp", lambda e: e.dma_start(out=xnT_dbg, in_=xnT), reads=t_xnT)
        for (kind, c0, meta) in blocks:
            wb = cnt["w"] % 2
            cnt["w"] += 1
            S.dma("pool", lambda e, wb=wb, c0=c0: e.dma_start(
                out=r3(wblk[wb], BW), in_=w_in[:, c0:c0 + BW].rearrange("(kc p) c -> p kc c", p=128)),
                writes=[t_wblk[wb]])
            precast(PC1)
            tiles = range(G) if kind in ("k", "v") else range(1, G, 2)
            for tt in tiles:
                t = g * G + tt
                j = (t - 1) // 2
                pb = PROJ_BANKS[cnt["ps"] % 3]
                cnt["ps"] += 1
                for kc in range(KC):
                    S.op("pe", lambda e, kc=kc, tt=tt, wb=wb, pb=pb: e.matmul(
                        bank(pb, BW), xnT3[:, kc, tt * 128:(tt + 1) * 128], r3(wblk[wb], BW)[:, kc, :],
                        start=(kc == 0), stop=(kc == KC - 1)),
                        reads=[t_xnT[tt], t_wblk[wb]], writes=[psk[pb]], signal=(kc == KC - 1))
                if kind in ("q", "k"):
                    row, h0 = meta
                    qb = cnt["q"] % 2
                    cnt["q"] += 1
                    S.op("act", lambda e, qb=qb, pb=pb: e.activation(out=qf[qb], in_=bank(pb, BW), func=AF.Copy),
                         reads=[psk[pb]], writes=[t_qf[qb]])
                    for hh in range(HPB):
                        S.op("dve", lambda e, qb=qb, hh=hh: e.scalar_tensor_tensor(out=qn[qb][:, hh * 128:(hh + 1) * 128], in0=qf[qb][:, hh * 128:(hh + 1) * 128], scalar=1.0, in1=qf[qb][:, hh * 128:(hh + 1) * 128], op0=ALU.mult, op1=ALU.mult, accum_out=ssq[qb][:, hh:hh + 1]),
                            reads=[t_qf[qb]], writes=[t_sq[qb], t_qn[qb]])
                    S.op("dve", lambda e, qb=qb: e.tensor_scalar(out=ssq[qb], in0=ssq[qb], scalar1=1.0 / 128,
                                                                 scalar2=NORM_EPS, op0=ALU.mult, op1=ALU.add),
                         reads=[t_sq[qb]], writes=[t_sq[qb]])
                    S.op("act", lambda e, qb=qb: e.activation(out=ssq[qb], in_=ssq[qb], func=AF.Sqrt),
                         reads=[t_sq[qb]], writes=[t_sq[qb]])
                    S.op("dve", lambda e, qb=qb: e.reciprocal(out=rq[qb], in_=ssq[qb]),
                         reads=[t_sq[qb]], writes=[t_sq[qb]])
                    for hh in range(HPB):
                        S.op("dve", lambda e, qb=qb, hh=hh, row=row: e.scalar_tensor_tensor(
                            out=qn[qb][:, hh * 128:(hh + 1) * 128], in0=qf[qb][:, hh * 128:(hh + 1) * 128],
                            scalar=rq[qb][:, hh:hh + 1], in1=qkn_b[:, row * BW + hh * 128:row * BW + (hh + 1) * 128],
                            op0=ALU.mult, op1=ALU.mult),
                            reads=[t_qf[qb], t_sq[qb], t_c1], writes=[t_qn[qb]])
                    if kind == "k":
                        col = tt
                    else:
                        col = (tt - 1) // 2
                    def _tr(qb=qb, col=col):
                        transposes_to(qn[qb], t_qn[qb], HPB,
                                      lambda c, n, col=col: stageT3[:, c:c + n, col * 128:(col + 1) * 128],
                                      t_stT, (5, 6), True, ident_b)
                    if pend1[0] is not None:
                        pend1[0]()
                    pend1[0] = _tr
                elif kind == "v":
                    sv = cnt["sv"] % 2
                    cnt["sv"] += 1
                    S.op("act", lambda e, sv=sv, pb=pb: e.activation(out=stageV[sv], in_=bank(pb, BW), func=AF.Copy),
                         reads=[psk[pb]], writes=[t_stV[sv]])
                    S.dma("act", lambda e, sv=sv, t=t, meta=meta: e.dma_start(
                        out=V_d[t * 128:(t + 1) * 128, meta:meta + BW], in_=stageV[sv]),
                        reads=[t_stV[sv]], writes=[t_dram1])
                else:
                    sv = cnt["sv"] % 2
                    cnt["sv"] += 1
                    S.op("act", lambda e, sv=sv, pb=pb: e.activation(out=stageV[sv], in_=bank(pb, BW), func=AF.Sigmoid),
                         reads=[psk[pb]], writes=[t_stV[sv]])
                    S.dma("act", lambda e, sv=sv, j=j, meta=meta: e.dma_start(
                        out=sg_d[j * 128:(j + 1) * 128, meta:meta + BW], in_=stageV[sv]),
                        reads=[t_stV[sv]], writes=[t_dram1])
            if pend1[0] is not None:
                pend1[0]()
                pend1[0] = None
            if kind == "k":
                row, h0 = meta
                S.dma("sp", lambda e, h0=h0, g=g: e.dma_start(
                    out=kT_d[h0:h0 + HPB, :, g * G * 128:(g + 1) * G * 128].rearrange("h p t -> p h t"),
                    in_=stageT3), reads=[t_stT], writes=[t_dram1])
            elif kind == "q":
                row, h0 = meta
                GH = G // 2
                S.dma("sp", lambda e, h0=h0, g=g, GH=GH: e.dma_start(
                    out=qT_d[h0:h0 + HPB, :, g * GH * 128:(g + 1) * GH * 128].rearrange("h p t -> p h t"),
                    in_=stageT3[:, :, 0:GH * 128]), reads=[t_stT], writes=[t_dram1])

    S.op("act", lambda e: e.activation(out=lf, in_=f_all, func=AF.Sigmoid), reads=[t_fall], writes=[t_fall])
    S.op("act", lambda e: e.activation(out=lf, in_=lf, func=AF.Ln), reads=[t_fall], writes=[t_fall])
    S.barrier()
    psk = new_ps()
    A.release(m1)

    lf3 = r3(lf, HB)
    cum3 = r3(cum, NS)
    base3 = r3(base, HB)
    t_cum = S.tok("cum")
    t_base = S.tok("base")
    S.op("dve", lambda e: e.memset(base3[:, 0, :], 0.0), writes=[t_base])
    for i in range(NS):
        bk = i % 2
        S.op("pe", lambda e, i=i, bk=bk: e.matmul(bank(bk, HB), tri_incl_f, lf3[:, i, :], start=True, stop=True),
             writes=[psk[bk]], signal=False)
        S.op("pe", lambda e, i=i, bk=bk: e.matmul(bank(bk, HB, HB), ones_f, lf3[:, i, :], start=True, stop=True),
             writes=[psk[bk]])
        S.op("dve", lambda e, i=i, bk=bk: e.tensor_tensor(out=cum3[:, :, i], in0=bank(bk, HB), in1=base3[:, i, :],
                                                          op=ALU.add),
             reads=[psk[bk], t_base], writes=[t_cum])
        S.op("dve", lambda e, i=i, bk=bk: e.tensor_tensor(out=base3[:, i + 1, :], in0=bank(bk, HB, HB),
                                                          in1=base3[:, i, :], op=ALU.add),
             reads=[psk[bk], t_base], writes=[t_base])
    if debug:
        S.dma("sp", lambda e: e.dma_start(out=cum_dbg, in_=cum), reads=[t_cum])
    S.barrier()
    psk = new_ps()

    m3 = A.mark()
    yT_all = A.bf16(H * NQ * 128)
    yT3 = r3(yT_all, NQ * 128)
    m3b = A.mark()
    Etab = A.bf16(HA * 640)
    Etab3 = r3(Etab, 640)
    tmpE = [A.f32(640) for _ in range(2)]
    VW = 130
    vaug = [A.bf16(NS * VW) for _ in range(2)]
    kTh = [A.bf16(NS * 128) for _ in range(2)]
    qTh = [A.bf16(NQ * 128) for _ in range(2)]
    pexp = [A.bf16(640) for _ in range(2)]
    pT = [A.bf16(640) for _ in range(2)]
    biasB = [A.f32(NS) for _ in range(2)]
    rinv = [A.f32(2) for _ in range(2)]
    ytile = [A.bf16(128) for _ in range(2)]

    t_E = S.tok("E")
    t_tmpE = S.toks_n(2, "tmpE")
    t_vaug = S.toks_n(2, "vaug")
    t_kTh = S.toks_n(2, "kTh")
    t_qTh = S.toks_n(2, "qTh")
    t_pexp = S.toks_n(2, "pexp")
    t_pT = S.toks_n(2, "pT")
    t_bias = S.toks_n(2, "bias")
    t_rinv = S.toks_n(2, "rinv")
    t_yt = S.toks_n(2, "yt")
    t_yT = S.toks_n(NQ, "yT")

    for h in range(HA):
        b = h % 2
        S.dma("sp", lambda e, b=b, h=h: e.dma_start(out=tmpE[b], in_=btab[:, h, :]), writes=[t_tmpE[b]])
        S.op("act", lambda e, b=b, h=h: e.activation(out=Etab3[:, h, :], in_=tmpE[b], func=AF.Exp),
             reads=[t_tmpE[b]], writes=[t_E])
    for b in range(2):
        S.op("dve", lambda e, b=b: e.tensor_copy(out=r3(vaug[b], VW)[:, :, 128], in_=valid_s), writes=[t_vaug[b]])

    cnt3 = {"s": 0, "o": 0, "p": 0, "y": 0, "b": 0}

    def load_head(hidx, b):
        S.dma("sp", lambda e: e.dma_start(out=kTh[b], in_=kT_d[hidx, :, :]), writes=[t_kTh[b]])
        S.dma("sp", lambda e: e.dma_start(out=qTh[b], in_=qT_d[hidx, :, :]), writes=[t_qTh[b]])
        S.dma("sp", lambda e: e.dma_start(
            out=r3(vaug[b], VW)[:, :, 0:128],
            in_=V_d[:, hidx * 128:(hidx + 1) * 128].rearrange("(i p) d -> p i d", p=128)),
            writes=[t_vaug[b]])

    pending = [None]
    pending2 = [None]
    PC3 = (max(0, npc - PC1 * nblk1) // 2 + H - 1) // H

    def flush():
        f2 = pending2[0]
        pending2[0] = None
        f1 = pending[0]
        pending[0] = None
        if f1 is not None:
            pending2[0] = f1()
        if f2 is not None:
            f2()

    def finalize(hidx, j, ob):
        yb = cnt3["y"] % 2
        cnt3["y"] += 1
        okb = 4 + ob
        S.op("dve", lambda e: e.reciprocal(out=rinv[yb][:, 0:1], in_=bank(okb, 1, 128)),
             reads=[psk[okb]], writes=[t_rinv[yb]])
        S.op("dve", lambda e: e.tensor_scalar(out=ytile[yb], in0=bank(okb, 128), scalar1=rinv[yb][:, 0:1],
                                              scalar2=None, op0=ALU.mult),
             reads=[psk[okb], t_rinv[yb]], writes=[t_yt[yb]])
        tbk = 6 + (yb % 2)

        def fin2():
            S.op("pe", lambda e: e.transpose(bank_bf(tbk, 128), ytile[yb], ident_b), reads=[t_yt[yb]],
                 writes=[psk[tbk]])
            S.op("act", lambda e: e.activation(out=yT3[:, hidx, j * 128:(j + 1) * 128], in_=bank_bf(tbk, 128),
                                               func=AF.Copy),
                 reads=[psk[tbk]], writes=[t_yT[j]])
        return fin2

    for hidx in range(H):
        b = hidx % 2
        load_head(hidx, b)
        precast(PC3)
        isA = hidx < HA
        for j in range(NQ):
            iq = 2 * j + 1
            ob = cnt3["o"] % 2
            cnt3["o"] += 1
            if isA:
                rs = [r for r in range(5) if iq - 4 + r >= 0]
                groups = [[(iq - 4 + r, r) for r in rs]]
            else:
                hb_ = hidx - HA
                bb = cnt3["b"] % 2
                cnt3["b"] += 1
                S.op("dve", lambda e, hb_=hb_, bb=bb, j=j: e.tensor_scalar(
                    out=biasB[bb], in0=cum3[:, hb_, :], scalar1=-1.0, scalar2=base3[:, 2 * j + 2, hb_:hb_ + 1],
                    op0=ALU.mult, op1=ALU.add), writes=[t_bias[bb]])
                ks = list(range(0, iq + 1))
                groups = [[(i, i - g0) for i in ks[g0:g0 + 4]] for g0 in range(0, len(ks), 4)]
            ngr = len(groups)
            for gi, grp in enumerate(groups):
                sb = cnt3["s"] % 2
                cnt3["s"] += 1
                pb_ = cnt3["p"] % 2
                cnt3["p"] += 1
                for (i, r) in grp:
                    bkk = 2 * sb + (1 if r >= 4 else 0)
                    off = (r % 4) * 128
                    S.op("pe", lambda e, i=i, bkk=bkk, off=off, b=b, j=j: e.matmul(
                        bank(bkk, 128, off), kTh[b][:, i * 128:(i + 1) * 128], qTh[b][:, j * 128:(j + 1) * 128],
                        start=True, stop=True),
                        reads=[t_kTh[b], t_qTh[b]], writes=[psk[bkk]], signal=True)
                r0 = grp[0][1]
                r1 = grp[-1][1]
                if isA:
                    lo = r0 * 128
                    hi = min(r1 + 1, 4) * 128
                    S.op("act", lambda e, sb=sb, pb_=pb_, lo=lo, hi=hi: e.activation(
                        out=pexp[pb_][:, lo:hi], in_=ps_t[:, 2 * sb * 512 + lo:2 * sb * 512 + hi], func=AF.Exp,
                        scale=SCALE), reads=[psk[2 * sb]], writes=[t_pexp[pb_]])
                    if r1 >= 4:
                        S.op("act", lambda e, sb=sb, pb_=pb_: e.activation(
                            out=pexp[pb_][:, 512:640], in_=bank(2 * sb + 1, 128), func=AF.Exp, scale=SCALE),
                            reads=[psk[2 * sb + 1]], writes=[t_pexp[pb_]])
                    S.op("dve", lambda e, pb_=pb_, lo=lo, r1=r1, hidx=hidx: e.tensor_tensor(
                        out=pT[pb_][:, lo:(r1 + 1) * 128], in0=pexp[pb_][:, lo:(r1 + 1) * 128],
                        in1=Etab3[:, hidx, lo:(r1 + 1) * 128], op=ALU.mult),
                        reads=[t_pexp[pb_], t_E], writes=[t_pT[pb_]])
                else:
                    for (i, r) in grp:
                        S.op("act", lambda e, i=i, r=r, sb=sb, pb_=pb_, bb=bb: e.activation(
                            out=pT[pb_][:, r * 128:(r + 1) * 128], in_=bank(2 * sb, 128, r * 128), func=AF.Exp,
                            bias=biasB[bb][:, i:i + 1], scale=SCALE),
                            reads=[psk[2 * sb], t_bias[bb]], writes=[t_pT[pb_]])
                    if grp[-1][0] == iq:
                        r = grp[-1][1]
                        S.op("dve", lambda e, r=r, pb_=pb_: e.tensor_tensor(
                            out=pT[pb_][:, r * 128:(r + 1) * 128], in0=pT[pb_][:, r * 128:(r + 1) * 128],
                            in1=tri_incl_b, op=ALU.mult), reads=[t_pT[pb_]], writes=[t_pT[pb_]])

                def pv(grp=grp, gi=gi, ngr=ngr, pb_=pb_, ob=ob, b=b, hidx=hidx, j=j):
                    okb = 4 + ob
                    for (i, r) in grp:
                        first = (gi == 0 and (i, r) == grp[0])
                        last = (gi == ngr - 1 and (i, r) == grp[-1])
                        S.op("pe", lambda e, i=i, r=r, first=first, last=last: e.matmul(
                            bank(okb, 129), pT[pb_][:, r * 128:(r + 1) * 128], r3(vaug[b], VW)[:, i, 0:129],
                            start=first, stop=last),
                            reads=[t_pT[pb_], t_vaug[b]], writes=[psk[okb]], signal=((i, r) == grp[-1]))
                    if gi == ngr - 1:
                        return finalize(hidx, j, ob)
                    return None
                flush()
                pending[0] = pv
    flush()
    flush()
    if debug:
        S.dma("sp", lambda e: e.dma_start(out=yT_dbg, in_=yT_all), reads=t_yT)
    S.barrier()
    psk = new_ps()
    A.release(m3b)

    t_yT = S.toks_n(NQ, "yT")
    Wa = A.bf16(HA * D)
    Wb = A.bf16(HB * D)
    Wa3 = r3(Wa, D)
    Wb3 = r3(Wb, D)
    sgt = [A.bf16(2 * D) for _ in range(2)]
    zt = [A.bf16(D) for _ in range(2)]
    t1 = [A.f32(BW) for _ in range(2)]
    t2 = [A.f32(BW) for _ in range(2)]
    t_W = S.tok("Wab")
    t_sgt = S.toks_n(2, "sgt")
    t_zt = S.toks_n(2, "zt")
    t_t1 = S.toks_n(2, "t1")
    t_t2 = S.toks_n(2, "t2")
    for c in range(HA):
        S.dma("pool", lambda e, c=c: e.dma_start(out=Wa3[:, c, :], in_=w_ba[c * 128:(c + 1) * 128, :]), writes=[t_W])
    for c in range(HB):
        S.dma("pool", lambda e, c=c: e.dma_start(out=Wb3[:, c, :], in_=w_bb[c * 128:(c + 1) * 128, :]), writes=[t_W])
    c4 = 0
    pend4 = [None]
    for j in range(NQ):
        b = j % 2
        S.dma("sp", lambda e, b=b, j=j: e.dma_start(out=sgt[b], in_=sg_d[j * 128:(j + 1) * 128, :]),
              writes=[t_sgt[b]])
        precast((len(pc_list) - pc_pos[0] + (NQ - j) - 1) // (NQ - j))
        for n in range(NB):
            ab = (c4 % 2) * 2
            tb = c4 % 2
            c4 += 1
            for c in range(HA):
                S.op("pe", lambda e, c=c, j=j, n=n, ab=ab: e.matmul(
                    bank(ab, BW), yT3[:, c, j * 128:(j + 1) * 128], Wa3[:, c, n * BW:(n + 1) * BW],
                    start=(c == 0), stop=(c == HA - 1)),
                    reads=[t_yT[j], t_W], writes=[psk[ab]], signal=(c == HA - 1))
            for c in range(HB):
                S.op("pe", lambda e, c=c, j=j, n=n, ab=ab: e.matmul(
                    bank(ab + 1, BW), yT3[:, HA + c, j * 128:(j + 1) * 128], Wb3[:, c, n * BW:(n + 1) * BW],
                    start=(c == 0), stop=(c == HB - 1)),
                    reads=[t_yT[j], t_W], writes=[psk[ab + 1]], signal=(c == HB - 1))
            S.op("dve", lambda e, b=b, n=n, ab=ab, tb=tb: e.tensor_tensor(
                out=t1[tb], in0=bank(ab, BW), in1=sgt[b][:, n * BW:(n + 1) * BW], op=ALU.mult),
                reads=[psk[ab], t_sgt[b]], writes=[t_t1[tb]])
            S.op("dve", lambda e, b=b, n=n, ab=ab, tb=tb: e.tensor_tensor(
                out=t2[tb], in0=bank(ab + 1, BW), in1=sgt[b][:, D + n * BW:D + (n + 1) * BW], op=ALU.mult),
                reads=[psk[ab + 1], t_sgt[b]], writes=[t_t2[tb]])
            S.op("dve", lambda e, b=b, n=n, tb=tb: e.tensor_tensor(
                out=zt[b][:, n * BW:(n + 1) * BW], in0=t1[tb], in1=t2[tb], op=ALU.add),
                reads=[t_t1[tb], t_t2[tb]], writes=[t_zt[b]])
        def _trz(b=b, j=j):
            transposes_to(zt[b], t_zt[b], KC, lambda c, n, j=j: yT3[:, c:c + n, j * 128:(j + 1) * 128],
                          t_yT[j], (4, 5), True, ident_b)
        if pend4[0] is not None:
            pend4[0]()
        pend4[0] = _trz
    pend4[0]()
    S.barrier()
    psk = new_ps()
    A.release(m3b)

    t_zT = S.toks_n(NQ, "zT")
    Wo = A.bf16(KC * D)
    Wo3 = r3(Wo, D)
    wr_f = A.f32(KC * E)
    wr3 = r3(wr_f, E)
    br_b = A.f32(E)
    fnw_b = A.f32(D)
    cb = [A.f32(E) for _ in range(2)]
    xt4 = [A.f32(D)] * 2
    hh_ = [A.f32(D) for _ in range(2)]
    hnf = hh_
    hnb = [A.bf16(D)] * 2
    hnT = A.f32(KC * 128)
    hnT3 = r3(hnT, 128)
    logit = A.f32(E)
    top8 = A.f32(8)
    negm = A.f32(2)
    mask = A.f32(E)
    ex = A.f32(E)
    exm = A.f32(E)
    ssum = A.f32(2)
    dest_e = A.f32(E)
    oh = A.f32(E)
    junkE = A.f32(E)
    destf = A.f32(4)
    ss4 = [A.f32(2) for _ in range(2)]
    rstd4 = [A.f32(2) for _ in range(2)]

    t_c4 = S.tok("c4")
    t_Wo = S.tok("Wo")
    for c in range(KC):
        S.dma("pool", lambda e, c=c: e.dma_start(out=Wo3[:, c, :], in_=w_out[c * 128:(c + 1) * 128, :]), writes=[t_Wo])
    S.dma("sp", lambda e: e.dma_start(out=wr3, in_=w_router.rearrange("(kc p) c -> p kc c", p=128)), writes=[t_c4])
    S.dma("sp", lambda e: e.dma_start(out=br_b, in_=b_router.partition_broadcast(128)), writes=[t_c4])
    S.dma("sp", lambda e: e.dma_start(out=fnw_b, in_=fnw.partition_broadcast(128)), writes=[t_c4])
    S.dma("sp", lambda e: e.dma_start(out=cb[0], in_=ebase_d), writes=[t_c4])
    t_xt4 = [S.tok("xt")] * 2
    t_h = S.toks_n(2, "h")
    t_hnf = t_h
    t_hnb = [S.tok("hnb")] * 2
    t_ss4 = S.toks_n(2, "ss4")
    t_hnT = S.tok("hnT")
    t_r = S.tok("route")
    t_cb = S.toks_n(2, "cb")
    t_ga = S.tok("gate_all")
    t_di = S.tok("dest_i")
    t_hbuf = S.tok("hbuf")
    t_out = S.tok("out")
    t_GdT = S.tok("GdT")
    gate3 = r3(gate_all, 4)
    desti3 = r3(dest_i, 4)
    for j in range(NQ):
        b = j % 2
        S.dma("sp", lambda e, b=b, j=j: e.dma_start(out=xt4[b], in_=xkv[(2 * j + 1) * 128:(2 * j + 2) * 128, :]),
              writes=[t_xt4[b]])
        for n in range(NB):
            for kc in range(KC):
                S.op("pe", lambda e, kc=kc, n=n, j=j: e.matmul(
                    bank(n, BW), yT3[:, kc, j * 128:(j + 1) * 128], Wo3[:, kc, n * BW:(n + 1) * BW],
                    start=(kc == 0), stop=(kc == KC - 1)),
                    reads=[t_zT[j], t_Wo], writes=[psk[n]], signal=(kc == KC - 1))
            S.op("dve", lambda e, b=b, n=n: e.tensor_tensor(
                out=hh_[b][:, n * BW:(n + 1) * BW], in0=bank(n, BW), in1=xt4[b][:, n * BW:(n + 1) * BW], op=ALU.add),
                reads=[psk[n], t_xt4[b]], writes=[t_h[b]])
        S.dma("sp", lambda e, b=b, j=j: e.dma_start(out=out[j * 128:(j + 1) * 128, :], in_=hh_[b]),
              reads=[t_h[b]], writes=[t_out])
        rms_rstd(hh_[b], ss4[b], rstd4[b], D, [t_h[b]], t_ss4[b], hnb[b], t_hnb[b])
        S.op("dve", lambda e, b=b: e.scalar_tensor_tensor(out=hnf[b], in0=hh_[b], scalar=rstd4[b][:, 0:1],
                                                         in1=fnw_b, op0=ALU.mult, op1=ALU.mult),
             reads=[t_h[b], t_ss4[b], t_c4], writes=[t_hnf[b]])
        S.op("act", lambda e, b=b: e.activation(out=hnb[b], in_=hnf[b], func=AF.Copy),
             reads=[t_hnf[b]], writes=[t_hnb[b]])
        tb_banks = (4, 5, 6, 7)
        transposes_to(hnf[b], t_hnf[b], KC, lambda c, n: hnT3[:, c:c + n, :], t_hnT, tb_banks, False, ident_f)
        LB = 4
        for kc in range(KC):
            S.op("pe", lambda e, kc=kc: e.matmul(bank(LB, E), hnT3[:, kc, :], wr3[:, kc, :],
                                                 start=(kc == 0), stop=(kc == KC - 1)),
                 reads=[t_hnT, t_c4], writes=[psk[LB]], signal=(kc == KC - 1))
        S.op("dve", lambda e: e.tensor_tensor(out=logit, in0=bank(LB, E), in1=br_b, op=ALU.add),
             reads=[psk[LB], t_c4], writes=[t_r])
        S.op("dve", lambda e: e.max(out=top8, in_=logit), reads=[t_r], writes=[t_r])
        S.op("dve", lambda e: e.tensor_scalar(out=mask, in0=logit, scalar1=top8[:, 3:4], scalar2=None,
                                              op0=ALU.is_ge), reads=[t_r], writes=[t_r])
        S.op("dve", lambda e: e.tensor_scalar(out=negm[:, 0:1], in0=top8[:, 0:1], scalar1=-1.0, scalar2=None,
                                              op0=ALU.mult), reads=[t_r], writes=[t_r])
        S.op("act", lambda e: e.activation(out=ex, in_=logit, func=AF.Exp, bias=negm[:, 0:1], scale=1.0),
             reads=[t_r], writes=[t_r])
        S.op("dve", lambda e: e.scalar_tensor_tensor(out=exm, in0=ex, scalar=1.0, in1=mask, op0=ALU.mult, op1=ALU.mult, accum_out=ssum[:, 0:1]),
             reads=[t_r], writes=[t_r])
        S.op("dve", lambda e: e.reciprocal(out=ssum[:, 0:1], in_=ssum[:, 0:1]), reads=[t_r], writes=[t_r])
        S.op("dve", lambda e, j=j: e.tensor_scalar(out=Gd3[:, j, :], in0=exm, scalar1=ssum[:, 0:1], scalar2=None,
                                                   op0=ALU.mult),
             reads=[t_r], writes=[t_r])
        PB = 5
        S.op("pe", lambda e: e.matmul(bank(PB, E), tri_strict_f, mask, start=True, stop=True),
             reads=[t_r], writes=[psk[PB]], signal=False)
        S.op("pe", lambda e: e.matmul(bank(PB, E, E), ones_f, mask, start=True, stop=True),
             reads=[t_r], writes=[psk[PB]])
        c0_, c1_ = cb[j % 2], cb[(j + 1) % 2]
        S.op("dve", lambda e, c0_=c0_: e.tensor_tensor(out=dest_e, in0=bank(PB, E), in1=c0_, op=ALU.add),
             reads=[psk[PB], t_cb[j % 2], t_c4], writes=[t_r])
        S.op("dve", lambda e, c0_=c0_, c1_=c1_: e.tensor_tensor(out=c1_, in0=bank(PB, E, E), in1=c0_, op=ALU.add),
             reads=[psk[PB], t_cb[j % 2], t_c4], writes=[t_cb[(j + 1) % 2]])
        for k in range(4):
            S.op("dve", lambda e, k=k: e.tensor_scalar(out=oh, in0=logit, scalar1=top8[:, k:k + 1], scalar2=None,
                                                       op0=ALU.is_equal), reads=[t_r], writes=[t_r])
            S.op("dve", lambda e, k=k: e.scalar_tensor_tensor(out=junkE, in0=oh, scalar=1.0, in1=dest_e, op0=ALU.mult, op1=ALU.mult, accum_out=destf[:, k:k + 1]),
                 reads=[t_r], writes=[t_r])
            S.op("dve", lambda e, k=k, j=j: e.scalar_tensor_tensor(out=junkE, in0=oh, scalar=1.0, in1=Gd3[:, j, :], op0=ALU.mult, op1=ALU.mult, accum_out=gate3[:, j, k:k + 1]),
                 reads=[t_r], writes=[t_r, t_ga])
        S.op("dve", lambda e, j=j: e.tensor_copy(out=desti3[:, j, :], in_=destf), reads=[t_r], writes=[t_di])
        for k in range(4):
            S.dma("pool", lambda e, b=b, j=j, k=k: e.indirect_dma_start(
                out=hbuf[:, :], out_offset=bass.IndirectOffsetOnAxis(ap=desti3[:, j, k:k + 1], axis=0),
                in_=hnb[b], in_offset=None), reads=[t_hnb[b], t_di], writes=[t_hbuf])
        if debug:
            S.dma("sp", lambda e, b=b, j=j: e.dma_start(out=h_dbg[j * 128:(j + 1) * 128, :], in_=hnf[b]),
                  reads=[t_hnf[b]])
    if debug:
        S.dma("sp", lambda e: e.dma_start(out=gate_dbg, in_=gate_all), reads=[t_ga])
        S.dma("sp", lambda e: e.dma_start(out=dest_dbg, in_=dest_i), reads=[t_di])
    S.barrier()
    psk = new_ps()
    A.release(m3)

    m5 = A.mark()
    NWB = 3
    wbk = [A.bf16(KC * BW) for _ in range(NWB)]
    xe = A.bf16(CT * D)
    xe3 = r3(xe, D)
    hTe = [A.bf16(KC * C)] * 2
    hbT = [A.bf16(KCE * C)] * 2
    SW = BW // 2
    NSTG = 4
    stg = [A.f32(KC * SW) for _ in range(NSTG)]
    t_stg = S.toks_n(NSTG, "stg")
    gsb = [A.f32(HPB * C) for _ in range(2)]
    gtmp = [A.f32(C) for _ in range(2)]
    stmp = [A.f32(C) for _ in range(2)]
    utmp = [A.f32(C) for _ in range(2)]
    ystage = [A.f32(BW) for _ in range(3)]
    bgu = A.f32(E * NGU)
    bgu3 = r3(bgu, NGU)
    t_bgu = S.tok("bgu")
    S.dma("sp", lambda e: e.dma_start(out=bgu, in_=bgu_t), writes=[t_bgu])
    t_wbk = S.toks_n(NWB, "wbk")
    t_xe = S.tok("xe")
    t_hTe = [S.tok("hTe")] * 2
    t_hbT = [S.tok("hbT")] * 2
    t_gsb = S.toks_n(2, "gsb")
    t_g = S.toks_n(2, "g")
    t_s = S.toks_n(2, "s")
    t_u = S.toks_n(2, "u")
    t_ys = S.toks_n(3, "ys")
    t_ybuf = S.tok("ybuf")
    c5 = {"w": 0, "gu": 0, "dn": 0, "t": 0, "ys": 0, "tr": 0}
    GU_BANKS = (0, 1, 2, 3)
    DN_BANKS = (4, 5)
    wsrc = []
    for ex2 in range(E):
        for bidx2 in range(DE // BW):
            for part2 in range(2):
                c0_ = part2 * DE + bidx2 * BW
                wsrc.append((w_gu, wgu_bf, ex2, c0_))
        for n2 in range(NB):
            wsrc.append((w_dn, wdn_bf, ex2, n2 * BW))
    wst = {"issued": 0, "half": 0}

    def ensure_weights(upto):
        while wst["issued"] <= min(upto, len(wsrc) - 1):
            i_ = wst["issued"]
            wst["issued"] += 1
            wsrc_f, wsrc_b, ex2, c0_ = wsrc[i_]
            wi2 = i_ % NWB
            if ex2 >= E - NP:
                S.dma("sp", lambda e, wi2=wi2, ex2=ex2, c0_=c0_, wsrc_b=wsrc_b: e.dma_start(
                    out=r3(wbk[wi2], BW),
                    in_=wsrc_b[ex2 - (E - NP), :, c0_:c0_ + BW].rearrange("(kc p) c -> p kc c", p=128)),
                    writes=[t_wbk[wi2]])
                continue
            for h2 in range(2):
                hc = wst["half"]
                wst["half"] += 1
                si = hc % NSTG
                q = "sp" if hc % 2 == 0 else "act"
                S.dma(q, lambda e, si=si, ex2=ex2, c0_=c0_, h2=h2, wsrc_f=wsrc_f: e.dma_start(
                    out=r3(stg[si], SW),
                    in_=wsrc_f[ex2, :, c0_ + h2 * SW:c0_ + (h2 + 1) * SW].rearrange("(kc p) c -> p kc c", p=128)),
                    writes=[t_stg[si]])
                dst = r3(wbk[wi2], BW)[:, :, h2 * SW:(h2 + 1) * SW]
                if hc % 3 == 2:
                    S.op("pool", lambda e, si=si, dst=dst: e.tensor_copy(out=dst, in_=r3(stg[si], SW)),
                         reads=[t_stg[si]], writes=[t_wbk[wi2]])
                else:
                    S.op("act", lambda e, si=si, dst=dst: e.activation(out=dst, in_=r3(stg[si], SW), func=AF.Copy),
                         reads=[t_stg[si]], writes=[t_wbk[wi2]])

    widx = [0]
    for ex_ in range(E):
        eb = ex_ % 2
        nfull = C // 128
        if nfull:
            S.dma("sp", lambda e, ex_=ex_: e.dma_start(
                out=xe3[:, 0:nfull, :],
                in_=hbuf[ex_ * C:ex_ * C + nfull * 128, :].rearrange("(s p) d -> p s d", p=128)), writes=[t_xe])
        if C % 128:
            S.dma("sp", lambda e, ex_=ex_: e.dma_start(
                out=xe3[0:C % 128, nfull, :], in_=hbuf[ex_ * C + nfull * 128:(ex_ + 1) * C, :]), writes=[t_xe])
        he3 = r3(hTe[eb], C)
        for s_ in range(CT):
            np_ = min(128, C - s_ * 128)
            transposes_to(xe3[:, s_, :], t_xe, KC,
                          lambda c, n, s_=s_, he3=he3, np_=np_: he3[:, c:c + n, s_ * 128:s_ * 128 + np_],
                          t_hTe[eb], (6, 7), True, ident_b, npart=np_)
        hb3 = r3(hbT[eb], C)
        nblk = DE // BW
        for bidx in range(nblk):
            gsi = c5["t"] % 2
            c5["t"] += 1
            gs3 = r3(gsb[gsi], C)
            for part in range(2):
                wi = widx[0] % NWB
                ensure_weights(widx[0] + 2)
                widx[0] += 1
                for m in range(HPB):
                    gb_ = GU_BANKS[c5["gu"] % 4]
                    c5["gu"] += 1
                    for kc in range(KC):
                        S.op("pe", lambda e, kc=kc, wi=wi, m=m, gb_=gb_, he3=he3: e.matmul(
                            bank(gb_, C), r3(wbk[wi], BW)[:, kc, m * 128:(m + 1) * 128], he3[:, kc, :],
                            start=(kc == 0), stop=(kc == KC - 1)),
                            reads=[t_wbk[wi], t_hTe[eb]], writes=[psk[gb_]], signal=(kc == KC - 1))
                    chunk = bidx * HPB + m
                    ti = (c5["gu"]) % 2
                    if part == 0:
                        bcol = bgu3[:, ex_, chunk:chunk + 1]
                        S.op("dve", lambda e, gb_=gb_, ti=ti, bcol=bcol: e.tensor_scalar(
                            out=gtmp[ti], in0=bank(gb_, C), scalar1=bcol, scalar2=SWIGLU_LIMIT, op0=ALU.add,
                            op1=ALU.min), reads=[psk[gb_], t_bgu], writes=[t_g[ti]])
                        S.op("act", lambda e, ti=ti: e.activation(out=stmp[ti], in_=gtmp[ti], func=AF.Sigmoid,
                                                                  scale=SWIGLU_ALPHA),
                             reads=[t_g[ti]], writes=[t_s[ti]])
                        S.op("dve", lambda e, ti=ti, m=m, gs3=gs3: e.tensor_tensor(
                            out=gs3[:, m, :], in0=gtmp[ti], in1=stmp[ti], op=ALU.mult),
                            reads=[t_g[ti], t_s[ti]], writes=[t_gsb[gsi]])
                    else:
                        bcol = bgu3[:, ex_, KCE + chunk:KCE + chunk + 1]
                        S.op("dve", lambda e, gb_=gb_, ti=ti, bcol=bcol: e.tensor_scalar(
                            out=utmp[ti], in0=bank(gb_, C), scalar1=bcol, scalar2=SWIGLU_LIMIT, op0=ALU.add,
                            op1=ALU.min), reads=[psk[gb_], t_bgu], writes=[t_u[ti]])
                        S.op("dve", lambda e, ti=ti: e.tensor_scalar(
                            out=utmp[ti], in0=utmp[ti], scalar1=-SWIGLU_LIMIT, scalar2=1.0, op0=ALU.max,
                            op1=ALU.add), reads=[t_u[ti]], writes=[t_u[ti]])
                        S.op("dve", lambda e, ti=ti, m=m, gs3=gs3, chunk=chunk, hb3=hb3: e.tensor_tensor(
                            out=hb3[:, chunk, :], in0=utmp[ti], in1=gs3[:, m, :], op=ALU.mult),
                            reads=[t_u[ti], t_gsb[gsi]], writes=[t_hbT[eb]])
        for n in range(NB):
            wi = widx[0] % NWB
            ensure_weights(widx[0] + 2)
            widx[0] += 1
            for s_ in range(CT):
                db = DN_BANKS[c5["dn"] % 2]
                c5["dn"] += 1
                np_ = min(128, C - s_ * 128)
                for kc in range(KCE):
                    S.op("pe", lambda e, kc=kc, wi=wi, s_=s_, db=db, hb3=hb3, np_=np_: e.matmul(
                        bank(db, BW)[0:np_, :], hb3[:, kc, s_ * 128:s_ * 128 + np_], r3(wbk[wi], BW)[:, kc, :],
                        start=(kc == 0), stop=(kc == KCE - 1)),
                        reads=[t_wbk[wi], t_hbT[eb]], writes=[psk[db]], signal=(kc == KCE - 1))
                yi = c5["ys"] % 3
                c5["ys"] += 1
                S.op("act", lambda e, yi=yi, db=db, np_=np_: e.activation(
                    out=ystage[yi][0:np_, :], in_=bank(db, BW)[0:np_, :], func=AF.Copy),
                     reads=[psk[db]], writes=[t_ys[yi]])
                S.dma("act", lambda e, yi=yi, ex_=ex_, s_=s_, n=n, np_=np_: e.dma_start(
                    out=ybuf[ex_ * C + s_ * 128:ex_ * C + s_ * 128 + np_, n * BW:(n + 1) * BW],
                    in_=ystage[yi][0:np_, :]),
                    reads=[t_ys[yi]], writes=[t_ybuf])
    S.barrier()
    psk = new_ps()
    A.release(m5)

    hp = [A.f32(D) for _ in range(2)]
    yk = [[A.f32(D) for _ in range(4)] for _ in range(2)]
    bdn = A.f32(D)
    GdT = A.f32(128)
    t_bdn = S.tok("bdn")
    t_GdT = S.tok("GdT")
    S.dma("sp", lambda e: e.dma_start(out=bdn[0:E, :], in_=b_dn), writes=[t_bdn])
    t_hp = S.toks_n(2, "hp")
    t_yk = [S.toks_n(4, "yk%d" % i) for i in range(2)]
    t_out = S.tok("out")
    for j in range(NQ):
        b = j % 2
        S.dma("sp", lambda e, b=b, j=j: e.dma_start(out=hp[b], in_=out[j * 128:(j + 1) * 128, :]), writes=[t_hp[b]])
        GB = 6
        S.op("pe", lambda e, j=j: e.transpose(bank(GB, 128)[0:E, :], Gd3[:, j, :], ident_f), writes=[psk[GB]])
        S.op("act", lambda e: e.activation(out=GdT[0:E, :], in_=bank(GB, 128)[0:E, :], func=AF.Copy),
             reads=[psk[GB]], writes=[t_GdT])
        for n in range(NB):
            S.op("pe", lambda e, n=n: e.matmul(bank(n, BW), GdT[0:E, :], bdn[0:E, n * BW:(n + 1) * BW],
                                               start=True, stop=True),
                 reads=[t_GdT, t_bdn], writes=[psk[n]])
            S.op("dve", lambda e, n=n, b=b: e.tensor_tensor(
                out=hp[b][:, n * BW:(n + 1) * BW], in0=bank(n, BW), in1=hp[b][:, n * BW:(n + 1) * BW], op=ALU.add),
                reads=[psk[n], t_hp[b]], writes=[t_hp[b]])
        for k in range(4):
            S.dma("pool", lambda e, b=b, j=j, k=k: e.indirect_dma_start(
                out=yk[b][k], out_offset=None, in_=ybuf[:, :],
                in_offset=bass.IndirectOffsetOnAxis(ap=desti3[:, j, k:k + 1], axis=0)), writes=[t_yk[b][k]])
            S.op("dve", lambda e, b=b, j=j, k=k: e.scalar_tensor_tensor(
                out=hp[b], in0=yk[b][k], scalar=gate3[:, j, k:k + 1], in1=hp[b], op0=ALU.mult, op1=ALU.add),
                reads=[t_yk[b][k], t_hp[b]], writes=[t_hp[b]])
        S.dma("sp", lambda e, b=b, j=j: e.dma_start(out=out[j * 128:(j + 1) * 128, :], in_=hp[b]),
              reads=[t_hp[b]], writes=[t_out])
    S.barrier()

    blk = st.enter_context(nc.Block())

    @blk.tensor
    def _(e):
        for f in S.ops["pe"]:
            f(e)

    @blk.scalar
    def _(e):
        for f in S.ops["act"]:
            f(e)

    @blk.vector
    def _(e):
        for f in S.ops["dve"]:
            f(e)

    @blk.gpsimd
    def _(e):
        for f in S.ops["pool"]:
            f(e)

    @blk.sync
    def _(e):
        for f in S.ops["sp"]:
            f(e)

    st.close()
    return nc


def make_consts(cfg):
    t = np.arange(128)
    ident = np.eye(128, dtype=np.float32)
    tri_incl = (t[:, None] <= t[None, :]).astype(np.float32)
    tri_strict = (t[:, None] < t[None, :]).astype(np.float32)
    ones = np.ones((128, 128), np.float32)
    cst = np.concatenate([ident, tri_incl, tri_strict, ones], axis=1)
    ebase = np.tile((np.arange(cfg.E) * cfg.C).astype(np.float32)[None, :], (128, 1))
    return cst, ebase


def make_btab(rel_bias, cfg):
    HA = cfg.HA
    kk = np.arange(128)[:, None, None]
    r = np.arange(5)[None, :, None]
    qi = np.arange(128)[None, None, :]
    dist = (4 - r) * 128 + qi - kk
    qo = qi % 64
    ok = (dist >= -(63 - qo)) & (dist <= qo + 512)
    idx = np.clip(dist, -63, 256) + 63
    tab = rel_bias[:, idx]
    tab = np.where(ok[None], tab, np.float32(NEG)).astype(np.float32)
    return np.ascontiguousarray(tab.transpose(1, 0, 2, 3).reshape(128, HA, 640))


def prepare(cfg, inp):
    D, NS, E = cfg.D, cfg.NS, cfg.E
    x = np.asarray(inp["x"], np.float32)
    B = x.shape[0]
    cst, ebase = make_consts(cfg)
    btab = make_btab(np.asarray(inp["rel_bias"], np.float32), cfg)
    qkn = np.concatenate([np.tile(np.asarray(inp[k], np.float32), cfg.HPB)
                          for k in ("qn_a", "kn_a", "qn_b", "kn_b")])
    NGU = 2 * D // 128
    bgu_t = np.ascontiguousarray(
        np.asarray(inp["b_gate_up"], np.float32).reshape(E, NGU, 128).transpose(2, 0, 1).reshape(128, E * NGU))
    shared = {
        "w_in": np.asarray(inp["w_in"], np.float32),
        "attn_norm_w": np.asarray(inp["attn_norm_w"], np.float32),
        "ffn_norm_w": np.asarray(inp["ffn_norm_w"], np.float32),
        "qkn": qkn,
        "b_forget": np.asarray(inp["b_forget"], np.float32),
        "btab": btab,
        "w_branch_a": np.asarray(inp["w_branch_a"], np.float32),
        "w_branch_b": np.asarray(inp["w_branch_b"], np.float32),
        "w_out": np.asarray(inp["w_out"], np.float32),
        "w_router": np.asarray(inp["w_router"], np.float32),
        "b_router": np.asarray(inp["b_router"], np.float32),
        "w_gate_up": np.asarray(inp["w_gate_up"], np.float32),
        "bgu_t": bgu_t,
        "w_down": np.asarray(inp["w_down"], np.float32),
        "b_down": np.asarray(inp["b_down"], np.float32),
        "cst": cst,
        "ebase": ebase,
    }
    in_maps = []
    for c in range(2 * B):
        b, p = c // 2, c % 2
        if p == 0:
            xk = np.concatenate([np.zeros((128, D), np.float32), x[b, :(NS - 1) * 128]], axis=0)
            valid = np.ones((128, NS), np.float32)
            valid[:, 0] = 0.0
        else:
            xk = x[b, :NS * 128]
            valid = np.ones((128, NS), np.float32)
        m = dict(shared)
        m["xkv"] = np.ascontiguousarray(xk)
        m["valid"] = valid
        in_maps.append(m)
    return in_maps


def assemble(cfg, results, B):
    D, NS, NQ = cfg.D, cfg.NS, cfg.NQ
    y = np.zeros((B, NS * 128, D), np.float32)
    for c in range(2 * B):
        b, p = c // 2, c % 2
        o = np.asarray(results[c]["out"]).reshape(NQ, 128, D)
        yv = y[b].reshape(NS, 128, D)
        for j in range(NQ):
            yv[2 * j + p] = o[j]
    return y


_CACHE = {}


def kernel(**inputs):
    cfg = Cfg()
    if "nc" not in _CACHE:
        _CACHE["nc"] = build(cfg)
    nc = _CACHE["nc"]
    in_maps = prepare(cfg, inputs)
    res = run_bass_kernel_spmd(nc, in_maps, core_ids=list(range(cfg.n_cores)))
    return assemble(cfg, res.results, 4)
```

```python
import numpy as np
from contextlib import ExitStack
import concourse.bass as bass
import concourse.mybir as mybir
from concourse.alu_op_type import AluOpType as ALU
from concourse.bass_utils import run_bass_kernel_spmd

F32 = mybir.dt.float32
BF16 = mybir.dt.bfloat16
I32 = mybir.dt.int32
AF = mybir.ActivationFunctionType
AX = mybir.AxisListType

NORM_EPS = 1e-5
SWIGLU_LIMIT = 7.0
SWIGLU_ALPHA = 1.702
NEG = -30000.0


class Cfg:
    def __init__(self, D=2048, HA=8, HB=8, NS=32, E=32, C=320, G=16, n_cores=8, NP=0):
        self.D = D
        self.KC = D // 128
        self.HA = HA
        self.HB = HB
        self.H = HA + HB
        self.NS = NS
        self.NQ = NS // 2
        self.E = E
        self.C = C
        self.CT = (C + 127) // 128
        self.NP = NP
        self.G = G
        self.WA = HA * 128
        self.WB = HB * 128
        self.BW = min(512, self.WA, self.WB, D)
        self.HPB = self.BW // 128
        self.NB = D // self.BW
        self.INC = 3 * self.WA + 3 * self.WB + HB + 2 * D
        self.n_cores = n_cores


class Tok:
    __slots__ = ("w", "r", "name")

    def __init__(self, name=""):
        self.w = None
        self.r = []
        self.name = name


ENGS = ("pe", "act", "dve", "pool", "sp")
NDMA = 8


class Sched:
    def __init__(self, nc, st):
        self.nc = nc
        self.ops = {e: [] for e in ENGS}
        self.seq = {e: 0 for e in ENGS}
        self.sem = {e: st.enter_context(nc.semaphore("s_" + e)) for e in ENGS}
        self.semid = {}
        self.waited = {e: {} for e in ENGS}
        self.dsem = {}
        for q in ("sp", "act", "pool", "poolpc"):
            self.dsem[q] = [[st.enter_context(nc.semaphore("d_%s%d" % (q, i))), 0] for i in range(NDMA)]
        self.drr = {q: 0 for q in self.dsem}
        self.unsig = {e: False for e in ENGS}
        self.toks = []

    def tok(self, name=""):
        t = Tok(name)
        self.toks.append(t)
        return t

    def toks_n(self, n, name=""):
        return [self.tok(name + str(i)) for i in range(n)]

    def _key(self, sem):
        return id(sem)

    def _need(self, eng, deps):
        for (sem, val, src) in deps:
            if src == "pe" and eng == "pe":
                continue
            k = self._key(sem)
            if self.waited[eng].get(k, 0) < val:
                self.waited[eng][k] = val
                self.ops[eng].append(lambda e, sem=sem, val=val: e.wait_ge(sem, val))

    def _deps(self, reads, writes):
        deps = []
        for b in reads:
            if b.w is not None:
                deps.append(b.w)
        for b in writes:
            if b.w is not None:
                deps.append(b.w)
            deps.extend(b.r)
        return deps

    def _mark(self, reads, writes, t):
        for b in reads:
            b.r.append(t)
        for b in writes:
            b.w = t
            b.r = []

    def op(self, eng, fn, reads=(), writes=(), signal=True):
        self._need(eng, self._deps(reads, writes))
        sem = self.sem[eng]
        if signal:
            self.seq[eng] += 1
            t = (sem, self.seq[eng], eng)
            self.ops[eng].append(lambda e, fn=fn, sem=sem: fn(e).then_inc(sem, 1))
            self.unsig[eng] = False
        else:
            t = (sem, self.seq[eng] + 1, eng)
            self.ops[eng].append(lambda e, fn=fn: fn(e))
            self.unsig[eng] = True
        self._mark(reads, writes, t)

    def dma(self, q, fn, reads=(), writes=(), key=None):
        key = key or q
        self._need(q, self._deps(reads, writes))
        slot = self.dsem[key][self.drr[key] % NDMA]
        self.drr[key] += 1
        sem, uses = slot
        if uses > 0:
            self._need(q, [(sem, 16 * uses, "dma")])
        slot[1] = uses + 1
        t = (sem, 16 * (uses + 1), "dma")
        self.ops[q].append(lambda e, fn=fn, sem=sem: fn(e).then_inc(sem, 16))
        self._mark(reads, writes, t)

    def barrier(self):
        for e in ENGS:
            assert not self.unsig[e], e
        deps = []
        for e in ENGS:
            if self.seq[e] > 0:
                deps.append((self.sem[e], self.seq[e], "x"))
        for q in self.dsem:
            for sem, uses in self.dsem[q]:
                if uses > 0:
                    deps.append((sem, 16 * uses, "dma"))
        for e in ENGS:
            self._need(e, deps)
        for t in self.toks:
            t.w = None
            t.r = []
        self.toks = []


class Arena:
    def __init__(self, ap, nfloats):
        self.ap = ap
        self.n = nfloats
        self.off = 0

    def mark(self):
        return self.off

    def release(self, m):
        self.off = m

    def f32(self, n):
        n = (n + 1) // 2 * 2
        assert self.off + n <= self.n, ("arena overflow", self.off, n, self.n)
        a = self.ap[:, self.off:self.off + n]
        self.off += n
        return a

    def bf16(self, n):
        n = (n + 3) // 4 * 4
        return self.f32(n // 2).bitcast(BF16)

    def i32(self, n):
        return self.f32(n).bitcast(I32)


def r3(ap, b):
    return ap.rearrange("p (a b) -> p a b", b=b)


def build(cfg, debug=False):
    D, KC, HA, HB, H, NS, NQ, E, C, CT, G = (cfg.D, cfg.KC, cfg.HA, cfg.HB, cfg.H, cfg.NS, cfg.NQ,
                                             cfg.E, cfg.C, cfg.CT, cfg.G)
    WA, WB, BW, HPB, NB, INC = cfg.WA, cfg.WB, cfg.BW, cfg.HPB, cfg.NB, cfg.INC
    DE = D
    KCE = DE // 128
    NGU = 2 * DE // 128
    SCALE = 128.0 ** -0.5
    nc = bass.Bass("TRN2", target_bir_lowering=False)

    def din(name, shape, dt=F32):
        return nc.dram_tensor(name, list(shape), dt, kind="ExternalInput").ap()

    def dscr(name, shape, dt):
        kind = "ExternalOutput" if debug else "Internal"
        return nc.dram_tensor(name, list(shape), dt, kind=kind).ap()

    xkv = din("xkv", [NS * 128, D])
    valid_d = din("valid", [128, NS])
    w_in = din("w_in", [D, INC])
    anw = din("attn_norm_w", [D])
    fnw = din("ffn_norm_w", [D])
    qkn = din("qkn", [4 * BW])
    b_forget = din("b_forget", [HB])
    btab = din("btab", [128, HA, 5 * 128])
    w_ba = din("w_branch_a", [WA, D])
    w_bb = din("w_branch_b", [WB, D])
    w_out = din("w_out", [D, D])
    w_router = din("w_router", [D, E])
    b_router = din("b_router", [E])
    w_gu = din("w_gate_up", [E, D, 2 * DE])
    bgu_t = din("bgu_t", [128, E * NGU])
    w_dn = din("w_down", [E, DE, D])
    b_dn = din("b_down", [E, D])
    cst = din("cst", [128, 4 * 128])
    ebase_d = din("ebase", [128, E])
    out = nc.dram_tensor("out", [NQ * 128, D], F32, kind="ExternalOutput").ap()

    kT_d = dscr("kT_d", [H, 128, NS * 128], BF16)
    qT_d = dscr("qT_d", [H, 128, NQ * 128], BF16)
    V_d = dscr("V_d", [NS * 128, WA + WB], BF16)
    sg_d = dscr("sg_d", [NQ * 128, 2 * D], BF16)
    hbuf = dscr("hbuf", [E * C + 128, D], BF16)
    ybuf = dscr("ybuf", [E * C + 128, D], F32)
    NP = cfg.NP
    wgu_bf = nc.dram_tensor("wgu_bf", [max(NP, 1), D, 2 * DE], BF16, kind="Internal").ap()
    wdn_bf = nc.dram_tensor("wdn_bf", [max(NP, 1), DE, D], BF16, kind="Internal").ap()
    if debug:
        yT_dbg = dscr("yT_dbg", [128, H * NQ * 128], BF16)
        h_dbg = dscr("h_dbg", [NQ * 128, D], F32)
        gate_dbg = dscr("gate_dbg", [128, NQ * 4], F32)
        dest_dbg = dscr("dest_dbg", [128, NQ * 4], I32)
        cum_dbg = dscr("cum_dbg", [128, HB * NS], F32)
        xnT_dbg = dscr("xnT_dbg", [128, KC * G * 128], BF16)

    st = ExitStack()
    ARENA_F = 47000
    arena_t = st.enter_context(nc.sbuf_tensor("arena", [128, ARENA_F], F32))
    ps_t = st.enter_context(nc.psum_tensor("ps", [128, 4096], F32))
    S = Sched(nc, st)
    A = Arena(arena_t, ARENA_F)

    def bank(i, n=512, off=0):
        return ps_t[:, i * 512 + off:i * 512 + off + n]

    def bank_bf(i, n=1024, off=0):
        return ps_t[:, i * 512:(i + 1) * 512].bitcast(BF16)[:, off:off + n]

    psk = S.toks_n(8, "ps")

    pc_list = []
    for ep in range(NP):
        e_ = E - NP + ep
        cw = min(2048, 2 * DE)
        for q4 in range(4):
            r0, r1 = q4 * D // 4, (q4 + 1) * D // 4
            pc_list.append((wgu_bf[ep, r0:r1, :].rearrange("r (a b) -> r a b", b=cw),
                            w_gu[e_, r0:r1, :].rearrange("r (a b) -> r a b", b=cw)))
        cw = min(2048, D)
        for q2 in range(2):
            r0, r1 = q2 * DE // 2, (q2 + 1) * DE // 2
            pc_list.append((wdn_bf[ep, r0:r1, :].rearrange("r (a b) -> r a b", b=cw),
                            w_dn[e_, r0:r1, :].rearrange("r (a b) -> r a b", b=cw)))
    pc_pos = [0]

    def precast(n):
        for _ in range(n):
            if pc_pos[0] >= len(pc_list):
                return
            o_, i_ = pc_list[pc_pos[0]]
            pc_pos[0] += 1
            S.dma("pool", lambda e, o_=o_, i_=i_: e.dma_start(out=o_, in_=i_), key="poolpc")

    def new_ps():
        return S.toks_n(8, "ps")

    ident_f = A.f32(128)
    tri_incl_f = A.f32(128)
    tri_strict_f = A.f32(128)
    ones_f = A.f32(128)
    ident_b = A.bf16(128)
    tri_incl_b = A.bf16(128)
    valid_s = A.f32(NS)
    lf = A.f32(NS * HB)
    cum = A.f32(HB * NS)
    base = A.f32((NS + 1) * HB)
    gate_all = A.f32(NQ * 4)
    dest_i = A.i32(NQ * 4)
    Gd_all = A.f32(NQ * E)
    Gd3 = r3(Gd_all, E)
    tk_const = S.tok("const")

    cst_tmp = [ident_f, tri_incl_f, tri_strict_f, ones_f]
    for i, a in enumerate(cst_tmp):
        S.dma("sp", lambda e, a=a, i=i: e.dma_start(out=a, in_=cst[:, i * 128:(i + 1) * 128]), writes=[tk_const])
    S.dma("sp", lambda e: e.dma_start(out=valid_s, in_=valid_d), writes=[tk_const])
    S.op("dve", lambda e: e.tensor_copy(out=ident_b, in_=ident_f), reads=[tk_const], writes=[tk_const])
    S.op("dve", lambda e: e.tensor_copy(out=tri_incl_b, in_=tri_incl_f), reads=[tk_const], writes=[tk_const])
    S.barrier()
    psk = new_ps()

    m1 = A.mark()
    anw_b = A.f32(D)
    qkn_b = A.f32(4 * BW)
    bf_b = A.f32(HB)
    wf = A.bf16(KC * HB)
    xnT = A.bf16(KC * G * 128)
    xnT3 = r3(xnT, G * 128)
    xt = [A.f32(D) for _ in range(2)]
    xn = [A.bf16(D) for _ in range(2)]
    wblk = [A.bf16(KC * BW) for _ in range(2)]
    qf = [A.f32(BW) for _ in range(2)]
    qn = [A.bf16(BW) for _ in range(2)]
    stageT = A.bf16(HPB * G * 128)
    stageT3 = r3(stageT, G * 128)
    stageV = [A.bf16(BW) for _ in range(2)]
    ss = [A.f32(2) for _ in range(2)]
    rstd = [A.f32(2) for _ in range(2)]
    ssq = [A.f32(HPB) for _ in range(2)]
    rq = [A.f32(HPB) for _ in range(2)]
    f_all = A.f32(NS * HB)
    f_all3 = r3(f_all, HB)

    t_c1 = S.tok("c1")
    S.dma("sp", lambda e: e.dma_start(out=anw_b, in_=anw.partition_broadcast(128)), writes=[t_c1])
    S.dma("sp", lambda e: e.dma_start(out=qkn_b, in_=qkn.partition_broadcast(128)), writes=[t_c1])
    S.dma("sp", lambda e: e.dma_start(out=bf_b, in_=b_forget.partition_broadcast(128)), writes=[t_c1])
    fcol = 3 * WA + 3 * WB
    S.dma("pool", lambda e: e.dma_start(out=r3(wf, HB),
                                        in_=w_in[:, fcol:fcol + HB].rearrange("(kc p) c -> p kc c", p=128)),
          writes=[t_c1])

    t_xt = S.toks_n(2, "xt")
    t_xn = S.toks_n(2, "xn")
    t_ss = S.toks_n(2, "ss")
    t_xnT = S.toks_n(G, "xnT")
    t_wblk = S.toks_n(2, "wblk")
    t_qf = S.toks_n(2, "qf")
    t_qn = S.toks_n(2, "qn")
    t_sq = S.toks_n(2, "sq")
    t_stT = S.tok("stT")
    t_stV = S.toks_n(2, "stV")
    t_fall = S.tok("fall")
    t_dram1 = S.tok("dram1")

    blocks = []
    for hb_ in range(WA // BW):
        blocks.append(("q", 0 * WA + hb_ * BW, (0, hb_ * HPB)))
    for hb_ in range(WA // BW):
        blocks.append(("k", 1 * WA + hb_ * BW, (1, hb_ * HPB)))
    for hb_ in range(WA // BW):
        blocks.append(("v", 2 * WA + hb_ * BW, hb_ * BW))
    for hb_ in range(WB // BW):
        blocks.append(("q", 3 * WA + hb_ * BW, (2, HA + hb_ * HPB)))
    for hb_ in range(WB // BW):
        blocks.append(("k", 3 * WA + WB + hb_ * BW, (3, HA + hb_ * HPB)))
    for hb_ in range(WB // BW):
        blocks.append(("v", 3 * WA + 2 * WB + hb_ * BW, WA + hb_ * BW))
    gcol = 3 * WA + 3 * WB + HB
    for nb_ in range(2 * D // BW):
        blocks.append(("g", gcol + nb_ * BW, nb_ * BW))

    cnt = {"x": 0, "w": 0, "ps": 0, "q": 0, "sv": 0}
    PROJ_BANKS = (2, 3, 4)

    def rms_rstd(eng_src_ap, ss_ap, rstd_ap, n, toks_r, tok_ss, junk_ap, tok_junk):
        S.op("dve", lambda e: e.scalar_tensor_tensor(out=junk_ap, in0=eng_src_ap, scalar=1.0, in1=eng_src_ap, op0=ALU.mult, op1=ALU.mult, accum_out=ss_ap[:, 0:1]),
             reads=toks_r, writes=[tok_ss, tok_junk])
        S.op("dve", lambda e: e.tensor_scalar(out=ss_ap[:, 0:1], in0=ss_ap[:, 0:1], scalar1=1.0 / n,
                                              scalar2=NORM_EPS, op0=ALU.mult, op1=ALU.add),
             reads=[tok_ss], writes=[tok_ss])
        S.op("act", lambda e: e.activation(out=ss_ap[:, 0:1], in_=ss_ap[:, 0:1], func=AF.Sqrt),
             reads=[tok_ss], writes=[tok_ss])
        S.op("dve", lambda e: e.reciprocal(out=rstd_ap[:, 0:1], in_=ss_ap[:, 0:1]),
             reads=[tok_ss], writes=[tok_ss])

    def transposes_to(src_ap, src_tok, ncol_chunks, dst_fn, dst_tok, banks, dt_bf=True, ident=None, npart=128):
        per = 8 if dt_bf else 4
        c = 0
        bi = 0
        while c < ncol_chunks:
            n = min(per, ncol_chunks - c)
            bk = banks[bi % len(banks)]
            bi += 1
            for k in range(n):
                if dt_bf:
                    o = bank_bf(bk, 128, k * 128)
                else:
                    o = bank(bk, 128, k * 128)
                S.op("pe", lambda e, o=o, k=k, c=c: e.transpose(
                    o[:, 0:npart], src_ap[0:npart, (c + k) * 128:(c + k + 1) * 128], ident[0:npart, 0:npart]),
                     reads=[src_tok], writes=[psk[bk]], signal=(k == n - 1))
            if dt_bf:
                srcp = r3(bank_bf(bk, n * 128), 128)[:, :, 0:npart]
            else:
                srcp = r3(bank(bk, n * 128), 128)[:, :, 0:npart]
            S.op("act", lambda e, srcp=srcp, c=c, n=n: e.activation(out=dst_fn(c, n), in_=srcp, func=AF.Copy),
                 reads=[psk[bk]], writes=[dst_tok])
            c += n

    ngroups = NS // G
    pend1 = [None]
    npc = len(pc_list)
    nblk1 = len(blocks) * ngroups
    PC1 = max(1, (npc * 5 // 9 + nblk1 - 1) // nblk1) if npc else 0
    for g in range(ngroups):
        for tt in range(G):
            t = g * G + tt
            b = cnt["x"] % 2
            cnt["x"] += 1
            S.dma("sp", lambda e, b=b, t=t: e.dma_start(out=xt[b], in_=xkv[t * 128:(t + 1) * 128, :]),
                  writes=[t_xt[b]])
            rms_rstd(xt[b], ss[b], rstd[b], D, [t_xt[b]], t_ss[b], xn[b], t_xn[b])
            S.op("dve", lambda e, b=b: e.scalar_tensor_tensor(out=xn[b], in0=xt[b], scalar=rstd[b][:, 0:1],
                                                             in1=anw_b, op0=ALU.mult, op1=ALU.mult),
                 reads=[t_xt[b], t_ss[b], t_c1], writes=[t_xn[b]])
            transposes_to(xn[b], t_xn[b], KC,
                          lambda c, n, tt=tt: xnT3[:, c:c + n, tt * 128:(tt + 1) * 128],
                          t_xnT[tt], (0, 1), True, ident_b)
            for kc in range(KC):
                S.op("pe", lambda e, kc=kc, tt=tt: e.matmul(bank(7, HB), xnT3[:, kc, tt * 128:(tt + 1) * 128],
                                                            r3(wf, HB)[:, kc, :], start=(kc == 0), stop=(kc == KC - 1)),
                     reads=[t_xnT[tt], t_c1], writes=[psk[7]], signal=(kc == KC - 1))
            S.op("dve", lambda e, t=t: e.tensor_tensor(out=f_all3[:, t, :], in0=bank(7, HB), in1=bf_b, op=ALU.add),
                 reads=[psk[7], t_c1], writes=[t_fall])
        if debug and g == 0:
            S.dma("sp", lambda e: e.dma_start(out=xnT_dbg, in_=xnT), reads=t_xnT)
        for (kind, c0, meta) in blocks:
            wb = cnt["w"] % 2
            cnt["w"] += 1
            S.dma("pool", lambda e, wb=wb, c0=c0: e.dma_start(
                out=r3(wblk[wb], BW), in_=w_in[:, c0:c0 + BW].rearrange("(kc p) c -> p kc c", p=128)),
                writes=[t_wblk[wb]])
            precast(PC1)
            tiles = range(G) if kind in ("k", "v") else range(1, G, 2)
            for tt in tiles:
                t = g * G + tt
                j = (t - 1) // 2
                pb = PROJ_BANKS[cnt["ps"] % 3]
                cnt["ps"] += 1
                for kc in range(KC):
                    S.op("pe", lambda e, kc=kc, tt=tt, wb=wb, pb=pb: e.matmul(
                        bank(pb, BW), xnT3[:, kc, tt * 128:(tt + 1) * 128], r3(wblk[wb], BW)[:, kc, :],
                        start=(kc == 0), stop=(kc == KC - 1)),
                        reads=[t_xnT[tt], t_wblk[wb]], writes=[psk[pb]], signal=(kc == KC - 1))
                if kind in ("q", "k"):
                    row, h0 = meta
                    qb = cnt["q"] % 2
                    cnt["q"] += 1
                    S.op("act", lambda e, qb=qb, pb=pb: e.activation(out=qf[qb], in_=bank(pb, BW), func=AF.Copy),
                         reads=[psk[pb]], writes=[t_qf[qb]])
                    for hh in range(HPB):
                        S.op("dve", lambda e, qb=qb, hh=hh: e.scalar_tensor_tensor(out=qn[qb][:, hh * 128:(hh + 1) * 128], in0=qf[qb][:, hh * 128:(hh + 1) * 128], scalar=1.0, in1=qf[qb][:, hh * 128:(hh + 1) * 128], op0=ALU.mult, op1=ALU.mult, accum_out=ssq[qb][:, hh:hh + 1]),
                            reads=[t_qf[qb]], writes=[t_sq[qb], t_qn[qb]])
                    S.op("dve", lambda e, qb=qb: e.tensor_scalar(out=ssq[qb], in0=ssq[qb], scalar1=1.0 / 128,
                                                                 scalar2=NORM_EPS, op0=ALU.mult, op1=ALU.add),
                         reads=[t_sq[qb]], writes=[t_sq[qb]])
                    S.op("act", lambda e, qb=qb: e.activation(out=ssq[qb], in_=ssq[qb], func=AF.Sqrt),
                         reads=[t_sq[qb]], writes=[t_sq[qb]])
                    S.op("dve", lambda e, qb=qb: e.reciprocal(out=rq[qb], in_=ssq[qb]),
                         reads=[t_sq[qb]], writes=[t_sq[qb]])
                    for hh in range(HPB):
                        S.op("dve", lambda e, qb=qb, hh=hh, row=row: e.scalar_tensor_tensor(
                            out=qn[qb][:, hh * 128:(hh + 1) * 128], in0=qf[qb][:, hh * 128:(hh + 1) * 128],
                            scalar=rq[qb][:, hh:hh + 1], in1=qkn_b[:, row * BW + hh * 128:row * BW + (hh + 1) * 128],
                            op0=ALU.mult, op1=ALU.mult),
                            reads=[t_qf[qb], t_sq[qb], t_c1], writes=[t_qn[qb]])
                    if kind == "k":
                        col = tt
                    else:
                        col = (tt - 1) // 2
                    def _tr(qb=qb, col=col):
                        transposes_to(qn[qb], t_qn[qb], HPB,
                                      lambda c, n, col=col: stageT3[:, c:c + n, col * 128:(col + 1) * 128],
                                      t_stT, (5, 6), True, ident_b)
                    if pend1[0] is not None:
                        pend1[0]()
                    pend1[0] = _tr
                elif kind == "v":
                    sv = cnt["sv"] % 2
                    cnt["sv"] += 1
                    S.op("act", lambda e, sv=sv, pb=pb: e.activation(out=stageV[sv], in_=bank(pb, BW), func=AF.Copy),
                         reads=[psk[pb]], writes=[t_stV[sv]])
                    S.dma("act", lambda e, sv=sv, t=t, meta=meta: e.dma_start(
                        out=V_d[t * 128:(t + 1) * 128, meta:meta + BW], in_=stageV[sv]),
                        reads=[t_stV[sv]], writes=[t_dram1])
                else:
                    sv = cnt["sv"] % 2
                    cnt["sv"] += 1
                    S.op("act", lambda e, sv=sv, pb=pb: e.activation(out=stageV[sv], in_=bank(pb, BW), func=AF.Sigmoid),
                         reads=[psk[pb]], writes=[t_stV[sv]])
                    S.dma("act", lambda e, sv=sv, j=j, meta=meta: e.dma_start(
                        out=sg_d[j * 128:(j + 1) * 128, meta:meta + BW], in_=stageV[sv]),
                        reads=[t_stV[sv]], writes=[t_dram1])
            if pend1[0] is not None:
                pend1[0]()
                pend1[0] = None
            if kind == "k":
                row, h0 = meta
                S.dma("sp", lambda e, h0=h0, g=g: e.dma_start(
                    out=kT_d[h0:h0 + HPB, :, g * G * 128:(g + 1) * G * 128].rearrange("h p t -> p h t"),
                    in_=stageT3), reads=[t_stT], writes=[t_dram1])
            elif kind == "q":
                row, h0 = meta
                GH = G // 2
                S.dma("sp", lambda e, h0=h0, g=g, GH=GH: e.dma_start(
                    out=qT_d[h0:h0 + HPB, :, g * GH * 128:(g + 1) * GH * 128].rearrange("h p t -> p h t"),
                    in_=stageT3[:, :, 0:GH * 128]), reads=[t_stT], writes=[t_dram1])

    S.op("act", lambda e: e.activation(out=lf, in_=f_all, func=AF.Sigmoid), reads=[t_fall], writes=[t_fall])
    S.op("act", lambda e: e.activation(out=lf, in_=lf, func=AF.Ln), reads=[t_fall], writes=[t_fall])
    S.barrier()
    psk = new_ps()
    A.release(m1)

    lf3 = r3(lf, HB)
    cum3 = r3(cum, NS)
    base3 = r3(base, HB)
    t_cum = S.tok("cum")
    t_base = S.tok("base")
    S.op("dve", lambda e: e.memset(base3[:, 0, :], 0.0), writes=[t_base])
    for i in range(NS):
        bk = i % 2
        S.op("pe", lambda e, i=i, bk=bk: e.matmul(bank(bk, HB), tri_incl_f, lf3[:, i, :], start=True, stop=True),
             writes=[psk[bk]], signal=False)
        S.op("pe", lambda e, i=i, bk=bk: e.matmul(bank(bk, HB, HB), ones_f, lf3[:, i, :], start=True, stop=True),
             writes=[psk[bk]])
        S.op("dve", lambda e, i=i, bk=bk: e.tensor_tensor(out=cum3[:, :, i], in0=bank(bk, HB), in1=base3[:, i, :],
                                                          op=ALU.add),
             reads=[psk[bk], t_base], writes=[t_cum])
        S.op("dve", lambda e, i=i, bk=bk: e.tensor_tensor(out=base3[:, i + 1, :], in0=bank(bk, HB, HB),
                                                          in1=base3[:, i, :], op=ALU.add),
             reads=[psk[bk], t_base], writes=[t_base])
    if debug:
        S.dma("sp", lambda e: e.dma_start(out=cum_dbg, in_=cum), reads=[t_cum])
    S.barrier()
    psk = new_ps()

    m3 = A.mark()
    yT_all = A.bf16(H * NQ * 128)
    yT3 = r3(yT_all, NQ * 128)
    m3b = A.mark()
    Etab = A.bf16(HA * 640)
    Etab3 = r3(Etab, 640)
    tmpE = [A.f32(640) for _ in range(2)]
    VW = 130
    vaug = [A.bf16(NS * VW) for _ in range(2)]
    kTh = [A.bf16(NS * 128) for _ in range(2)]
    qTh = [A.bf16(NQ * 128) for _ in range(2)]
    pexp = [A.bf16(640) for _ in range(2)]
    pT = [A.bf16(640) for _ in range(2)]
    biasB = [A.f32(NS) for _ in range(2)]
    rinv = [A.f32(2) for _ in range(2)]
    ytile = [A.bf16(128) for _ in range(2)]

    t_E = S.tok("E")
    t_tmpE = S.toks_n(2, "tmpE")
    t_vaug = S.toks_n(2, "vaug")
    t_kTh = S.toks_n(2, "kTh")
    t_qTh = S.toks_n(2, "qTh")
    t_pexp = S.toks_n(2, "pexp")
    t_pT = S.toks_n(2, "pT")
    t_bias = S.toks_n(2, "bias")
    t_rinv = S.toks_n(2, "rinv")
    t_yt = S.toks_n(2, "yt")
    t_yT = S.toks_n(NQ, "yT")

    for h in range(HA):
        b = h % 2
        S.dma("sp", lambda e, b=b, h=h: e.dma_start(out=tmpE[b], in_=btab[:, h, :]), writes=[t_tmpE[b]])
        S.op("act", lambda e, b=b, h=h: e.activation(out=Etab3[:, h, :], in_=tmpE[b], func=AF.Exp),
             reads=[t_tmpE[b]], writes=[t_E])
    for b in range(2):
        S.op("dve", lambda e, b=b: e.tensor_copy(out=r3(vaug[b], VW)[:, :, 128], in_=valid_s), writes=[t_vaug[b]])

    cnt3 = {"s": 0, "o": 0, "p": 0, "y": 0, "b": 0}

    def load_head(hidx, b):
        S.dma("sp", lambda e: e.dma_start(out=kTh[b], in_=kT_d[hidx, :, :]), writes=[t_kTh[b]])
        S.dma("sp", lambda e: e.dma_start(out=qTh[b], in_=qT_d[hidx, :, :]), writes=[t_qTh[b]])
        S.dma("sp", lambda e: e.dma_start(
            out=r3(vaug[b], VW)[:, :, 0:128],
            in_=V_d[:, hidx * 128:(hidx + 1) * 128].rearrange("(i p) d -> p i d", p=128)),
            writes=[t_vaug[b]])

    pending = [None]
    pending2 = [None]
    PC3 = (max(0, npc - PC1 * nblk1) // 2 + H - 1) // H

    def flush():
        f2 = pending2[0]
        pending2[0] = None
        f1 = pending[0]
        pending[0] = None
        if f1 is not None:
            pending2[0] = f1()
        if f2 is not None:
            f2()

    def finalize(hidx, j, ob):
        yb = cnt3["y"] % 2
        cnt3["y"] += 1
        okb = 4 + ob
        S.op("dve", lambda e: e.reciprocal(out=rinv[yb][:, 0:1], in_=bank(okb, 1, 128)),
             reads=[psk[okb]], writes=[t_rinv[yb]])
        S.op("dve", lambda e: e.tensor_scalar(out=ytile[yb], in0=bank(okb, 128), scalar1=rinv[yb][:, 0:1],
                                              scalar2=None, op0=ALU.mult),
             reads=[psk[okb], t_rinv[yb]], writes=[t_yt[yb]])
        tbk = 6 + (yb % 2)

        def fin2():
            S.op("pe", lambda e: e.transpose(bank_bf(tbk, 128), ytile[yb], ident_b), reads=[t_yt[yb]],
                 writes=[psk[tbk]])
            S.op("act", lambda e: e.activation(out=yT3[:, hidx, j * 128:(j + 1) * 128], in_=bank_bf(tbk, 128),
                                               func=AF.Copy),
                 reads=[psk[tbk]], writes=[t_yT[j]])
        return fin2

    for hidx in range(H):
        b = hidx % 2
        load_head(hidx, b)
        precast(PC3)
        isA = hidx < HA
        for j in range(NQ):
            iq = 2 * j + 1
            ob = cnt3["o"] % 2
            cnt3["o"] += 1
            if isA:
                rs = [r for r in range(5) if iq - 4 + r >= 0]
                groups = [[(iq - 4 + r, r) for r in rs]]
            else:
                hb_ = hidx - HA
                bb = cnt3["b"] % 2
                cnt3["b"] += 1
                S.op("dve", lambda e, hb_=hb_, bb=bb, j=j: e.tensor_scalar(
                    out=biasB[bb], in0=cum3[:, hb_, :], scalar1=-1.0, scalar2=base3[:, 2 * j + 2, hb_:hb_ + 1],
                    op0=ALU.mult, op1=ALU.add), writes=[t_bias[bb]])
                ks = list(range(0, iq + 1))
                groups = [[(i, i - g0) for i in ks[g0:g0 + 4]] for g0 in range(0, len(ks), 4)]
            ngr = len(groups)
            for gi, grp in enumerate(groups):
                sb = cnt3["s"] % 2
                cnt3["s"] += 1
                pb_ = cnt3["p"] % 2
                cnt3["p"] += 1
                for (i, r) in grp:
                    bkk = 2 * sb + (1 if r >= 4 else 0)
                    off = (r % 4) * 128
                    S.op("pe", lambda e, i=i, bkk=bkk, off=off, b=b, j=j: e.matmul(
                        bank(bkk, 128, off), kTh[b][:, i * 128:(i + 1) * 128], qTh[b][:, j * 128:(j + 1) * 128],
                        start=True, stop=True),
                        reads=[t_kTh[b], t_qTh[b]], writes=[psk[bkk]], signal=True)
                r0 = grp[0][1]
                r1 = grp[-1][1]
                if isA:
                    lo = r0 * 128
                    hi = min(r1 + 1, 4) * 128
                    S.op("act", lambda e, sb=sb, pb_=pb_, lo=lo, hi=hi: e.activation(
                        out=pexp[pb_][:, lo:hi], in_=ps_t[:, 2 * sb * 512 + lo:2 * sb * 512 + hi], func=AF.Exp,
                        scale=SCALE), reads=[psk[2 * sb]], writes=[t_pexp[pb_]])
                    if r1 >= 4:
                        S.op("act", lambda e, sb=sb, pb_=pb_: e.activation(
                            out=pexp[pb_][:, 512:640], in_=bank(2 * sb + 1, 128), func=AF.Exp, scale=SCALE),
                            reads=[psk[2 * sb + 1]], writes=[t_pexp[pb_]])
                    S.op("dve", lambda e, pb_=pb_, lo=lo, r1=r1, hidx=hidx: e.tensor_tensor(
                        out=pT[pb_][:, lo:(r1 + 1) * 128], in0=pexp[pb_][:, lo:(r1 + 1) * 128],
                        in1=Etab3[:, hidx, lo:(r1 + 1) * 128], op=ALU.mult),
                        reads=[t_pexp[pb_], t_E], writes=[t_pT[pb_]])
                else:
                    for (i, r) in grp:
                        S.op("act", lambda e, i=i, r=r, sb=sb, pb_=pb_, bb=bb: e.activation(
                            out=pT[pb_][:, r * 128:(r + 1) * 128], in_=bank(2 * sb, 128, r * 128), func=AF.Exp,
                            bias=biasB[bb][:, i:i + 1], scale=SCALE),
                            reads=[psk[2 * sb], t_bias[bb]], writes=[t_pT[pb_]])
                    if grp[-1][0] == iq:
                        r = grp[-1][1]
                        S.op("dve", lambda e, r=r, pb_=pb_: e.tensor_tensor(
                            out=pT[pb_][:, r * 128:(r + 1) * 128], in0=pT[pb_][:, r * 128:(r + 1) * 128],
                            in1=tri_incl_b, op=ALU.mult), reads=[t_pT[pb_]], writes=[t_pT[pb_]])

                def pv(grp=grp, gi=gi, ngr=ngr, pb_=pb_, ob=ob, b=b, hidx=hidx, j=j):
                    okb = 4 + ob
                    for (i, r) in grp:
                        first = (gi == 0 and (i, r) == grp[0])
                        last = (gi == ngr - 1 and (i, r) == grp[-1])
                        S.op("pe", lambda e, i=i, r=r, first=first, last=last: e.matmul(
                            bank(okb, 129), pT[pb_][:, r * 128:(r + 1) * 128], r3(vaug[b], VW)[:, i, 0:129],
                            start=first, stop=last),
                            reads=[t_pT[pb_], t_vaug[b]], writes=[psk[okb]], signal=((i, r) == grp[-1]))
                    if gi == ngr - 1:
                        return finalize(hidx, j, ob)
                    return None
                flush()
                pending[0] = pv
    flush()
    flush()
    if debug:
        S.dma("sp", lambda e: e.dma_start(out=yT_dbg, in_=yT_all), reads=t_yT)
    S.barrier()
    psk = new_ps()
    A.release(m3b)

    t_yT = S.toks_n(NQ, "yT")
    Wa = A.bf16(HA * D)
    Wb = A.bf16(HB * D)
    Wa3 = r3(Wa, D)
    Wb3 = r3(Wb, D)
    sgt = [A.bf16(2 * D) for _ in range(2)]
    zt = [A.bf16(D) for _ in range(2)]
    t1 = [A.f32(BW) for _ in range(2)]
    t2 = [A.f32(BW) for _ in range(2)]
    t_W = S.tok("Wab")
    t_sgt = S.toks_n(2, "sgt")
    t_zt = S.toks_n(2, "zt")
    t_t1 = S.toks_n(2, "t1")
    t_t2 = S.toks_n(2, "t2")
    for c in range(HA):
        S.dma("pool", lambda e, c=c: e.dma_start(out=Wa3[:, c, :], in_=w_ba[c * 128:(c + 1) * 128, :]), writes=[t_W])
    for c in range(HB):
        S.dma("pool", lambda e, c=c: e.dma_start(out=Wb3[:, c, :], in_=w_bb[c * 128:(c + 1) * 128, :]), writes=[t_W])
    c4 = 0
    pend4 = [None]
    for j in range(NQ):
        b = j % 2
        S.dma("sp", lambda e, b=b, j=j: e.dma_start(out=sgt[b], in_=sg_d[j * 128:(j + 1) * 128, :]),
              writes=[t_sgt[b]])
        precast((len(pc_list) - pc_pos[0] + (NQ - j) - 1) // (NQ - j))
        for n in range(NB):
            ab = (c4 % 2) * 2
            tb = c4 % 2
            c4 += 1
            for c in range(HA):
                S.op("pe", lambda e, c=c, j=j, n=n, ab=ab: e.matmul(
                    bank(ab, BW), yT3[:, c, j * 128:(j + 1) * 128], Wa3[:, c, n * BW:(n + 1) * BW],
                    start=(c == 0), stop=(c == HA - 1)),
                    reads=[t_yT[j], t_W], writes=[psk[ab]], signal=(c == HA - 1))
            for c in range(HB):
                S.op("pe", lambda e, c=c, j=j, n=n, ab=ab: e.matmul(
                    bank(ab + 1, BW), yT3[:, HA + c, j * 128:(j + 1) * 128], Wb3[:, c, n * BW:(n + 1) * BW],
                    start=(c == 0), stop=(c == HB - 1)),
                    reads=[t_yT[j], t_W], writes=[psk[ab + 1]], signal=(c == HB - 1))
            S.op("dve", lambda e, b=b, n=n, ab=ab, tb=tb: e.tensor_tensor(
                out=t1[tb], in0=bank(ab, BW), in1=sgt[b][:, n * BW:(n + 1) * BW], op=ALU.mult),
                reads=[psk[ab], t_sgt[b]], writes=[t_t1[tb]])
            S.op("dve", lambda e, b=b, n=n, ab=ab, tb=tb: e.tensor_tensor(
                out=t2[tb], in0=bank(ab + 1, BW), in1=sgt[b][:, D + n * BW:D + (n + 1) * BW], op=ALU.mult),
                reads=[psk[ab + 1], t_sgt[b]], writes=[t_t2[tb]])
            S.op("dve", lambda e, b=b, n=n, tb=tb: e.tensor_tensor(
                out=zt[b][:, n * BW:(n + 1) * BW], in0=t1[tb], in1=t2[tb], op=ALU.add),
                reads=[t_t1[tb], t_t2[tb]], writes=[t_zt[b]])
        def _trz(b=b, j=j):
            transposes_to(zt[b], t_zt[b], KC, lambda c, n, j=j: yT3[:, c:c + n, j * 128:(j + 1) * 128],
                          t_yT[j], (4, 5), True, ident_b)
        if pend4[0] is not None:
            pend4[0]()
        pend4[0] = _trz
    pend4[0]()
    S.barrier()
    psk = new_ps()
    A.release(m3b)

    t_zT = S.toks_n(NQ, "zT")
    Wo = A.bf16(KC * D)
    Wo3 = r3(Wo, D)
    wr_f = A.f32(KC * E)
    wr3 = r3(wr_f, E)
    br_b = A.f32(E)
    fnw_b = A.f32(D)
    cb = [A.f32(E) for _ in range(2)]
    xt4 = [A.f32(D)] * 2
    hh_ = [A.f32(D) for _ in range(2)]
    hnf = hh_
    hnb = [A.bf16(D)] * 2
    hnT = A.f32(KC * 128)
    hnT3 = r3(hnT, 128)
    logit = A.f32(E)
    top8 = A.f32(8)
    negm = A.f32(2)
    mask = A.f32(E)
    ex = A.f32(E)
    exm = A.f32(E)
    ssum = A.f32(2)
    dest_e = A.f32(E)
    oh = A.f32(E)
    junkE = A.f32(E)
    destf = A.f32(4)
    ss4 = [A.f32(2) for _ in range(2)]
    rstd4 = [A.f32(2) for _ in range(2)]

    t_c4 = S.tok("c4")
    t_Wo = S.tok("Wo")
    for c in range(KC):
        S.dma("pool", lambda e, c=c: e.dma_start(out=Wo3[:, c, :], in_=w_out[c * 128:(c + 1) * 128, :]), writes=[t_Wo])
    S.dma("sp", lambda e: e.dma_start(out=wr3, in_=w_router.rearrange("(kc p) c -> p kc c", p=128)), writes=[t_c4])
    S.dma("sp", lambda e: e.dma_start(out=br_b, in_=b_router.partition_broadcast(128)), writes=[t_c4])
    S.dma("sp", lambda e: e.dma_start(out=fnw_b, in_=fnw.partition_broadcast(128)), writes=[t_c4])
    S.dma("sp", lambda e: e.dma_start(out=cb[0], in_=ebase_d), writes=[t_c4])
    t_xt4 = [S.tok("xt")] * 2
    t_h = S.toks_n(2, "h")
    t_hnf = t_h
    t_hnb = [S.tok("hnb")] * 2
    t_ss4 = S.toks_n(2, "ss4")
    t_hnT = S.tok("hnT")
    t_r = S.tok("route")
    t_cb = S.toks_n(2, "cb")
    t_ga = S.tok("gate_all")
    t_di = S.tok("dest_i")
    t_hbuf = S.tok("hbuf")
    t_out = S.tok("out")
    t_GdT = S.tok("GdT")
    gate3 = r3(gate_all, 4)
    desti3 = r3(dest_i, 4)
    for j in range(NQ):
        b = j % 2
        S.dma("sp", lambda e, b=b, j=j: e.dma_start(out=xt4[b], in_=xkv[(2 * j + 1) * 128:(2 * j + 2) * 128, :]),
              writes=[t_xt4[b]])
        for n in range(NB):
            for kc in range(KC):
                S.op("pe", lambda e, kc=kc, n=n, j=j: e.matmul(
                    bank(n, BW), yT3[:, kc, j * 128:(j + 1) * 128], Wo3[:, kc, n * BW:(n + 1) * BW],
                    start=(kc == 0), stop=(kc == KC - 1)),
                    reads=[t_zT[j], t_Wo], writes=[psk[n]], signal=(kc == KC - 1))
            S.op("dve", lambda e, b=b, n=n: e.tensor_tensor(
                out=hh_[b][:, n * BW:(n + 1) * BW], in0=bank(n, BW), in1=xt4[b][:, n * BW:(n + 1) * BW], op=ALU.add),
                reads=[psk[n], t_xt4[b]], writes=[t_h[b]])
        S.dma("sp", lambda e, b=b, j=j: e.dma_start(out=out[j * 128:(j + 1) * 128, :], in_=hh_[b]),
              reads=[t_h[b]], writes=[t_out])
        rms_rstd(hh_[b], ss4[b], rstd4[b], D, [t_h[b]], t_ss4[b], hnb[b], t_hnb[b])
        S.op("dve", lambda e, b=b: e.scalar_tensor_tensor(out=hnf[b], in0=hh_[b], scalar=rstd4[b][:, 0:1],
                                                         in1=fnw_b, op0=ALU.mult, op1=ALU.mult),
             reads=[t_h[b], t_ss4[b], t_c4], writes=[t_hnf[b]])
        S.op("act", lambda e, b=b: e.activation(out=hnb[b], in_=hnf[b], func=AF.Copy),
             reads=[t_hnf[b]], writes=[t_hnb[b]])
        tb_banks = (4, 5, 6, 7)
        transposes_to(hnf[b], t_hnf[b], KC, lambda c, n: hnT3[:, c:c + n, :], t_hnT, tb_banks, False, ident_f)
        LB = 4
        for kc in range(KC):
            S.op("pe", lambda e, kc=kc: e.matmul(bank(LB, E), hnT3[:, kc, :], wr3[:, kc, :],
                                                 start=(kc == 0), stop=(kc == KC - 1)),
                 reads=[t_hnT, t_c4], writes=[psk[LB]], signal=(kc == KC - 1))
        S.op("dve", lambda e: e.tensor_tensor(out=logit, in0=bank(LB, E), in1=br_b, op=ALU.add),
             reads=[psk[LB], t_c4], writes=[t_r])
        S.op("dve", lambda e: e.max(out=top8, in_=logit), reads=[t_r], writes=[t_r])
        S.op("dve", lambda e: e.tensor_scalar(out=mask, in0=logit, scalar1=top8[:, 3:4], scalar2=None,
                                              op0=ALU.is_ge), reads=[t_r], writes=[t_r])
        S.op("dve", lambda e: e.tensor_scalar(out=negm[:, 0:1], in0=top8[:, 0:1], scalar1=-1.0, scalar2=None,
                                              op0=ALU.mult), reads=[t_r], writes=[t_r])
        S.op("act", lambda e: e.activation(out=ex, in_=logit, func=AF.Exp, bias=negm[:, 0:1], scale=1.0),
             reads=[t_r], writes=[t_r])
        S.op("dve", lambda e: e.scalar_tensor_tensor(out=exm, in0=ex, scalar=1.0, in1=mask, op0=ALU.mult, op1=ALU.mult, accum_out=ssum[:, 0:1]),
             reads=[t_r], writes=[t_r])
        S.op("dve", lambda e: e.reciprocal(out=ssum[:, 0:1], in_=ssum[:, 0:1]), reads=[t_r], writes=[t_r])
        S.op("dve", lambda e, j=j: e.tensor_scalar(out=Gd3[:, j, :], in0=exm, scalar1=ssum[:, 0:1], scalar2=None,
                                                   op0=ALU.mult),
             reads=[t_r], writes=[t_r])
        PB = 5
        S.op("pe", lambda e: e.matmul(bank(PB, E), tri_strict_f, mask, start=True, stop=True),
             reads=[t_r], writes=[psk[PB]], signal=False)
        S.op("pe", lambda e: e.matmul(bank(PB, E, E), ones_f, mask, start=True, stop=True),
             reads=[t_r], writes=[psk[PB]])
        c0_, c1_ = cb[j % 2], cb[(j + 1) % 2]
        S.op("dve", lambda e, c0_=c0_: e.tensor_tensor(out=dest_e, in0=bank(PB, E), in1=c0_, op=ALU.add),
             reads=[psk[PB], t_cb[j % 2], t_c4], writes=[t_r])
        S.op("dve", lambda e, c0_=c0_, c1_=c1_: e.tensor_tensor(out=c1_, in0=bank(PB, E, E), in1=c0_, op=ALU.add),
             reads=[psk[PB], t_cb[j % 2], t_c4], writes=[t_cb[(j + 1) % 2]])
        for k in range(4):
            S.op("dve", lambda e, k=k: e.tensor_scalar(out=oh, in0=logit, scalar1=top8[:, k:k + 1], scalar2=None,
                                                       op0=ALU.is_equal), reads=[t_r], writes=[t_r])
            S.op("dve", lambda e, k=k: e.scalar_tensor_tensor(out=junkE, in0=oh, scalar=1.0, in1=dest_e, op0=ALU.mult, op1=ALU.mult, accum_out=destf[:, k:k + 1]),
                 reads=[t_r], writes=[t_r])
            S.op("dve", lambda e, k=k, j=j: e.scalar_tensor_tensor(out=junkE, in0=oh, scalar=1.0, in1=Gd3[:, j, :], op0=ALU.mult, op1=ALU.mult, accum_out=gate3[:, j, k:k + 1]),
                 reads=[t_r], writes=[t_r, t_ga])
        S.op("dve", lambda e, j=j: e.tensor_copy(out=desti3[:, j, :], in_=destf), reads=[t_r], writes=[t_di])
        for k in range(4):
            S.dma("pool", lambda e, b=b, j=j, k=k: e.indirect_dma_start(
                out=hbuf[:, :], out_offset=bass.IndirectOffsetOnAxis(ap=desti3[:, j, k:k + 1], axis=0),
                in_=hnb[b], in_offset=None), reads=[t_hnb[b], t_di], writes=[t_hbuf])
        if debug:
            S.dma("sp", lambda e, b=b, j=j: e.dma_start(out=h_dbg[j * 128:(j + 1) * 128, :], in_=hnf[b]),
                  reads=[t_hnf[b]])
    if debug:
        S.dma("sp", lambda e: e.dma_start(out=gate_dbg, in_=gate_all), reads=[t_ga])
        S.dma("sp", lambda e: e.dma_start(out=dest_dbg, in_=dest_i), reads=[t_di])
    S.barrier()
    psk = new_ps()
    A.release(m3)

    m5 = A.mark()
    NWB = 3
    wbk = [A.bf16(KC * BW) for _ in range(NWB)]
    xe = A.bf16(CT * D)
    xe3 = r3(xe, D)
    hTe = [A.bf16(KC * C)] * 2
    hbT = [A.bf16(KCE * C)] * 2
    SW = BW // 2
    NSTG = 4
    stg = [A.f32(KC * SW) for _ in range(NSTG)]
    t_stg = S.toks_n(NSTG, "stg")
    gsb = [A.f32(HPB * C) for _ in range(2)]
    gtmp = [A.f32(C) for _ in range(2)]
    stmp = [A.f32(C) for _ in range(2)]
    utmp = [A.f32(C) for _ in range(2)]
    ystage = [A.f32(BW) for _ in range(3)]
    bgu = A.f32(E * NGU)
    bgu3 = r3(bgu, NGU)
    t_bgu = S.tok("bgu")
    S.dma("sp", lambda e: e.dma_start(out=bgu, in_=bgu_t), writes=[t_bgu])
    t_wbk = S.toks_n(NWB, "wbk")
    t_xe = S.tok("xe")
    t_hTe = [S.tok("hTe")] * 2
    t_hbT = [S.tok("hbT")] * 2
    t_gsb = S.toks_n(2, "gsb")
    t_g = S.toks_n(2, "g")
    t_s = S.toks_n(2, "s")
    t_u = S.toks_n(2, "u")
    t_ys = S.toks_n(3, "ys")
    t_ybuf = S.tok("ybuf")
    c5 = {"w": 0, "gu": 0, "dn": 0, "t": 0, "ys": 0, "tr": 0}
    GU_BANKS = (0, 1, 2, 3)
    DN_BANKS = (4, 5)
    wsrc = []
    for ex2 in range(E):
        for bidx2 in range(DE // BW):
            for part2 in range(2):
                c0_ = part2 * DE + bidx2 * BW
                wsrc.append((w_gu, wgu_bf, ex2, c0_))
        for n2 in range(NB):
            wsrc.append((w_dn, wdn_bf, ex2, n2 * BW))
    wst = {"issued": 0, "half": 0}

    def ensure_weights(upto):
        while wst["issued"] <= min(upto, len(wsrc) - 1):
            i_ = wst["issued"]
            wst["issued"] += 1
            wsrc_f, wsrc_b, ex2, c0_ = wsrc[i_]
            wi2 = i_ % NWB
            if ex2 >= E - NP:
                S.dma("sp", lambda e, wi2=wi2, ex2=ex2, c0_=c0_, wsrc_b=wsrc_b: e.dma_start(
                    out=r3(wbk[wi2], BW),
                    in_=wsrc_b[ex2 - (E - NP), :, c0_:c0_ + BW].rearrange("(kc p) c -> p kc c", p=128)),
                    writes=[t_wbk[wi2]])
                continue
            for h2 in range(2):
                hc = wst["half"]
                wst["half"] += 1
                si = hc % NSTG
                q = "sp" if hc % 2 == 0 else "act"
                S.dma(q, lambda e, si=si, ex2=ex2, c0_=c0_, h2=h2, wsrc_f=wsrc_f: e.dma_start(
                    out=r3(stg[si], SW),
                    in_=wsrc_f[ex2, :, c0_ + h2 * SW:c0_ + (h2 + 1) * SW].rearrange("(kc p) c -> p kc c", p=128)),
                    writes=[t_stg[si]])
                dst = r3(wbk[wi2], BW)[:, :, h2 * SW:(h2 + 1) * SW]
                if hc % 3 == 2:
                    S.op("dve", lambda e, si=si, dst=dst: e.tensor_copy(out=dst, in_=r3(stg[si], SW)),
                         reads=[t_stg[si]], writes=[t_wbk[wi2]])
                else:
                    S.op("act", lambda e, si=si, dst=dst: e.activation(out=dst, in_=r3(stg[si], SW), func=AF.Copy),
                         reads=[t_stg[si]], writes=[t_wbk[wi2]])

    widx = [0]
    for ex_ in range(E):
        eb = ex_ % 2
        nfull = C // 128
        if nfull:
            S.dma("sp", lambda e, ex_=ex_: e.dma_start(
                out=xe3[:, 0:nfull, :],
                in_=hbuf[ex_ * C:ex_ * C + nfull * 128, :].rearrange("(s p) d -> p s d", p=128)), writes=[t_xe])
        if C % 128:
            S.dma("sp", lambda e, ex_=ex_: e.dma_start(
                out=xe3[0:C % 128, nfull, :], in_=hbuf[ex_ * C + nfull * 128:(ex_ + 1) * C, :]), writes=[t_xe])
        he3 = r3(hTe[eb], C)
        for s_ in range(CT):
            np_ = min(128, C - s_ * 128)
            transposes_to(xe3[:, s_, :], t_xe, KC,
                          lambda c, n, s_=s_, he3=he3, np_=np_: he3[:, c:c + n, s_ * 128:s_ * 128 + np_],
                          t_hTe[eb], (6, 7), True, ident_b, npart=np_)
        hb3 = r3(hbT[eb], C)
        nblk = DE // BW
        for bidx in range(nblk):
            gsi = c5["t"] % 2
            c5["t"] += 1
            gs3 = r3(gsb[gsi], C)
            for part in range(2):
                wi = widx[0] % NWB
                ensure_weights(widx[0] + 2)
                widx[0] += 1
                for m in range(HPB):
                    gb_ = GU_BANKS[c5["gu"] % 4]
                    c5["gu"] += 1
                    for kc in range(KC):
                        S.op("pe", lambda e, kc=kc, wi=wi, m=m, gb_=gb_, he3=he3: e.matmul(
                            bank(gb_, C), r3(wbk[wi], BW)[:, kc, m * 128:(m + 1) * 128], he3[:, kc, :],
                            start=(kc == 0), stop=(kc == KC - 1)),
                            reads=[t_wbk[wi], t_hTe[eb]], writes=[psk[gb_]], signal=(kc == KC - 1))
                    chunk = bidx * HPB + m
                    ti = (c5["gu"]) % 2
                    if part == 0:
                        bcol = bgu3[:, ex_, chunk:chunk + 1]
                        S.op("dve", lambda e, gb_=gb_, ti=ti, bcol=bcol: e.tensor_scalar(
                            out=gtmp[ti], in0=bank(gb_, C), scalar1=bcol, scalar2=SWIGLU_LIMIT, op0=ALU.add,
                            op1=ALU.min), reads=[psk[gb_], t_bgu], writes=[t_g[ti]])
                        S.op("act", lambda e, ti=ti: e.activation(out=stmp[ti], in_=gtmp[ti], func=AF.Sigmoid,
                                                                  scale=SWIGLU_ALPHA),
                             reads=[t_g[ti]], writes=[t_s[ti]])
                        S.op("dve", lambda e, ti=ti, m=m, gs3=gs3: e.tensor_tensor(
                            out=gs3[:, m, :], in0=gtmp[ti], in1=stmp[ti], op=ALU.mult),
                            reads=[t_g[ti], t_s[ti]], writes=[t_gsb[gsi]])
                    else:
                        bcol = bgu3[:, ex_, KCE + chunk:KCE + chunk + 1]
                        S.op("dve", lambda e, gb_=gb_, ti=ti, bcol=bcol: e.tensor_scalar(
                            out=utmp[ti], in0=bank(gb_, C), scalar1=bcol, scalar2=SWIGLU_LIMIT, op0=ALU.add,
                            op1=ALU.min), reads=[psk[gb_], t_bgu], writes=[t_u[ti]])
                        S.op("dve", lambda e, ti=ti: e.tensor_scalar(
                            out=utmp[ti], in0=utmp[ti], scalar1=-SWIGLU_LIMIT, scalar2=1.0, op0=ALU.max,
                            op1=ALU.add), reads=[t_u[ti]], writes=[t_u[ti]])
                        S.op("dve", lambda e, ti=ti, m=m, gs3=gs3, chunk=chunk, hb3=hb3: e.tensor_tensor(
                            out=hb3[:, chunk, :], in0=utmp[ti], in1=gs3[:, m, :], op=ALU.mult),
                            reads=[t_u[ti], t_gsb[gsi]], writes=[t_hbT[eb]])
        for n in range(NB):
            wi = widx[0] % NWB
            ensure_weights(widx[0] + 2)
            widx[0] += 1
            for s_ in range(CT):
                db = DN_BANKS[c5["dn"] % 2]
                c5["dn"] += 1
                np_ = min(128, C - s_ * 128)
                for kc in range(KCE):
                    S.op("pe", lambda e, kc=kc, wi=wi, s_=s_, db=db, hb3=hb3, np_=np_: e.matmul(
                        bank(db, BW)[0:np_, :], hb3[:, kc, s_ * 128:s_ * 128 + np_], r3(wbk[wi], BW)[:, kc, :],
                        start=(kc == 0), stop=(kc == KCE - 1)),
                        reads=[t_wbk[wi], t_hbT[eb]], writes=[psk[db]], signal=(kc == KCE - 1))
                yi = c5["ys"] % 3
                c5["ys"] += 1
                S.op("act", lambda e, yi=yi, db=db, np_=np_: e.activation(
                    out=ystage[yi][0:np_, :], in_=bank(db, BW)[0:np_, :], func=AF.Copy),
                     reads=[psk[db]], writes=[t_ys[yi]])
                S.dma("act", lambda e, yi=yi, ex_=ex_, s_=s_, n=n, np_=np_: e.dma_start(
                    out=ybuf[ex_ * C + s_ * 128:ex_ * C + s_ * 128 + np_, n * BW:(n + 1) * BW],
                    in_=ystage[yi][0:np_, :]),
                    reads=[t_ys[yi]], writes=[t_ybuf])
    S.barrier()
    psk = new_ps()
    A.release(m5)

    hp = [A.f32(D) for _ in range(2)]
    yk = [[A.f32(D) for _ in range(4)] for _ in range(2)]
    bdn = A.f32(D)
    GdT = A.f32(128)
    t_bdn = S.tok("bdn")
    t_GdT = S.tok("GdT")
    S.dma("sp", lambda e: e.dma_start(out=bdn[0:E, :], in_=b_dn), writes=[t_bdn])
    t_hp = S.toks_n(2, "hp")
    t_yk = [S.toks_n(4, "yk%d" % i) for i in range(2)]
    t_out = S.tok("out")
    for j in range(NQ):
        b = j % 2
        S.dma("sp", lambda e, b=b, j=j: e.dma_start(out=hp[b], in_=out[j * 128:(j + 1) * 128, :]), writes=[t_hp[b]])
        GB = 6
        S.op("pe", lambda e, j=j: e.transpose(bank(GB, 128)[0:E, :], Gd3[:, j, :], ident_f), writes=[psk[GB]])
        S.op("act", lambda e: e.activation(out=GdT[0:E, :], in_=bank(GB, 128)[0:E, :], func=AF.Copy),
             reads=[psk[GB]], writes=[t_GdT])
        for n in range(NB):
            S.op("pe", lambda e, n=n: e.matmul(bank(n, BW), GdT[0:E, :], bdn[0:E, n * BW:(n + 1) * BW],
                                               start=True, stop=True),
                 reads=[t_GdT, t_bdn], writes=[psk[n]])
            S.op("dve", lambda e, n=n, b=b: e.tensor_tensor(
                out=hp[b][:, n * BW:(n + 1) * BW], in0=bank(n, BW), in1=hp[b][:, n * BW:(n + 1) * BW], op=ALU.add),
                reads=[psk[n], t_hp[b]], writes=[t_hp[b]])
        for k in range(4):
            S.dma("pool", lambda e, b=b, j=j, k=k: e.indirect_dma_start(
                out=yk[b][k], out_offset=None, in_=ybuf[:, :],
                in_offset=bass.IndirectOffsetOnAxis(ap=desti3[:, j, k:k + 1], axis=0)), writes=[t_yk[b][k]])
            S.op("dve", lambda e, b=b, j=j, k=k: e.scalar_tensor_tensor(
                out=hp[b], in0=yk[b][k], scalar=gate3[:, j, k:k + 1], in1=hp[b], op0=ALU.mult, op1=ALU.add),
                reads=[t_yk[b][k], t_hp[b]], writes=[t_hp[b]])
        S.dma("sp", lambda e, b=b, j=j: e.dma_start(out=out[j * 128:(j + 1) * 128, :], in_=hp[b]),
              reads=[t_hp[b]], writes=[t_out])
    S.barrier()

    blk = st.enter_context(nc.Block())

    @blk.tensor
    def _(e):
        for f in S.ops["pe"]:
            f(e)

    @blk.scalar
    def _(e):
        for f in S.ops["act"]:
            f(e)

    @blk.vector
    def _(e):
        for f in S.ops["dve"]:
            f(e)

    @blk.gpsimd
    def _(e):
        for f in S.ops["pool"]:
            f(e)

    @blk.sync
    def _(e):
        for f in S.ops["sp"]:
            f(e)

    st.close()
    return nc


def make_consts(cfg):
    t = np.arange(128)
    ident = np.eye(128, dtype=np.float32)
    tri_incl = (t[:, None] <= t[None, :]).astype(np.float32)
    tri_strict = (t[:, None] < t[None, :]).astype(np.float32)
    ones = np.ones((128, 128), np.float32)
    cst = np.concatenate([ident, tri_incl, tri_strict, ones], axis=1)
    ebase = np.tile((np.arange(cfg.E) * cfg.C).astype(np.float32)[None, :], (128, 1))
    return cst, ebase


def make_btab(rel_bias, cfg):
    HA = cfg.HA
    kk = np.arange(128)[:, None, None]
    r = np.arange(5)[None, :, None]
    qi = np.arange(128)[None, None, :]
    dist = (4 - r) * 128 + qi - kk
    qo = qi % 64
    ok = (dist >= -(63 - qo)) & (dist <= qo + 512)
    idx = np.clip(dist, -63, 256) + 63
    tab = rel_bias[:, idx]
    tab = np.where(ok[None], tab, np.float32(NEG)).astype(np.float32)
    return np.ascontiguousarray(tab.transpose(1, 0, 2, 3).reshape(128, HA, 640))


def prepare(cfg, inp):
    D, NS, E = cfg.D, cfg.NS, cfg.E
    x = np.asarray(inp["x"], np.float32)
    B = x.shape[0]
    cst, ebase = make_consts(cfg)
    btab = make_btab(np.asarray(inp["rel_bias"], np.float32), cfg)
    qkn = np.concatenate([np.tile(np.asarray(inp[k], np.float32), cfg.HPB)
                          for k in ("qn_a", "kn_a", "qn_b", "kn_b")])
    NGU = 2 * D // 128
    bgu_t = np.ascontiguousarray(
        np.asarray(inp["b_gate_up"], np.float32).reshape(E, NGU, 128).transpose(2, 0, 1).reshape(128, E * NGU))
    shared = {
        "w_in": np.asarray(inp["w_in"], np.float32),
        "attn_norm_w": np.asarray(inp["attn_norm_w"], np.float32),
        "ffn_norm_w": np.asarray(inp["ffn_norm_w"], np.float32),
        "qkn": qkn,
        "b_forget": np.asarray(inp["b_forget"], np.float32),
        "btab": btab,
        "w_branch_a": np.asarray(inp["w_branch_a"], np.float32),
        "w_branch_b": np.asarray(inp["w_branch_b"], np.float32),
        "w_out": np.asarray(inp["w_out"], np.float32),
        "w_router": np.asarray(inp["w_router"], np.float32),
        "b_router": np.asarray(inp["b_router"], np.float32),
        "w_gate_up": np.asarray(inp["w_gate_up"], np.float32),
        "bgu_t": bgu_t,
        "w_down": np.asarray(inp["w_down"], np.float32),
        "b_down": np.asarray(inp["b_down"], np.float32),
        "cst": cst,
        "ebase": ebase,
    }
    in_maps = []
    for c in range(2 * B):
        b, p = c // 2, c % 2
        if p == 0:
            xk = np.concatenate([np.zeros((128, D), np.float32), x[b, :(NS - 1) * 128]], axis=0)
            valid = np.ones((128, NS), np.float32)
            valid[:, 0] = 0.0
        else:
            xk = x[b, :NS * 128]
            valid = np.ones((128, NS), np.float32)
        m = dict(shared)
        m["xkv"] = np.ascontiguousarray(xk)
        m["valid"] = valid
        in_maps.append(m)
    return in_maps


def assemble(cfg, results, B):
    D, NS, NQ = cfg.D, cfg.NS, cfg.NQ
    y = np.zeros((B, NS * 128, D), np.float32)
    for c in range(2 * B):
        b, p = c // 2, c % 2
        o = np.asarray(results[c]["out"]).reshape(NQ, 128, D)
        yv = y[b].reshape(NS, 128, D)
        for j in range(NQ):
            yv[2 * j + p] = o[j]
    return y


_CACHE = {}


def kernel(**inputs):
    cfg = Cfg()
    if "nc" not in _CACHE:
        _CACHE["nc"] = build(cfg)
    nc = _CACHE["nc"]
    in_maps = prepare(cfg, inputs)
    res = run_bass_kernel_spmd(nc, in_maps, core_ids=list(range(cfg.n_cores)))
    return assemble(cfg, res.results, 4)
```

```python
import numpy as np
from contextlib import ExitStack
import concourse.bass as bass
import concourse.mybir as mybir
from concourse.alu_op_type import AluOpType as ALU
from concourse.bass_utils import run_bass_kernel_spmd

F32 = mybir.dt.float32
BF16 = mybir.dt.bfloat16
I32 = mybir.dt.int32
AF = mybir.ActivationFunctionType
AX = mybir.AxisListType

NORM_EPS = 1e-5
SWIGLU_LIMIT = 7.0
SWIGLU_ALPHA = 1.702
NEG = -30000.0


class Cfg:
    def __init__(self, D=2048, HA=8, HB=8, NS=32, E=32, C=320, G=16, n_cores=8, NP=0):
        self.D = D
        self.KC = D // 128
        self.HA = HA
        self.HB = HB
        self.H = HA + HB
        self.NS = NS
        self.NQ = NS // 2
        self.E = E
        self.C = C
        self.CT = (C + 127) // 128
        self.NP = NP
        self.G = G
        self.WA = HA * 128
        self.WB = HB * 128
        self.BW = min(512, self.WA, self.WB, D)
        self.HPB = self.BW // 128
        self.NB = D // self.BW
        self.INC = 3 * self.WA + 3 * self.WB + HB + 2 * D
        self.n_cores = n_cores


class Tok:
    __slots__ = ("w", "r", "name")

    def __init__(self, name=""):
        self.w = None
        self.r = []
        self.name = name


ENGS = ("pe", "act", "dve", "pool", "sp")
NDMA = 8


class Sched:
    def __init__(self, nc, st):
        self.nc = nc
        self.ops = {e: [] for e in ENGS}
        self.seq = {e: 0 for e in ENGS}
        self.sem = {e: st.enter_context(nc.semaphore("s_" + e)) for e in ENGS}
        self.semid = {}
        self.waited = {e: {} for e in ENGS}
        self.dsem = {}
        for q in ("sp", "act", "pool", "poolpc"):
            self.dsem[q] = [[st.enter_context(nc.semaphore("d_%s%d" % (q, i))), 0] for i in range(NDMA)]
        self.drr = {q: 0 for q in self.dsem}
        self.unsig = {e: False for e in ENGS}
        self.toks = []

    def tok(self, name=""):
        t = Tok(name)
        self.toks.append(t)
        return t

    def toks_n(self, n, name=""):
        return [self.tok(name + str(i)) for i in range(n)]

    def _key(self, sem):
        return id(sem)

    def _need(self, eng, deps):
        for (sem, val, src) in deps:
            if src == "pe" and eng == "pe":
                continue
            k = self._key(sem)
            if self.waited[eng].get(k, 0) < val:
                self.waited[eng][k] = val
                self.ops[eng].append(lambda e, sem=sem, val=val: e.wait_ge(sem, val))

    def _deps(self, reads, writes):
        deps = []
        for b in reads:
            if b.w is not None:
                deps.append(b.w)
        for b in writes:
            if b.w is not None:
                deps.append(b.w)
            deps.extend(b.r)
        return deps

    def _mark(self, reads, writes, t):
        for b in reads:
            b.r.append(t)
        for b in writes:
            b.w = t
            b.r = []

    def op(self, eng, fn, reads=(), writes=(), signal=True):
        self._need(eng, self._deps(reads, writes))
        sem = self.sem[eng]
        if signal:
            self.seq[eng] += 1
            t = (sem, self.seq[eng], eng)
            self.ops[eng].append(lambda e, fn=fn, sem=sem: fn(e).then_inc(sem, 1))
            self.unsig[eng] = False
        else:
            t = (sem, self.seq[eng] + 1, eng)
            self.ops[eng].append(lambda e, fn=fn: fn(e))
            self.unsig[eng] = True
        self._mark(reads, writes, t)

    def dma(self, q, fn, reads=(), writes=(), key=None):
        key = key or q
        self._need(q, self._deps(reads, writes))
        slot = self.dsem[key][self.drr[key] % NDMA]
        self.drr[key] += 1
        sem, uses = slot
        if uses > 0:
            self._need(q, [(sem, 16 * uses, "dma")])
        slot[1] = uses + 1
        t = (sem, 16 * (uses + 1), "dma")
        self.ops[q].append(lambda e, fn=fn, sem=sem: fn(e).then_inc(sem, 16))
        self._mark(reads, writes, t)

    def barrier(self):
        for e in ENGS:
            assert not self.unsig[e], e
        deps = []
        for e in ENGS:
            if self.seq[e] > 0:
                deps.append((self.sem[e], self.seq[e], "x"))
        for q in self.dsem:
            for sem, uses in self.dsem[q]:
                if uses > 0:
                    deps.append((sem, 16 * uses, "dma"))
        for e in ENGS:
            self._need(e, deps)
        for t in self.toks:
            t.w = None
            t.r = []
        self.toks = []


class Arena:
    def __init__(self, ap, nfloats):
        self.ap = ap
        self.n = nfloats
        self.off = 0

    def mark(self):
        return self.off

    def release(self, m):
        self.off = m

    def f32(self, n):
        n = (n + 1) // 2 * 2
        assert self.off + n <= self.n, ("arena overflow", self.off, n, self.n)
        a = self.ap[:, self.off:self.off + n]
        self.off += n
        return a

    def bf16(self, n):
        n = (n + 3) // 4 * 4
        return self.f32(n // 2).bitcast(BF16)

    def i32(self, n):
        return self.f32(n).bitcast(I32)


def r3(ap, b):
    return ap.rearrange("p (a b) -> p a b", b=b)


def build(cfg, debug=False):
    D, KC, HA, HB, H, NS, NQ, E, C, CT, G = (cfg.D, cfg.KC, cfg.HA, cfg.HB, cfg.H, cfg.NS, cfg.NQ,
                                             cfg.E, cfg.C, cfg.CT, cfg.G)
    WA, WB, BW, HPB, NB, INC = cfg.WA, cfg.WB, cfg.BW, cfg.HPB, cfg.NB, cfg.INC
    DE = D
    KCE = DE // 128
    NGU = 2 * DE // 128
    SCALE = 128.0 ** -0.5
    nc = bass.Bass("TRN2", target_bir_lowering=False)

    def din(name, shape, dt=F32):
        return nc.dram_tensor(name, list(shape), dt, kind="ExternalInput").ap()

    def dscr(name, shape, dt):
        kind = "ExternalOutput" if debug else "Internal"
        return nc.dram_tensor(name, list(shape), dt, kind=kind).ap()

    xkv = din("xkv", [NS * 128, D])
    valid_d = din("valid", [128, NS])
    w_in = din("w_in", [D, INC])
    anw = din("attn_norm_w", [D])
    fnw = din("ffn_norm_w", [D])
    qkn = din("qkn", [4 * BW])
    b_forget = din("b_forget", [HB])
    btab = din("btab", [128, HA, 5 * 128])
    w_ba = din("w_branch_a", [WA, D])
    w_bb = din("w_branch_b", [WB, D])
    w_out = din("w_out", [D, D])
    w_router = din("w_router", [D, E])
    b_router = din("b_router", [E])
    w_gu = din("w_gate_up", [E, D, 2 * DE])
    bgu_t = din("bgu_t", [128, E * NGU])
    w_dn = din("w_down", [E, DE, D])
    b_dn = din("b_down", [E, D])
    cst = din("cst", [128, 4 * 128])
    ebase_d = din("ebase", [128, E])
    out = nc.dram_tensor("out", [NQ * 128, D], F32, kind="ExternalOutput").ap()

    kT_d = dscr("kT_d", [H, 128, NS * 128], BF16)
    qT_d = dscr("qT_d", [H, 128, NQ * 128], BF16)
    V_d = dscr("V_d", [NS * 128, WA + WB], BF16)
    sg_d = dscr("sg_d", [NQ * 128, 2 * D], BF16)
    hbuf = dscr("hbuf", [E * C + 128, D], BF16)
    ybuf = dscr("ybuf", [E * C + 128, D], F32)
    NP = cfg.NP
    wgu_bf = nc.dram_tensor("wgu_bf", [max(NP, 1), D, 2 * DE], BF16, kind="Internal").ap()
    wdn_bf = nc.dram_tensor("wdn_bf", [max(NP, 1), DE, D], BF16, kind="Internal").ap()
    if debug:
        yT_dbg = dscr("yT_dbg", [128, H * NQ * 128], BF16)
        h_dbg = dscr("h_dbg", [NQ * 128, D], F32)
        gate_dbg = dscr("gate_dbg", [128, NQ * 4], F32)
        dest_dbg = dscr("dest_dbg", [128, NQ * 4], I32)
        cum_dbg = dscr("cum_dbg", [128, HB * NS], F32)
        xnT_dbg = dscr("xnT_dbg", [128, KC * G * 128], BF16)

    st = ExitStack()
    ARENA_F = 47000
    arena_t = st.enter_context(nc.sbuf_tensor("arena", [128, ARENA_F], F32))
    ps_t = st.enter_context(nc.psum_tensor("ps", [128, 4096], F32))
    S = Sched(nc, st)
    A = Arena(arena_t, ARENA_F)

    def bank(i, n=512, off=0):
        return ps_t[:, i * 512 + off:i * 512 + off + n]

    def bank_bf(i, n=1024, off=0):
        return ps_t[:, i * 512:(i + 1) * 512].bitcast(BF16)[:, off:off + n]

    psk = S.toks_n(8, "ps")

    pc_list = []
    for ep in range(NP):
        e_ = E - NP + ep
        cw = min(2048, 2 * DE)
        for q4 in range(4):
            r0, r1 = q4 * D // 4, (q4 + 1) * D // 4
            pc_list.append((wgu_bf[ep, r0:r1, :].rearrange("r (a b) -> r a b", b=cw),
                            w_gu[e_, r0:r1, :].rearrange("r (a b) -> r a b", b=cw)))
        cw = min(2048, D)
        for q2 in range(2):
            r0, r1 = q2 * DE // 2, (q2 + 1) * DE // 2
            pc_list.append((wdn_bf[ep, r0:r1, :].rearrange("r (a b) -> r a b", b=cw),
                            w_dn[e_, r0:r1, :].rearrange("r (a b) -> r a b", b=cw)))
    pc_pos = [0]

    def precast(n):
        for _ in range(n):
            if pc_pos[0] >= len(pc_list):
                return
            o_, i_ = pc_list[pc_pos[0]]
            pc_pos[0] += 1
            S.dma("pool", lambda e, o_=o_, i_=i_: e.dma_start(out=o_, in_=i_), key="poolpc")

    def new_ps():
        return S.toks_n(8, "ps")

    ident_f = A.f32(128)
    tri_incl_f = A.f32(128)
    tri_strict_f = A.f32(128)
    ones_f = A.f32(128)
    ident_b = A.bf16(128)
    tri_incl_b = A.bf16(128)
    valid_s = A.f32(NS)
    lf = A.f32(NS * HB)
    cum = A.f32(HB * NS)
    base = A.f32((NS + 1) * HB)
    gate_all = A.f32(NQ * 4)
    dest_i = A.i32(NQ * 4)
    Gd_all = A.f32(NQ * E)
    Gd3 = r3(Gd_all, E)
    tk_const = S.tok("const")

    cst_tmp = [ident_f, tri_incl_f, tri_strict_f, ones_f]
    for i, a in enumerate(cst_tmp):
        S.dma("sp", lambda e, a=a, i=i: e.dma_start(out=a, in_=cst[:, i * 128:(i + 1) * 128]), writes=[tk_const])
    S.dma("sp", lambda e: e.dma_start(out=valid_s, in_=valid_d), writes=[tk_const])
    S.op("dve", lambda e: e.tensor_copy(out=ident_b, in_=ident_f), reads=[tk_const], writes=[tk_const])
    S.op("dve", lambda e: e.tensor_copy(out=tri_incl_b, in_=tri_incl_f), reads=[tk_const], writes=[tk_const])
    S.barrier()
    psk = new_ps()

    m1 = A.mark()
    anw_b = A.f32(D)
    qkn_b = A.f32(4 * BW)
    bf_b = A.f32(HB)
    wf = A.bf16(KC * HB)
    xnT = A.bf16(KC * G * 128)
    xnT3 = r3(xnT, G * 128)
    xt = [A.f32(D) for _ in range(2)]
    xn = [A.bf16(D) for _ in range(2)]
    wblk = [A.bf16(KC * BW) for _ in range(2)]
    qf = [A.f32(BW) for _ in range(2)]
    qn = [A.bf16(BW) for _ in range(2)]
    stageT = A.bf16(HPB * G * 128)
    stageT3 = r3(stageT, G * 128)
    stageV = [A.bf16(BW) for _ in range(2)]
    ss = [A.f32(2) for _ in range(2)]
    rstd = [A.f32(2) for _ in range(2)]
    ssq = [A.f32(HPB) for _ in range(2)]
    rq = [A.f32(HPB) for _ in range(2)]
    f_all = A.f32(NS * HB)
    f_all3 = r3(f_all, HB)

    t_c1 = S.tok("c1")
    S.dma("sp", lambda e: e.dma_start(out=anw_b, in_=anw.partition_broadcast(128)), writes=[t_c1])
    S.dma("sp", lambda e: e.dma_start(out=qkn_b, in_=qkn.partition_broadcast(128)), writes=[t_c1])
    S.dma("sp", lambda e: e.dma_start(out=bf_b, in_=b_forget.partition_broadcast(128)), writes=[t_c1])
    fcol = 3 * WA + 3 * WB
    S.dma("pool", lambda e: e.dma_start(out=r3(wf, HB),
                                        in_=w_in[:, fcol:fcol + HB].rearrange("(kc p) c -> p kc c", p=128)),
          writes=[t_c1])

    t_xt = S.toks_n(2, "xt")
    t_xn = S.toks_n(2, "xn")
    t_ss = S.toks_n(2, "ss")
    t_xnT = S.toks_n(G, "xnT")
    t_wblk = S.toks_n(2, "wblk")
    t_qf = S.toks_n(2, "qf")
    t_qn = S.toks_n(2, "qn")
    t_sq = S.toks_n(2, "sq")
    t_stT = S.tok("stT")
    t_stV = S.toks_n(2, "stV")
    t_fall = S.tok("fall")
    t_dram1 = S.tok("dram1")

    blocks = []
    for hb_ in range(WA // BW):
        blocks.append(("q", 0 * WA + hb_ * BW, (0, hb_ * HPB)))
    for hb_ in range(WA // BW):
        blocks.append(("k", 1 * WA + hb_ * BW, (1, hb_ * HPB)))
    for hb_ in range(WA // BW):
        blocks.append(("v", 2 * WA + hb_ * BW, hb_ * BW))
    for hb_ in range(WB // BW):
        blocks.append(("q", 3 * WA + hb_ * BW, (2, HA + hb_ * HPB)))
    for hb_ in range(WB // BW):
        blocks.append(("k", 3 * WA + WB + hb_ * BW, (3, HA + hb_ * HPB)))
    for hb_ in range(WB // BW):
        blocks.append(("v", 3 * WA + 2 * WB + hb_ * BW, WA + hb_ * BW))
    gcol = 3 * WA + 3 * WB + HB
    for nb_ in range(2 * D // BW):
        blocks.append(("g", gcol + nb_ * BW, nb_ * BW))

    cnt = {"x": 0, "w": 0, "ps": 0, "q": 0, "sv": 0}
    PROJ_BANKS = (2, 3, 4)

    def rms_rstd(eng_src_ap, ss_ap, rstd_ap, n, toks_r, tok_ss, junk_ap, tok_junk):
        S.op("dve", lambda e: e.scalar_tensor_tensor(out=junk_ap, in0=eng_src_ap, scalar=1.0, in1=eng_src_ap, op0=ALU.mult, op1=ALU.mult, accum_out=ss_ap[:, 0:1]),
             reads=toks_r, writes=[tok_ss, tok_junk])
        S.op("dve", lambda e: e.tensor_scalar(out=ss_ap[:, 0:1], in0=ss_ap[:, 0:1], scalar1=1.0 / n,
                                              scalar2=NORM_EPS, op0=ALU.mult, op1=ALU.add),
             reads=[tok_ss], writes=[tok_ss])
        S.op("act", lambda e: e.activation(out=ss_ap[:, 0:1], in_=ss_ap[:, 0:1], func=AF.Sqrt),
             reads=[tok_ss], writes=[tok_ss])
        S.op("dve", lambda e: e.reciprocal(out=rstd_ap[:, 0:1], in_=ss_ap[:, 0:1]),
             reads=[tok_ss], writes=[tok_ss])

    def transposes_to(src_ap, src_tok, ncol_chunks, dst_fn, dst_tok, banks, dt_bf=True, ident=None, npart=128):
        per = 8 if dt_bf else 4
        c = 0
        bi = 0
        while c < ncol_chunks:
            n = min(per, ncol_chunks - c)
            bk = banks[bi % len(banks)]
            bi += 1
            for k in range(n):
                if dt_bf:
                    o = bank_bf(bk, 128, k * 128)
                else:
                    o = bank(bk, 128, k * 128)
                S.op("pe", lambda e, o=o, k=k, c=c: e.transpose(
                    o[:, 0:npart], src_ap[0:npart, (c + k) * 128:(c + k + 1) * 128], ident[0:npart, 0:npart]),
                     reads=[src_tok], writes=[psk[bk]], signal=(k == n - 1))
            if dt_bf:
                srcp = r3(bank_bf(bk, n * 128), 128)[:, :, 0:npart]
            else:
                srcp = r3(bank(bk, n * 128), 128)[:, :, 0:npart]
            S.op("act", lambda e, srcp=srcp, c=c, n=n: e.activation(out=dst_fn(c, n), in_=srcp, func=AF.Copy),
                 reads=[psk[bk]], writes=[dst_tok])
            c += n

    ngroups = NS // G
    pend1 = [None]
    npc = len(pc_list)
    nblk1 = len(blocks) * ngroups
    PC1 = max(1, (npc * 5 // 9 + nblk1 - 1) // nblk1) if npc else 0
    for g in range(ngroups):
        for tt in range(G):
            t = g * G + tt
            b = cnt["x"] % 2
            cnt["x"] += 1
            S.dma("sp", lambda e, b=b, t=t: e.dma_start(out=xt[b], in_=xkv[t * 128:(t + 1) * 128, :]),
                  writes=[t_xt[b]])
            rms_rstd(xt[b], ss[b], rstd[b], D, [t_xt[b]], t_ss[b], xn[b], t_xn[b])
            S.op("dve", lambda e, b=b: e.scalar_tensor_tensor(out=xn[b], in0=xt[b], scalar=rstd[b][:, 0:1],
                                                             in1=anw_b, op0=ALU.mult, op1=ALU.mult),
                 reads=[t_xt[b], t_ss[b], t_c1], writes=[t_xn[b]])
            transposes_to(xn[b], t_xn[b], KC,
                          lambda c, n, tt=tt: xnT3[:, c:c + n, tt * 128:(tt + 1) * 128],
                          t_xnT[tt], (0, 1), True, ident_b)
            for kc in range(KC):
                S.op("pe", lambda e, kc=kc, tt=tt: e.matmul(bank(7, HB), xnT3[:, kc, tt * 128:(tt + 1) * 128],
                                                            r3(wf, HB)[:, kc, :], start=(kc == 0), stop=(kc == KC - 1)),
                     reads=[t_xnT[tt], t_c1], writes=[psk[7]], signal=(kc == KC - 1))
            S.op("dve", lambda e, t=t: e.tensor_tensor(out=f_all3[:, t, :], in0=bank(7, HB), in1=bf_b, op=ALU.add),
                 reads=[psk[7], t_c1], writes=[t_fall])
        if debug and g == 0:
            S.dma("sp", lambda e: e.dma_start(out=xnT_dbg, in_=xnT), reads=t_xnT)
        for (kind, c0, meta) in blocks:
            wb = cnt["w"] % 2
            cnt["w"] += 1
            S.dma("pool", lambda e, wb=wb, c0=c0: e.dma_start(
                out=r3(wblk[wb], BW), in_=w_in[:, c0:c0 + BW].rearrange("(kc p) c -> p kc c", p=128)),
                writes=[t_wblk[wb]])
            precast(PC1)
            tiles = range(G) if kind in ("k", "v") else range(1, G, 2)
            for tt in tiles:
                t = g * G + tt
                j = (t - 1) // 2
                pb = PROJ_BANKS[cnt["ps"] % 3]
                cnt["ps"] += 1
                for kc in range(KC):
                    S.op("pe", lambda e, kc=kc, tt=tt, wb=wb, pb=pb: e.matmul(
                        bank(pb, BW), xnT3[:, kc, tt * 128:(tt + 1) * 128], r3(wblk[wb], BW)[:, kc, :],
                        start=(kc == 0), stop=(kc == KC - 1)),
                        reads=[t_xnT[tt], t_wblk[wb]], writes=[psk[pb]], signal=(kc == KC - 1))
                if kind in ("q", "k"):
                    row, h0 = meta
                    qb = cnt["q"] % 2
                    cnt["q"] += 1
                    S.op("act", lambda e, qb=qb, pb=pb: e.activation(out=qf[qb], in_=bank(pb, BW), func=AF.Copy),
                         reads=[psk[pb]], writes=[t_qf[qb]])
                    for hh in range(HPB):
                        S.op("dve", lambda e, qb=qb, hh=hh: e.scalar_tensor_tensor(out=qn[qb][:, hh * 128:(hh + 1) * 128], in0=qf[qb][:, hh * 128:(hh + 1) * 128], scalar=1.0, in1=qf[qb][:, hh * 128:(hh + 1) * 128], op0=ALU.mult, op1=ALU.mult, accum_out=ssq[qb][:, hh:hh + 1]),
                            reads=[t_qf[qb]], writes=[t_sq[qb], t_qn[qb]])
                    S.op("dve", lambda e, qb=qb: e.tensor_scalar(out=ssq[qb], in0=ssq[qb], scalar1=1.0 / 128,
                                                                 scalar2=NORM_EPS, op0=ALU.mult, op1=ALU.add),
                         reads=[t_sq[qb]], writes=[t_sq[qb]])
                    S.op("act", lambda e, qb=qb: e.activation(out=ssq[qb], in_=ssq[qb], func=AF.Sqrt),
                         reads=[t_sq[qb]], writes=[t_sq[qb]])
                    S.op("dve", lambda e, qb=qb: e.reciprocal(out=rq[qb], in_=ssq[qb]),
                         reads=[t_sq[qb]], writes=[t_sq[qb]])
                    for hh in range(HPB):
                        S.op("dve", lambda e, qb=qb, hh=hh, row=row: e.scalar_tensor_tensor(
                            out=qn[qb][:, hh * 128:(hh + 1) * 128], in0=qf[qb][:, hh * 128:(hh + 1) * 128],
                            scalar=rq[qb][:, hh:hh + 1], in1=qkn_b[:, row * BW + hh * 128:row * BW + (hh + 1) * 128],
                            op0=ALU.mult, op1=ALU.mult),
                            reads=[t_qf[qb], t_sq[qb], t_c1], writes=[t_qn[qb]])
                    if kind == "k":
                        col = tt
                    else:
                        col = (tt - 1) // 2
                    def _tr(qb=qb, col=col):
                        transposes_to(qn[qb], t_qn[qb], HPB,
                                      lambda c, n, col=col: stageT3[:, c:c + n, col * 128:(col + 1) * 128],
                                      t_stT, (5, 6), True, ident_b)
                    if pend1[0] is not None:
                        pend1[0]()
                    pend1[0] = _tr
                elif kind == "v":
                    sv = cnt["sv"] % 2
                    cnt["sv"] += 1
                    S.op("act", lambda e, sv=sv, pb=pb: e.activation(out=stageV[sv], in_=bank(pb, BW), func=AF.Copy),
                         reads=[psk[pb]], writes=[t_stV[sv]])
                    S.dma("act", lambda e, sv=sv, t=t, meta=meta: e.dma_start(
                        out=V_d[t * 128:(t + 1) * 128, meta:meta + BW], in_=stageV[sv]),
                        reads=[t_stV[sv]], writes=[t_dram1])
                else:
                    sv = cnt["sv"] % 2
                    cnt["sv"] += 1
                    S.op("act", lambda e, sv=sv, pb=pb: e.activation(out=stageV[sv], in_=bank(pb, BW), func=AF.Sigmoid),
                         reads=[psk[pb]], writes=[t_stV[sv]])
                    S.dma("act", lambda e, sv=sv, j=j, meta=meta: e.dma_start(
                        out=sg_d[j * 128:(j + 1) * 128, meta:meta + BW], in_=stageV[sv]),
                        reads=[t_stV[sv]], writes=[t_dram1])
            if pend1[0] is not None:
                pend1[0]()
                pend1[0] = None
            if kind == "k":
                row, h0 = meta
                S.dma("sp", lambda e, h0=h0, g=g: e.dma_start(
                    out=kT_d[h0:h0 + HPB, :, g * G * 128:(g + 1) * G * 128].rearrange("h p t -> p h t"),
                    in_=stageT3), reads=[t_stT], writes=[t_dram1])
            elif kind == "q":
                row, h0 = meta
                GH = G // 2
                S.dma("sp", lambda e, h0=h0, g=g, GH=GH: e.dma_start(
                    out=qT_d[h0:h0 + HPB, :, g * GH * 128:(g + 1) * GH * 128].rearrange("h p t -> p h t"),
                    in_=stageT3[:, :, 0:GH * 128]), reads=[t_stT], writes=[t_dram1])

    S.op("act", lambda e: e.activation(out=lf, in_=f_all, func=AF.Sigmoid), reads=[t_fall], writes=[t_fall])
    S.op("act", lambda e: e.activation(out=lf, in_=lf, func=AF.Ln), reads=[t_fall], writes=[t_fall])
    S.barrier()
    psk = new_ps()
    A.release(m1)

    lf3 = r3(lf, HB)
    cum3 = r3(cum, NS)
    base3 = r3(base, HB)
    t_cum = S.tok("cum")
    t_base = S.tok("base")
    S.op("dve", lambda e: e.memset(base3[:, 0, :], 0.0), writes=[t_base])
    for i in range(NS):
        bk = i % 2
        S.op("pe", lambda e, i=i, bk=bk: e.matmul(bank(bk, HB), tri_incl_f, lf3[:, i, :], start=True, stop=True),
             writes=[psk[bk]], signal=False)
        S.op("pe", lambda e, i=i, bk=bk: e.matmul(bank(bk, HB, HB), ones_f, lf3[:, i, :], start=True, stop=True),
             writes=[psk[bk]])
        S.op("dve", lambda e, i=i, bk=bk: e.tensor_tensor(out=cum3[:, :, i], in0=bank(bk, HB), in1=base3[:, i, :],
                                                          op=ALU.add),
             reads=[psk[bk], t_base], writes=[t_cum])
        S.op("dve", lambda e, i=i, bk=bk: e.tensor_tensor(out=base3[:, i + 1, :], in0=bank(bk, HB, HB),
                                                          in1=base3[:, i, :], op=ALU.add),
             reads=[psk[bk], t_base], writes=[t_base])
    if debug:
        S.dma("sp", lambda e: e.dma_start(out=cum_dbg, in_=cum), reads=[t_cum])
    S.barrier()
    psk = new_ps()

    m3 = A.mark()
    yT_all = A.bf16(H * NQ * 128)
    yT3 = r3(yT_all, NQ * 128)
    m3b = A.mark()
    Etab = A.bf16(HA * 640)
    Etab3 = r3(Etab, 640)
    tmpE = [A.f32(640) for _ in range(2)]
    VW = 130
    vaug = [A.bf16(NS * VW) for _ in range(2)]
    kTh = [A.bf16(NS * 128) for _ in range(2)]
    qTh = [A.bf16(NQ * 128) for _ in range(2)]
    pexp = [A.bf16(640) for _ in range(2)]
    pT = [A.bf16(640) for _ in range(2)]
    biasB = [A.f32(NS) for _ in range(2)]
    rinv = [A.f32(2) for _ in range(2)]
    ytile = [A.bf16(128) for _ in range(2)]

    t_E = S.tok("E")
    t_tmpE = S.toks_n(2, "tmpE")
    t_vaug = S.toks_n(2, "vaug")
    t_kTh = S.toks_n(2, "kTh")
    t_qTh = S.toks_n(2, "qTh")
    t_pexp = S.toks_n(2, "pexp")
    t_pT = S.toks_n(2, "pT")
    t_bias = S.toks_n(2, "bias")
    t_rinv = S.toks_n(2, "rinv")
    t_yt = S.toks_n(2, "yt")
    t_yT = S.toks_n(NQ, "yT")

    for h in range(HA):
        b = h % 2
        S.dma("sp", lambda e, b=b, h=h: e.dma_start(out=tmpE[b], in_=btab[:, h, :]), writes=[t_tmpE[b]])
        S.op("act", lambda e, b=b, h=h: e.activation(out=Etab3[:, h, :], in_=tmpE[b], func=AF.Exp),
             reads=[t_tmpE[b]], writes=[t_E])
    for b in range(2):
        S.op("dve", lambda e, b=b: e.tensor_copy(out=r3(vaug[b], VW)[:, :, 128], in_=valid_s), writes=[t_vaug[b]])

    cnt3 = {"s": 0, "o": 0, "p": 0, "y": 0, "b": 0}

    def load_head(hidx, b):
        S.dma("sp", lambda e: e.dma_start(out=kTh[b], in_=kT_d[hidx, :, :]), writes=[t_kTh[b]])
        S.dma("sp", lambda e: e.dma_start(out=qTh[b], in_=qT_d[hidx, :, :]), writes=[t_qTh[b]])
        S.dma("sp", lambda e: e.dma_start(
            out=r3(vaug[b], VW)[:, :, 0:128],
            in_=V_d[:, hidx * 128:(hidx + 1) * 128].rearrange("(i p) d -> p i d", p=128)),
            writes=[t_vaug[b]])

    pending = [None]
    pending2 = [None]
    PC3 = (max(0, npc - PC1 * nblk1) // 2 + H - 1) // H

    def flush():
        f2 = pending2[0]
        pending2[0] = None
        f1 = pending[0]
        pending[0] = None
        if f1 is not None:
            pending2[0] = f1()
        if f2 is not None:
            f2()

    def finalize(hidx, j, ob):
        yb = cnt3["y"] % 2
        cnt3["y"] += 1
        okb = 4 + ob
        S.op("dve", lambda e: e.reciprocal(out=rinv[yb][:, 0:1], in_=bank(okb, 1, 128)),
             reads=[psk[okb]], writes=[t_rinv[yb]])
        S.op("dve", lambda e: e.tensor_scalar(out=ytile[yb], in0=bank(okb, 128), scalar1=rinv[yb][:, 0:1],
                                              scalar2=None, op0=ALU.mult),
             reads=[psk[okb], t_rinv[yb]], writes=[t_yt[yb]])
        tbk = 6 + (yb % 2)

        def fin2():
            S.op("pe", lambda e: e.transpose(bank_bf(tbk, 128), ytile[yb], ident_b), reads=[t_yt[yb]],
                 writes=[psk[tbk]])
            S.op("act", lambda e: e.activation(out=yT3[:, hidx, j * 128:(j + 1) * 128], in_=bank_bf(tbk, 128),
                                               func=AF.Copy),
                 reads=[psk[tbk]], writes=[t_yT[j]])
        return fin2

    for hidx in range(H):
        b = hidx % 2
        load_head(hidx, b)
        precast(PC3)
        isA = hidx < HA
        for j in range(NQ):
            iq = 2 * j + 1
            ob = cnt3["o"] % 2
            cnt3["o"] += 1
            if isA:
                rs = [r for r in range(5) if iq - 4 + r >= 0]
                groups = [[(iq - 4 + r, r) for r in rs]]
            else:
                hb_ = hidx - HA
                bb = cnt3["b"] % 2
                cnt3["b"] += 1
                S.op("dve", lambda e, hb_=hb_, bb=bb, j=j: e.tensor_scalar(
                    out=biasB[bb], in0=cum3[:, hb_, :], scalar1=-1.0, scalar2=base3[:, 2 * j + 2, hb_:hb_ + 1],
                    op0=ALU.mult, op1=ALU.add), writes=[t_bias[bb]])
                ks = list(range(0, iq + 1))
                groups = [[(i, i - g0) for i in ks[g0:g0 + 4]] for g0 in range(0, len(ks), 4)]
            ngr = len(groups)
            for gi, grp in enumerate(groups):
                sb = cnt3["s"] % 2
                cnt3["s"] += 1
                pb_ = cnt3["p"] % 2
                cnt3["p"] += 1
                for (i, r) in grp:
                    bkk = 2 * sb + (1 if r >= 4 else 0)
                    off = (r % 4) * 128
                    S.op("pe", lambda e, i=i, bkk=bkk, off=off, b=b, j=j: e.matmul(
                        bank(bkk, 128, off), kTh[b][:, i * 128:(i + 1) * 128], qTh[b][:, j * 128:(j + 1) * 128],
                        start=True, stop=True),
                        reads=[t_kTh[b], t_qTh[b]], writes=[psk[bkk]], signal=True)
                r0 = grp[0][1]
                r1 = grp[-1][1]
                if isA:
                    lo = r0 * 128
                    hi = min(r1 + 1, 4) * 128
                    S.op("act", lambda e, sb=sb, pb_=pb_, lo=lo, hi=hi: e.activation(
                        out=pexp[pb_][:, lo:hi], in_=ps_t[:, 2 * sb * 512 + lo:2 * sb * 512 + hi], func=AF.Exp,
                        scale=SCALE), reads=[psk[2 * sb]], writes=[t_pexp[pb_]])
                    if r1 >= 4:
                        S.op("act", lambda e, sb=sb, pb_=pb_: e.activation(
                            out=pexp[pb_][:, 512:640], in_=bank(2 * sb + 1, 128), func=AF.Exp, scale=SCALE),
                            reads=[psk[2 * sb + 1]], writes=[t_pexp[pb_]])
                    S.op("dve", lambda e, pb_=pb_, lo=lo, r1=r1, hidx=hidx: e.tensor_tensor(
                        out=pT[pb_][:, lo:(r1 + 1) * 128], in0=pexp[pb_][:, lo:(r1 + 1) * 128],
                        in1=Etab3[:, hidx, lo:(r1 + 1) * 128], op=ALU.mult),
                        reads=[t_pexp[pb_], t_E], writes=[t_pT[pb_]])
                else:
                    for (i, r) in grp:
                        S.op("act", lambda e, i=i, r=r, sb=sb, pb_=pb_, bb=bb: e.activation(
                            out=pT[pb_][:, r * 128:(r + 1) * 128], in_=bank(2 * sb, 128, r * 128), func=AF.Exp,
                            bias=biasB[bb][:, i:i + 1], scale=SCALE),
                            reads=[psk[2 * sb], t_bias[bb]], writes=[t_pT[pb_]])
                    if grp[-1][0] == iq:
                        r = grp[-1][1]
                        S.op("dve", lambda e, r=r, pb_=pb_: e.tensor_tensor(
                            out=pT[pb_][:, r * 128:(r + 1) * 128], in0=pT[pb_][:, r * 128:(r + 1) * 128],
                            in1=tri_incl_b, op=ALU.mult), reads=[t_pT[pb_]], writes=[t_pT[pb_]])

                def pv(grp=grp, gi=gi, ngr=ngr, pb_=pb_, ob=ob, b=b, hidx=hidx, j=j):
                    okb = 4 + ob
                    for (i, r) in grp:
                        first = (gi == 0 and (i, r) == grp[0])
                        last = (gi == ngr - 1 and (i, r) == grp[-1])
                        S.op("pe", lambda e, i=i, r=r, first=first, last=last: e.matmul(
                            bank(okb, 129), pT[pb_][:, r * 128:(r + 1) * 128], r3(vaug[b], VW)[:, i, 0:129],
                            start=first, stop=last),
                            reads=[t_pT[pb_], t_vaug[b]], writes=[psk[okb]], signal=((i, r) == grp[-1]))
                    if gi == ngr - 1:
                        return finalize(hidx, j, ob)
                    return None
                flush()
                pending[0] = pv
    flush()
    flush()
    if debug:
        S.dma("sp", lambda e: e.dma_start(out=yT_dbg, in_=yT_all), reads=t_yT)
    S.barrier()
    psk = new_ps()
    A.release(m3b)

    t_yT = S.toks_n(NQ, "yT")
    Wa = A.bf16(HA * D)
    Wb = A.bf16(HB * D)
    Wa3 = r3(Wa, D)
    Wb3 = r3(Wb, D)
    sgt = [A.bf16(2 * D) for _ in range(2)]
    zt = [A.bf16(D) for _ in range(2)]
    t1 = [A.f32(BW) for _ in range(2)]
    t2 = [A.f32(BW) for _ in range(2)]
    t_W = S.tok("Wab")
    t_sgt = S.toks_n(2, "sgt")
    t_zt = S.toks_n(2, "zt")
    t_t1 = S.toks_n(2, "t1")
    t_t2 = S.toks_n(2, "t2")
    for c in range(HA):
        S.dma("pool", lambda e, c=c: e.dma_start(out=Wa3[:, c, :], in_=w_ba[c * 128:(c + 1) * 128, :]), writes=[t_W])
    for c in range(HB):
        S.dma("pool", lambda e, c=c: e.dma_start(out=Wb3[:, c, :], in_=w_bb[c * 128:(c + 1) * 128, :]), writes=[t_W])
    c4 = 0
    pend4 = [None]
    for j in range(NQ):
        b = j % 2
        S.dma("sp", lambda e, b=b, j=j: e.dma_start(out=sgt[b], in_=sg_d[j * 128:(j + 1) * 128, :]),
              writes=[t_sgt[b]])
        precast((len(pc_list) - pc_pos[0] + (NQ - j) - 1) // (NQ - j))
        for n in range(NB):
            ab = (c4 % 2) * 2
            tb = c4 % 2
            c4 += 1
            for c in range(HA):
                S.op("pe", lambda e, c=c, j=j, n=n, ab=ab: e.matmul(
                    bank(ab, BW), yT3[:, c, j * 128:(j + 1) * 128], Wa3[:, c, n * BW:(n + 1) * BW],
                    start=(c == 0), stop=(c == HA - 1)),
                    reads=[t_yT[j], t_W], writes=[psk[ab]], signal=(c == HA - 1))
            for c in range(HB):
                S.op("pe", lambda e, c=c, j=j, n=n, ab=ab: e.matmul(
                    bank(ab + 1, BW), yT3[:, HA + c, j * 128:(j + 1) * 128], Wb3[:, c, n * BW:(n + 1) * BW],
                    start=(c == 0), stop=(c == HB - 1)),
                    reads=[t_yT[j], t_W], writes=[psk[ab + 1]], signal=(c == HB - 1))
            S.op("dve", lambda e, b=b, n=n, ab=ab, tb=tb: e.tensor_tensor(
                out=t1[tb], in0=bank(ab, BW), in1=sgt[b][:, n * BW:(n + 1) * BW], op=ALU.mult),
                reads=[psk[ab], t_sgt[b]], writes=[t_t1[tb]])
            S.op("dve", lambda e, b=b, n=n, ab=ab, tb=tb: e.tensor_tensor(
                out=t2[tb], in0=bank(ab + 1, BW), in1=sgt[b][:, D + n * BW:D + (n + 1) * BW], op=ALU.mult),
                reads=[psk[ab + 1], t_sgt[b]], writes=[t_t2[tb]])
            S.op("dve", lambda e, b=b, n=n, tb=tb: e.tensor_tensor(
                out=zt[b][:, n * BW:(n + 1) * BW], in0=t1[tb], in1=t2[tb], op=ALU.add),
                reads=[t_t1[tb], t_t2[tb]], writes=[t_zt[b]])
        def _trz(b=b, j=j):
            transposes_to(zt[b], t_zt[b], KC, lambda c, n, j=j: yT3[:, c:c + n, j * 128:(j + 1) * 128],
                          t_yT[j], (4, 5), True, ident_b)
        if pend4[0] is not None:
            pend4[0]()
        pend4[0] = _trz
    pend4[0]()
    S.barrier()
    psk = new_ps()
    A.release(m3b)

    t_zT = S.toks_n(NQ, "zT")
    Wo = A.bf16(KC * D)
    Wo3 = r3(Wo, D)
    wr_f = A.f32(KC * E)
    wr3 = r3(wr_f, E)
    br_b = A.f32(E)
    fnw_b = A.f32(D)
    cb = [A.f32(E) for _ in range(2)]
    xt4 = [A.f32(D)] * 2
    hh_ = [A.f32(D) for _ in range(2)]
    hnf = hh_
    hnb = [A.bf16(D)] * 2
    hnT = A.f32(KC * 128)
    hnT3 = r3(hnT, 128)
    logit = A.f32(E)
    top8 = A.f32(8)
    negm = A.f32(2)
    mask = A.f32(E)
    ex = A.f32(E)
    exm = A.f32(E)
    ssum = A.f32(2)
    dest_e = A.f32(E)
    oh = A.f32(E)
    junkE = A.f32(E)
    destf = A.f32(4)
    ss4 = [A.f32(2) for _ in range(2)]
    rstd4 = [A.f32(2) for _ in range(2)]

    t_c4 = S.tok("c4")
    t_Wo = S.tok("Wo")
    for c in range(KC):
        S.dma("pool", lambda e, c=c: e.dma_start(out=Wo3[:, c, :], in_=w_out[c * 128:(c + 1) * 128, :]), writes=[t_Wo])
    S.dma("sp", lambda e: e.dma_start(out=wr3, in_=w_router.rearrange("(kc p) c -> p kc c", p=128)), writes=[t_c4])
    S.dma("sp", lambda e: e.dma_start(out=br_b, in_=b_router.partition_broadcast(128)), writes=[t_c4])
    S.dma("sp", lambda e: e.dma_start(out=fnw_b, in_=fnw.partition_broadcast(128)), writes=[t_c4])
    S.dma("sp", lambda e: e.dma_start(out=cb[0], in_=ebase_d), writes=[t_c4])
    t_xt4 = [S.tok("xt")] * 2
    t_h = S.toks_n(2, "h")
    t_hnf = t_h
    t_hnb = [S.tok("hnb")] * 2
    t_ss4 = S.toks_n(2, "ss4")
    t_hnT = S.tok("hnT")
    t_r = S.tok("route")
    t_cb = S.toks_n(2, "cb")
    t_ga = S.tok("gate_all")
    t_di = S.tok("dest_i")
    t_hbuf = S.tok("hbuf")
    t_out = S.tok("out")
    t_GdT = S.tok("GdT")
    gate3 = r3(gate_all, 4)
    desti3 = r3(dest_i, 4)
    for j in range(NQ):
        b = j % 2
        S.dma("sp", lambda e, b=b, j=j: e.dma_start(out=xt4[b], in_=xkv[(2 * j + 1) * 128:(2 * j + 2) * 128, :]),
              writes=[t_xt4[b]])
        for n in range(NB):
            for kc in range(KC):
                S.op("pe", lambda e, kc=kc, n=n, j=j: e.matmul(
                    bank(n, BW), yT3[:, kc, j * 128:(j + 1) * 128], Wo3[:, kc, n * BW:(n + 1) * BW],
                    start=(kc == 0), stop=(kc == KC - 1)),
                    reads=[t_zT[j], t_Wo], writes=[psk[n]], signal=(kc == KC - 1))
            S.op("dve", lambda e, b=b, n=n: e.tensor_tensor(
                out=hh_[b][:, n * BW:(n + 1) * BW], in0=bank(n, BW), in1=xt4[b][:, n * BW:(n + 1) * BW], op=ALU.add),
                reads=[psk[n], t_xt4[b]], writes=[t_h[b]])
        S.dma("sp", lambda e, b=b, j=j: e.dma_start(out=out[j * 128:(j + 1) * 128, :], in_=hh_[b]),
              reads=[t_h[b]], writes=[t_out])
        rms_rstd(hh_[b], ss4[b], rstd4[b], D, [t_h[b]], t_ss4[b], hnb[b], t_hnb[b])
        S.op("dve", lambda e, b=b: e.scalar_tensor_tensor(out=hnf[b], in0=hh_[b], scalar=rstd4[b][:, 0:1],
                                                         in1=fnw_b, op0=ALU.mult, op1=ALU.mult),
             reads=[t_h[b], t_ss4[b], t_c4], writes=[t_hnf[b]])
        S.op("act", lambda e, b=b: e.activation(out=hnb[b], in_=hnf[b], func=AF.Copy),
             reads=[t_hnf[b]], writes=[t_hnb[b]])
        tb_banks = (4, 5, 6, 7)
        transposes_to(hnf[b], t_hnf[b], KC, lambda c, n: hnT3[:, c:c + n, :], t_hnT, tb_banks, False, ident_f)
        LB = 4
        for kc in range(KC):
            S.op("pe", lambda e, kc=kc: e.matmul(bank(LB, E), hnT3[:, kc, :], wr3[:, kc, :],
                                                 start=(kc == 0), stop=(kc == KC - 1)),
                 reads=[t_hnT, t_c4], writes=[psk[LB]], signal=(kc == KC - 1))
        S.op("dve", lambda e: e.tensor_tensor(out=logit, in0=bank(LB, E), in1=br_b, op=ALU.add),
             reads=[psk[LB], t_c4], writes=[t_r])
        S.op("dve", lambda e: e.max(out=top8, in_=logit), reads=[t_r], writes=[t_r])
        S.op("dve", lambda e: e.tensor_scalar(out=mask, in0=logit, scalar1=top8[:, 3:4], scalar2=None,
                                              op0=ALU.is_ge), reads=[t_r], writes=[t_r])
        S.op("dve", lambda e: e.tensor_scalar(out=negm[:, 0:1], in0=top8[:, 0:1], scalar1=-1.0, scalar2=None,
                                              op0=ALU.mult), reads=[t_r], writes=[t_r])
        S.op("act", lambda e: e.activation(out=ex, in_=logit, func=AF.Exp, bias=negm[:, 0:1], scale=1.0),
             reads=[t_r], writes=[t_r])
        S.op("dve", lambda e: e.scalar_tensor_tensor(out=exm, in0=ex, scalar=1.0, in1=mask, op0=ALU.mult, op1=ALU.mult, accum_out=ssum[:, 0:1]),
             reads=[t_r], writes=[t_r])
        S.op("dve", lambda e: e.reciprocal(out=ssum[:, 0:1], in_=ssum[:, 0:1]), reads=[t_r], writes=[t_r])
        S.op("dve", lambda e, j=j: e.tensor_scalar(out=Gd3[:, j, :], in0=exm, scalar1=ssum[:, 0:1], scalar2=None,
                                                   op0=ALU.mult),
             reads=[t_r], writes=[t_r])
        PB = 5
        S.op("pe", lambda e: e.matmul(bank(PB, E), tri_strict_f, mask, start=True, stop=True),
             reads=[t_r], writes=[psk[PB]], signal=False)
        S.op("pe", lambda e: e.matmul(bank(PB, E, E), ones_f, mask, start=True, stop=True),
             reads=[t_r], writes=[psk[PB]])
        c0_, c1_ = cb[j % 2], cb[(j + 1) % 2]
        S.op("dve", lambda e, c0_=c0_: e.tensor_tensor(out=dest_e, in0=bank(PB, E), in1=c0_, op=ALU.add),
             reads=[psk[PB], t_cb[j % 2], t_c4], writes=[t_r])
        S.op("dve", lambda e, c0_=c0_, c1_=c1_: e.tensor_tensor(out=c1_, in0=bank(PB, E, E), in1=c0_, op=ALU.add),
             reads=[psk[PB], t_cb[j % 2], t_c4], writes=[t_cb[(j + 1) % 2]])
        for k in range(4):
            S.op("dve", lambda e, k=k: e.tensor_scalar(out=oh, in0=logit, scalar1=top8[:, k:k + 1], scalar2=None,
                                                       op0=ALU.is_equal), reads=[t_r], writes=[t_r])
            S.op("dve", lambda e, k=k: e.scalar_tensor_tensor(out=junkE, in0=oh, scalar=1.0, in1=dest_e, op0=ALU.mult, op1=ALU.mult, accum_out=destf[:, k:k + 1]),
                 reads=[t_r], writes=[t_r])
            S.op("dve", lambda e, k=k, j=j: e.scalar_tensor_tensor(out=junkE, in0=oh, scalar=1.0, in1=Gd3[:, j, :], op0=ALU.mult, op1=ALU.mult, accum_out=gate3[:, j, k:k + 1]),
                 reads=[t_r], writes=[t_r, t_ga])
        S.op("dve", lambda e, j=j: e.tensor_copy(out=desti3[:, j, :], in_=destf), reads=[t_r], writes=[t_di])
        for k in range(4):
            S.dma("pool", lambda e, b=b, j=j, k=k: e.indirect_dma_start(
                out=hbuf[:, :], out_offset=bass.IndirectOffsetOnAxis(ap=desti3[:, j, k:k + 1], axis=0),
                in_=hnb[b], in_offset=None), reads=[t_hnb[b], t_di], writes=[t_hbuf])
        if debug:
            S.dma("sp", lambda e, b=b, j=j: e.dma_start(out=h_dbg[j * 128:(j + 1) * 128, :], in_=hnf[b]),
                  reads=[t_hnf[b]])
    if debug:
        S.dma("sp", lambda e: e.dma_start(out=gate_dbg, in_=gate_all), reads=[t_ga])
        S.dma("sp", lambda e: e.dma_start(out=dest_dbg, in_=dest_i), reads=[t_di])
    S.barrier()
    psk = new_ps()
    A.release(m3)

    m5 = A.mark()
    NWB = 5
    wbk = [A.bf16(KC * BW) for _ in range(NWB)]
    xe = A.bf16(CT * D)
    xe3 = r3(xe, D)
    hTe = [A.bf16(KC * C)] * 2
    hbT = [A.bf16(KCE * C)] * 2
    SW = BW // 2
    NSTG = 4

    gsb = [A.f32(HPB * C) for _ in range(2)]
    gtmp = [A.f32(C) for _ in range(2)]
    stmp = [A.f32(C) for _ in range(2)]
    utmp = [A.f32(C) for _ in range(2)]
    ystage = [A.f32(BW) for _ in range(3)]
    bgu = A.f32(E * NGU)
    bgu3 = r3(bgu, NGU)
    t_bgu = S.tok("bgu")
    S.dma("sp", lambda e: e.dma_start(out=bgu, in_=bgu_t), writes=[t_bgu])
    t_wbk = S.toks_n(NWB, "wbk")
    t_xe = S.tok("xe")
    t_hTe = [S.tok("hTe")] * 2
    t_hbT = [S.tok("hbT")] * 2
    t_gsb = S.toks_n(2, "gsb")
    t_g = S.toks_n(2, "g")
    t_s = S.toks_n(2, "s")
    t_u = S.toks_n(2, "u")
    t_ys = S.toks_n(3, "ys")
    t_ybuf = S.tok("ybuf")
    c5 = {"w": 0, "gu": 0, "dn": 0, "t": 0, "ys": 0, "tr": 0}
    GU_BANKS = (0, 1, 2, 3)
    DN_BANKS = (4, 5)
    wsrc = []
    for ex2 in range(E):
        for bidx2 in range(DE // BW):
            for part2 in range(2):
                c0_ = part2 * DE + bidx2 * BW
                wsrc.append((w_gu, wgu_bf, ex2, c0_))
        for n2 in range(NB):
            wsrc.append((w_dn, wdn_bf, ex2, n2 * BW))
    wst = {"issued": 0, "half": 0}

    def ensure_weights(upto):
        while wst["issued"] <= min(upto, len(wsrc) - 1):
            i_ = wst["issued"]
            wst["issued"] += 1
            wsrc_f, wsrc_b, ex2, c0_ = wsrc[i_]
            wi2 = i_ % NWB
            if ex2 >= E - NP:
                S.dma("sp", lambda e, wi2=wi2, ex2=ex2, c0_=c0_, wsrc_b=wsrc_b: e.dma_start(
                    out=r3(wbk[wi2], BW),
                    in_=wsrc_b[ex2 - (E - NP), :, c0_:c0_ + BW].rearrange("(kc p) c -> p kc c", p=128)),
                    writes=[t_wbk[wi2]])
                continue
            S.dma("pool", lambda e, wi2=wi2, ex2=ex2, c0_=c0_, wsrc_f=wsrc_f: e.dma_start(
                out=r3(wbk[wi2], BW),
                in_=wsrc_f[ex2, :, c0_:c0_ + BW].rearrange("(kc p) c -> p kc c", p=128)),
                writes=[t_wbk[wi2]])

    widx = [0]
    for ex_ in range(E):
        eb = ex_ % 2
        nfull = C // 128
        if nfull:
            S.dma("sp", lambda e, ex_=ex_: e.dma_start(
                out=xe3[:, 0:nfull, :],
                in_=hbuf[ex_ * C:ex_ * C + nfull * 128, :].rearrange("(s p) d -> p s d", p=128)), writes=[t_xe])
        if C % 128:
            S.dma("sp", lambda e, ex_=ex_: e.dma_start(
                out=xe3[0:C % 128, nfull, :], in_=hbuf[ex_ * C + nfull * 128:(ex_ + 1) * C, :]), writes=[t_xe])
        he3 = r3(hTe[eb], C)
        for s_ in range(CT):
            np_ = min(128, C - s_ * 128)
            transposes_to(xe3[:, s_, :], t_xe, KC,
                          lambda c, n, s_=s_, he3=he3, np_=np_: he3[:, c:c + n, s_ * 128:s_ * 128 + np_],
                          t_hTe[eb], (6, 7), True, ident_b, npart=np_)
        hb3 = r3(hbT[eb], C)
        nblk = DE // BW
        for bidx in range(nblk):
            gsi = c5["t"] % 2
            c5["t"] += 1
            gs3 = r3(gsb[gsi], C)
            for part in range(2):
                wi = widx[0] % NWB
                ensure_weights(widx[0] + NWB - 1)
                widx[0] += 1
                for m in range(HPB):
                    gb_ = GU_BANKS[c5["gu"] % 4]
                    c5["gu"] += 1
                    for kc in range(KC):
                        S.op("pe", lambda e, kc=kc, wi=wi, m=m, gb_=gb_, he3=he3: e.matmul(
                            bank(gb_, C), r3(wbk[wi], BW)[:, kc, m * 128:(m + 1) * 128], he3[:, kc, :],
                            start=(kc == 0), stop=(kc == KC - 1)),
                            reads=[t_wbk[wi], t_hTe[eb]], writes=[psk[gb_]], signal=(kc == KC - 1))
                    chunk = bidx * HPB + m
                    ti = (c5["gu"]) % 2
                    if part == 0:
                        bcol = bgu3[:, ex_, chunk:chunk + 1]
                        S.op("dve", lambda e, gb_=gb_, ti=ti, bcol=bcol: e.tensor_scalar(
                            out=gtmp[ti], in0=bank(gb_, C), scalar1=bcol, scalar2=SWIGLU_LIMIT, op0=ALU.add,
                            op1=ALU.min), reads=[psk[gb_], t_bgu], writes=[t_g[ti]])
                        S.op("act", lambda e, ti=ti: e.activation(out=stmp[ti], in_=gtmp[ti], func=AF.Sigmoid,
                                                                  scale=SWIGLU_ALPHA),
                             reads=[t_g[ti]], writes=[t_s[ti]])
                        S.op("dve", lambda e, ti=ti, m=m, gs3=gs3: e.tensor_tensor(
                            out=gs3[:, m, :], in0=gtmp[ti], in1=stmp[ti], op=ALU.mult),
                            reads=[t_g[ti], t_s[ti]], writes=[t_gsb[gsi]])
                    else:
                        bcol = bgu3[:, ex_, KCE + chunk:KCE + chunk + 1]
                        S.op("dve", lambda e, gb_=gb_, ti=ti, bcol=bcol: e.tensor_scalar(
                            out=utmp[ti], in0=bank(gb_, C), scalar1=bcol, scalar2=SWIGLU_LIMIT, op0=ALU.add,
                            op1=ALU.min), reads=[psk[gb_], t_bgu], writes=[t_u[ti]])
                        S.op("dve", lambda e, ti=ti: e.tensor_scalar(
                            out=utmp[ti], in0=utmp[ti], scalar1=-SWIGLU_LIMIT, scalar2=1.0, op0=ALU.max,
                            op1=ALU.add), reads=[t_u[ti]], writes=[t_u[ti]])
                        S.op("dve", lambda e, ti=ti, m=m, gs3=gs3, chunk=chunk, hb3=hb3: e.tensor_tensor(
                            out=hb3[:, chunk, :], in0=utmp[ti], in1=gs3[:, m, :], op=ALU.mult),
                            reads=[t_u[ti], t_gsb[gsi]], writes=[t_hbT[eb]])
        for n in range(NB):
            wi = widx[0] % NWB
            ensure_weights(widx[0] + NWB - 1)
            widx[0] += 1
            for s_ in range(CT):
                db = DN_BANKS[c5["dn"] % 2]
                c5["dn"] += 1
                np_ = min(128, C - s_ * 128)
                for kc in range(KCE):
                    S.op("pe", lambda e, kc=kc, wi=wi, s_=s_, db=db, hb3=hb3, np_=np_: e.matmul(
                        bank(db, BW)[0:np_, :], hb3[:, kc, s_ * 128:s_ * 128 + np_], r3(wbk[wi], BW)[:, kc, :],
                        start=(kc == 0), stop=(kc == KCE - 1)),
                        reads=[t_wbk[wi], t_hbT[eb]], writes=[psk[db]], signal=(kc == KCE - 1))
                yi = c5["ys"] % 3
                c5["ys"] += 1
                S.op("act", lambda e, yi=yi, db=db, np_=np_: e.activation(
                    out=ystage[yi][0:np_, :], in_=bank(db, BW)[0:np_, :], func=AF.Copy),
                     reads=[psk[db]], writes=[t_ys[yi]])
                S.dma("act", lambda e, yi=yi, ex_=ex_, s_=s_, n=n, np_=np_: e.dma_start(
                    out=ybuf[ex_ * C + s_ * 128:ex_ * C + s_ * 128 + np_, n * BW:(n + 1) * BW],
                    in_=ystage[yi][0:np_, :]),
                    reads=[t_ys[yi]], writes=[t_ybuf])
    S.barrier()
    psk = new_ps()
    A.release(m5)

    hp = [A.f32(D) for _ in range(2)]
    yk = [[A.f32(D) for _ in range(4)] for _ in range(2)]
    bdn = A.f32(D)
    GdT = A.f32(128)
    t_bdn = S.tok("bdn")
    t_GdT = S.tok("GdT")
    S.dma("sp", lambda e: e.dma_start(out=bdn[0:E, :], in_=b_dn), writes=[t_bdn])
    t_hp = S.toks_n(2, "hp")
    t_yk = [S.toks_n(4, "yk%d" % i) for i in range(2)]
    t_out = S.tok("out")
    for j in range(NQ):
        b = j % 2
        S.dma("sp", lambda e, b=b, j=j: e.dma_start(out=hp[b], in_=out[j * 128:(j + 1) * 128, :]), writes=[t_hp[b]])
        GB = 6
        S.op("pe", lambda e, j=j: e.transpose(bank(GB, 128)[0:E, :], Gd3[:, j, :], ident_f), writes=[psk[GB]])
        S.op("act", lambda e: e.activation(out=GdT[0:E, :], in_=bank(GB, 128)[0:E, :], func=AF.Copy),
             reads=[psk[GB]], writes=[t_GdT])
        for n in range(NB):
            S.op("pe", lambda e, n=n: e.matmul(bank(n, BW), GdT[0:E, :], bdn[0:E, n * BW:(n + 1) * BW],
                                               start=True, stop=True),
                 reads=[t_GdT, t_bdn], writes=[psk[n]])
            S.op("dve", lambda e, n=n, b=b: e.tensor_tensor(
                out=hp[b][:, n * BW:(n + 1) * BW], in0=bank(n, BW), in1=hp[b][:, n * BW:(n + 1) * BW], op=ALU.add),
                reads=[psk[n], t_hp[b]], writes=[t_hp[b]])
        for k in range(4):
            S.dma("pool", lambda e, b=b, j=j, k=k: e.indirect_dma_start(
                out=yk[b][k], out_offset=None, in_=ybuf[:, :],
                in_offset=bass.IndirectOffsetOnAxis(ap=desti3[:, j, k:k + 1], axis=0)), writes=[t_yk[b][k]])
            S.op("dve", lambda e, b=b, j=j, k=k: e.scalar_tensor_tensor(
                out=hp[b], in0=yk[b][k], scalar=gate3[:, j, k:k + 1], in1=hp[b], op0=ALU.mult, op1=ALU.add),
                reads=[t_yk[b][k], t_hp[b]], writes=[t_hp[b]])
        S.dma("sp", lambda e, b=b, j=j: e.dma_start(out=out[j * 128:(j + 1) * 128, :], in_=hp[b]),
              reads=[t_hp[b]], writes=[t_out])
    S.barrier()

    blk = st.enter_context(nc.Block())

    @blk.tensor
    def _(e):
        for f in S.ops["pe"]:
            f(e)

    @blk.scalar
    def _(e):
        for f in S.ops["act"]:
            f(e)

    @blk.vector
    def _(e):
        for f in S.ops["dve"]:
            f(e)

    @blk.gpsimd
    def _(e):
        for f in S.ops["pool"]:
            f(e)

    @blk.sync
    def _(e):
        for f in S.ops["sp"]:
            f(e)

    st.close()
    return nc


def make_consts(cfg):
    t = np.arange(128)
    ident = np.eye(128, dtype=np.float32)
    tri_incl = (t[:, None] <= t[None, :]).astype(np.float32)
    tri_strict = (t[:, None] < t[None, :]).astype(np.float32)
    ones = np.ones((128, 128), np.float32)
    cst = np.concatenate([ident, tri_incl, tri_strict, ones], axis=1)
    ebase = np.tile((np.arange(cfg.E) * cfg.C).astype(np.float32)[None, :], (128, 1))
    return cst, ebase


def make_btab(rel_bias, cfg):
    HA = cfg.HA
    kk = np.arange(128)[:, None, None]
    r = np.arange(5)[None, :, None]
    qi = np.arange(128)[None, None, :]
    dist = (4 - r) * 128 + qi - kk
    qo = qi % 64
    ok = (dist >= -(63 - qo)) & (dist <= qo + 512)
    idx = np.clip(dist, -63, 256) + 63
    tab = rel_bias[:, idx]
    tab = np.where(ok[None], tab, np.float32(NEG)).astype(np.float32)
    return np.ascontiguousarray(tab.transpose(1, 0, 2, 3).reshape(128, HA, 640))


def prepare(cfg, inp):
    D, NS, E = cfg.D, cfg.NS, cfg.E
    x = np.asarray(inp["x"], np.float32)
    B = x.shape[0]
    cst, ebase = make_consts(cfg)
    btab = make_btab(np.asarray(inp["rel_bias"], np.float32), cfg)
    qkn = np.concatenate([np.tile(np.asarray(inp[k], np.float32), cfg.HPB)
                          for k in ("qn_a", "kn_a", "qn_b", "kn_b")])
    NGU = 2 * D // 128
    bgu_t = np.ascontiguousarray(
        np.asarray(inp["b_gate_up"], np.float32).reshape(E, NGU, 128).transpose(2, 0, 1).reshape(128, E * NGU))
    shared = {
        "w_in": np.asarray(inp["w_in"], np.float32),
        "attn_norm_w": np.asarray(inp["attn_norm_w"], np.float32),
        "ffn_norm_w": np.asarray(inp["ffn_norm_w"], np.float32),
        "qkn": qkn,
        "b_forget": np.asarray(inp["b_forget"], np.float32),
        "btab": btab,
        "w_branch_a": np.asarray(inp["w_branch_a"], np.float32),
        "w_branch_b": np.asarray(inp["w_branch_b"], np.float32),
        "w_out": np.asarray(inp["w_out"], np.float32),
        "w_router": np.asarray(inp["w_router"], np.float32),
        "b_router": np.asarray(inp["b_router"], np.float32),
        "w_gate_up": np.asarray(inp["w_gate_up"], np.float32),
        "bgu_t": bgu_t,
        "w_down": np.asarray(inp["w_down"], np.float32),
        "b_down": np.asarray(inp["b_down"], np.float32),
        "cst": cst,
        "ebase": ebase,
    }
    in_maps = []
    for c in range(2 * B):
        b, p = c // 2, c % 2
        if p == 0:
            xk = np.concatenate([np.zeros((128, D), np.float32), x[b, :(NS - 1) * 128]], axis=0)
            valid = np.ones((128, NS), np.float32)
            valid[:, 0] = 0.0
        else:
            xk = x[b, :NS * 128]
            valid = np.ones((128, NS), np.float32)
        m = dict(shared)
        m["xkv"] = np.ascontiguousarray(xk)
        m["valid"] = valid
        in_maps.append(m)
    return in_maps


def assemble(cfg, results, B):
    D, NS, NQ = cfg.D, cfg.NS, cfg.NQ
    y = np.zeros((B, NS * 128, D), np.float32)
    for c in range(2 * B):
        b, p = c // 2, c % 2
        o = np.asarray(results[c]["out"]).reshape(NQ, 128, D)
        yv = y[b].reshape(NS, 128, D)
        for j in range(NQ):
            yv[2 * j + p] = o[j]
    return y


_CACHE = {}


def kernel(**inputs):
    cfg = Cfg()
    if "nc" not in _CACHE:
        _CACHE["nc"] = build(cfg)
    nc = _CACHE["nc"]
    in_maps = prepare(cfg, inputs)
    res = run_bass_kernel_spmd(nc, in_maps, core_ids=list(range(cfg.n_cores)))
    return assemble(cfg, res.results, 4)
```

```python
import numpy as np
from contextlib import ExitStack
import concourse.bass as bass
import concourse.mybir as mybir
from concourse.alu_op_type import AluOpType as ALU
from concourse.bass_utils import run_bass_kernel_spmd

F32 = mybir.dt.float32
BF16 = mybir.dt.bfloat16
I32 = mybir.dt.int32
AF = mybir.ActivationFunctionType
AX = mybir.AxisListType

NORM_EPS = 1e-5
SWIGLU_LIMIT = 7.0
SWIGLU_ALPHA = 1.702
NEG = -30000.0


class Cfg:
    def __init__(self, D=2048, HA=8, HB=8, NS=32, E=32, C=320, G=16, n_cores=8, NP=0):
        self.D = D
        self.KC = D // 128
        self.HA = HA
        self.HB = HB
        self.H = HA + HB
        self.NS = NS
        self.NQ = NS // 2
        self.E = E
        self.C = C
        self.CT = (C + 127) // 128
        self.NP = NP
        self.G = G
        self.WA = HA * 128
        self.WB = HB * 128
        self.BW = min(512, self.WA, self.WB, D)
        self.HPB = self.BW // 128
        self.NB = D // self.BW
        self.INC = 3 * self.WA + 3 * self.WB + HB + 2 * D
        self.n_cores = n_cores


class Tok:
    __slots__ = ("w", "r", "name")

    def __init__(self, name=""):
        self.w = None
        self.r = []
        self.name = name


ENGS = ("pe", "act", "dve", "pool", "sp")
NDMA = 8


class Sched:
    def __init__(self, nc, st):
        self.nc = nc
        self.ops = {e: [] for e in ENGS}
        self.seq = {e: 0 for e in ENGS}
        self.sem = {e: st.enter_context(nc.semaphore("s_" + e)) for e in ENGS}
        self.semid = {}
        self.waited = {e: {} for e in ENGS}
        self.dsem = {}
        for q in ("sp", "act", "pool", "poolpc"):
            self.dsem[q] = [[st.enter_context(nc.semaphore("d_%s%d" % (q, i))), 0] for i in range(NDMA)]
        self.drr = {q: 0 for q in self.dsem}
        self.unsig = {e: False for e in ENGS}
        self.toks = []

    def tok(self, name=""):
        t = Tok(name)
        self.toks.append(t)
        return t

    def toks_n(self, n, name=""):
        return [self.tok(name + str(i)) for i in range(n)]

    def _key(self, sem):
        return id(sem)

    def _need(self, eng, deps):
        for (sem, val, src) in deps:
            if src == "pe" and eng == "pe":
                continue
            k = self._key(sem)
            if self.waited[eng].get(k, 0) < val:
                self.waited[eng][k] = val
                self.ops[eng].append(lambda e, sem=sem, val=val: e.wait_ge(sem, val))

    def _deps(self, reads, writes):
        deps = []
        for b in reads:
            if b.w is not None:
                deps.append(b.w)
        for b in writes:
            if b.w is not None:
                deps.append(b.w)
            deps.extend(b.r)
        return deps

    def _mark(self, reads, writes, t):
        for b in reads:
            b.r.append(t)
        for b in writes:
            b.w = t
            b.r = []

    def op(self, eng, fn, reads=(), writes=(), signal=True):
        self._need(eng, self._deps(reads, writes))
        sem = self.sem[eng]
        if signal:
            self.seq[eng] += 1
            t = (sem, self.seq[eng], eng)
            self.ops[eng].append(lambda e, fn=fn, sem=sem: fn(e).then_inc(sem, 1))
            self.unsig[eng] = False
        else:
            t = (sem, self.seq[eng] + 1, eng)
            self.ops[eng].append(lambda e, fn=fn: fn(e))
            self.unsig[eng] = True
        self._mark(reads, writes, t)

    def dma(self, q, fn, reads=(), writes=(), key=None):
        key = key or q
        self._need(q, self._deps(reads, writes))
        slot = self.dsem[key][self.drr[key] % NDMA]
        self.drr[key] += 1
        sem, uses = slot
        if uses > 0:
            self._need(q, [(sem, 16 * uses, "dma")])
        slot[1] = uses + 1
        t = (sem, 16 * (uses + 1), "dma")
        self.ops[q].append(lambda e, fn=fn, sem=sem: fn(e).then_inc(sem, 16))
        self._mark(reads, writes, t)

    def barrier(self):
        for e in ENGS:
            assert not self.unsig[e], e
        deps = []
        for e in ENGS:
            if self.seq[e] > 0:
                deps.append((self.sem[e], self.seq[e], "x"))
        for q in self.dsem:
            for sem, uses in self.dsem[q]:
                if uses > 0:
                    deps.append((sem, 16 * uses, "dma"))
        for e in ENGS:
            self._need(e, deps)
        for t in self.toks:
            t.w = None
            t.r = []
        self.toks = []


class Arena:
    def __init__(self, ap, nfloats):
        self.ap = ap
        self.n = nfloats
        self.off = 0

    def mark(self):
        return self.off

    def release(self, m):
        self.off = m

    def f32(self, n):
        n = (n + 1) // 2 * 2
        assert self.off + n <= self.n, ("arena overflow", self.off, n, self.n)
        a = self.ap[:, self.off:self.off + n]
        self.off += n
        return a

    def bf16(self, n):
        n = (n + 3) // 4 * 4
        return self.f32(n // 2).bitcast(BF16)

    def i32(self, n):
        return self.f32(n).bitcast(I32)


def r3(ap, b):
    return ap.rearrange("p (a b) -> p a b", b=b)


def build(cfg, debug=False):
    D, KC, HA, HB, H, NS, NQ, E, C, CT, G = (cfg.D, cfg.KC, cfg.HA, cfg.HB, cfg.H, cfg.NS, cfg.NQ,
                                             cfg.E, cfg.C, cfg.CT, cfg.G)
    WA, WB, BW, HPB, NB, INC = cfg.WA, cfg.WB, cfg.BW, cfg.HPB, cfg.NB, cfg.INC
    DE = D
    KCE = DE // 128
    NGU = 2 * DE // 128
    SCALE = 128.0 ** -0.5
    nc = bass.Bass("TRN2", target_bir_lowering=False)

    def din(name, shape, dt=F32):
        return nc.dram_tensor(name, list(shape), dt, kind="ExternalInput").ap()

    def dscr(name, shape, dt):
        kind = "ExternalOutput" if debug else "Internal"
        return nc.dram_tensor(name, list(shape), dt, kind=kind).ap()

    xkv = din("xkv", [NS * 128, D])
    valid_d = din("valid", [128, NS])
    w_in = din("w_in", [D, INC])
    anw = din("attn_norm_w", [D])
    fnw = din("ffn_norm_w", [D])
    qkn = din("qkn", [4 * BW])
    b_forget = din("b_forget", [HB])
    btab = din("btab", [128, HA, 5 * 128])
    w_ba = din("w_branch_a", [WA, D])
    w_bb = din("w_branch_b", [WB, D])
    w_out = din("w_out", [D, D])
    w_router = din("w_router", [D, E])
    b_router = din("b_router", [E])
    w_gu = din("w_gate_up", [E, D, 2 * DE])
    bgu_t = din("bgu_t", [128, E * NGU])
    w_dn = din("w_down", [E, DE, D])
    b_dn = din("b_down", [E, D])
    cst = din("cst", [128, 4 * 128])
    ebase_d = din("ebase", [128, E])
    out = nc.dram_tensor("out", [NQ * 128, D], F32, kind="ExternalOutput").ap()

    kT_d = dscr("kT_d", [H, 128, NS * 128], BF16)
    qT_d = dscr("qT_d", [H, 128, NQ * 128], BF16)
    V_d = dscr("V_d", [NS * 128, WA + WB], BF16)
    sg_d = dscr("sg_d", [NQ * 128, 2 * D], BF16)
    hbuf = dscr("hbuf", [E * C + 128, D], BF16)
    ybuf = dscr("ybuf", [E * C + 128, D], F32)
    NP = cfg.NP
    wgu_bf = nc.dram_tensor("wgu_bf", [max(NP, 1), D, 2 * DE], BF16, kind="Internal").ap()
    wdn_bf = nc.dram_tensor("wdn_bf", [max(NP, 1), DE, D], BF16, kind="Internal").ap()
    if debug:
        yT_dbg = dscr("yT_dbg", [128, H * NQ * 128], BF16)
        h_dbg = dscr("h_dbg", [NQ * 128, D], F32)
        gate_dbg = dscr("gate_dbg", [128, NQ * 4], F32)
        dest_dbg = dscr("dest_dbg", [128, NQ * 4], I32)
        cum_dbg = dscr("cum_dbg", [128, HB * NS], F32)
        xnT_dbg = dscr("xnT_dbg", [128, KC * G * 128], BF16)

    st = ExitStack()
    ARENA_F = 47000
    arena_t = st.enter_context(nc.sbuf_tensor("arena", [128, ARENA_F], F32))
    ps_t = st.enter_context(nc.psum_tensor("ps", [128, 4096], F32))
    S = Sched(nc, st)
    A = Arena(arena_t, ARENA_F)

    def bank(i, n=512, off=0):
        return ps_t[:, i * 512 + off:i * 512 + off + n]

    def bank_bf(i, n=1024, off=0):
        return ps_t[:, i * 512:(i + 1) * 512].bitcast(BF16)[:, off:off + n]

    psk = S.toks_n(8, "ps")

    pc_list = []
    for ep in range(NP):
        e_ = E - NP + ep
        cw = min(2048, 2 * DE)
        for q4 in range(4):
            r0, r1 = q4 * D // 4, (q4 + 1) * D // 4
            pc_list.append((wgu_bf[ep, r0:r1, :].rearrange("r (a b) -> r a b", b=cw),
                            w_gu[e_, r0:r1, :].rearrange("r (a b) -> r a b", b=cw)))
        cw = min(2048, D)
        for q2 in range(2):
            r0, r1 = q2 * DE // 2, (q2 + 1) * DE // 2
            pc_list.append((wdn_bf[ep, r0:r1, :].rearrange("r (a b) -> r a b", b=cw),
                            w_dn[e_, r0:r1, :].rearrange("r (a b) -> r a b", b=cw)))
    pc_pos = [0]

    def precast(n):
        for _ in range(n):
            if pc_pos[0] >= len(pc_list):
                return
            o_, i_ = pc_list[pc_pos[0]]
            pc_pos[0] += 1
            S.dma("pool", lambda e, o_=o_, i_=i_: e.dma_start(out=o_, in_=i_), key="poolpc")

    def new_ps():
        return S.toks_n(8, "ps")

    ident_f = A.f32(128)
    tri_incl_f = A.f32(128)
    tri_strict_f = A.f32(128)
    ones_f = A.f32(128)
    ident_b = A.bf16(128)
    tri_incl_b = A.bf16(128)
    valid_s = A.f32(NS)
    lf = A.f32(NS * HB)
    cum = A.f32(HB * NS)
    base = A.f32((NS + 1) * HB)
    gate_all = A.f32(NQ * 4)
    dest_i = A.i32(NQ * 4)
    Gd_all = A.f32(NQ * E)
    Gd3 = r3(Gd_all, E)
    tk_const = S.tok("const")

    cst_tmp = [ident_f, tri_incl_f, tri_strict_f, ones_f]
    for i, a in enumerate(cst_tmp):
        S.dma("sp", lambda e, a=a, i=i: e.dma_start(out=a, in_=cst[:, i * 128:(i + 1) * 128]), writes=[tk_const])
    S.dma("sp", lambda e: e.dma_start(out=valid_s, in_=valid_d), writes=[tk_const])
    S.op("dve", lambda e: e.tensor_copy(out=ident_b, in_=ident_f), reads=[tk_const], writes=[tk_const])
    S.op("dve", lambda e: e.tensor_copy(out=tri_incl_b, in_=tri_incl_f), reads=[tk_const], writes=[tk_const])
    S.barrier()
    psk = new_ps()

    m1 = A.mark()
    anw_b = A.f32(D)
    qkn_b = A.f32(4 * BW)
    bf_b = A.f32(HB)
    wf = A.bf16(KC * HB)
    xnT = A.bf16(KC * G * 128)
    xnT3 = r3(xnT, G * 128)
    xt = [A.f32(D) for _ in range(2)]
    xn = [A.bf16(D) for _ in range(2)]
    wblk = [A.bf16(KC * BW) for _ in range(2)]
    qf = [A.f32(BW) for _ in range(2)]
    qn = [A.bf16(BW) for _ in range(2)]
    stageT = A.bf16(HPB * G * 128)
    stageT3 = r3(stageT, G * 128)
    stageV = [A.bf16(BW) for _ in range(2)]
    ss = [A.f32(2) for _ in range(2)]
    rstd = [A.f32(2) for _ in range(2)]
    ssq = [A.f32(HPB) for _ in range(2)]
    rq = [A.f32(HPB) for _ in range(2)]
    f_all = A.f32(NS * HB)
    f_all3 = r3(f_all, HB)

    t_c1 = S.tok("c1")
    S.dma("sp", lambda e: e.dma_start(out=anw_b, in_=anw.partition_broadcast(128)), writes=[t_c1])
    S.dma("sp", lambda e: e.dma_start(out=qkn_b, in_=qkn.partition_broadcast(128)), writes=[t_c1])
    S.dma("sp", lambda e: e.dma_start(out=bf_b, in_=b_forget.partition_broadcast(128)), writes=[t_c1])
    fcol = 3 * WA + 3 * WB
    S.dma("pool", lambda e: e.dma_start(out=r3(wf, HB),
                                        in_=w_in[:, fcol:fcol + HB].rearrange("(kc p) c -> p kc c", p=128)),
          writes=[t_c1])

    t_xt = S.toks_n(2, "xt")
    t_xn = S.toks_n(2, "xn")
    t_ss = S.toks_n(2, "ss")
    t_xnT = S.toks_n(G, "xnT")
    t_wblk = S.toks_n(2, "wblk")
    t_qf = S.toks_n(2, "qf")
    t_qn = S.toks_n(2, "qn")
    t_sq = S.toks_n(2, "sq")
    t_stT = S.tok("stT")
    t_stV = S.toks_n(2, "stV")
    t_fall = S.tok("fall")
    t_dram1 = S.tok("dram1")

    blocks = []
    for hb_ in range(WA // BW):
        blocks.append(("q", 0 * WA + hb_ * BW, (0, hb_ * HPB)))
    for hb_ in range(WA // BW):
        blocks.append(("k", 1 * WA + hb_ * BW, (1, hb_ * HPB)))
    for hb_ in range(WA // BW):
        blocks.append(("v", 2 * WA + hb_ * BW, hb_ * BW))
    for hb_ in range(WB // BW):
        blocks.append(("q", 3 * WA + hb_ * BW, (2, HA + hb_ * HPB)))
    for hb_ in range(WB // BW):
        blocks.append(("k", 3 * WA + WB + hb_ * BW, (3, HA + hb_ * HPB)))
    for hb_ in range(WB // BW):
        blocks.append(("v", 3 * WA + 2 * WB + hb_ * BW, WA + hb_ * BW))
    gcol = 3 * WA + 3 * WB + HB
    for nb_ in range(2 * D // BW):
        blocks.append(("g", gcol + nb_ * BW, nb_ * BW))

    cnt = {"x": 0, "w": 0, "ps": 0, "q": 0, "sv": 0}
    PROJ_BANKS = (2, 3, 4)

    def rms_rstd(eng_src_ap, ss_ap, rstd_ap, n, toks_r, tok_ss, junk_ap, tok_junk):
        S.op("dve", lambda e: e.scalar_tensor_tensor(out=junk_ap, in0=eng_src_ap, scalar=1.0, in1=eng_src_ap, op0=ALU.mult, op1=ALU.mult, accum_out=ss_ap[:, 0:1]),
             reads=toks_r, writes=[tok_ss, tok_junk])
        S.op("dve", lambda e: e.tensor_scalar(out=ss_ap[:, 0:1], in0=ss_ap[:, 0:1], scalar1=1.0 / n,
                                              scalar2=NORM_EPS, op0=ALU.mult, op1=ALU.add),
             reads=[tok_ss], writes=[tok_ss])
        S.op("act", lambda e: e.activation(out=ss_ap[:, 0:1], in_=ss_ap[:, 0:1], func=AF.Sqrt),
             reads=[tok_ss], writes=[tok_ss])
        S.op("dve", lambda e: e.reciprocal(out=rstd_ap[:, 0:1], in_=ss_ap[:, 0:1]),
             reads=[tok_ss], writes=[tok_ss])

    def transposes_to(src_ap, src_tok, ncol_chunks, dst_fn, dst_tok, banks, dt_bf=True, ident=None, npart=128):
        per = 8 if dt_bf else 4
        c = 0
        bi = 0
        while c < ncol_chunks:
            n = min(per, ncol_chunks - c)
            bk = banks[bi % len(banks)]
            bi += 1
            for k in range(n):
                if dt_bf:
                    o = bank_bf(bk, 128, k * 128)
                else:
                    o = bank(bk, 128, k * 128)
                S.op("pe", lambda e, o=o, k=k, c=c: e.transpose(
                    o[:, 0:npart], src_ap[0:npart, (c + k) * 128:(c + k + 1) * 128], ident[0:npart, 0:npart]),
                     reads=[src_tok], writes=[psk[bk]], signal=(k == n - 1))
            if dt_bf:
                srcp = r3(bank_bf(bk, n * 128), 128)[:, :, 0:npart]
            else:
                srcp = r3(bank(bk, n * 128), 128)[:, :, 0:npart]
            S.op("act", lambda e, srcp=srcp, c=c, n=n: e.activation(out=dst_fn(c, n), in_=srcp, func=AF.Copy),
                 reads=[psk[bk]], writes=[dst_tok])
            c += n

    ngroups = NS // G
    pend1 = [None]
    npc = len(pc_list)
    nblk1 = len(blocks) * ngroups
    PC1 = max(1, (npc * 5 // 9 + nblk1 - 1) // nblk1) if npc else 0
    for g in range(ngroups):
        for tt in range(G):
            t = g * G + tt
            b = cnt["x"] % 2
            cnt["x"] += 1
            S.dma("sp", lambda e, b=b, t=t: e.dma_start(out=xt[b], in_=xkv[t * 128:(t + 1) * 128, :]),
                  writes=[t_xt[b]])
            rms_rstd(xt[b], ss[b], rstd[b], D, [t_xt[b]], t_ss[b], xn[b], t_xn[b])
            S.op("dve", lambda e, b=b: e.scalar_tensor_tensor(out=xn[b], in0=xt[b], scalar=rstd[b][:, 0:1],
                                                             in1=anw_b, op0=ALU.mult, op1=ALU.mult),
                 reads=[t_xt[b], t_ss[b], t_c1], writes=[t_xn[b]])
            transposes_to(xn[b], t_xn[b], KC,
                          lambda c, n, tt=tt: xnT3[:, c:c + n, tt * 128:(tt + 1) * 128],
                          t_xnT[tt], (0, 1), True, ident_b)
            for kc in range(KC):
                S.op("pe", lambda e, kc=kc, tt=tt: e.matmul(bank(7, HB), xnT3[:, kc, tt * 128:(tt + 1) * 128],
                                                            r3(wf, HB)[:, kc, :], start=(kc == 0), stop=(kc == KC - 1)),
                     reads=[t_xnT[tt], t_c1], writes=[psk[7]], signal=(kc == KC - 1))
            S.op("dve", lambda e, t=t: e.tensor_tensor(out=f_all3[:, t, :], in0=bank(7, HB), in1=bf_b, op=ALU.add),
                 reads=[psk[7], t_c1], writes=[t_fall])
        if debug and g == 0:
            S.dma("sp", lambda e: e.dma_start(out=xnT_dbg, in_=xnT), reads=t_xnT)
        for (kind, c0, meta) in blocks:
            wb = cnt["w"] % 2
            cnt["w"] += 1
            S.dma("pool", lambda e, wb=wb, c0=c0: e.dma_start(
                out=r3(wblk[wb], BW), in_=w_in[:, c0:c0 + BW].rearrange("(kc p) c -> p kc c", p=128)),
                writes=[t_wblk[wb]])
            precast(PC1)
            tiles = range(G) if kind in ("k", "v") else range(1, G, 2)
            for tt in tiles:
                t = g * G + tt
                j = (t - 1) // 2
                pb = PROJ_BANKS[cnt["ps"] % 3]
                cnt["ps"] += 1
                for kc in range(KC):
                    S.op("pe", lambda e, kc=kc, tt=tt, wb=wb, pb=pb: e.matmul(
                        bank(pb, BW), xnT3[:, kc, tt * 128:(tt + 1) * 128], r3(wblk[wb], BW)[:, kc, :],
                        start=(kc == 0), stop=(kc == KC - 1)),
                        reads=[t_xnT[tt], t_wblk[wb]], writes=[psk[pb]], signal=(kc == KC - 1))
                if kind in ("q", "k"):
                    row, h0 = meta
                    qb = cnt["q"] % 2
                    cnt["q"] += 1
                    S.op("act", lambda e, qb=qb, pb=pb: e.activation(out=qf[qb], in_=bank(pb, BW), func=AF.Copy),
                         reads=[psk[pb]], writes=[t_qf[qb]])
                    for hh in range(HPB):
                        S.op("dve", lambda e, qb=qb, hh=hh: e.scalar_tensor_tensor(out=qn[qb][:, hh * 128:(hh + 1) * 128], in0=qf[qb][:, hh * 128:(hh + 1) * 128], scalar=1.0, in1=qf[qb][:, hh * 128:(hh + 1) * 128], op0=ALU.mult, op1=ALU.mult, accum_out=ssq[qb][:, hh:hh + 1]),
                            reads=[t_qf[qb]], writes=[t_sq[qb], t_qn[qb]])
                    S.op("dve", lambda e, qb=qb: e.tensor_scalar(out=ssq[qb], in0=ssq[qb], scalar1=1.0 / 128,
                                                                 scalar2=NORM_EPS, op0=ALU.mult, op1=ALU.add),
                         reads=[t_sq[qb]], writes=[t_sq[qb]])
                    S.op("act", lambda e, qb=qb: e.activation(out=ssq[qb], in_=ssq[qb], func=AF.Sqrt),
                         reads=[t_sq[qb]], writes=[t_sq[qb]])
                    S.op("dve", lambda e, qb=qb: e.reciprocal(out=rq[qb], in_=ssq[qb]),
                         reads=[t_sq[qb]], writes=[t_sq[qb]])
                    for hh in range(HPB):
                        S.op("dve", lambda e, qb=qb, hh=hh, row=row: e.scalar_tensor_tensor(
                            out=qn[qb][:, hh * 128:(hh + 1) * 128], in0=qf[qb][:, hh * 128:(hh + 1) * 128],
                            scalar=rq[qb][:, hh:hh + 1], in1=qkn_b[:, row * BW + hh * 128:row * BW + (hh + 1) * 128],
                            op0=ALU.mult, op1=ALU.mult),
                            reads=[t_qf[qb], t_sq[qb], t_c1], writes=[t_qn[qb]])
                    if kind == "k":
                        col = tt
                    else:
                        col = (tt - 1) // 2
                    def _tr(qb=qb, col=col):
                        transposes_to(qn[qb], t_qn[qb], HPB,
                                      lambda c, n, col=col: stageT3[:, c:c + n, col * 128:(col + 1) * 128],
                                      t_stT, (5, 6), True, ident_b)
                    if pend1[0] is not None:
                        pend1[0]()
                    pend1[0] = _tr
                elif kind == "v":
                    sv = cnt["sv"] % 2
                    cnt["sv"] += 1
                    S.op("act", lambda e, sv=sv, pb=pb: e.activation(out=stageV[sv], in_=bank(pb, BW), func=AF.Copy),
                         reads=[psk[pb]], writes=[t_stV[sv]])
                    S.dma("act", lambda e, sv=sv, t=t, meta=meta: e.dma_start(
                        out=V_d[t * 128:(t + 1) * 128, meta:meta + BW], in_=stageV[sv]),
                        reads=[t_stV[sv]], writes=[t_dram1])
                else:
                    sv = cnt["sv"] % 2
                    cnt["sv"] += 1
                    S.op("act", lambda e, sv=sv, pb=pb: e.activation(out=stageV[sv], in_=bank(pb, BW), func=AF.Sigmoid),
                         reads=[psk[pb]], writes=[t_stV[sv]])
                    S.dma("act", lambda e, sv=sv, j=j, meta=meta: e.dma_start(
                        out=sg_d[j * 128:(j + 1) * 128, meta:meta + BW], in_=stageV[sv]),
                        reads=[t_stV[sv]], writes=[t_dram1])
            if pend1[0] is not None:
                pend1[0]()
                pend1[0] = None
            if kind == "k":
                row, h0 = meta
                S.dma("sp", lambda e, h0=h0, g=g: e.dma_start(
                    out=kT_d[h0:h0 + HPB, :, g * G * 128:(g + 1) * G * 128].rearrange("h p t -> p h t"),
                    in_=stageT3), reads=[t_stT], writes=[t_dram1])
            elif kind == "q":
                row, h0 = meta
                GH = G // 2
                S.dma("sp", lambda e, h0=h0, g=g, GH=GH: e.dma_start(
                    out=qT_d[h0:h0 + HPB, :, g * GH * 128:(g + 1) * GH * 128].rearrange("h p t -> p h t"),
                    in_=stageT3[:, :, 0:GH * 128]), reads=[t_stT], writes=[t_dram1])

    S.op("act", lambda e: e.activation(out=lf, in_=f_all, func=AF.Sigmoid), reads=[t_fall], writes=[t_fall])
    S.op("act", lambda e: e.activation(out=lf, in_=lf, func=AF.Ln), reads=[t_fall], writes=[t_fall])
    S.barrier()
    psk = new_ps()
    A.release(m1)

    lf3 = r3(lf, HB)
    cum3 = r3(cum, NS)
    base3 = r3(base, HB)
    t_cum = S.tok("cum")
    t_base = S.tok("base")
    S.op("dve", lambda e: e.memset(base3[:, 0, :], 0.0), writes=[t_base])
    for i in range(NS):
        bk = i % 2
        S.op("pe", lambda e, i=i, bk=bk: e.matmul(bank(bk, HB), tri_incl_f, lf3[:, i, :], start=True, stop=True),
             writes=[psk[bk]], signal=False)
        S.op("pe", lambda e, i=i, bk=bk: e.matmul(bank(bk, HB, HB), ones_f, lf3[:, i, :], start=True, stop=True),
             writes=[psk[bk]])
        S.op("dve", lambda e, i=i, bk=bk: e.tensor_tensor(out=cum3[:, :, i], in0=bank(bk, HB), in1=base3[:, i, :],
                                                          op=ALU.add),
             reads=[psk[bk], t_base], writes=[t_cum])
        S.op("dve", lambda e, i=i, bk=bk: e.tensor_tensor(out=base3[:, i + 1, :], in0=bank(bk, HB, HB),
                                                          in1=base3[:, i, :], op=ALU.add),
             reads=[psk[bk], t_base], writes=[t_base])
    if debug:
        S.dma("sp", lambda e: e.dma_start(out=cum_dbg, in_=cum), reads=[t_cum])
    S.barrier()
    psk = new_ps()

    m3 = A.mark()
    yT_all = A.bf16(H * NQ * 128)
    yT3 = r3(yT_all, NQ * 128)
    m3b = A.mark()
    Etab = A.bf16(HA * 640)
    Etab3 = r3(Etab, 640)
    tmpE = [A.f32(640) for _ in range(2)]
    VW = 130
    vaug = [A.bf16(NS * VW) for _ in range(2)]
    kTh = [A.bf16(NS * 128) for _ in range(2)]
    qTh = [A.bf16(NQ * 128) for _ in range(2)]
    pexp = [A.bf16(640) for _ in range(2)]
    pT = [A.bf16(640) for _ in range(2)]
    biasB = [A.f32(NS) for _ in range(2)]
    rinv = [A.f32(2) for _ in range(2)]
    ytile = [A.bf16(128) for _ in range(2)]

    t_E = S.tok("E")
    t_tmpE = S.toks_n(2, "tmpE")
    t_vaug = S.toks_n(2, "vaug")
    t_kTh = S.toks_n(2, "kTh")
    t_qTh = S.toks_n(2, "qTh")
    t_pexp = S.toks_n(2, "pexp")
    t_pT = S.toks_n(2, "pT")
    t_bias = S.toks_n(2, "bias")
    t_rinv = S.toks_n(2, "rinv")
    t_yt = S.toks_n(2, "yt")
    t_yT = S.toks_n(NQ, "yT")

    for h in range(HA):
        b = h % 2
        S.dma("sp", lambda e, b=b, h=h: e.dma_start(out=tmpE[b], in_=btab[:, h, :]), writes=[t_tmpE[b]])
        S.op("act", lambda e, b=b, h=h: e.activation(out=Etab3[:, h, :], in_=tmpE[b], func=AF.Exp),
             reads=[t_tmpE[b]], writes=[t_E])
    for b in range(2):
        S.op("dve", lambda e, b=b: e.tensor_copy(out=r3(vaug[b], VW)[:, :, 128], in_=valid_s), writes=[t_vaug[b]])

    cnt3 = {"s": 0, "o": 0, "p": 0, "y": 0, "b": 0}

    def load_head(hidx, b):
        S.dma("sp", lambda e: e.dma_start(out=kTh[b], in_=kT_d[hidx, :, :]), writes=[t_kTh[b]])
        S.dma("sp", lambda e: e.dma_start(out=qTh[b], in_=qT_d[hidx, :, :]), writes=[t_qTh[b]])
        S.dma("sp", lambda e: e.dma_start(
            out=r3(vaug[b], VW)[:, :, 0:128],
            in_=V_d[:, hidx * 128:(hidx + 1) * 128].rearrange("(i p) d -> p i d", p=128)),
            writes=[t_vaug[b]])

    pending = [None]
    pending2 = [None]
    PC3 = (max(0, npc - PC1 * nblk1) // 2 + H - 1) // H

    def flush():
        f2 = pending2[0]
        pending2[0] = None
        f1 = pending[0]
        pending[0] = None
        if f1 is not None:
            pending2[0] = f1()
        if f2 is not None:
            f2()

    def finalize(hidx, j, ob):
        yb = cnt3["y"] % 2
        cnt3["y"] += 1
        okb = 4 + ob
        S.op("dve", lambda e: e.reciprocal(out=rinv[yb][:, 0:1], in_=bank(okb, 1, 128)),
             reads=[psk[okb]], writes=[t_rinv[yb]])
        S.op("dve", lambda e: e.tensor_scalar(out=ytile[yb], in0=bank(okb, 128), scalar1=rinv[yb][:, 0:1],
                                              scalar2=None, op0=ALU.mult),
             reads=[psk[okb], t_rinv[yb]], writes=[t_yt[yb]])
        tbk = 6 + (yb % 2)

        def fin2():
            S.op("pe", lambda e: e.transpose(bank_bf(tbk, 128), ytile[yb], ident_b), reads=[t_yt[yb]],
                 writes=[psk[tbk]])
            S.op("dve", lambda e: e.tensor_copy(out=yT3[:, hidx, j * 128:(j + 1) * 128], in_=bank_bf(tbk, 128)),
                 reads=[psk[tbk]], writes=[t_yT[j]])
        return fin2

    for hidx in range(H):
        b = hidx % 2
        load_head(hidx, b)
        precast(PC3)
        isA = hidx < HA
        for j in range(NQ):
            iq = 2 * j + 1
            ob = cnt3["o"] % 2
            cnt3["o"] += 1
            if isA:
                rs = [r for r in range(5) if iq - 4 + r >= 0]
                groups = [[(iq - 4 + r, r) for r in rs]]
            else:
                hb_ = hidx - HA
                bb = cnt3["b"] % 2
                cnt3["b"] += 1
                S.op("dve", lambda e, hb_=hb_, bb=bb, j=j: e.tensor_scalar(
                    out=biasB[bb], in0=cum3[:, hb_, :], scalar1=-1.0, scalar2=base3[:, 2 * j + 2, hb_:hb_ + 1],
                    op0=ALU.mult, op1=ALU.add), writes=[t_bias[bb]])
                S.op("act", lambda e, bb=bb, iq=iq: e.activation(out=biasB[bb][:, 0:iq + 1], in_=biasB[bb][:, 0:iq + 1],
                                                                func=AF.Exp),
                     reads=[t_bias[bb]], writes=[t_bias[bb]])
                ks = list(range(0, iq + 1))
                groups = [[(i, i - g0) for i in ks[g0:g0 + 4]] for g0 in range(0, len(ks), 4)]
            ngr = len(groups)
            for gi, grp in enumerate(groups):
                sb = cnt3["s"] % 2
                cnt3["s"] += 1
                pb_ = cnt3["p"] % 2
                cnt3["p"] += 1
                for (i, r) in grp:
                    bkk = 2 * sb + (1 if r >= 4 else 0)
                    off = (r % 4) * 128
                    S.op("pe", lambda e, i=i, bkk=bkk, off=off, b=b, j=j: e.matmul(
                        bank(bkk, 128, off), kTh[b][:, i * 128:(i + 1) * 128], qTh[b][:, j * 128:(j + 1) * 128],
                        start=True, stop=True),
                        reads=[t_kTh[b], t_qTh[b]], writes=[psk[bkk]], signal=True)
                r0 = grp[0][1]
                r1 = grp[-1][1]
                if isA:
                    lo = r0 * 128
                    hi = min(r1 + 1, 4) * 128
                    S.op("act", lambda e, sb=sb, pb_=pb_, lo=lo, hi=hi: e.activation(
                        out=pexp[pb_][:, lo:hi], in_=ps_t[:, 2 * sb * 512 + lo:2 * sb * 512 + hi], func=AF.Exp,
                        scale=SCALE), reads=[psk[2 * sb]], writes=[t_pexp[pb_]])
                    if r1 >= 4:
                        S.op("act", lambda e, sb=sb, pb_=pb_: e.activation(
                            out=pexp[pb_][:, 512:640], in_=bank(2 * sb + 1, 128), func=AF.Exp, scale=SCALE),
                            reads=[psk[2 * sb + 1]], writes=[t_pexp[pb_]])
                    S.op("dve", lambda e, pb_=pb_, lo=lo, r1=r1, hidx=hidx: e.tensor_tensor(
                        out=pT[pb_][:, lo:(r1 + 1) * 128], in0=pexp[pb_][:, lo:(r1 + 1) * 128],
                        in1=Etab3[:, hidx, lo:(r1 + 1) * 128], op=ALU.mult),
                        reads=[t_pexp[pb_], t_E], writes=[t_pT[pb_]])
                else:
                    ng_ = len(grp)
                    S.op("act", lambda e, sb=sb, pb_=pb_, ng_=ng_: e.activation(
                        out=pexp[pb_][:, 0:ng_ * 128], in_=bank(2 * sb, ng_ * 128), func=AF.Exp, scale=SCALE),
                        reads=[psk[2 * sb]], writes=[t_pexp[pb_]])
                    for (i, r) in grp:
                        if i == iq:
                            S.op("dve", lambda e, i=i, r=r, pb_=pb_, bb=bb: e.scalar_tensor_tensor(
                                out=pT[pb_][:, r * 128:(r + 1) * 128], in0=pexp[pb_][:, r * 128:(r + 1) * 128],
                                scalar=biasB[bb][:, i:i + 1], in1=tri_incl_b, op0=ALU.mult, op1=ALU.mult),
                                reads=[t_pexp[pb_], t_bias[bb]], writes=[t_pT[pb_]])
                        else:
                            S.op("dve", lambda e, i=i, r=r, pb_=pb_, bb=bb: e.tensor_scalar(
                                out=pT[pb_][:, r * 128:(r + 1) * 128], in0=pexp[pb_][:, r * 128:(r + 1) * 128],
                                scalar1=biasB[bb][:, i:i + 1], scalar2=None, op0=ALU.mult),
                                reads=[t_pexp[pb_], t_bias[bb]], writes=[t_pT[pb_]])

                def pv(grp=grp, gi=gi, ngr=ngr, pb_=pb_, ob=ob, b=b, hidx=hidx, j=j):
                    okb = 4 + ob
                    for (i, r) in grp:
                        first = (gi == 0 and (i, r) == grp[0])
                        last = (gi == ngr - 1 and (i, r) == grp[-1])
                        S.op("pe", lambda e, i=i, r=r, first=first, last=last: e.matmul(
                            bank(okb, 129), pT[pb_][:, r * 128:(r + 1) * 128], r3(vaug[b], VW)[:, i, 0:129],
                            start=first, stop=last),
                            reads=[t_pT[pb_], t_vaug[b]], writes=[psk[okb]], signal=((i, r) == grp[-1]))
                    if gi == ngr - 1:
                        return finalize(hidx, j, ob)
                    return None
                flush()
                pending[0] = pv
    flush()
    flush()
    if debug:
        S.dma("sp", lambda e: e.dma_start(out=yT_dbg, in_=yT_all), reads=t_yT)
    S.barrier()
    psk = new_ps()
    A.release(m3b)

    t_yT = S.toks_n(NQ, "yT")
    Wa = A.bf16(HA * D)
    Wb = A.bf16(HB * D)
    Wa3 = r3(Wa, D)
    Wb3 = r3(Wb, D)
    sgt = [A.bf16(2 * D) for _ in range(2)]
    zt = [A.bf16(D) for _ in range(2)]
    t1 = [A.f32(BW) for _ in range(2)]
    t2 = [A.f32(BW) for _ in range(2)]
    t_W = S.tok("Wab")
    t_sgt = S.toks_n(2, "sgt")
    t_zt = S.toks_n(2, "zt")
    t_t1 = S.toks_n(2, "t1")
    t_t2 = S.toks_n(2, "t2")
    for c in range(HA):
        S.dma("pool", lambda e, c=c: e.dma_start(out=Wa3[:, c, :], in_=w_ba[c * 128:(c + 1) * 128, :]), writes=[t_W])
    for c in range(HB):
        S.dma("pool", lambda e, c=c: e.dma_start(out=Wb3[:, c, :], in_=w_bb[c * 128:(c + 1) * 128, :]), writes=[t_W])
    c4 = 0
    pend4 = [None]
    for j in range(NQ):
        b = j % 2
        S.dma("sp", lambda e, b=b, j=j: e.dma_start(out=sgt[b], in_=sg_d[j * 128:(j + 1) * 128, :]),
              writes=[t_sgt[b]])
        precast((len(pc_list) - pc_pos[0] + (NQ - j) - 1) // (NQ - j))
        for n in range(NB):
            ab = (c4 % 2) * 2
            tb = c4 % 2
            c4 += 1
            for c in range(HA):
                S.op("pe", lambda e, c=c, j=j, n=n, ab=ab: e.matmul(
                    bank(ab, BW), yT3[:, c, j * 128:(j + 1) * 128], Wa3[:, c, n * BW:(n + 1) * BW],
                    start=(c == 0), stop=(c == HA - 1)),
                    reads=[t_yT[j], t_W], writes=[psk[ab]], signal=(c == HA - 1))
            for c in range(HB):
                S.op("pe", lambda e, c=c, j=j, n=n, ab=ab: e.matmul(
                    bank(ab + 1, BW), yT3[:, HA + c, j * 128:(j + 1) * 128], Wb3[:, c, n * BW:(n + 1) * BW],
                    start=(c == 0), stop=(c == HB - 1)),
                    reads=[t_yT[j], t_W], writes=[psk[ab + 1]], signal=(c == HB - 1))
            S.op("dve", lambda e, b=b, n=n, ab=ab, tb=tb: e.tensor_tensor(
                out=t1[tb], in0=bank(ab, BW), in1=sgt[b][:, n * BW:(n + 1) * BW], op=ALU.mult),
                reads=[psk[ab], t_sgt[b]], writes=[t_t1[tb]])
            S.op("dve", lambda e, b=b, n=n, ab=ab, tb=tb: e.tensor_tensor(
                out=t2[tb], in0=bank(ab + 1, BW), in1=sgt[b][:, D + n * BW:D + (n + 1) * BW], op=ALU.mult),
                reads=[psk[ab + 1], t_sgt[b]], writes=[t_t2[tb]])
            S.op("dve", lambda e, b=b, n=n, tb=tb: e.tensor_tensor(
                out=zt[b][:, n * BW:(n + 1) * BW], in0=t1[tb], in1=t2[tb], op=ALU.add),
                reads=[t_t1[tb], t_t2[tb]], writes=[t_zt[b]])
        def _trz(b=b, j=j):
            transposes_to(zt[b], t_zt[b], KC, lambda c, n, j=j: yT3[:, c:c + n, j * 128:(j + 1) * 128],
                          t_yT[j], (4, 5), True, ident_b)
        if pend4[0] is not None:
            pend4[0]()
        pend4[0] = _trz
    pend4[0]()
    S.barrier()
    psk = new_ps()
    A.release(m3b)

    t_zT = S.toks_n(NQ, "zT")
    Wo = A.bf16(KC * D)
    Wo3 = r3(Wo, D)
    wr_f = A.f32(KC * E)
    wr3 = r3(wr_f, E)
    br_b = A.f32(E)
    fnw_b = A.f32(D)
    cb = [A.f32(E) for _ in range(2)]
    xt4 = [A.f32(D)] * 2
    hh_ = [A.f32(D) for _ in range(2)]
    hnf = hh_
    hnb = [A.bf16(D)] * 2
    hnT = A.f32(KC * 128)
    hnT3 = r3(hnT, 128)
    logit = A.f32(E)
    top8 = A.f32(8)
    negm = A.f32(2)
    mask = A.f32(E)
    ex = A.f32(E)
    exm = A.f32(E)
    ssum = A.f32(2)
    dest_e = A.f32(E)
    oh = A.f32(E)
    junkE = A.f32(E)
    destf = A.f32(4)
    ss4 = [A.f32(2) for _ in range(2)]
    rstd4 = [A.f32(2) for _ in range(2)]

    t_c4 = S.tok("c4")
    t_Wo = S.tok("Wo")
    for c in range(KC):
        S.dma("pool", lambda e, c=c: e.dma_start(out=Wo3[:, c, :], in_=w_out[c * 128:(c + 1) * 128, :]), writes=[t_Wo])
    S.dma("sp", lambda e: e.dma_start(out=wr3, in_=w_router.rearrange("(kc p) c -> p kc c", p=128)), writes=[t_c4])
    S.dma("sp", lambda e: e.dma_start(out=br_b, in_=b_router.partition_broadcast(128)), writes=[t_c4])
    S.dma("sp", lambda e: e.dma_start(out=fnw_b, in_=fnw.partition_broadcast(128)), writes=[t_c4])
    S.dma("sp", lambda e: e.dma_start(out=cb[0], in_=ebase_d), writes=[t_c4])
    t_xt4 = [S.tok("xt")] * 2
    t_h = S.toks_n(2, "h")
    t_hnf = t_h
    t_hnb = [S.tok("hnb")] * 2
    t_ss4 = S.toks_n(2, "ss4")
    t_hnT = S.tok("hnT")
    t_r = S.tok("route")
    t_cb = S.toks_n(2, "cb")
    t_ga = S.tok("gate_all")
    t_di = S.tok("dest_i")
    t_hbuf = S.tok("hbuf")
    t_out = S.tok("out")
    t_GdT = S.tok("GdT")
    gate3 = r3(gate_all, 4)
    desti3 = r3(dest_i, 4)
    for j in range(NQ):
        b = j % 2
        S.dma("sp", lambda e, b=b, j=j: e.dma_start(out=xt4[b], in_=xkv[(2 * j + 1) * 128:(2 * j + 2) * 128, :]),
              writes=[t_xt4[b]])
        for n in range(NB):
            for kc in range(KC):
                S.op("pe", lambda e, kc=kc, n=n, j=j: e.matmul(
                    bank(n, BW), yT3[:, kc, j * 128:(j + 1) * 128], Wo3[:, kc, n * BW:(n + 1) * BW],
                    start=(kc == 0), stop=(kc == KC - 1)),
                    reads=[t_zT[j], t_Wo], writes=[psk[n]], signal=(kc == KC - 1))
            S.op("dve", lambda e, b=b, n=n: e.tensor_tensor(
                out=hh_[b][:, n * BW:(n + 1) * BW], in0=bank(n, BW), in1=xt4[b][:, n * BW:(n + 1) * BW], op=ALU.add),
                reads=[psk[n], t_xt4[b]], writes=[t_h[b]])
        S.dma("sp", lambda e, b=b, j=j: e.dma_start(out=out[j * 128:(j + 1) * 128, :], in_=hh_[b]),
              reads=[t_h[b]], writes=[t_out])
        rms_rstd(hh_[b], ss4[b], rstd4[b], D, [t_h[b]], t_ss4[b], hnb[b], t_hnb[b])
        S.op("dve", lambda e, b=b: e.scalar_tensor_tensor(out=hnf[b], in0=hh_[b], scalar=rstd4[b][:, 0:1],
                                                         in1=fnw_b, op0=ALU.mult, op1=ALU.mult),
             reads=[t_h[b], t_ss4[b], t_c4], writes=[t_hnf[b]])
        S.op("act", lambda e, b=b: e.activation(out=hnb[b], in_=hnf[b], func=AF.Copy),
             reads=[t_hnf[b]], writes=[t_hnb[b]])
        tb_banks = (4, 5, 6, 7)
        transposes_to(hnf[b], t_hnf[b], KC, lambda c, n: hnT3[:, c:c + n, :], t_hnT, tb_banks, False, ident_f)
        LB = 4
        for kc in range(KC):
            S.op("pe", lambda e, kc=kc: e.matmul(bank(LB, E), hnT3[:, kc, :], wr3[:, kc, :],
                                                 start=(kc == 0), stop=(kc == KC - 1)),
                 reads=[t_hnT, t_c4], writes=[psk[LB]], signal=(kc == KC - 1))
        S.op("dve", lambda e: e.tensor_tensor(out=logit, in0=bank(LB, E), in1=br_b, op=ALU.add),
             reads=[psk[LB], t_c4], writes=[t_r])
        S.op("dve", lambda e: e.max(out=top8, in_=logit), reads=[t_r], writes=[t_r])
        S.op("dve", lambda e: e.tensor_scalar(out=mask, in0=logit, scalar1=top8[:, 3:4], scalar2=None,
                                              op0=ALU.is_ge), reads=[t_r], writes=[t_r])
        S.op("dve", lambda e: e.tensor_scalar(out=negm[:, 0:1], in0=top8[:, 0:1], scalar1=-1.0, scalar2=None,
                                              op0=ALU.mult), reads=[t_r], writes=[t_r])
        S.op("act", lambda e: e.activation(out=ex, in_=logit, func=AF.Exp, bias=negm[:, 0:1], scale=1.0),
             reads=[t_r], writes=[t_r])
        S.op("dve", lambda e: e.scalar_tensor_tensor(out=exm, in0=ex, scalar=1.0, in1=mask, op0=ALU.mult, op1=ALU.mult, accum_out=ssum[:, 0:1]),
             reads=[t_r], writes=[t_r])
        S.op("dve", lambda e: e.reciprocal(out=ssum[:, 0:1], in_=ssum[:, 0:1]), reads=[t_r], writes=[t_r])
        S.op("dve", lambda e, j=j: e.tensor_scalar(out=Gd3[:, j, :], in0=exm, scalar1=ssum[:, 0:1], scalar2=None,
                                                   op0=ALU.mult),
             reads=[t_r], writes=[t_r])
        PB = 5
        S.op("pe", lambda e: e.matmul(bank(PB, E), tri_strict_f, mask, start=True, stop=True),
             reads=[t_r], writes=[psk[PB]], signal=False)
        S.op("pe", lambda e: e.matmul(bank(PB, E, E), ones_f, mask, start=True, stop=True),
             reads=[t_r], writes=[psk[PB]])
        c0_, c1_ = cb[j % 2], cb[(j + 1) % 2]
        S.op("dve", lambda e, c0_=c0_: e.tensor_tensor(out=dest_e, in0=bank(PB, E), in1=c0_, op=ALU.add),
             reads=[psk[PB], t_cb[j % 2], t_c4], writes=[t_r])
        S.op("dve", lambda e, c0_=c0_, c1_=c1_: e.tensor_tensor(out=c1_, in0=bank(PB, E, E), in1=c0_, op=ALU.add),
             reads=[psk[PB], t_cb[j % 2], t_c4], writes=[t_cb[(j + 1) % 2]])
        for k in range(4):
            S.op("dve", lambda e, k=k: e.tensor_scalar(out=oh, in0=logit, scalar1=top8[:, k:k + 1], scalar2=None,
                                                       op0=ALU.is_equal), reads=[t_r], writes=[t_r])
            S.op("dve", lambda e, k=k: e.scalar_tensor_tensor(out=junkE, in0=oh, scalar=1.0, in1=dest_e, op0=ALU.mult, op1=ALU.mult, accum_out=destf[:, k:k + 1]),
                 reads=[t_r], writes=[t_r])
            S.op("dve", lambda e, k=k, j=j: e.scalar_tensor_tensor(out=junkE, in0=oh, scalar=1.0, in1=Gd3[:, j, :], op0=ALU.mult, op1=ALU.mult, accum_out=gate3[:, j, k:k + 1]),
                 reads=[t_r], writes=[t_r, t_ga])
        S.op("dve", lambda e, j=j: e.tensor_copy(out=desti3[:, j, :], in_=destf), reads=[t_r], writes=[t_di])
        for k in range(4):
            S.dma("pool", lambda e, b=b, j=j, k=k: e.indirect_dma_start(
                out=hbuf[:, :], out_offset=bass.IndirectOffsetOnAxis(ap=desti3[:, j, k:k + 1], axis=0),
                in_=hnb[b], in_offset=None), reads=[t_hnb[b], t_di], writes=[t_hbuf])
        if debug:
            S.dma("sp", lambda e, b=b, j=j: e.dma_start(out=h_dbg[j * 128:(j + 1) * 128, :], in_=hnf[b]),
                  reads=[t_hnf[b]])
    if debug:
        S.dma("sp", lambda e: e.dma_start(out=gate_dbg, in_=gate_all), reads=[t_ga])
        S.dma("sp", lambda e: e.dma_start(out=dest_dbg, in_=dest_i), reads=[t_di])
    S.barrier()
    psk = new_ps()
    A.release(m3)

    m5 = A.mark()
    NWB = 5
    wbk = [A.bf16(KC * BW) for _ in range(NWB)]
    xe = A.bf16(CT * D)
    xe3 = r3(xe, D)
    hTe = [A.bf16(KC * C)] * 2
    hbT = [A.bf16(KCE * C)] * 2
    SW = BW // 2
    NSTG = 4

    gsb = [A.f32(HPB * C) for _ in range(2)]
    gtmp = [A.f32(C) for _ in range(2)]
    stmp = [A.f32(C) for _ in range(2)]
    utmp = [A.f32(C) for _ in range(2)]
    ystage = [A.f32(BW) for _ in range(3)]
    bgu = A.f32(E * NGU)
    bgu3 = r3(bgu, NGU)
    t_bgu = S.tok("bgu")
    S.dma("sp", lambda e: e.dma_start(out=bgu, in_=bgu_t), writes=[t_bgu])
    t_wbk = S.toks_n(NWB, "wbk")
    t_xe = S.tok("xe")
    t_hTe = [S.tok("hTe")] * 2
    t_hbT = [S.tok("hbT")] * 2
    t_gsb = S.toks_n(2, "gsb")
    t_g = S.toks_n(2, "g")
    t_s = S.toks_n(2, "s")
    t_u = S.toks_n(2, "u")
    t_ys = S.toks_n(3, "ys")
    t_ybuf = S.tok("ybuf")
    c5 = {"w": 0, "gu": 0, "dn": 0, "t": 0, "ys": 0, "tr": 0}
    GU_BANKS = (0, 1, 2, 3)
    DN_BANKS = (4, 5)
    wsrc = []
    for ex2 in range(E):
        for bidx2 in range(DE // BW):
            for part2 in range(2):
                c0_ = part2 * DE + bidx2 * BW
                wsrc.append((w_gu, wgu_bf, ex2, c0_))
        for n2 in range(NB):
            wsrc.append((w_dn, wdn_bf, ex2, n2 * BW))
    wst = {"issued": 0, "half": 0}

    def ensure_weights(upto):
        while wst["issued"] <= min(upto, len(wsrc) - 1):
            i_ = wst["issued"]
            wst["issued"] += 1
            wsrc_f, wsrc_b, ex2, c0_ = wsrc[i_]
            wi2 = i_ % NWB
            if ex2 >= E - NP:
                S.dma("sp", lambda e, wi2=wi2, ex2=ex2, c0_=c0_, wsrc_b=wsrc_b: e.dma_start(
                    out=r3(wbk[wi2], BW),
                    in_=wsrc_b[ex2 - (E - NP), :, c0_:c0_ + BW].rearrange("(kc p) c -> p kc c", p=128)),
                    writes=[t_wbk[wi2]])
                continue
            S.dma("pool", lambda e, wi2=wi2, ex2=ex2, c0_=c0_, wsrc_f=wsrc_f: e.dma_start(
                out=r3(wbk[wi2], BW),
                in_=wsrc_f[ex2, :, c0_:c0_ + BW].rearrange("(kc p) c -> p kc c", p=128)),
                writes=[t_wbk[wi2]])

    widx = [0]
    for ex_ in range(E):
        eb = ex_ % 2
        nfull = C // 128
        if nfull:
            S.dma("sp", lambda e, ex_=ex_: e.dma_start(
                out=xe3[:, 0:nfull, :],
                in_=hbuf[ex_ * C:ex_ * C + nfull * 128, :].rearrange("(s p) d -> p s d", p=128)), writes=[t_xe])
        if C % 128:
            S.dma("sp", lambda e, ex_=ex_: e.dma_start(
                out=xe3[0:C % 128, nfull, :], in_=hbuf[ex_ * C + nfull * 128:(ex_ + 1) * C, :]), writes=[t_xe])
        he3 = r3(hTe[eb], C)
        for s_ in range(CT):
            np_ = min(128, C - s_ * 128)
            transposes_to(xe3[:, s_, :], t_xe, KC,
                          lambda c, n, s_=s_, he3=he3, np_=np_: he3[:, c:c + n, s_ * 128:s_ * 128 + np_],
                          t_hTe[eb], (6, 7), True, ident_b, npart=np_)
        hb3 = r3(hbT[eb], C)
        nblk = DE // BW
        for bidx in range(nblk):
            gsi = c5["t"] % 2
            c5["t"] += 1
            gs3 = r3(gsb[gsi], C)
            for part in range(2):
                wi = widx[0] % NWB
                ensure_weights(widx[0] + NWB - 1)
                widx[0] += 1
                for m in range(HPB):
                    gb_ = GU_BANKS[c5["gu"] % 4]
                    c5["gu"] += 1
                    for kc in range(KC):
                        S.op("pe", lambda e, kc=kc, wi=wi, m=m, gb_=gb_, he3=he3: e.matmul(
                            bank(gb_, C), r3(wbk[wi], BW)[:, kc, m * 128:(m + 1) * 128], he3[:, kc, :],
                            start=(kc == 0), stop=(kc == KC - 1)),
                            reads=[t_wbk[wi], t_hTe[eb]], writes=[psk[gb_]], signal=(kc == KC - 1))
                    chunk = bidx * HPB + m
                    ti = (c5["gu"]) % 2
                    if part == 0:
                        bcol = bgu3[:, ex_, chunk:chunk + 1]
                        S.op("dve", lambda e, gb_=gb_, ti=ti, bcol=bcol: e.tensor_scalar(
                            out=gtmp[ti], in0=bank(gb_, C), scalar1=bcol, scalar2=SWIGLU_LIMIT, op0=ALU.add,
                            op1=ALU.min), reads=[psk[gb_], t_bgu], writes=[t_g[ti]])
                        S.op("act", lambda e, ti=ti: e.activation(out=stmp[ti], in_=gtmp[ti], func=AF.Sigmoid,
                                                                  scale=SWIGLU_ALPHA),
                             reads=[t_g[ti]], writes=[t_s[ti]])
                        S.op("dve", lambda e, ti=ti, m=m, gs3=gs3: e.tensor_tensor(
                            out=gs3[:, m, :], in0=gtmp[ti], in1=stmp[ti], op=ALU.mult),
                            reads=[t_g[ti], t_s[ti]], writes=[t_gsb[gsi]])
                    else:
                        bcol = bgu3[:, ex_, KCE + chunk:KCE + chunk + 1]
                        S.op("dve", lambda e, gb_=gb_, ti=ti, bcol=bcol: e.tensor_scalar(
                            out=utmp[ti], in0=bank(gb_, C), scalar1=bcol, scalar2=SWIGLU_LIMIT, op0=ALU.add,
                            op1=ALU.min), reads=[psk[gb_], t_bgu], writes=[t_u[ti]])
                        S.op("dve", lambda e, ti=ti: e.tensor_scalar(
                            out=utmp[ti], in0=utmp[ti], scalar1=-SWIGLU_LIMIT, scalar2=1.0, op0=ALU.max,
                            op1=ALU.add), reads=[t_u[ti]], writes=[t_u[ti]])
                        S.op("dve", lambda e, ti=ti, m=m, gs3=gs3, chunk=chunk, hb3=hb3: e.tensor_tensor(
                            out=hb3[:, chunk, :], in0=utmp[ti], in1=gs3[:, m, :], op=ALU.mult),
                            reads=[t_u[ti], t_gsb[gsi]], writes=[t_hbT[eb]])
        for n in range(NB):
            wi = widx[0] % NWB
            ensure_weights(widx[0] + NWB - 1)
            widx[0] += 1
            for s_ in range(CT):
                db = DN_BANKS[c5["dn"] % 2]
                c5["dn"] += 1
                np_ = min(128, C - s_ * 128)
                for kc in range(KCE):
                    S.op("pe", lambda e, kc=kc, wi=wi, s_=s_, db=db, hb3=hb3, np_=np_: e.matmul(
                        bank(db, BW)[0:np_, :], hb3[:, kc, s_ * 128:s_ * 128 + np_], r3(wbk[wi], BW)[:, kc, :],
                        start=(kc == 0), stop=(kc == KCE - 1)),
                        reads=[t_wbk[wi], t_hbT[eb]], writes=[psk[db]], signal=(kc == KCE - 1))
                yi = c5["ys"] % 3
                c5["ys"] += 1
                S.op("act", lambda e, yi=yi, db=db, np_=np_: e.activation(
                    out=ystage[yi][0:np_, :], in_=bank(db, BW)[0:np_, :], func=AF.Copy),
                     reads=[psk[db]], writes=[t_ys[yi]])
                S.dma("act", lambda e, yi=yi, ex_=ex_, s_=s_, n=n, np_=np_: e.dma_start(
                    out=ybuf[ex_ * C + s_ * 128:ex_ * C + s_ * 128 + np_, n * BW:(n + 1) * BW],
                    in_=ystage[yi][0:np_, :]),
                    reads=[t_ys[yi]], writes=[t_ybuf])
    S.barrier()
    psk = new_ps()
    A.release(m5)

    hp = [A.f32(D) for _ in range(2)]
    yk = [[A.f32(D) for _ in range(4)] for _ in range(2)]
    bdn = A.f32(D)
    GdT = A.f32(128)
    t_bdn = S.tok("bdn")
    t_GdT = S.tok("GdT")
    S.dma("sp", lambda e: e.dma_start(out=bdn[0:E, :], in_=b_dn), writes=[t_bdn])
    t_hp = S.toks_n(2, "hp")
    t_yk = [S.toks_n(4, "yk%d" % i) for i in range(2)]
    t_out = S.tok("out")
    for j in range(NQ):
        b = j % 2
        S.dma("sp", lambda e, b=b, j=j: e.dma_start(out=hp[b], in_=out[j * 128:(j + 1) * 128, :]), writes=[t_hp[b]])
        GB = 6
        S.op("pe", lambda e, j=j: e.transpose(bank(GB, 128)[0:E, :], Gd3[:, j, :], ident_f), writes=[psk[GB]])
        S.op("act", lambda e: e.activation(out=GdT[0:E, :], in_=bank(GB, 128)[0:E, :], func=AF.Copy),
             reads=[psk[GB]], writes=[t_GdT])
        for n in range(NB):
            S.op("pe", lambda e, n=n: e.matmul(bank(n, BW), GdT[0:E, :], bdn[0:E, n * BW:(n + 1) * BW],
                                               start=True, stop=True),
                 reads=[t_GdT, t_bdn], writes=[psk[n]])
            S.op("dve", lambda e, n=n, b=b: e.tensor_tensor(
                out=hp[b][:, n * BW:(n + 1) * BW], in0=bank(n, BW), in1=hp[b][:, n * BW:(n + 1) * BW], op=ALU.add),
                reads=[psk[n], t_hp[b]], writes=[t_hp[b]])
        for k in range(4):
            S.dma("pool", lambda e, b=b, j=j, k=k: e.indirect_dma_start(
                out=yk[b][k], out_offset=None, in_=ybuf[:, :],
                in_offset=bass.IndirectOffsetOnAxis(ap=desti3[:, j, k:k + 1], axis=0)), writes=[t_yk[b][k]])
            S.op("dve", lambda e, b=b, j=j, k=k: e.scalar_tensor_tensor(
                out=hp[b], in0=yk[b][k], scalar=gate3[:, j, k:k + 1], in1=hp[b], op0=ALU.mult, op1=ALU.add),
                reads=[t_yk[b][k], t_hp[b]], writes=[t_hp[b]])
        S.dma("sp", lambda e, b=b, j=j: e.dma_start(out=out[j * 128:(j + 1) * 128, :], in_=hp[b]),
              reads=[t_hp[b]], writes=[t_out])
    S.barrier()

    blk = st.enter_context(nc.Block())

    @blk.tensor
    def _(e):
        for f in S.ops["pe"]:
            f(e)

    @blk.scalar
    def _(e):
        for f in S.ops["act"]:
            f(e)

    @blk.vector
    def _(e):
        for f in S.ops["dve"]:
            f(e)

    @blk.gpsimd
    def _(e):
        for f in S.ops["pool"]:
            f(e)

    @blk.sync
    def _(e):
        for f in S.ops["sp"]:
            f(e)

    st.close()
    return nc


def make_consts(cfg):
    t = np.arange(128)
    ident = np.eye(128, dtype=np.float32)
    tri_incl = (t[:, None] <= t[None, :]).astype(np.float32)
    tri_strict = (t[:, None] < t[None, :]).astype(np.float32)
    ones = np.ones((128, 128), np.float32)
    cst = np.concatenate([ident, tri_incl, tri_strict, ones], axis=1)
    ebase = np.tile((np.arange(cfg.E) * cfg.C).astype(np.float32)[None, :], (128, 1))
    return cst, ebase


def make_btab(rel_bias, cfg):
    HA = cfg.HA
    kk = np.arange(128)[:, None, None]
    r = np.arange(5)[None, :, None]
    qi = np.arange(128)[None, None, :]
    dist = (4 - r) * 128 + qi - kk
    qo = qi % 64
    ok = (dist >= -(63 - qo)) & (dist <= qo + 512)
    idx = np.clip(dist, -63, 256) + 63
    tab = rel_bias[:, idx]
    tab = np.where(ok[None], tab, np.float32(NEG)).astype(np.float32)
    return np.ascontiguousarray(tab.transpose(1, 0, 2, 3).reshape(128, HA, 640))


def prepare(cfg, inp):
    D, NS, E = cfg.D, cfg.NS, cfg.E
    x = np.asarray(inp["x"], np.float32)
    B = x.shape[0]
    cst, ebase = make_consts(cfg)
    btab = make_btab(np.asarray(inp["rel_bias"], np.float32), cfg)
    qkn = np.concatenate([np.tile(np.asarray(inp[k], np.float32), cfg.HPB)
                          for k in ("qn_a", "kn_a", "qn_b", "kn_b")])
    NGU = 2 * D // 128
    bgu_t = np.ascontiguousarray(
        np.asarray(inp["b_gate_up"], np.float32).reshape(E, NGU, 128).transpose(2, 0, 1).reshape(128, E * NGU))
    shared = {
        "w_in": np.asarray(inp["w_in"], np.float32),
        "attn_norm_w": np.asarray(inp["attn_norm_w"], np.float32),
        "ffn_norm_w": np.asarray(inp["ffn_norm_w"], np.float32),
        "qkn": qkn,
        "b_forget": np.asarray(inp["b_forget"], np.float32),
        "btab": btab,
        "w_branch_a": np.asarray(inp["w_branch_a"], np.float32),
        "w_branch_b": np.asarray(inp["w_branch_b"], np.float32),
        "w_out": np.asarray(inp["w_out"], np.float32),
        "w_router": np.asarray(inp["w_router"], np.float32),
        "b_router": np.asarray(inp["b_router"], np.float32),
        "w_gate_up": np.asarray(inp["w_gate_up"], np.float32),
        "bgu_t": bgu_t,
        "w_down": np.asarray(inp["w_down"], np.float32),
        "b_down": np.asarray(inp["b_down"], np.float32),
        "cst": cst,
        "ebase": ebase,
    }
    in_maps = []
    for c in range(2 * B):
        b, p = c // 2, c % 2
        if p == 0:
            xk = np.concatenate([np.zeros((128, D), np.float32), x[b, :(NS - 1) * 128]], axis=0)
            valid = np.ones((128, NS), np.float32)
            valid[:, 0] = 0.0
        else:
            xk = x[b, :NS * 128]
            valid = np.ones((128, NS), np.float32)
        m = dict(shared)
        m["xkv"] = np.ascontiguousarray(xk)
        m["valid"] = valid
        in_maps.append(m)
    return in_maps


def assemble(cfg, results, B):
    D, NS, NQ = cfg.D, cfg.NS, cfg.NQ
    y = np.zeros((B, NS * 128, D), np.float32)
    for c in range(2 * B):
        b, p = c // 2, c % 2
        o = np.asarray(results[c]["out"]).reshape(NQ, 128, D)
        yv = y[b].reshape(NS, 128, D)
        for j in range(NQ):
            yv[2 * j + p] = o[j]
    return y


_CACHE = {}


def kernel(**inputs):
    cfg = Cfg()
    if "nc" not in _CACHE:
        _CACHE["nc"] = build(cfg)
    nc = _CACHE["nc"]
    in_maps = prepare(cfg, inputs)
    res = run_bass_kernel_spmd(nc, in_maps, core_ids=list(range(cfg.n_cores)))
    return assemble(cfg, res.results, 4)
```

```python
import numpy as np
from contextlib import ExitStack
import concourse.bass as bass
import concourse.mybir as mybir
from concourse.alu_op_type import AluOpType as ALU
from concourse.bass_utils import run_bass_kernel_spmd

F32 = mybir.dt.float32
BF16 = mybir.dt.bfloat16
I32 = mybir.dt.int32
AF = mybir.ActivationFunctionType
AX = mybir.AxisListType

NORM_EPS = 1e-5
SWIGLU_LIMIT = 7.0
SWIGLU_ALPHA = 1.702
NEG = -30000.0


class Cfg:
    def __init__(self, D=2048, HA=8, HB=8, NS=32, E=32, C=320, G=16, n_cores=8, NP=0):
        self.D = D
        self.KC = D // 128
        self.HA = HA
        self.HB = HB
        self.H = HA + HB
        self.NS = NS
        self.NQ = NS // 2
        self.E = E
        self.C = C
        self.CT = (C + 127) // 128
        self.NP = NP
        self.G = G
        self.WA = HA * 128
        self.WB = HB * 128
        self.BW = min(512, self.WA, self.WB, D)
        self.HPB = self.BW // 128
        self.NB = D // self.BW
        self.INC = 3 * self.WA + 3 * self.WB + HB + 2 * D
        self.n_cores = n_cores


class Tok:
    __slots__ = ("w", "r", "name")

    def __init__(self, name=""):
        self.w = None
        self.r = []
        self.name = name


ENGS = ("pe", "act", "dve", "pool", "sp")
NDMA = 8


class Sched:
    def __init__(self, nc, st):
        self.nc = nc
        self.ops = {e: [] for e in ENGS}
        self.seq = {e: 0 for e in ENGS}
        self.sem = {e: st.enter_context(nc.semaphore("s_" + e)) for e in ENGS}
        self.semid = {}
        self.waited = {e: {} for e in ENGS}
        self.dsem = {}
        for q in ("sp", "act", "pool", "poolpc"):
            self.dsem[q] = [[st.enter_context(nc.semaphore("d_%s%d" % (q, i))), 0] for i in range(NDMA)]
        self.drr = {q: 0 for q in self.dsem}
        self.unsig = {e: False for e in ENGS}
        self.toks = []

    def tok(self, name=""):
        t = Tok(name)
        self.toks.append(t)
        return t

    def toks_n(self, n, name=""):
        return [self.tok(name + str(i)) for i in range(n)]

    def _key(self, sem):
        return id(sem)

    def _need(self, eng, deps):
        for (sem, val, src) in deps:
            if src == "pe" and eng == "pe":
                continue
            k = self._key(sem)
            if self.waited[eng].get(k, 0) < val:
                self.waited[eng][k] = val
                self.ops[eng].append(lambda e, sem=sem, val=val: e.wait_ge(sem, val))

    def _deps(self, reads, writes):
        deps = []
        for b in reads:
            if b.w is not None:
                deps.append(b.w)
        for b in writes:
            if b.w is not None:
                deps.append(b.w)
            deps.extend(b.r)
        return deps

    def _mark(self, reads, writes, t):
        for b in reads:
            b.r.append(t)
        for b in writes:
            b.w = t
            b.r = []

    def op(self, eng, fn, reads=(), writes=(), signal=True):
        self._need(eng, self._deps(reads, writes))
        sem = self.sem[eng]
        if signal:
            self.seq[eng] += 1
            t = (sem, self.seq[eng], eng)
            self.ops[eng].append(lambda e, fn=fn, sem=sem: fn(e).then_inc(sem, 1))
            self.unsig[eng] = False
        else:
            t = (sem, self.seq[eng] + 1, eng)
            self.ops[eng].append(lambda e, fn=fn: fn(e))
            self.unsig[eng] = True
        self._mark(reads, writes, t)

    def dma(self, q, fn, reads=(), writes=(), key=None):
        key = key or q
        self._need(q, self._deps(reads, writes))
        slot = self.dsem[key][self.drr[key] % NDMA]
        self.drr[key] += 1
        sem, uses = slot
        if uses > 0:
            self._need(q, [(sem, 16 * uses, "dma")])
        slot[1] = uses + 1
        t = (sem, 16 * (uses + 1), "dma")
        self.ops[q].append(lambda e, fn=fn, sem=sem: fn(e).then_inc(sem, 16))
        self._mark(reads, writes, t)

    def barrier(self):
        for e in ENGS:
            assert not self.unsig[e], e
        deps = []
        for e in ENGS:
            if self.seq[e] > 0:
                deps.append((self.sem[e], self.seq[e], "x"))
        for q in self.dsem:
            for sem, uses in self.dsem[q]:
                if uses > 0:
                    deps.append((sem, 16 * uses, "dma"))
        for e in ENGS:
            self._need(e, deps)
        for t in self.toks:
            t.w = None
            t.r = []
        self.toks = []


class Arena:
    def __init__(self, ap, nfloats):
        self.ap = ap
        self.n = nfloats
        self.off = 0

    def mark(self):
        return self.off

    def release(self, m):
        self.off = m

    def f32(self, n):
        n = (n + 1) // 2 * 2
        assert self.off + n <= self.n, ("arena overflow", self.off, n, self.n)
        a = self.ap[:, self.off:self.off + n]
        self.off += n
        return a

    def bf16(self, n):
        n = (n + 3) // 4 * 4
        return self.f32(n // 2).bitcast(BF16)

    def i32(self, n):
        return self.f32(n).bitcast(I32)


def r3(ap, b):
    return ap.rearrange("p (a b) -> p a b", b=b)


def build(cfg, debug=False):
    D, KC, HA, HB, H, NS, NQ, E, C, CT, G = (cfg.D, cfg.KC, cfg.HA, cfg.HB, cfg.H, cfg.NS, cfg.NQ,
                                             cfg.E, cfg.C, cfg.CT, cfg.G)
    WA, WB, BW, HPB, NB, INC = cfg.WA, cfg.WB, cfg.BW, cfg.HPB, cfg.NB, cfg.INC
    DE = D
    KCE = DE // 128
    NGU = 2 * DE // 128
    SCALE = 128.0 ** -0.5
    nc = bass.Bass("TRN2", target_bir_lowering=False)

    def din(name, shape, dt=F32):
        return nc.dram_tensor(name, list(shape), dt, kind="ExternalInput").ap()

    def dscr(name, shape, dt):
        kind = "ExternalOutput" if debug else "Internal"
        return nc.dram_tensor(name, list(shape), dt, kind=kind).ap()

    xkv = din("xkv", [NS * 128, D])
    valid_d = din("valid", [128, NS])
    w_in = din("w_in", [D, INC])
    anw = din("attn_norm_w", [D])
    fnw = din("ffn_norm_w", [D])
    qkn = din("qkn", [4 * BW])
    b_forget = din("b_forget", [HB])
    btab = din("btab", [128, HA, 5 * 128])
    w_ba = din("w_branch_a", [WA, D])
    w_bb = din("w_branch_b", [WB, D])
    w_out = din("w_out", [D, D])
    w_router = din("w_router", [D, E])
    b_router = din("b_router", [E])
    w_gu = din("w_gate_up", [E, D, 2 * DE])
    bgu_t = din("bgu_t", [128, E * NGU])
    w_dn = din("w_down", [E, DE, D])
    b_dn = din("b_down", [E, D])
    cst = din("cst", [128, 4 * 128])
    ebase_d = din("ebase", [128, E])
    out = nc.dram_tensor("out", [NQ * 128, D], F32, kind="ExternalOutput").ap()

    kT_d = dscr("kT_d", [H, 128, NS * 128], BF16)
    qT_d = dscr("qT_d", [H, 128, NQ * 128], BF16)
    V_d = dscr("V_d", [NS * 128, WA + WB], BF16)
    sg_d = dscr("sg_d", [NQ * 128, 2 * D], BF16)
    hbuf = dscr("hbuf", [E * C + 128, D], BF16)
    ybuf = dscr("ybuf", [E * C + 128, D], F32)
    NP = cfg.NP
    wgu_bf = nc.dram_tensor("wgu_bf", [max(NP, 1), D, 2 * DE], BF16, kind="Internal").ap()
    wdn_bf = nc.dram_tensor("wdn_bf", [max(NP, 1), DE, D], BF16, kind="Internal").ap()
    if debug:
        yT_dbg = dscr("yT_dbg", [128, H * NQ * 128], BF16)
        h_dbg = dscr("h_dbg", [NQ * 128, D], F32)
        gate_dbg = dscr("gate_dbg", [128, NQ * 4], F32)
        dest_dbg = dscr("dest_dbg", [128, NQ * 4], I32)
        cum_dbg = dscr("cum_dbg", [128, HB * NS], F32)
        xnT_dbg = dscr("xnT_dbg", [128, KC * G * 128], BF16)

    st = ExitStack()
    ARENA_F = 47000
    arena_t = st.enter_context(nc.sbuf_tensor("arena", [128, ARENA_F], F32))
    ps_t = st.enter_context(nc.psum_tensor("ps", [128, 4096], F32))
    S = Sched(nc, st)
    A = Arena(arena_t, ARENA_F)

    def bank(i, n=512, off=0):
        return ps_t[:, i * 512 + off:i * 512 + off + n]

    def bank_bf(i, n=1024, off=0):
        return ps_t[:, i * 512:(i + 1) * 512].bitcast(BF16)[:, off:off + n]

    psk = S.toks_n(8, "ps")

    pc_list = []
    for ep in range(NP):
        e_ = E - NP + ep
        cw = min(2048, 2 * DE)
        for q4 in range(4):
            r0, r1 = q4 * D // 4, (q4 + 1) * D // 4
            pc_list.append((wgu_bf[ep, r0:r1, :].rearrange("r (a b) -> r a b", b=cw),
                            w_gu[e_, r0:r1, :].rearrange("r (a b) -> r a b", b=cw)))
        cw = min(2048, D)
        for q2 in range(2):
            r0, r1 = q2 * DE // 2, (q2 + 1) * DE // 2
            pc_list.append((wdn_bf[ep, r0:r1, :].rearrange("r (a b) -> r a b", b=cw),
                            w_dn[e_, r0:r1, :].rearrange("r (a b) -> r a b", b=cw)))
    pc_pos = [0]

    def precast(n):
        for _ in range(n):
            if pc_pos[0] >= len(pc_list):
                return
            o_, i_ = pc_list[pc_pos[0]]
            pc_pos[0] += 1
            S.dma("pool", lambda e, o_=o_, i_=i_: e.dma_start(out=o_, in_=i_), key="poolpc")

    def new_ps():
        return S.toks_n(8, "ps")

    ident_f = A.f32(128)
    tri_incl_f = A.f32(128)
    tri_strict_f = A.f32(128)
    ones_f = A.f32(128)
    ident_b = A.bf16(128)
    tri_incl_b = A.bf16(128)
    valid_s = A.f32(NS)
    lf = A.f32(NS * HB)
    cum = A.f32(HB * NS)
    base = A.f32((NS + 1) * HB)
    gate_all = A.f32(NQ * 4)
    dest_i = A.i32(NQ * 4)
    Gd_all = A.f32(NQ * E)
    Gd3 = r3(Gd_all, E)
    tk_const = S.tok("const")

    cst_tmp = [ident_f, tri_incl_f, tri_strict_f, ones_f]
    for i, a in enumerate(cst_tmp):
        S.dma("sp", lambda e, a=a, i=i: e.dma_start(out=a, in_=cst[:, i * 128:(i + 1) * 128]), writes=[tk_const])
    S.dma("sp", lambda e: e.dma_start(out=valid_s, in_=valid_d), writes=[tk_const])
    S.op("dve", lambda e: e.tensor_copy(out=ident_b, in_=ident_f), reads=[tk_const], writes=[tk_const])
    S.op("dve", lambda e: e.tensor_copy(out=tri_incl_b, in_=tri_incl_f), reads=[tk_const], writes=[tk_const])
    S.barrier()
    psk = new_ps()

    m1 = A.mark()
    anw_b = A.f32(D)
    qkn_b = A.f32(4 * BW)
    bf_b = A.f32(HB)
    wf = A.bf16(KC * HB)
    xnT = A.bf16(KC * G * 128)
    xnT3 = r3(xnT, G * 128)
    xt = [A.f32(D) for _ in range(2)]
    xn = [A.bf16(D) for _ in range(2)]
    wblk = [A.bf16(KC * BW) for _ in range(2)]
    qf = [A.f32(BW) for _ in range(2)]
    qn = [A.bf16(BW) for _ in range(2)]
    stageT = A.bf16(HPB * G * 128)
    stageT3 = r3(stageT, G * 128)
    stageV = [A.bf16(BW) for _ in range(2)]
    ss = [A.f32(2) for _ in range(2)]
    rstd = [A.f32(2) for _ in range(2)]
    ssq = [A.f32(HPB) for _ in range(2)]
    rq = [A.f32(HPB) for _ in range(2)]
    f_all = A.f32(NS * HB)
    f_all3 = r3(f_all, HB)

    t_c1 = S.tok("c1")
    S.dma("sp", lambda e: e.dma_start(out=anw_b, in_=anw.partition_broadcast(128)), writes=[t_c1])
    S.dma("sp", lambda e: e.dma_start(out=qkn_b, in_=qkn.partition_broadcast(128)), writes=[t_c1])
    S.dma("sp", lambda e: e.dma_start(out=bf_b, in_=b_forget.partition_broadcast(128)), writes=[t_c1])
    fcol = 3 * WA + 3 * WB
    S.dma("pool", lambda e: e.dma_start(out=r3(wf, HB),
                                        in_=w_in[:, fcol:fcol + HB].rearrange("(kc p) c -> p kc c", p=128)),
          writes=[t_c1])

    t_xt = S.toks_n(2, "xt")
    t_xn = S.toks_n(2, "xn")
    t_ss = S.toks_n(2, "ss")
    t_xnT = S.toks_n(G, "xnT")
    t_wblk = S.toks_n(2, "wblk")
    t_qf = S.toks_n(2, "qf")
    t_qn = S.toks_n(2, "qn")
    t_sq = S.toks_n(2, "sq")
    t_stT = S.tok("stT")
    t_stV = S.toks_n(2, "stV")
    t_fall = S.tok("fall")
    t_dram1 = S.tok("dram1")

    blocks = []
    for hb_ in range(WA // BW):
        blocks.append(("q", 0 * WA + hb_ * BW, (0, hb_ * HPB)))
    for hb_ in range(WA // BW):
        blocks.append(("k", 1 * WA + hb_ * BW, (1, hb_ * HPB)))
    for hb_ in range(WA // BW):
        blocks.append(("v", 2 * WA + hb_ * BW, hb_ * BW))
    for hb_ in range(WB // BW):
        blocks.append(("q", 3 * WA + hb_ * BW, (2, HA + hb_ * HPB)))
    for hb_ in range(WB // BW):
        blocks.append(("k", 3 * WA + WB + hb_ * BW, (3, HA + hb_ * HPB)))
    for hb_ in range(WB // BW):
        blocks.append(("v", 3 * WA + 2 * WB + hb_ * BW, WA + hb_ * BW))
    gcol = 3 * WA + 3 * WB + HB
    for nb_ in range(2 * D // BW):
        blocks.append(("g", gcol + nb_ * BW, nb_ * BW))

    cnt = {"x": 0, "w": 0, "ps": 0, "q": 0, "sv": 0}
    PROJ_BANKS = (2, 3, 4)

    def rms_rstd(eng_src_ap, ss_ap, rstd_ap, n, toks_r, tok_ss, junk_ap, tok_junk):
        S.op("dve", lambda e: e.scalar_tensor_tensor(out=junk_ap, in0=eng_src_ap, scalar=1.0, in1=eng_src_ap, op0=ALU.mult, op1=ALU.mult, accum_out=ss_ap[:, 0:1]),
             reads=toks_r, writes=[tok_ss, tok_junk])
        S.op("dve", lambda e: e.tensor_scalar(out=ss_ap[:, 0:1], in0=ss_ap[:, 0:1], scalar1=1.0 / n,
                                              scalar2=NORM_EPS, op0=ALU.mult, op1=ALU.add),
             reads=[tok_ss], writes=[tok_ss])
        S.op("act", lambda e: e.activation(out=ss_ap[:, 0:1], in_=ss_ap[:, 0:1], func=AF.Sqrt),
             reads=[tok_ss], writes=[tok_ss])
        S.op("dve", lambda e: e.reciprocal(out=rstd_ap[:, 0:1], in_=ss_ap[:, 0:1]),
             reads=[tok_ss], writes=[tok_ss])

    def transposes_to(src_ap, src_tok, ncol_chunks, dst_fn, dst_tok, banks, dt_bf=True, ident=None, npart=128):
        per = 8 if dt_bf else 4
        c = 0
        bi = 0
        while c < ncol_chunks:
            n = min(per, ncol_chunks - c)
            bk = banks[bi % len(banks)]
            bi += 1
            for k in range(n):
                if dt_bf:
                    o = bank_bf(bk, 128, k * 128)
                else:
                    o = bank(bk, 128, k * 128)
                S.op("pe", lambda e, o=o, k=k, c=c: e.transpose(
                    o[:, 0:npart], src_ap[0:npart, (c + k) * 128:(c + k + 1) * 128], ident[0:npart, 0:npart]),
                     reads=[src_tok], writes=[psk[bk]], signal=(k == n - 1))
            if dt_bf:
                srcp = r3(bank_bf(bk, n * 128), 128)[:, :, 0:npart]
            else:
                srcp = r3(bank(bk, n * 128), 128)[:, :, 0:npart]
            S.op("act", lambda e, srcp=srcp, c=c, n=n: e.activation(out=dst_fn(c, n), in_=srcp, func=AF.Copy),
                 reads=[psk[bk]], writes=[dst_tok])
            c += n

    ngroups = NS // G
    pend1 = [None]
    npc = len(pc_list)
    nblk1 = len(blocks) * ngroups
    PC1 = max(1, (npc * 5 // 9 + nblk1 - 1) // nblk1) if npc else 0
    for g in range(ngroups):
        for tt in range(G):
            t = g * G + tt
            b = cnt["x"] % 2
            cnt["x"] += 1
            S.dma("sp", lambda e, b=b, t=t: e.dma_start(out=xt[b], in_=xkv[t * 128:(t + 1) * 128, :]),
                  writes=[t_xt[b]])
            rms_rstd(xt[b], ss[b], rstd[b], D, [t_xt[b]], t_ss[b], xn[b], t_xn[b])
            S.op("dve", lambda e, b=b: e.scalar_tensor_tensor(out=xn[b], in0=xt[b], scalar=rstd[b][:, 0:1],
                                                             in1=anw_b, op0=ALU.mult, op1=ALU.mult),
                 reads=[t_xt[b], t_ss[b], t_c1], writes=[t_xn[b]])
            transposes_to(xn[b], t_xn[b], KC,
                          lambda c, n, tt=tt: xnT3[:, c:c + n, tt * 128:(tt + 1) * 128],
                          t_xnT[tt], (0, 1), True, ident_b)
            for kc in range(KC):
                S.op("pe", lambda e, kc=kc, tt=tt: e.matmul(bank(7, HB), xnT3[:, kc, tt * 128:(tt + 1) * 128],
                                                            r3(wf, HB)[:, kc, :], start=(kc == 0), stop=(kc == KC - 1)),
                     reads=[t_xnT[tt], t_c1], writes=[psk[7]], signal=(kc == KC - 1))
            S.op("dve", lambda e, t=t: e.tensor_tensor(out=f_all3[:, t, :], in0=bank(7, HB), in1=bf_b, op=ALU.add),
                 reads=[psk[7], t_c1], writes=[t_fall])
        if debug and g == 0:
            S.dma("sp", lambda e: e.dma_start(out=xnT_dbg, in_=xnT), reads=t_xnT)
        for (kind, c0, meta) in blocks:
            wb = cnt["w"] % 2
            cnt["w"] += 1
            S.dma("pool", lambda e, wb=wb, c0=c0: e.dma_start(
                out=r3(wblk[wb], BW), in_=w_in[:, c0:c0 + BW].rearrange("(kc p) c -> p kc c", p=128)),
                writes=[t_wblk[wb]])
            precast(PC1)
            tiles = range(G) if kind in ("k", "v") else range(1, G, 2)
            for tt in tiles:
                t = g * G + tt
                j = (t - 1) // 2
                pb = PROJ_BANKS[cnt["ps"] % 3]
                cnt["ps"] += 1
                for kc in range(KC):
                    S.op("pe", lambda e, kc=kc, tt=tt, wb=wb, pb=pb: e.matmul(
                        bank(pb, BW), xnT3[:, kc, tt * 128:(tt + 1) * 128], r3(wblk[wb], BW)[:, kc, :],
                        start=(kc == 0), stop=(kc == KC - 1)),
                        reads=[t_xnT[tt], t_wblk[wb]], writes=[psk[pb]], signal=(kc == KC - 1))
                if kind in ("q", "k"):
                    row, h0 = meta
                    qb = cnt["q"] % 2
                    cnt["q"] += 1
                    S.op("act", lambda e, qb=qb, pb=pb: e.activation(out=qf[qb], in_=bank(pb, BW), func=AF.Copy),
                         reads=[psk[pb]], writes=[t_qf[qb]])
                    for hh in range(HPB):
                        S.op("dve", lambda e, qb=qb, hh=hh: e.scalar_tensor_tensor(out=qn[qb][:, hh * 128:(hh + 1) * 128], in0=qf[qb][:, hh * 128:(hh + 1) * 128], scalar=1.0, in1=qf[qb][:, hh * 128:(hh + 1) * 128], op0=ALU.mult, op1=ALU.mult, accum_out=ssq[qb][:, hh:hh + 1]),
                            reads=[t_qf[qb]], writes=[t_sq[qb], t_qn[qb]])
                    S.op("dve", lambda e, qb=qb: e.tensor_scalar(out=ssq[qb], in0=ssq[qb], scalar1=1.0 / 128,
                                                                 scalar2=NORM_EPS, op0=ALU.mult, op1=ALU.add),
                         reads=[t_sq[qb]], writes=[t_sq[qb]])
                    S.op("act", lambda e, qb=qb: e.activation(out=ssq[qb], in_=ssq[qb], func=AF.Sqrt),
                         reads=[t_sq[qb]], writes=[t_sq[qb]])
                    S.op("dve", lambda e, qb=qb: e.reciprocal(out=rq[qb], in_=ssq[qb]),
                         reads=[t_sq[qb]], writes=[t_sq[qb]])
                    for hh in range(HPB):
                        S.op("dve", lambda e, qb=qb, hh=hh, row=row: e.scalar_tensor_tensor(
                            out=qn[qb][:, hh * 128:(hh + 1) * 128], in0=qf[qb][:, hh * 128:(hh + 1) * 128],
                            scalar=rq[qb][:, hh:hh + 1], in1=qkn_b[:, row * BW + hh * 128:row * BW + (hh + 1) * 128],
                            op0=ALU.mult, op1=ALU.mult),
                            reads=[t_qf[qb], t_sq[qb], t_c1], writes=[t_qn[qb]])
                    if kind == "k":
                        col = tt
                    else:
                        col = (tt - 1) // 2
                    def _tr(qb=qb, col=col):
                        transposes_to(qn[qb], t_qn[qb], HPB,
                                      lambda c, n, col=col: stageT3[:, c:c + n, col * 128:(col + 1) * 128],
                                      t_stT, (5, 6), True, ident_b)
                    if pend1[0] is not None:
                        pend1[0]()
                    pend1[0] = _tr
                elif kind == "v":
                    sv = cnt["sv"] % 2
                    cnt["sv"] += 1
                    S.op("act", lambda e, sv=sv, pb=pb: e.activation(out=stageV[sv], in_=bank(pb, BW), func=AF.Copy),
                         reads=[psk[pb]], writes=[t_stV[sv]])
                    S.dma("act", lambda e, sv=sv, t=t, meta=meta: e.dma_start(
                        out=V_d[t * 128:(t + 1) * 128, meta:meta + BW], in_=stageV[sv]),
                        reads=[t_stV[sv]], writes=[t_dram1])
                else:
                    sv = cnt["sv"] % 2
                    cnt["sv"] += 1
                    S.op("act", lambda e, sv=sv, pb=pb: e.activation(out=stageV[sv], in_=bank(pb, BW), func=AF.Sigmoid),
                         reads=[psk[pb]], writes=[t_stV[sv]])
                    S.dma("act", lambda e, sv=sv, j=j, meta=meta: e.dma_start(
                        out=sg_d[j * 128:(j + 1) * 128, meta:meta + BW], in_=stageV[sv]),
                        reads=[t_stV[sv]], writes=[t_dram1])
            if pend1[0] is not None:
                pend1[0]()
                pend1[0] = None
            if kind == "k":
                row, h0 = meta
                S.dma("sp", lambda e, h0=h0, g=g: e.dma_start(
                    out=kT_d[h0:h0 + HPB, :, g * G * 128:(g + 1) * G * 128].rearrange("h p t -> p h t"),
                    in_=stageT3), reads=[t_stT], writes=[t_dram1])
            elif kind == "q":
                row, h0 = meta
                GH = G // 2
                S.dma("sp", lambda e, h0=h0, g=g, GH=GH: e.dma_start(
                    out=qT_d[h0:h0 + HPB, :, g * GH * 128:(g + 1) * GH * 128].rearrange("h p t -> p h t"),
                    in_=stageT3[:, :, 0:GH * 128]), reads=[t_stT], writes=[t_dram1])

    S.op("act", lambda e: e.activation(out=lf, in_=f_all, func=AF.Sigmoid), reads=[t_fall], writes=[t_fall])
    S.op("act", lambda e: e.activation(out=lf, in_=lf, func=AF.Ln), reads=[t_fall], writes=[t_fall])
    S.barrier()
    psk = new_ps()
    A.release(m1)

    lf3 = r3(lf, HB)
    cum3 = r3(cum, NS)
    base3 = r3(base, HB)
    t_cum = S.tok("cum")
    t_base = S.tok("base")
    S.op("dve", lambda e: e.memset(base3[:, 0, :], 0.0), writes=[t_base])
    for i in range(NS):
        bk = i % 2
        S.op("pe", lambda e, i=i, bk=bk: e.matmul(bank(bk, HB), tri_incl_f, lf3[:, i, :], start=True, stop=True),
             writes=[psk[bk]], signal=False)
        S.op("pe", lambda e, i=i, bk=bk: e.matmul(bank(bk, HB, HB), ones_f, lf3[:, i, :], start=True, stop=True),
             writes=[psk[bk]])
        S.op("dve", lambda e, i=i, bk=bk: e.tensor_tensor(out=cum3[:, :, i], in0=bank(bk, HB), in1=base3[:, i, :],
                                                          op=ALU.add),
             reads=[psk[bk], t_base], writes=[t_cum])
        S.op("dve", lambda e, i=i, bk=bk: e.tensor_tensor(out=base3[:, i + 1, :], in0=bank(bk, HB, HB),
                                                          in1=base3[:, i, :], op=ALU.add),
             reads=[psk[bk], t_base], writes=[t_base])
    if debug:
        S.dma("sp", lambda e: e.dma_start(out=cum_dbg, in_=cum), reads=[t_cum])
    S.barrier()
    psk = new_ps()

    m3 = A.mark()
    yT_all = A.bf16(H * NQ * 128)
    yT3 = r3(yT_all, NQ * 128)
    m3b = A.mark()
    Etab = A.bf16(HA * 640)
    Etab3 = r3(Etab, 640)
    tmpE = [A.f32(640) for _ in range(2)]
    VW = 130
    vaug = [A.bf16(NS * VW) for _ in range(2)]
    kTh = [A.bf16(NS * 128) for _ in range(2)]
    qTh = [A.bf16(NQ * 128) for _ in range(2)]
    pexp = [A.bf16(640) for _ in range(4)]
    pT = [A.bf16(640) for _ in range(4)]
    biasB = [A.f32(NS) for _ in range(2)]
    rinv = [A.f32(2) for _ in range(2)]
    ytile = [A.bf16(128) for _ in range(2)]

    t_E = S.tok("E")
    t_tmpE = S.toks_n(2, "tmpE")
    t_vaug = S.toks_n(2, "vaug")
    t_kTh = S.toks_n(2, "kTh")
    t_qTh = S.toks_n(2, "qTh")
    t_pexp = S.toks_n(4, "pexp")
    t_pT = S.toks_n(4, "pT")
    t_bias = S.toks_n(2, "bias")
    t_rinv = S.toks_n(2, "rinv")
    t_yt = S.toks_n(2, "yt")
    t_yT = S.toks_n(NQ, "yT")

    for h in range(HA):
        b = h % 2
        S.dma("sp", lambda e, b=b, h=h: e.dma_start(out=tmpE[b], in_=btab[:, h, :]), writes=[t_tmpE[b]])
        S.op("act", lambda e, b=b, h=h: e.activation(out=Etab3[:, h, :], in_=tmpE[b], func=AF.Exp),
             reads=[t_tmpE[b]], writes=[t_E])
    for b in range(2):
        S.op("dve", lambda e, b=b: e.tensor_copy(out=r3(vaug[b], VW)[:, :, 128], in_=valid_s), writes=[t_vaug[b]])

    cnt3 = {"s": 0, "o": 0, "p": 0, "y": 0, "b": 0}

    def load_head(hidx, b):
        S.dma("sp", lambda e: e.dma_start(out=kTh[b], in_=kT_d[hidx, :, :]), writes=[t_kTh[b]])
        S.dma("sp", lambda e: e.dma_start(out=qTh[b], in_=qT_d[hidx, :, :]), writes=[t_qTh[b]])
        S.dma("sp", lambda e: e.dma_start(
            out=r3(vaug[b], VW)[:, :, 0:128],
            in_=V_d[:, hidx * 128:(hidx + 1) * 128].rearrange("(i p) d -> p i d", p=128)),
            writes=[t_vaug[b]])

    pending = [None]
    pending2 = [None]
    PC3 = (max(0, npc - PC1 * nblk1) // 2 + H - 1) // H

    pvq = []

    def flush(depth=0):
        while len(pvq) > depth:
            f2 = pending2[0]
            pending2[0] = None
            f1 = pvq.pop(0)
            pending2[0] = f1()
            if f2 is not None:
                f2()
        if depth == 0 and pending2[0] is not None:
            f2 = pending2[0]
            pending2[0] = None
            f2()

    def finalize(hidx, j, ob):
        yb = cnt3["y"] % 2
        cnt3["y"] += 1
        okb = 4 + ob
        S.op("dve", lambda e: e.reciprocal(out=rinv[yb][:, 0:1], in_=bank(okb, 1, 128)),
             reads=[psk[okb]], writes=[t_rinv[yb]])
        S.op("dve", lambda e: e.tensor_scalar(out=ytile[yb], in0=bank(okb, 128), scalar1=rinv[yb][:, 0:1],
                                              scalar2=None, op0=ALU.mult),
             reads=[psk[okb], t_rinv[yb]], writes=[t_yt[yb]])
        tbk = 6 + (yb % 2)

        def fin2():
            S.op("pe", lambda e: e.transpose(bank_bf(tbk, 128), ytile[yb], ident_b), reads=[t_yt[yb]],
                 writes=[psk[tbk]])
            S.op("dve", lambda e: e.tensor_copy(out=yT3[:, hidx, j * 128:(j + 1) * 128], in_=bank_bf(tbk, 128)),
                 reads=[psk[tbk]], writes=[t_yT[j]])
        return fin2

    for hidx in range(H):
        b = hidx % 2
        load_head(hidx, b)
        precast(PC3)
        isA = hidx < HA
        for j in range(NQ):
            iq = 2 * j + 1
            ob = cnt3["o"] % 2
            cnt3["o"] += 1
            if isA:
                rs = [r for r in range(5) if iq - 4 + r >= 0]
                groups = [[(iq - 4 + r, r) for r in rs]]
            else:
                hb_ = hidx - HA
                bb = cnt3["b"] % 2
                cnt3["b"] += 1
                S.op("dve", lambda e, hb_=hb_, bb=bb, j=j: e.tensor_scalar(
                    out=biasB[bb], in0=cum3[:, hb_, :], scalar1=-1.0, scalar2=base3[:, 2 * j + 2, hb_:hb_ + 1],
                    op0=ALU.mult, op1=ALU.add), writes=[t_bias[bb]])
                S.op("act", lambda e, bb=bb, iq=iq: e.activation(out=biasB[bb][:, 0:iq + 1], in_=biasB[bb][:, 0:iq + 1],
                                                                func=AF.Exp),
                     reads=[t_bias[bb]], writes=[t_bias[bb]])
                ks = list(range(0, iq + 1))
                groups = [[(i, i - g0) for i in ks[g0:g0 + 4]] for g0 in range(0, len(ks), 4)]
            ngr = len(groups)
            for gi, grp in enumerate(groups):
                sbase = 2 * (cnt3["s"] % 2) if isA else cnt3["s"] % 4
                cnt3["s"] += 1
                pb_ = cnt3["p"] % 4
                cnt3["p"] += 1
                for (i, r) in grp:
                    bkk = sbase + (1 if r >= 4 else 0)
                    off = (r % 4) * 128
                    S.op("pe", lambda e, i=i, bkk=bkk, off=off, b=b, j=j: e.matmul(
                        bank(bkk, 128, off), kTh[b][:, i * 128:(i + 1) * 128], qTh[b][:, j * 128:(j + 1) * 128],
                        start=True, stop=True),
                        reads=[t_kTh[b], t_qTh[b]], writes=[psk[bkk]], signal=True)
                r0 = grp[0][1]
                r1 = grp[-1][1]
                if isA:
                    lo = r0 * 128
                    hi = min(r1 + 1, 4) * 128
                    S.op("act", lambda e, sbase=sbase, pb_=pb_, lo=lo, hi=hi: e.activation(
                        out=pexp[pb_][:, lo:hi], in_=ps_t[:, sbase * 512 + lo:sbase * 512 + hi], func=AF.Exp,
                        scale=SCALE), reads=[psk[sbase]], writes=[t_pexp[pb_]])
                    if r1 >= 4:
                        S.op("act", lambda e, sbase=sbase, pb_=pb_: e.activation(
                            out=pexp[pb_][:, 512:640], in_=bank(sbase + 1, 128), func=AF.Exp, scale=SCALE),
                            reads=[psk[sbase + 1]], writes=[t_pexp[pb_]])
                    S.op("dve", lambda e, pb_=pb_, lo=lo, r1=r1, hidx=hidx: e.tensor_tensor(
                        out=pT[pb_][:, lo:(r1 + 1) * 128], in0=pexp[pb_][:, lo:(r1 + 1) * 128],
                        in1=Etab3[:, hidx, lo:(r1 + 1) * 128], op=ALU.mult),
                        reads=[t_pexp[pb_], t_E], writes=[t_pT[pb_]])
                else:
                    ng_ = len(grp)
                    S.op("act", lambda e, sbase=sbase, pb_=pb_, ng_=ng_: e.activation(
                        out=pexp[pb_][:, 0:ng_ * 128], in_=bank(sbase, ng_ * 128), func=AF.Exp, scale=SCALE),
                        reads=[psk[sbase]], writes=[t_pexp[pb_]])
                    for (i, r) in grp:
                        if i == iq:
                            S.op("dve", lambda e, i=i, r=r, pb_=pb_, bb=bb: e.scalar_tensor_tensor(
                                out=pT[pb_][:, r * 128:(r + 1) * 128], in0=pexp[pb_][:, r * 128:(r + 1) * 128],
                                scalar=biasB[bb][:, i:i + 1], in1=tri_incl_b, op0=ALU.mult, op1=ALU.mult),
                                reads=[t_pexp[pb_], t_bias[bb]], writes=[t_pT[pb_]])
                        else:
                            S.op("dve", lambda e, i=i, r=r, pb_=pb_, bb=bb: e.tensor_scalar(
                                out=pT[pb_][:, r * 128:(r + 1) * 128], in0=pexp[pb_][:, r * 128:(r + 1) * 128],
                                scalar1=biasB[bb][:, i:i + 1], scalar2=None, op0=ALU.mult),
                                reads=[t_pexp[pb_], t_bias[bb]], writes=[t_pT[pb_]])

                def pv(grp=grp, gi=gi, ngr=ngr, pb_=pb_, ob=ob, b=b, hidx=hidx, j=j):
                    okb = 4 + ob
                    for (i, r) in grp:
                        first = (gi == 0 and (i, r) == grp[0])
                        last = (gi == ngr - 1 and (i, r) == grp[-1])
                        S.op("pe", lambda e, i=i, r=r, first=first, last=last: e.matmul(
                            bank(okb, 129), pT[pb_][:, r * 128:(r + 1) * 128], r3(vaug[b], VW)[:, i, 0:129],
                            start=first, stop=last),
                            reads=[t_pT[pb_], t_vaug[b]], writes=[psk[okb]], signal=((i, r) == grp[-1]))
                    if gi == ngr - 1:
                        return finalize(hidx, j, ob)
                    return None
                pvq.append(pv)
                flush(1 if isA else 3)
        if hidx == HA - 1:
            flush(0)
    flush(0)
    if debug:
        S.dma("sp", lambda e: e.dma_start(out=yT_dbg, in_=yT_all), reads=t_yT)
    S.barrier()
    psk = new_ps()
    A.release(m3b)

    t_yT = S.toks_n(NQ, "yT")
    Wa = A.bf16(HA * D)
    Wb = A.bf16(HB * D)
    Wa3 = r3(Wa, D)
    Wb3 = r3(Wb, D)
    sgt = [A.bf16(2 * D) for _ in range(2)]
    zt = [A.bf16(D) for _ in range(2)]
    t1 = [A.f32(BW) for _ in range(2)]
    t2 = [A.f32(BW) for _ in range(2)]
    t_W = S.tok("Wab")
    t_sgt = S.toks_n(2, "sgt")
    t_zt = S.toks_n(2, "zt")
    t_t1 = S.toks_n(2, "t1")
    t_t2 = S.toks_n(2, "t2")
    for c in range(HA):
        S.dma("pool", lambda e, c=c: e.dma_start(out=Wa3[:, c, :], in_=w_ba[c * 128:(c + 1) * 128, :]), writes=[t_W])
    for c in range(HB):
        S.dma("pool", lambda e, c=c: e.dma_start(out=Wb3[:, c, :], in_=w_bb[c * 128:(c + 1) * 128, :]), writes=[t_W])
    c4 = 0
    pend4 = [None]
    for j in range(NQ):
        b = j % 2
        S.dma("sp", lambda e, b=b, j=j: e.dma_start(out=sgt[b], in_=sg_d[j * 128:(j + 1) * 128, :]),
              writes=[t_sgt[b]])
        precast((len(pc_list) - pc_pos[0] + (NQ - j) - 1) // (NQ - j))
        for n in range(NB):
            ab = (c4 % 2) * 2
            tb = c4 % 2
            c4 += 1
            for c in range(HA):
                S.op("pe", lambda e, c=c, j=j, n=n, ab=ab: e.matmul(
                    bank(ab, BW), yT3[:, c, j * 128:(j + 1) * 128], Wa3[:, c, n * BW:(n + 1) * BW],
                    start=(c == 0), stop=(c == HA - 1)),
                    reads=[t_yT[j], t_W], writes=[psk[ab]], signal=(c == HA - 1))
            for c in range(HB):
                S.op("pe", lambda e, c=c, j=j, n=n, ab=ab: e.matmul(
                    bank(ab + 1, BW), yT3[:, HA + c, j * 128:(j + 1) * 128], Wb3[:, c, n * BW:(n + 1) * BW],
                    start=(c == 0), stop=(c == HB - 1)),
                    reads=[t_yT[j], t_W], writes=[psk[ab + 1]], signal=(c == HB - 1))
            S.op("dve", lambda e, b=b, n=n, ab=ab, tb=tb: e.tensor_tensor(
                out=t1[tb], in0=bank(ab, BW), in1=sgt[b][:, n * BW:(n + 1) * BW], op=ALU.mult),
                reads=[psk[ab], t_sgt[b]], writes=[t_t1[tb]])
            S.op("dve", lambda e, b=b, n=n, ab=ab, tb=tb: e.tensor_tensor(
                out=t2[tb], in0=bank(ab + 1, BW), in1=sgt[b][:, D + n * BW:D + (n + 1) * BW], op=ALU.mult),
                reads=[psk[ab + 1], t_sgt[b]], writes=[t_t2[tb]])
            S.op("dve", lambda e, b=b, n=n, tb=tb: e.tensor_tensor(
                out=zt[b][:, n * BW:(n + 1) * BW], in0=t1[tb], in1=t2[tb], op=ALU.add),
                reads=[t_t1[tb], t_t2[tb]], writes=[t_zt[b]])
        def _trz(b=b, j=j):
            transposes_to(zt[b], t_zt[b], KC, lambda c, n, j=j: yT3[:, c:c + n, j * 128:(j + 1) * 128],
                          t_yT[j], (4, 5), True, ident_b)
        if pend4[0] is not None:
            pend4[0]()
        pend4[0] = _trz
    pend4[0]()
    S.barrier()
    psk = new_ps()
    A.release(m3b)

    t_zT = S.toks_n(NQ, "zT")
    Wo = A.bf16(KC * D)
    Wo3 = r3(Wo, D)
    wr_f = A.f32(KC * E)
    wr3 = r3(wr_f, E)
    br_b = A.f32(E)
    fnw_b = A.f32(D)
    cb = [A.f32(E) for _ in range(2)]
    xt4 = [A.f32(D)] * 2
    hh_ = [A.f32(D) for _ in range(2)]
    hnf = hh_
    hnb = [A.bf16(D)] * 2
    hnT = A.f32(KC * 128)
    hnT3 = r3(hnT, 128)
    logit = A.f32(E)
    top8 = A.f32(8)
    negm = A.f32(2)
    mask = A.f32(E)
    ex = A.f32(E)
    exm = A.f32(E)
    ssum = A.f32(2)
    dest_e = A.f32(E)
    oh = A.f32(E)
    junkE = A.f32(E)
    destf = A.f32(4)
    ss4 = [A.f32(2) for _ in range(2)]
    rstd4 = [A.f32(2) for _ in range(2)]

    t_c4 = S.tok("c4")
    t_Wo = S.tok("Wo")
    for c in range(KC):
        S.dma("pool", lambda e, c=c: e.dma_start(out=Wo3[:, c, :], in_=w_out[c * 128:(c + 1) * 128, :]), writes=[t_Wo])
    S.dma("sp", lambda e: e.dma_start(out=wr3, in_=w_router.rearrange("(kc p) c -> p kc c", p=128)), writes=[t_c4])
    S.dma("sp", lambda e: e.dma_start(out=br_b, in_=b_router.partition_broadcast(128)), writes=[t_c4])
    S.dma("sp", lambda e: e.dma_start(out=fnw_b, in_=fnw.partition_broadcast(128)), writes=[t_c4])
    S.dma("sp", lambda e: e.dma_start(out=cb[0], in_=ebase_d), writes=[t_c4])
    t_xt4 = [S.tok("xt")] * 2
    t_h = S.toks_n(2, "h")
    t_hnf = t_h
    t_hnb = [S.tok("hnb")] * 2
    t_ss4 = S.toks_n(2, "ss4")
    t_hnT = S.tok("hnT")
    t_r = S.tok("route")
    t_cb = S.toks_n(2, "cb")
    t_ga = S.tok("gate_all")
    t_di = S.tok("dest_i")
    t_hbuf = S.tok("hbuf")
    t_out = S.tok("out")
    t_GdT = S.tok("GdT")
    gate3 = r3(gate_all, 4)
    desti3 = r3(dest_i, 4)
    for j in range(NQ):
        b = j % 2
        S.dma("sp", lambda e, b=b, j=j: e.dma_start(out=xt4[b], in_=xkv[(2 * j + 1) * 128:(2 * j + 2) * 128, :]),
              writes=[t_xt4[b]])
        for n in range(NB):
            for kc in range(KC):
                S.op("pe", lambda e, kc=kc, n=n, j=j: e.matmul(
                    bank(n, BW), yT3[:, kc, j * 128:(j + 1) * 128], Wo3[:, kc, n * BW:(n + 1) * BW],
                    start=(kc == 0), stop=(kc == KC - 1)),
                    reads=[t_zT[j], t_Wo], writes=[psk[n]], signal=(kc == KC - 1))
            S.op("dve", lambda e, b=b, n=n: e.tensor_tensor(
                out=hh_[b][:, n * BW:(n + 1) * BW], in0=bank(n, BW), in1=xt4[b][:, n * BW:(n + 1) * BW], op=ALU.add),
                reads=[psk[n], t_xt4[b]], writes=[t_h[b]])
        S.dma("sp", lambda e, b=b, j=j: e.dma_start(out=out[j * 128:(j + 1) * 128, :], in_=hh_[b]),
              reads=[t_h[b]], writes=[t_out])
        rms_rstd(hh_[b], ss4[b], rstd4[b], D, [t_h[b]], t_ss4[b], hnb[b], t_hnb[b])
        S.op("dve", lambda e, b=b: e.scalar_tensor_tensor(out=hnf[b], in0=hh_[b], scalar=rstd4[b][:, 0:1],
                                                         in1=fnw_b, op0=ALU.mult, op1=ALU.mult),
             reads=[t_h[b], t_ss4[b], t_c4], writes=[t_hnf[b]])
        S.op("act", lambda e, b=b: e.activation(out=hnb[b], in_=hnf[b], func=AF.Copy),
             reads=[t_hnf[b]], writes=[t_hnb[b]])
        tb_banks = (4, 5, 6, 7)
        transposes_to(hnf[b], t_hnf[b], KC, lambda c, n: hnT3[:, c:c + n, :], t_hnT, tb_banks, False, ident_f)
        LB = 4
        for kc in range(KC):
            S.op("pe", lambda e, kc=kc: e.matmul(bank(LB, E), hnT3[:, kc, :], wr3[:, kc, :],
                                                 start=(kc == 0), stop=(kc == KC - 1)),
                 reads=[t_hnT, t_c4], writes=[psk[LB]], signal=(kc == KC - 1))
        S.op("dve", lambda e: e.tensor_tensor(out=logit, in0=bank(LB, E), in1=br_b, op=ALU.add),
             reads=[psk[LB], t_c4], writes=[t_r])
        S.op("dve", lambda e: e.max(out=top8, in_=logit), reads=[t_r], writes=[t_r])
        S.op("dve", lambda e: e.tensor_scalar(out=mask, in0=logit, scalar1=top8[:, 3:4], scalar2=None,
                                              op0=ALU.is_ge), reads=[t_r], writes=[t_r])
        S.op("dve", lambda e: e.tensor_scalar(out=negm[:, 0:1], in0=top8[:, 0:1], scalar1=-1.0, scalar2=None,
                                              op0=ALU.mult), reads=[t_r], writes=[t_r])
        S.op("act", lambda e: e.activation(out=ex, in_=logit, func=AF.Exp, bias=negm[:, 0:1], scale=1.0),
             reads=[t_r], writes=[t_r])
        S.op("dve", lambda e: e.scalar_tensor_tensor(out=exm, in0=ex, scalar=1.0, in1=mask, op0=ALU.mult, op1=ALU.mult, accum_out=ssum[:, 0:1]),
             reads=[t_r], writes=[t_r])
        S.op("dve", lambda e: e.reciprocal(out=ssum[:, 0:1], in_=ssum[:, 0:1]), reads=[t_r], writes=[t_r])
        S.op("dve", lambda e, j=j: e.tensor_scalar(out=Gd3[:, j, :], in0=exm, scalar1=ssum[:, 0:1], scalar2=None,
                                                   op0=ALU.mult),
             reads=[t_r], writes=[t_r])
        PB = 5
        S.op("pe", lambda e: e.matmul(bank(PB, E), tri_strict_f, mask, start=True, stop=True),
             reads=[t_r], writes=[psk[PB]], signal=False)
        S.op("pe", lambda e: e.matmul(bank(PB, E, E), ones_f, mask, start=True, stop=True),
             reads=[t_r], writes=[psk[PB]])
        c0_, c1_ = cb[j % 2], cb[(j + 1) % 2]
        S.op("dve", lambda e, c0_=c0_: e.tensor_tensor(out=dest_e, in0=bank(PB, E), in1=c0_, op=ALU.add),
             reads=[psk[PB], t_cb[j % 2], t_c4], writes=[t_r])
        S.op("dve", lambda e, c0_=c0_, c1_=c1_: e.tensor_tensor(out=c1_, in0=bank(PB, E, E), in1=c0_, op=ALU.add),
             reads=[psk[PB], t_cb[j % 2], t_c4], writes=[t_cb[(j + 1) % 2]])
        for k in range(4):
            S.op("dve", lambda e, k=k: e.tensor_scalar(out=oh, in0=logit, scalar1=top8[:, k:k + 1], scalar2=None,
                                                       op0=ALU.is_equal), reads=[t_r], writes=[t_r])
            S.op("dve", lambda e, k=k: e.scalar_tensor_tensor(out=junkE, in0=oh, scalar=1.0, in1=dest_e, op0=ALU.mult, op1=ALU.mult, accum_out=destf[:, k:k + 1]),
                 reads=[t_r], writes=[t_r])
            S.op("dve", lambda e, k=k, j=j: e.scalar_tensor_tensor(out=junkE, in0=oh, scalar=1.0, in1=Gd3[:, j, :], op0=ALU.mult, op1=ALU.mult, accum_out=gate3[:, j, k:k + 1]),
                 reads=[t_r], writes=[t_r, t_ga])
        S.op("dve", lambda e, j=j: e.tensor_copy(out=desti3[:, j, :], in_=destf), reads=[t_r], writes=[t_di])
        for k in range(4):
            S.dma("pool", lambda e, b=b, j=j, k=k: e.indirect_dma_start(
                out=hbuf[:, :], out_offset=bass.IndirectOffsetOnAxis(ap=desti3[:, j, k:k + 1], axis=0),
                in_=hnb[b], in_offset=None), reads=[t_hnb[b], t_di], writes=[t_hbuf])
        if debug:
            S.dma("sp", lambda e, b=b, j=j: e.dma_start(out=h_dbg[j * 128:(j + 1) * 128, :], in_=hnf[b]),
                  reads=[t_hnf[b]])
    if debug:
        S.dma("sp", lambda e: e.dma_start(out=gate_dbg, in_=gate_all), reads=[t_ga])
        S.dma("sp", lambda e: e.dma_start(out=dest_dbg, in_=dest_i), reads=[t_di])
    S.barrier()
    psk = new_ps()
    A.release(m3)

    m5 = A.mark()
    NWB = 5
    wbk = [A.bf16(KC * BW) for _ in range(NWB)]
    xe = A.bf16(CT * D)
    xe3 = r3(xe, D)
    hTe = [A.bf16(KC * C)] * 2
    hbT = [A.bf16(KCE * C)] * 2
    SW = BW // 2
    NSTG = 4

    gsb = [A.f32(HPB * C) for _ in range(2)]
    gtmp = [A.f32(C) for _ in range(2)]
    stmp = [A.f32(C) for _ in range(2)]
    utmp = [A.f32(C) for _ in range(2)]
    ystage = [A.f32(BW) for _ in range(3)]
    bgu = A.f32(E * NGU)
    bgu3 = r3(bgu, NGU)
    t_bgu = S.tok("bgu")
    S.dma("sp", lambda e: e.dma_start(out=bgu, in_=bgu_t), writes=[t_bgu])
    t_wbk = S.toks_n(NWB, "wbk")
    t_xe = S.tok("xe")
    t_hTe = [S.tok("hTe")] * 2
    t_hbT = [S.tok("hbT")] * 2
    t_gsb = S.toks_n(2, "gsb")
    t_g = S.toks_n(2, "g")
    t_s = S.toks_n(2, "s")
    t_u = S.toks_n(2, "u")
    t_ys = S.toks_n(3, "ys")
    t_ybuf = S.tok("ybuf")
    c5 = {"w": 0, "gu": 0, "dn": 0, "t": 0, "ys": 0, "tr": 0}
    GU_BANKS = (0, 1, 2, 3)
    DN_BANKS = (4, 5)
    wsrc = []
    for ex2 in range(E):
        for bidx2 in range(DE // BW):
            for part2 in range(2):
                c0_ = part2 * DE + bidx2 * BW
                wsrc.append((w_gu, wgu_bf, ex2, c0_))
        for n2 in range(NB):
            wsrc.append((w_dn, wdn_bf, ex2, n2 * BW))
    wst = {"issued": 0, "half": 0}

    def ensure_weights(upto):
        while wst["issued"] <= min(upto, len(wsrc) - 1):
            i_ = wst["issued"]
            wst["issued"] += 1
            wsrc_f, wsrc_b, ex2, c0_ = wsrc[i_]
            wi2 = i_ % NWB
            if ex2 >= E - NP:
                S.dma("sp", lambda e, wi2=wi2, ex2=ex2, c0_=c0_, wsrc_b=wsrc_b: e.dma_start(
                    out=r3(wbk[wi2], BW),
                    in_=wsrc_b[ex2 - (E - NP), :, c0_:c0_ + BW].rearrange("(kc p) c -> p kc c", p=128)),
                    writes=[t_wbk[wi2]])
                continue
            S.dma("pool", lambda e, wi2=wi2, ex2=ex2, c0_=c0_, wsrc_f=wsrc_f: e.dma_start(
                out=r3(wbk[wi2], BW),
                in_=wsrc_f[ex2, :, c0_:c0_ + BW].rearrange("(kc p) c -> p kc c", p=128)),
                writes=[t_wbk[wi2]])

    widx = [0]
    for ex_ in range(E):
        eb = ex_ % 2
        nfull = C // 128
        if nfull:
            S.dma("sp", lambda e, ex_=ex_: e.dma_start(
                out=xe3[:, 0:nfull, :],
                in_=hbuf[ex_ * C:ex_ * C + nfull * 128, :].rearrange("(s p) d -> p s d", p=128)), writes=[t_xe])
        if C % 128:
            S.dma("sp", lambda e, ex_=ex_: e.dma_start(
                out=xe3[0:C % 128, nfull, :], in_=hbuf[ex_ * C + nfull * 128:(ex_ + 1) * C, :]), writes=[t_xe])
        he3 = r3(hTe[eb], C)
        for s_ in range(CT):
            np_ = min(128, C - s_ * 128)
            transposes_to(xe3[:, s_, :], t_xe, KC,
                          lambda c, n, s_=s_, he3=he3, np_=np_: he3[:, c:c + n, s_ * 128:s_ * 128 + np_],
                          t_hTe[eb], (6, 7), True, ident_b, npart=np_)
        hb3 = r3(hbT[eb], C)
        nblk = DE // BW
        for bidx in range(nblk):
            gsi = c5["t"] % 2
            c5["t"] += 1
            gs3 = r3(gsb[gsi], C)
            for part in range(2):
                wi = widx[0] % NWB
                ensure_weights(widx[0] + NWB - 1)
                widx[0] += 1
                for m in range(HPB):
                    gb_ = GU_BANKS[c5["gu"] % 4]
                    c5["gu"] += 1
                    for kc in range(KC):
                        S.op("pe", lambda e, kc=kc, wi=wi, m=m, gb_=gb_, he3=he3: e.matmul(
                            bank(gb_, C), r3(wbk[wi], BW)[:, kc, m * 128:(m + 1) * 128], he3[:, kc, :],
                            start=(kc == 0), stop=(kc == KC - 1)),
                            reads=[t_wbk[wi], t_hTe[eb]], writes=[psk[gb_]], signal=(kc == KC - 1))
                    chunk = bidx * HPB + m
                    ti = (c5["gu"]) % 2
                    if part == 0:
                        bcol = bgu3[:, ex_, chunk:chunk + 1]
                        S.op("dve", lambda e, gb_=gb_, ti=ti, bcol=bcol: e.tensor_scalar(
                            out=gtmp[ti], in0=bank(gb_, C), scalar1=bcol, scalar2=SWIGLU_LIMIT, op0=ALU.add,
                            op1=ALU.min), reads=[psk[gb_], t_bgu], writes=[t_g[ti]])
                        S.op("act", lambda e, ti=ti: e.activation(out=stmp[ti], in_=gtmp[ti], func=AF.Sigmoid,
                                                                  scale=SWIGLU_ALPHA),
                             reads=[t_g[ti]], writes=[t_s[ti]])
                        S.op("dve", lambda e, ti=ti, m=m, gs3=gs3: e.tensor_tensor(
                            out=gs3[:, m, :], in0=gtmp[ti], in1=stmp[ti], op=ALU.mult),
                            reads=[t_g[ti], t_s[ti]], writes=[t_gsb[gsi]])
                    else:
                        bcol = bgu3[:, ex_, KCE + chunk:KCE + chunk + 1]
                        S.op("dve", lambda e, gb_=gb_, ti=ti, bcol=bcol: e.tensor_scalar(
                            out=utmp[ti], in0=bank(gb_, C), scalar1=bcol, scalar2=SWIGLU_LIMIT, op0=ALU.add,
                            op1=ALU.min), reads=[psk[gb_], t_bgu], writes=[t_u[ti]])
                        S.op("dve", lambda e, ti=ti: e.tensor_scalar(
                            out=utmp[ti], in0=utmp[ti], scalar1=-SWIGLU_LIMIT, scalar2=1.0, op0=ALU.max,
                            op1=ALU.add), reads=[t_u[ti]], writes=[t_u[ti]])
                        S.op("dve", lambda e, ti=ti, m=m, gs3=gs3, chunk=chunk, hb3=hb3: e.tensor_tensor(
                            out=hb3[:, chunk, :], in0=utmp[ti], in1=gs3[:, m, :], op=ALU.mult),
                            reads=[t_u[ti], t_gsb[gsi]], writes=[t_hbT[eb]])
        for n in range(NB):
            wi = widx[0] % NWB
            ensure_weights(widx[0] + NWB - 1)
            widx[0] += 1
            for s_ in range(CT):
                db = DN_BANKS[c5["dn"] % 2]
                c5["dn"] += 1
                np_ = min(128, C - s_ * 128)
                for kc in range(KCE):
                    S.op("pe", lambda e, kc=kc, wi=wi, s_=s_, db=db, hb3=hb3, np_=np_: e.matmul(
                        bank(db, BW)[0:np_, :], hb3[:, kc, s_ * 128:s_ * 128 + np_], r3(wbk[wi], BW)[:, kc, :],
                        start=(kc == 0), stop=(kc == KCE - 1)),
                        reads=[t_wbk[wi], t_hbT[eb]], writes=[psk[db]], signal=(kc == KCE - 1))
                yi = c5["ys"] % 3
                c5["ys"] += 1
                S.op("act", lambda e, yi=yi, db=db, np_=np_: e.activation(
                    out=ystage[yi][0:np_, :], in_=bank(db, BW)[0:np_, :], func=AF.Copy),
                     reads=[psk[db]], writes=[t_ys[yi]])
                S.dma("act", lambda e, yi=yi, ex_=ex_, s_=s_, n=n, np_=np_: e.dma_start(
                    out=ybuf[ex_ * C + s_ * 128:ex_ * C + s_ * 128 + np_, n * BW:(n + 1) * BW],
                    in_=ystage[yi][0:np_, :]),
                    reads=[t_ys[yi]], writes=[t_ybuf])
    S.barrier()
    psk = new_ps()
    A.release(m5)

    hp = [A.f32(D) for _ in range(2)]
    yk = [[A.f32(D) for _ in range(4)] for _ in range(2)]
    bdn = A.f32(D)
    GdT = A.f32(128)
    t_bdn = S.tok("bdn")
    t_GdT = S.tok("GdT")
    S.dma("sp", lambda e: e.dma_start(out=bdn[0:E, :], in_=b_dn), writes=[t_bdn])
    t_hp = S.toks_n(2, "hp")
    t_yk = [S.toks_n(4, "yk%d" % i) for i in range(2)]
    t_out = S.tok("out")
    for j in range(NQ):
        b = j % 2
        S.dma("sp", lambda e, b=b, j=j: e.dma_start(out=hp[b], in_=out[j * 128:(j + 1) * 128, :]), writes=[t_hp[b]])
        GB = 6
        S.op("pe", lambda e, j=j: e.transpose(bank(GB, 128)[0:E, :], Gd3[:, j, :], ident_f), writes=[psk[GB]])
        S.op("act", lambda e: e.activation(out=GdT[0:E, :], in_=bank(GB, 128)[0:E, :], func=AF.Copy),
             reads=[psk[GB]], writes=[t_GdT])
        for n in range(NB):
            S.op("pe", lambda e, n=n: e.matmul(bank(n, BW), GdT[0:E, :], bdn[0:E, n * BW:(n + 1) * BW],
                                               start=True, stop=True),
                 reads=[t_GdT, t_bdn], writes=[psk[n]])
            S.op("dve", lambda e, n=n, b=b: e.tensor_tensor(
                out=hp[b][:, n * BW:(n + 1) * BW], in0=bank(n, BW), in1=hp[b][:, n * BW:(n + 1) * BW], op=ALU.add),
                reads=[psk[n], t_hp[b]], writes=[t_hp[b]])
        for k in range(4):
            S.dma("pool", lambda e, b=b, j=j, k=k: e.indirect_dma_start(
                out=yk[b][k], out_offset=None, in_=ybuf[:, :],
                in_offset=bass.IndirectOffsetOnAxis(ap=desti3[:, j, k:k + 1], axis=0)), writes=[t_yk[b][k]])
            S.op("dve", lambda e, b=b, j=j, k=k: e.scalar_tensor_tensor(
                out=hp[b], in0=yk[b][k], scalar=gate3[:, j, k:k + 1], in1=hp[b], op0=ALU.mult, op1=ALU.add),
                reads=[t_yk[b][k], t_hp[b]], writes=[t_hp[b]])
        S.dma("sp", lambda e, b=b, j=j: e.dma_start(out=out[j * 128:(j + 1) * 128, :], in_=hp[b]),
              reads=[t_hp[b]], writes=[t_out])
    S.barrier()

    blk = st.enter_context(nc.Block())

    @blk.tensor
    def _(e):
        for f in S.ops["pe"]:
            f(e)

    @blk.scalar
    def _(e):
        for f in S.ops["act"]:
            f(e)

    @blk.vector
    def _(e):
        for f in S.ops["dve"]:
            f(e)

    @blk.gpsimd
    def _(e):
        for f in S.ops["pool"]:
            f(e)

    @blk.sync
    def _(e):
        for f in S.ops["sp"]:
            f(e)

    st.close()
    return nc


def make_consts(cfg):
    t = np.arange(128)
    ident = np.eye(128, dtype=np.float32)
    tri_incl = (t[:, None] <= t[None, :]).astype(np.float32)
    tri_strict = (t[:, None] < t[None, :]).astype(np.float32)
    ones = np.ones((128, 128), np.float32)
    cst = np.concatenate([ident, tri_incl, tri_strict, ones], axis=1)
    ebase = np.tile((np.arange(cfg.E) * cfg.C).astype(np.float32)[None, :], (128, 1))
    return cst, ebase


def make_btab(rel_bias, cfg):
    HA = cfg.HA
    kk = np.arange(128)[:, None, None]
    r = np.arange(5)[None, :, None]
    qi = np.arange(128)[None, None, :]
    dist = (4 - r) * 128 + qi - kk
    qo = qi % 64
    ok = (dist >= -(63 - qo)) & (dist <= qo + 512)
    idx = np.clip(dist, -63, 256) + 63
    tab = rel_bias[:, idx]
    tab = np.where(ok[None], tab, np.float32(NEG)).astype(np.float32)
    return np.ascontiguousarray(tab.transpose(1, 0, 2, 3).reshape(128, HA, 640))


def prepare(cfg, inp):
    D, NS, E = cfg.D, cfg.NS, cfg.E
    x = np.asarray(inp["x"], np.float32)
    B = x.shape[0]
    cst, ebase = make_consts(cfg)
    btab = make_btab(np.asarray(inp["rel_bias"], np.float32), cfg)
    qkn = np.concatenate([np.tile(np.asarray(inp[k], np.float32), cfg.HPB)
                          for k in ("qn_a", "kn_a", "qn_b", "kn_b")])
    NGU = 2 * D // 128
    bgu_t = np.ascontiguousarray(
        np.asarray(inp["b_gate_up"], np.float32).reshape(E, NGU, 128).transpose(2, 0, 1).reshape(128, E * NGU))
    shared = {
        "w_in": np.asarray(inp["w_in"], np.float32),
        "attn_norm_w": np.asarray(inp["attn_norm_w"], np.float32),
        "ffn_norm_w": np.asarray(inp["ffn_norm_w"], np.float32),
        "qkn": qkn,
        "b_forget": np.asarray(inp["b_forget"], np.float32),
        "btab": btab,
        "w_branch_a": np.asarray(inp["w_branch_a"], np.float32),
        "w_branch_b": np.asarray(inp["w_branch_b"], np.float32),
        "w_out": np.asarray(inp["w_out"], np.float32),
        "w_router": np.asarray(inp["w_router"], np.float32),
        "b_router": np.asarray(inp["b_router"], np.float32),
        "w_gate_up": np.asarray(inp["w_gate_up"], np.float32),
        "bgu_t": bgu_t,
        "w_down": np.asarray(inp["w_down"], np.float32),
        "b_down": np.asarray(inp["b_down"], np.float32),
        "cst": cst,
        "ebase": ebase,
    }
    in_maps = []
    for c in range(2 * B):
        b, p = c // 2, c % 2
        if p == 0:
            xk = np.concatenate([np.zeros((128, D), np.float32), x[b, :(NS - 1) * 128]], axis=0)
            valid = np.ones((128, NS), np.float32)
            valid[:, 0] = 0.0
        else:
            xk = x[b, :NS * 128]
            valid = np.ones((128, NS), np.float32)
        m = dict(shared)
        m["xkv"] = np.ascontiguousarray(xk)
        m["valid"] = valid
        in_maps.append(m)
    return in_maps


def assemble(cfg, results, B):
    D, NS, NQ = cfg.D, cfg.NS, cfg.NQ
    y = np.zeros((B, NS * 128, D), np.float32)
    for c in range(2 * B):
        b, p = c // 2, c % 2
        o = np.asarray(results[c]["out"]).reshape(NQ, 128, D)
        yv = y[b].reshape(NS, 128, D)
        for j in range(NQ):
            yv[2 * j + p] = o[j]
    return y


_CACHE = {}


def kernel(**inputs):
    cfg = Cfg()
    if "nc" not in _CACHE:
        _CACHE["nc"] = build(cfg)
    nc = _CACHE["nc"]
    in_maps = prepare(cfg, inputs)
    res = run_bass_kernel_spmd(nc, in_maps, core_ids=list(range(cfg.n_cores)))
    return assemble(cfg, res.results, 4)
```

```python
import numpy as np
from contextlib import ExitStack
import concourse.bass as bass
import concourse.mybir as mybir
from concourse.alu_op_type import AluOpType as ALU
from concourse.bass_utils import run_bass_kernel_spmd

F32 = mybir.dt.float32
BF16 = mybir.dt.bfloat16
I32 = mybir.dt.int32
AF = mybir.ActivationFunctionType
AX = mybir.AxisListType

NORM_EPS = 1e-5
SWIGLU_LIMIT = 7.0
SWIGLU_ALPHA = 1.702
NEG = -30000.0


class Cfg:
    def __init__(self, D=2048, HA=8, HB=8, NS=32, E=32, C=320, G=16, n_cores=8, NP=0):
        self.D = D
        self.KC = D // 128
        self.HA = HA
        self.HB = HB
        self.H = HA + HB
        self.NS = NS
        self.NQ = NS // 2
        self.E = E
        self.C = C
        self.CT = (C + 127) // 128
        self.NP = NP
        self.G = G
        self.WA = HA * 128
        self.WB = HB * 128
        self.BW = min(512, self.WA, self.WB, D)
        self.HPB = self.BW // 128
        self.NB = D // self.BW
        self.INC = 3 * self.WA + 3 * self.WB + HB + 2 * D
        self.n_cores = n_cores


class Tok:
    __slots__ = ("w", "r", "name")

    def __init__(self, name=""):
        self.w = None
        self.r = []
        self.name = name


ENGS = ("pe", "act", "dve", "pool", "sp")
NDMA = 8


class Sched:
    def __init__(self, nc, st):
        self.nc = nc
        self.ops = {e: [] for e in ENGS}
        self.seq = {e: 0 for e in ENGS}
        self.sem = {e: st.enter_context(nc.semaphore("s_" + e)) for e in ENGS}
        self.semid = {}
        self.waited = {e: {} for e in ENGS}
        self.dsem = {}
        for q in ("sp", "act", "pool", "poolpc"):
            self.dsem[q] = [[st.enter_context(nc.semaphore("d_%s%d" % (q, i))), 0] for i in range(NDMA)]
        self.drr = {q: 0 for q in self.dsem}
        self.unsig = {e: False for e in ENGS}
        self.toks = []

    def tok(self, name=""):
        t = Tok(name)
        self.toks.append(t)
        return t

    def toks_n(self, n, name=""):
        return [self.tok(name + str(i)) for i in range(n)]

    def _key(self, sem):
        return id(sem)

    def _need(self, eng, deps):
        for (sem, val, src) in deps:
            if src == "pe" and eng == "pe":
                continue
            k = self._key(sem)
            if self.waited[eng].get(k, 0) < val:
                self.waited[eng][k] = val
                self.ops[eng].append(lambda e, sem=sem, val=val: e.wait_ge(sem, val))

    def _deps(self, reads, writes):
        deps = []
        for b in reads:
            if b.w is not None:
                deps.append(b.w)
        for b in writes:
            if b.w is not None:
                deps.append(b.w)
            deps.extend(b.r)
        return deps

    def _mark(self, reads, writes, t):
        for b in reads:
            b.r.append(t)
        for b in writes:
            b.w = t
            b.r = []

    def op(self, eng, fn, reads=(), writes=(), signal=True):
        self._need(eng, self._deps(reads, writes))
        sem = self.sem[eng]
        if signal:
            self.seq[eng] += 1
            t = (sem, self.seq[eng], eng)
            self.ops[eng].append(lambda e, fn=fn, sem=sem: fn(e).then_inc(sem, 1))
            self.unsig[eng] = False
        else:
            t = (sem, self.seq[eng] + 1, eng)
            self.ops[eng].append(lambda e, fn=fn: fn(e))
            self.unsig[eng] = True
        self._mark(reads, writes, t)

    def dma(self, q, fn, reads=(), writes=(), key=None):
        key = key or q
        self._need(q, self._deps(reads, writes))
        slot = self.dsem[key][self.drr[key] % NDMA]
        self.drr[key] += 1
        sem, uses = slot
        if uses > 0:
            self._need(q, [(sem, 16 * uses, "dma")])
        slot[1] = uses + 1
        t = (sem, 16 * (uses + 1), "dma")
        self.ops[q].append(lambda e, fn=fn, sem=sem: fn(e).then_inc(sem, 16))
        self._mark(reads, writes, t)

    def barrier(self):
        for e in ENGS:
            assert not self.unsig[e], e
        deps = []
        for e in ENGS:
            if self.seq[e] > 0:
                deps.append((self.sem[e], self.seq[e], "x"))
        for q in self.dsem:
            for sem, uses in self.dsem[q]:
                if uses > 0:
                    deps.append((sem, 16 * uses, "dma"))
        for e in ENGS:
            self._need(e, deps)
        for t in self.toks:
            t.w = None
            t.r = []
        self.toks = []


class Arena:
    def __init__(self, ap, nfloats):
        self.ap = ap
        self.n = nfloats
        self.off = 0

    def mark(self):
        return self.off

    def release(self, m):
        self.off = m

    def f32(self, n):
        n = (n + 1) // 2 * 2
        assert self.off + n <= self.n, ("arena overflow", self.off, n, self.n)
        a = self.ap[:, self.off:self.off + n]
        self.off += n
        return a

    def bf16(self, n):
        n = (n + 3) // 4 * 4
        return self.f32(n // 2).bitcast(BF16)

    def i32(self, n):
        return self.f32(n).bitcast(I32)


def r3(ap, b):
    return ap.rearrange("p (a b) -> p a b", b=b)


def build(cfg, debug=False):
    D, KC, HA, HB, H, NS, NQ, E, C, CT, G = (cfg.D, cfg.KC, cfg.HA, cfg.HB, cfg.H, cfg.NS, cfg.NQ,
                                             cfg.E, cfg.C, cfg.CT, cfg.G)
    WA, WB, BW, HPB, NB, INC = cfg.WA, cfg.WB, cfg.BW, cfg.HPB, cfg.NB, cfg.INC
    DE = D
    KCE = DE // 128
    NGU = 2 * DE // 128
    SCALE = 128.0 ** -0.5
    nc = bass.Bass("TRN2", target_bir_lowering=False)

    def din(name, shape, dt=F32):
        return nc.dram_tensor(name, list(shape), dt, kind="ExternalInput").ap()

    def dscr(name, shape, dt):
        kind = "ExternalOutput" if debug else "Internal"
        return nc.dram_tensor(name, list(shape), dt, kind=kind).ap()

    xkv = din("xkv", [NS * 128, D])
    valid_d = din("valid", [128, NS])
    w_in = din("w_in", [D, INC])
    anw = din("attn_norm_w", [D])
    fnw = din("ffn_norm_w", [D])
    qkn = din("qkn", [4 * BW])
    b_forget = din("b_forget", [HB])
    btab = din("btab", [128, HA, 5 * 128])
    w_ba = din("w_branch_a", [WA, D])
    w_bb = din("w_branch_b", [WB, D])
    w_out = din("w_out", [D, D])
    w_router = din("w_router", [D, E])
    b_router = din("b_router", [E])
    w_gu = din("w_gate_up", [E, D, 2 * DE])
    bgu_t = din("bgu_t", [128, E * NGU])
    w_dn = din("w_down", [E, DE, D])
    b_dn = din("b_down", [E, D])
    cst = din("cst", [128, 4 * 128])
    ebase_d = din("ebase", [128, E])
    out = nc.dram_tensor("out", [NQ * 128, D], F32, kind="ExternalOutput").ap()

    kT_d = dscr("kT_d", [H, 128, NS * 128], BF16)
    qT_d = dscr("qT_d", [H, 128, NQ * 128], BF16)
    V_d = dscr("V_d", [NS * 128, WA + WB], BF16)
    sg_d = dscr("sg_d", [NQ * 128, 2 * D], BF16)
    hbuf = dscr("hbuf", [E * C + 128, D], BF16)
    ybuf = dscr("ybuf", [E * C + 128, D], F32)
    NP = cfg.NP
    wgu_bf = nc.dram_tensor("wgu_bf", [max(NP, 1), D, 2 * DE], BF16, kind="Internal").ap()
    wdn_bf = nc.dram_tensor("wdn_bf", [max(NP, 1), DE, D], BF16, kind="Internal").ap()
    if debug:
        yT_dbg = dscr("yT_dbg", [128, H * NQ * 128], BF16)
        h_dbg = dscr("h_dbg", [NQ * 128, D], F32)
        gate_dbg = dscr("gate_dbg", [128, NQ * 4], F32)
        dest_dbg = dscr("dest_dbg", [128, NQ * 4], I32)
        cum_dbg = dscr("cum_dbg", [128, HB * NS], F32)
        xnT_dbg = dscr("xnT_dbg", [128, KC * G * 128], BF16)

    st = ExitStack()
    ARENA_F = 47000
    arena_t = st.enter_context(nc.sbuf_tensor("arena", [128, ARENA_F], F32))
    ps_t = st.enter_context(nc.psum_tensor("ps", [128, 4096], F32))
    S = Sched(nc, st)
    A = Arena(arena_t, ARENA_F)

    def bank(i, n=512, off=0):
        return ps_t[:, i * 512 + off:i * 512 + off + n]

    def bank_bf(i, n=1024, off=0):
        return ps_t[:, i * 512:(i + 1) * 512].bitcast(BF16)[:, off:off + n]

    psk = S.toks_n(8, "ps")

    pc_list = []
    for ep in range(NP):
        e_ = E - NP + ep
        cw = min(2048, 2 * DE)
        for q4 in range(4):
            r0, r1 = q4 * D // 4, (q4 + 1) * D // 4
            pc_list.append((wgu_bf[ep, r0:r1, :].rearrange("r (a b) -> r a b", b=cw),
                            w_gu[e_, r0:r1, :].rearrange("r (a b) -> r a b", b=cw)))
        cw = min(2048, D)
        for q2 in range(2):
            r0, r1 = q2 * DE // 2, (q2 + 1) * DE // 2
            pc_list.append((wdn_bf[ep, r0:r1, :].rearrange("r (a b) -> r a b", b=cw),
                            w_dn[e_, r0:r1, :].rearrange("r (a b) -> r a b", b=cw)))
    pc_pos = [0]

    def precast(n):
        for _ in range(n):
            if pc_pos[0] >= len(pc_list):
                return
            o_, i_ = pc_list[pc_pos[0]]
            pc_pos[0] += 1
            S.dma("pool", lambda e, o_=o_, i_=i_: e.dma_start(out=o_, in_=i_), key="poolpc")

    def new_ps():
        return S.toks_n(8, "ps")

    ident_f = A.f32(128)
    tri_incl_f = A.f32(128)
    tri_strict_f = A.f32(128)
    ones_f = A.f32(128)
    ident_b = A.bf16(128)
    tri_incl_b = A.bf16(128)
    valid_s = A.f32(NS)
    lf = A.f32(NS * HB)
    cum = A.f32(HB * NS)
    base = A.f32((NS + 1) * HB)
    gate_all = A.f32(NQ * 4)
    dest_i = A.i32(NQ * 4)
    Gd_all = A.f32(NQ * E)
    Gd3 = r3(Gd_all, E)
    tk_const = S.tok("const")

    cst_tmp = [ident_f, tri_incl_f, tri_strict_f, ones_f]
    for i, a in enumerate(cst_tmp):
        S.dma("sp", lambda e, a=a, i=i: e.dma_start(out=a, in_=cst[:, i * 128:(i + 1) * 128]), writes=[tk_const])
    S.dma("sp", lambda e: e.dma_start(out=valid_s, in_=valid_d), writes=[tk_const])
    S.op("dve", lambda e: e.tensor_copy(out=ident_b, in_=ident_f), reads=[tk_const], writes=[tk_const])
    S.op("dve", lambda e: e.tensor_copy(out=tri_incl_b, in_=tri_incl_f), reads=[tk_const], writes=[tk_const])
    S.barrier()
    psk = new_ps()

    m1 = A.mark()
    anw_b = A.f32(D)
    qkn_b = A.f32(4 * BW)
    bf_b = A.f32(HB)
    wf = A.bf16(KC * HB)
    xnT = A.bf16(KC * G * 128)
    xnT3 = r3(xnT, G * 128)
    xt = [A.f32(D) for _ in range(2)]
    xn = [A.bf16(D) for _ in range(2)]
    wblk = [A.bf16(KC * BW) for _ in range(2)]
    qf = [A.f32(BW) for _ in range(2)]
    qn = [A.bf16(BW) for _ in range(2)]
    stageT = A.bf16(HPB * G * 128)
    stageT3 = r3(stageT, G * 128)
    stageV = [A.bf16(BW) for _ in range(2)]
    ss = [A.f32(2) for _ in range(2)]
    rstd = [A.f32(2) for _ in range(2)]
    ssq = [A.f32(HPB) for _ in range(2)]
    rq = [A.f32(HPB) for _ in range(2)]
    f_all = A.f32(NS * HB)
    f_all3 = r3(f_all, HB)

    t_c1 = S.tok("c1")
    S.dma("sp", lambda e: e.dma_start(out=anw_b, in_=anw.partition_broadcast(128)), writes=[t_c1])
    S.dma("sp", lambda e: e.dma_start(out=qkn_b, in_=qkn.partition_broadcast(128)), writes=[t_c1])
    S.dma("sp", lambda e: e.dma_start(out=bf_b, in_=b_forget.partition_broadcast(128)), writes=[t_c1])
    fcol = 3 * WA + 3 * WB
    S.dma("pool", lambda e: e.dma_start(out=r3(wf, HB),
                                        in_=w_in[:, fcol:fcol + HB].rearrange("(kc p) c -> p kc c", p=128)),
          writes=[t_c1])

    t_xt = S.toks_n(2, "xt")
    t_xn = S.toks_n(2, "xn")
    t_ss = S.toks_n(2, "ss")
    t_xnT = S.toks_n(G, "xnT")
    t_wblk = S.toks_n(2, "wblk")
    t_qf = S.toks_n(2, "qf")
    t_qn = S.toks_n(2, "qn")
    t_sq = S.toks_n(2, "sq")
    t_stT = S.tok("stT")
    t_stV = S.toks_n(2, "stV")
    t_fall = S.tok("fall")
    t_dram1 = S.tok("dram1")

    blocks = []
    for hb_ in range(WA // BW):
        blocks.append(("q", 0 * WA + hb_ * BW, (0, hb_ * HPB)))
    for hb_ in range(WA // BW):
        blocks.append(("k", 1 * WA + hb_ * BW, (1, hb_ * HPB)))
    for hb_ in range(WA // BW):
        blocks.append(("v", 2 * WA + hb_ * BW, hb_ * BW))
    for hb_ in range(WB // BW):
        blocks.append(("q", 3 * WA + hb_ * BW, (2, HA + hb_ * HPB)))
    for hb_ in range(WB // BW):
        blocks.append(("k", 3 * WA + WB + hb_ * BW, (3, HA + hb_ * HPB)))
    for hb_ in range(WB // BW):
        blocks.append(("v", 3 * WA + 2 * WB + hb_ * BW, WA + hb_ * BW))
    gcol = 3 * WA + 3 * WB + HB
    for nb_ in range(2 * D // BW):
        blocks.append(("g", gcol + nb_ * BW, nb_ * BW))

    cnt = {"x": 0, "w": 0, "ps": 0, "q": 0, "sv": 0}
    PROJ_BANKS = (2, 3, 4)

    def rms_rstd(eng_src_ap, ss_ap, rstd_ap, n, toks_r, tok_ss, junk_ap, tok_junk):
        S.op("dve", lambda e: e.scalar_tensor_tensor(out=junk_ap, in0=eng_src_ap, scalar=1.0, in1=eng_src_ap, op0=ALU.mult, op1=ALU.mult, accum_out=ss_ap[:, 0:1]),
             reads=toks_r, writes=[tok_ss, tok_junk])
        S.op("dve", lambda e: e.tensor_scalar(out=ss_ap[:, 0:1], in0=ss_ap[:, 0:1], scalar1=1.0 / n,
                                              scalar2=NORM_EPS, op0=ALU.mult, op1=ALU.add),
             reads=[tok_ss], writes=[tok_ss])
        S.op("act", lambda e: e.activation(out=ss_ap[:, 0:1], in_=ss_ap[:, 0:1], func=AF.Sqrt),
             reads=[tok_ss], writes=[tok_ss])
        S.op("dve", lambda e: e.reciprocal(out=rstd_ap[:, 0:1], in_=ss_ap[:, 0:1]),
             reads=[tok_ss], writes=[tok_ss])

    def transposes_to(src_ap, src_tok, ncol_chunks, dst_fn, dst_tok, banks, dt_bf=True, ident=None, npart=128):
        per = 8 if dt_bf else 4
        c = 0
        bi = 0
        while c < ncol_chunks:
            n = min(per, ncol_chunks - c)
            bk = banks[bi % len(banks)]
            bi += 1
            for k in range(n):
                if dt_bf:
                    o = bank_bf(bk, 128, k * 128)
                else:
                    o = bank(bk, 128, k * 128)
                S.op("pe", lambda e, o=o, k=k, c=c: e.transpose(
                    o[:, 0:npart], src_ap[0:npart, (c + k) * 128:(c + k + 1) * 128], ident[0:npart, 0:npart]),
                     reads=[src_tok], writes=[psk[bk]], signal=(k == n - 1))
            if dt_bf:
                srcp = r3(bank_bf(bk, n * 128), 128)[:, :, 0:npart]
            else:
                srcp = r3(bank(bk, n * 128), 128)[:, :, 0:npart]
            S.op("act", lambda e, srcp=srcp, c=c, n=n: e.activation(out=dst_fn(c, n), in_=srcp, func=AF.Copy),
                 reads=[psk[bk]], writes=[dst_tok])
            c += n

    ngroups = NS // G
    pend1 = [None]
    npc = len(pc_list)
    nblk1 = len(blocks) * ngroups
    PC1 = max(1, (npc * 5 // 9 + nblk1 - 1) // nblk1) if npc else 0
    for g in range(ngroups):
        for tt in range(G):
            t = g * G + tt
            b = cnt["x"] % 2
            cnt["x"] += 1
            S.dma("sp", lambda e, b=b, t=t: e.dma_start(out=xt[b], in_=xkv[t * 128:(t + 1) * 128, :]),
                  writes=[t_xt[b]])
            rms_rstd(xt[b], ss[b], rstd[b], D, [t_xt[b]], t_ss[b], xn[b], t_xn[b])
            S.op("dve", lambda e, b=b: e.scalar_tensor_tensor(out=xn[b], in0=xt[b], scalar=rstd[b][:, 0:1],
                                                             in1=anw_b, op0=ALU.mult, op1=ALU.mult),
                 reads=[t_xt[b], t_ss[b], t_c1], writes=[t_xn[b]])
            transposes_to(xn[b], t_xn[b], KC,
                          lambda c, n, tt=tt: xnT3[:, c:c + n, tt * 128:(tt + 1) * 128],
                          t_xnT[tt], (0, 1), True, ident_b)
            for kc in range(KC):
                S.op("pe", lambda e, kc=kc, tt=tt: e.matmul(bank(7, HB), xnT3[:, kc, tt * 128:(tt + 1) * 128],
                                                            r3(wf, HB)[:, kc, :], start=(kc == 0), stop=(kc == KC - 1)),
                     reads=[t_xnT[tt], t_c1], writes=[psk[7]], signal=(kc == KC - 1))
            S.op("dve", lambda e, t=t: e.tensor_tensor(out=f_all3[:, t, :], in0=bank(7, HB), in1=bf_b, op=ALU.add),
                 reads=[psk[7], t_c1], writes=[t_fall])
        if debug and g == 0:
            S.dma("sp", lambda e: e.dma_start(out=xnT_dbg, in_=xnT), reads=t_xnT)
        for (kind, c0, meta) in blocks:
            wb = cnt["w"] % 2
            cnt["w"] += 1
            S.dma("pool", lambda e, wb=wb, c0=c0: e.dma_start(
                out=r3(wblk[wb], BW), in_=w_in[:, c0:c0 + BW].rearrange("(kc p) c -> p kc c", p=128)),
                writes=[t_wblk[wb]])
            precast(PC1)
            tiles = range(G) if kind in ("k", "v") else range(1, G, 2)
            for tt in tiles:
                t = g * G + tt
                j = (t - 1) // 2
                pb = PROJ_BANKS[cnt["ps"] % 3]
                cnt["ps"] += 1
                for kc in range(KC):
                    S.op("pe", lambda e, kc=kc, tt=tt, wb=wb, pb=pb: e.matmul(
                        bank(pb, BW), xnT3[:, kc, tt * 128:(tt + 1) * 128], r3(wblk[wb], BW)[:, kc, :],
                        start=(kc == 0), stop=(kc == KC - 1)),
                        reads=[t_xnT[tt], t_wblk[wb]], writes=[psk[pb]], signal=(kc == KC - 1))
                if kind in ("q", "k"):
                    row, h0 = meta
                    qb = cnt["q"] % 2
                    cnt["q"] += 1
                    S.op("act", lambda e, qb=qb, pb=pb: e.activation(out=qf[qb], in_=bank(pb, BW), func=AF.Copy),
                         reads=[psk[pb]], writes=[t_qf[qb]])
                    for hh in range(HPB):
                        S.op("dve", lambda e, qb=qb, hh=hh: e.scalar_tensor_tensor(out=qn[qb][:, hh * 128:(hh + 1) * 128], in0=qf[qb][:, hh * 128:(hh + 1) * 128], scalar=1.0, in1=qf[qb][:, hh * 128:(hh + 1) * 128], op0=ALU.mult, op1=ALU.mult, accum_out=ssq[qb][:, hh:hh + 1]),
                            reads=[t_qf[qb]], writes=[t_sq[qb], t_qn[qb]])
                    S.op("dve", lambda e, qb=qb: e.tensor_scalar(out=ssq[qb], in0=ssq[qb], scalar1=1.0 / 128,
                                                                 scalar2=NORM_EPS, op0=ALU.mult, op1=ALU.add),
                         reads=[t_sq[qb]], writes=[t_sq[qb]])
                    S.op("act", lambda e, qb=qb: e.activation(out=ssq[qb], in_=ssq[qb], func=AF.Sqrt),
                         reads=[t_sq[qb]], writes=[t_sq[qb]])
                    S.op("dve", lambda e, qb=qb: e.reciprocal(out=rq[qb], in_=ssq[qb]),
                         reads=[t_sq[qb]], writes=[t_sq[qb]])
                    for hh in range(HPB):
                        S.op("dve", lambda e, qb=qb, hh=hh, row=row: e.scalar_tensor_tensor(
                            out=qn[qb][:, hh * 128:(hh + 1) * 128], in0=qf[qb][:, hh * 128:(hh + 1) * 128],
                            scalar=rq[qb][:, hh:hh + 1], in1=qkn_b[:, row * BW + hh * 128:row * BW + (hh + 1) * 128],
                            op0=ALU.mult, op1=ALU.mult),
                            reads=[t_qf[qb], t_sq[qb], t_c1], writes=[t_qn[qb]])
                    if kind == "k":
                        col = tt
                    else:
                        col = (tt - 1) // 2
                    def _tr(qb=qb, col=col):
                        transposes_to(qn[qb], t_qn[qb], HPB,
                                      lambda c, n, col=col: stageT3[:, c:c + n, col * 128:(col + 1) * 128],
                                      t_stT, (5, 6), True, ident_b)
                    if pend1[0] is not None:
                        pend1[0]()
                    pend1[0] = _tr
                elif kind == "v":
                    sv = cnt["sv"] % 2
                    cnt["sv"] += 1
                    S.op("act", lambda e, sv=sv, pb=pb: e.activation(out=stageV[sv], in_=bank(pb, BW), func=AF.Copy),
                         reads=[psk[pb]], writes=[t_stV[sv]])
                    S.dma("act", lambda e, sv=sv, t=t, meta=meta: e.dma_start(
                        out=V_d[t * 128:(t + 1) * 128, meta:meta + BW], in_=stageV[sv]),
                        reads=[t_stV[sv]], writes=[t_dram1])
                else:
                    sv = cnt["sv"] % 2
                    cnt["sv"] += 1
                    S.op("act", lambda e, sv=sv, pb=pb: e.activation(out=stageV[sv], in_=bank(pb, BW), func=AF.Sigmoid),
                         reads=[psk[pb]], writes=[t_stV[sv]])
                    S.dma("act", lambda e, sv=sv, j=j, meta=meta: e.dma_start(
                        out=sg_d[j * 128:(j + 1) * 128, meta:meta + BW], in_=stageV[sv]),
                        reads=[t_stV[sv]], writes=[t_dram1])
            if pend1[0] is not None:
                pend1[0]()
                pend1[0] = None
            if kind == "k":
                row, h0 = meta
                S.dma("sp", lambda e, h0=h0, g=g: e.dma_start(
                    out=kT_d[h0:h0 + HPB, :, g * G * 128:(g + 1) * G * 128].rearrange("h p t -> p h t"),
                    in_=stageT3), reads=[t_stT], writes=[t_dram1])
            elif kind == "q":
                row, h0 = meta
                GH = G // 2
                S.dma("sp", lambda e, h0=h0, g=g, GH=GH: e.dma_start(
                    out=qT_d[h0:h0 + HPB, :, g * GH * 128:(g + 1) * GH * 128].rearrange("h p t -> p h t"),
                    in_=stageT3[:, :, 0:GH * 128]), reads=[t_stT], writes=[t_dram1])

    S.op("act", lambda e: e.activation(out=lf, in_=f_all, func=AF.Sigmoid), reads=[t_fall], writes=[t_fall])
    S.op("act", lambda e: e.activation(out=lf, in_=lf, func=AF.Ln), reads=[t_fall], writes=[t_fall])
    S.barrier()
    psk = new_ps()
    A.release(m1)

    lf3 = r3(lf, HB)
    cum3 = r3(cum, NS)
    base3 = r3(base, HB)
    t_cum = S.tok("cum")
    t_base = S.tok("base")
    S.op("dve", lambda e: e.memset(base3[:, 0, :], 0.0), writes=[t_base])
    for i in range(NS):
        bk = i % 2
        S.op("pe", lambda e, i=i, bk=bk: e.matmul(bank(bk, HB), tri_incl_f, lf3[:, i, :], start=True, stop=True),
             writes=[psk[bk]], signal=False)
        S.op("pe", lambda e, i=i, bk=bk: e.matmul(bank(bk, HB, HB), ones_f, lf3[:, i, :], start=True, stop=True),
             writes=[psk[bk]])
        S.op("dve", lambda e, i=i, bk=bk: e.tensor_tensor(out=cum3[:, :, i], in0=bank(bk, HB), in1=base3[:, i, :],
                                                          op=ALU.add),
             reads=[psk[bk], t_base], writes=[t_cum])
        S.op("dve", lambda e, i=i, bk=bk: e.tensor_tensor(out=base3[:, i + 1, :], in0=bank(bk, HB, HB),
                                                          in1=base3[:, i, :], op=ALU.add),
             reads=[psk[bk], t_base], writes=[t_base])
    if debug:
        S.dma("sp", lambda e: e.dma_start(out=cum_dbg, in_=cum), reads=[t_cum])
    S.barrier()
    psk = new_ps()

    m3 = A.mark()
    yT_all = A.bf16(H * NQ * 128)
    yT3 = r3(yT_all, NQ * 128)
    m3b = A.mark()
    Etab = A.bf16(HA * 640)
    Etab3 = r3(Etab, 640)
    tmpE = [A.f32(640) for _ in range(2)]
    VW = 130
    vaug = [A.bf16(NS * VW) for _ in range(2)]
    kTh = [A.bf16(NS * 128) for _ in range(2)]
    qTh = [A.bf16(NQ * 128) for _ in range(2)]
    pexp = [A.bf16(640) for _ in range(4)]
    pT = [A.bf16(640) for _ in range(4)]
    biasB = [A.f32(NS) for _ in range(2)]
    rinv = [A.f32(2) for _ in range(2)]
    ytile = [A.bf16(128) for _ in range(2)]

    t_E = S.tok("E")
    t_tmpE = S.toks_n(2, "tmpE")
    t_vaug = S.toks_n(2, "vaug")
    t_kTh = S.toks_n(2, "kTh")
    t_qTh = S.toks_n(2, "qTh")
    t_pexp = S.toks_n(4, "pexp")
    t_pT = [S.toks_n(5, "pT%d_" % i) for i in range(4)]
    t_bias = S.toks_n(2, "bias")
    t_rinv = S.toks_n(2, "rinv")
    t_yt = S.toks_n(2, "yt")
    t_yT = S.toks_n(NQ, "yT")

    for h in range(HA):
        b = h % 2
        S.dma("sp", lambda e, b=b, h=h: e.dma_start(out=tmpE[b], in_=btab[:, h, :]), writes=[t_tmpE[b]])
        S.op("act", lambda e, b=b, h=h: e.activation(out=Etab3[:, h, :], in_=tmpE[b], func=AF.Exp),
             reads=[t_tmpE[b]], writes=[t_E])
    for b in range(2):
        S.op("dve", lambda e, b=b: e.tensor_copy(out=r3(vaug[b], VW)[:, :, 128], in_=valid_s), writes=[t_vaug[b]])

    cnt3 = {"s": 0, "o": 0, "p": 0, "y": 0, "b": 0}

    def load_head(hidx, b):
        S.dma("sp", lambda e: e.dma_start(out=kTh[b], in_=kT_d[hidx, :, :]), writes=[t_kTh[b]])
        S.dma("sp", lambda e: e.dma_start(out=qTh[b], in_=qT_d[hidx, :, :]), writes=[t_qTh[b]])
        S.dma("sp", lambda e: e.dma_start(
            out=r3(vaug[b], VW)[:, :, 0:128],
            in_=V_d[:, hidx * 128:(hidx + 1) * 128].rearrange("(i p) d -> p i d", p=128)),
            writes=[t_vaug[b]])

    pending = [None]
    pending2 = [None]
    PC3 = (max(0, npc - PC1 * nblk1) // 2 + H - 1) // H

    pvq = []

    def flush(depth=0):
        while len(pvq) > depth:
            f2 = pending2[0]
            pending2[0] = None
            f1 = pvq.pop(0)
            pending2[0] = f1()
            if f2 is not None:
                f2()
        if depth == 0 and pending2[0] is not None:
            f2 = pending2[0]
            pending2[0] = None
            f2()

    def finalize(hidx, j, ob):
        yb = cnt3["y"] % 2
        cnt3["y"] += 1
        okb = 4 + ob
        S.op("dve", lambda e: e.reciprocal(out=rinv[yb][:, 0:1], in_=bank(okb, 1, 128)),
             reads=[psk[okb]], writes=[t_rinv[yb]])
        S.op("dve", lambda e: e.tensor_scalar(out=ytile[yb], in0=bank(okb, 128), scalar1=rinv[yb][:, 0:1],
                                              scalar2=None, op0=ALU.mult),
             reads=[psk[okb], t_rinv[yb]], writes=[t_yt[yb]])
        tbk = 6 + (yb % 2)

        def fin2():
            S.op("pe", lambda e: e.transpose(bank_bf(tbk, 128), ytile[yb], ident_b), reads=[t_yt[yb]],
                 writes=[psk[tbk]])
            S.op("dve", lambda e: e.tensor_copy(out=yT3[:, hidx, j * 128:(j + 1) * 128], in_=bank_bf(tbk, 128)),
                 reads=[psk[tbk]], writes=[t_yT[j]])
        return fin2

    for hidx in range(H):
        b = hidx % 2
        load_head(hidx, b)
        precast(PC3)
        isA = hidx < HA
        for j in range(NQ):
            iq = 2 * j + 1
            ob = cnt3["o"] % 2
            cnt3["o"] += 1
            if isA:
                rs = [r for r in range(5) if iq - 4 + r >= 0]
                groups = [[(iq - 4 + r, r) for r in rs]]
            else:
                hb_ = hidx - HA
                bb = cnt3["b"] % 2
                cnt3["b"] += 1
                S.op("dve", lambda e, hb_=hb_, bb=bb, j=j: e.tensor_scalar(
                    out=biasB[bb], in0=cum3[:, hb_, :], scalar1=-1.0, scalar2=base3[:, 2 * j + 2, hb_:hb_ + 1],
                    op0=ALU.mult, op1=ALU.add), writes=[t_bias[bb]])
                S.op("act", lambda e, bb=bb, iq=iq: e.activation(out=biasB[bb][:, 0:iq + 1], in_=biasB[bb][:, 0:iq + 1],
                                                                func=AF.Exp),
                     reads=[t_bias[bb]], writes=[t_bias[bb]])
                ks = list(range(0, iq + 1))
                groups = [[(i, i - g0) for i in ks[g0:g0 + 4]] for g0 in range(0, len(ks), 4)]
            ngr = len(groups)
            for gi, grp in enumerate(groups):
                sbase = 2 * (cnt3["s"] % 2) if isA else cnt3["s"] % 4
                cnt3["s"] += 1
                pb_ = cnt3["p"] % 4
                cnt3["p"] += 1
                for (i, r) in grp:
                    bkk = sbase + (1 if r >= 4 else 0)
                    off = (r % 4) * 128
                    S.op("pe", lambda e, i=i, bkk=bkk, off=off, b=b, j=j: e.matmul(
                        bank(bkk, 128, off), kTh[b][:, i * 128:(i + 1) * 128], qTh[b][:, j * 128:(j + 1) * 128],
                        start=True, stop=True),
                        reads=[t_kTh[b], t_qTh[b]], writes=[psk[bkk]], signal=True)
                r0 = grp[0][1]
                r1 = grp[-1][1]
                if isA:
                    lo = r0 * 128
                    hi = min(r1 + 1, 4) * 128
                    S.op("act", lambda e, sbase=sbase, pb_=pb_, lo=lo, hi=hi: e.activation(
                        out=pexp[pb_][:, lo:hi], in_=ps_t[:, sbase * 512 + lo:sbase * 512 + hi], func=AF.Exp,
                        scale=SCALE), reads=[psk[sbase]], writes=[t_pexp[pb_]])
                    if r1 >= 4:
                        S.op("act", lambda e, sbase=sbase, pb_=pb_: e.activation(
                            out=pexp[pb_][:, 512:640], in_=bank(sbase + 1, 128), func=AF.Exp, scale=SCALE),
                            reads=[psk[sbase + 1]], writes=[t_pexp[pb_]])
                    S.op("dve", lambda e, pb_=pb_, lo=lo, r1=r1, hidx=hidx: e.tensor_tensor(
                        out=pT[pb_][:, lo:(r1 + 1) * 128], in0=pexp[pb_][:, lo:(r1 + 1) * 128],
                        in1=Etab3[:, hidx, lo:(r1 + 1) * 128], op=ALU.mult),
                        reads=[t_pexp[pb_], t_E], writes=[t_pT[pb_][r_] for (_, r_) in grp])
                else:
                    ng_ = len(grp)
                    S.op("act", lambda e, sbase=sbase, pb_=pb_, ng_=ng_: e.activation(
                        out=pexp[pb_][:, 0:ng_ * 128], in_=bank(sbase, ng_ * 128), func=AF.Exp, scale=SCALE),
                        reads=[psk[sbase]], writes=[t_pexp[pb_]])
                    for (i, r) in grp:
                        if i == iq:
                            S.op("dve", lambda e, i=i, r=r, pb_=pb_, bb=bb: e.scalar_tensor_tensor(
                                out=pT[pb_][:, r * 128:(r + 1) * 128], in0=pexp[pb_][:, r * 128:(r + 1) * 128],
                                scalar=biasB[bb][:, i:i + 1], in1=tri_incl_b, op0=ALU.mult, op1=ALU.mult),
                                reads=[t_pexp[pb_], t_bias[bb]], writes=[t_pT[pb_][r]])
                        else:
                            S.op("dve", lambda e, i=i, r=r, pb_=pb_, bb=bb: e.tensor_scalar(
                                out=pT[pb_][:, r * 128:(r + 1) * 128], in0=pexp[pb_][:, r * 128:(r + 1) * 128],
                                scalar1=biasB[bb][:, i:i + 1], scalar2=None, op0=ALU.mult),
                                reads=[t_pexp[pb_], t_bias[bb]], writes=[t_pT[pb_][r]])

                def pv(grp=grp, gi=gi, ngr=ngr, pb_=pb_, ob=ob, b=b, hidx=hidx, j=j):
                    okb = 4 + ob
                    for (i, r) in grp:
                        first = (gi == 0 and (i, r) == grp[0])
                        last = (gi == ngr - 1 and (i, r) == grp[-1])
                        S.op("pe", lambda e, i=i, r=r, first=first, last=last: e.matmul(
                            bank(okb, 129), pT[pb_][:, r * 128:(r + 1) * 128], r3(vaug[b], VW)[:, i, 0:129],
                            start=first, stop=last),
                            reads=[t_pT[pb_][r], t_vaug[b]], writes=[psk[okb]], signal=((i, r) == grp[-1]))
                    if gi == ngr - 1:
                        return finalize(hidx, j, ob)
                    return None
                pvq.append(pv)
                flush(1 if isA else 3)
        if hidx == HA - 1:
            flush(0)
    flush(0)
    if debug:
        S.dma("sp", lambda e: e.dma_start(out=yT_dbg, in_=yT_all), reads=t_yT)
    S.barrier()
    psk = new_ps()
    A.release(m3b)

    t_yT = S.toks_n(NQ, "yT")
    Wa = A.bf16(HA * D)
    Wb = A.bf16(HB * D)
    Wa3 = r3(Wa, D)
    Wb3 = r3(Wb, D)
    sgt = [A.bf16(2 * D) for _ in range(2)]
    zt = [A.bf16(D) for _ in range(2)]
    t1 = [A.f32(BW) for _ in range(2)]
    t2 = [A.f32(BW) for _ in range(2)]
    t_W = S.tok("Wab")
    t_sgt = S.toks_n(2, "sgt")
    t_zt = S.toks_n(2, "zt")
    t_t1 = S.toks_n(2, "t1")
    t_t2 = S.toks_n(2, "t2")
    for c in range(HA):
        S.dma("pool", lambda e, c=c: e.dma_start(out=Wa3[:, c, :], in_=w_ba[c * 128:(c + 1) * 128, :]), writes=[t_W])
    for c in range(HB):
        S.dma("pool", lambda e, c=c: e.dma_start(out=Wb3[:, c, :], in_=w_bb[c * 128:(c + 1) * 128, :]), writes=[t_W])
    c4 = 0
    pend4 = [None]
    for j in range(NQ):
        b = j % 2
        S.dma("sp", lambda e, b=b, j=j: e.dma_start(out=sgt[b], in_=sg_d[j * 128:(j + 1) * 128, :]),
              writes=[t_sgt[b]])
        precast((len(pc_list) - pc_pos[0] + (NQ - j) - 1) // (NQ - j))
        for n in range(NB):
            ab = (c4 % 2) * 2
            tb = c4 % 2
            c4 += 1
            for c in range(HA):
                S.op("pe", lambda e, c=c, j=j, n=n, ab=ab: e.matmul(
                    bank(ab, BW), yT3[:, c, j * 128:(j + 1) * 128], Wa3[:, c, n * BW:(n + 1) * BW],
                    start=(c == 0), stop=(c == HA - 1)),
                    reads=[t_yT[j], t_W], writes=[psk[ab]], signal=(c == HA - 1))
            for c in range(HB):
                S.op("pe", lambda e, c=c, j=j, n=n, ab=ab: e.matmul(
                    bank(ab + 1, BW), yT3[:, HA + c, j * 128:(j + 1) * 128], Wb3[:, c, n * BW:(n + 1) * BW],
                    start=(c == 0), stop=(c == HB - 1)),
                    reads=[t_yT[j], t_W], writes=[psk[ab + 1]], signal=(c == HB - 1))
            S.op("dve", lambda e, b=b, n=n, ab=ab, tb=tb: e.tensor_tensor(
                out=t1[tb], in0=bank(ab, BW), in1=sgt[b][:, n * BW:(n + 1) * BW], op=ALU.mult),
                reads=[psk[ab], t_sgt[b]], writes=[t_t1[tb]])
            S.op("dve", lambda e, b=b, n=n, ab=ab, tb=tb: e.tensor_tensor(
                out=t2[tb], in0=bank(ab + 1, BW), in1=sgt[b][:, D + n * BW:D + (n + 1) * BW], op=ALU.mult),
                reads=[psk[ab + 1], t_sgt[b]], writes=[t_t2[tb]])
            S.op("dve", lambda e, b=b, n=n, tb=tb: e.tensor_tensor(
                out=zt[b][:, n * BW:(n + 1) * BW], in0=t1[tb], in1=t2[tb], op=ALU.add),
                reads=[t_t1[tb], t_t2[tb]], writes=[t_zt[b]])
        def _trz(b=b, j=j):
            transposes_to(zt[b], t_zt[b], KC, lambda c, n, j=j: yT3[:, c:c + n, j * 128:(j + 1) * 128],
                          t_yT[j], (4, 5), True, ident_b)
        if pend4[0] is not None:
            pend4[0]()
        pend4[0] = _trz
    pend4[0]()
    S.barrier()
    psk = new_ps()
    A.release(m3b)

    t_zT = S.toks_n(NQ, "zT")
    Wo = A.bf16(KC * D)
    Wo3 = r3(Wo, D)
    wr_f = A.f32(KC * E)
    wr3 = r3(wr_f, E)
    br_b = A.f32(E)
    fnw_b = A.f32(D)
    cb = [A.f32(E) for _ in range(2)]
    xt4 = [A.f32(D)] * 2
    hh_ = [A.f32(D) for _ in range(2)]
    hnf = hh_
    hnb = [A.bf16(D)] * 2
    hnT = A.f32(KC * 128)
    hnT3 = r3(hnT, 128)
    logit = A.f32(E)
    top8 = A.f32(8)
    negm = A.f32(2)
    mask = A.f32(E)
    ex = A.f32(E)
    exm = A.f32(E)
    ssum = A.f32(2)
    dest_e = A.f32(E)
    oh = A.f32(E)
    junkE = A.f32(E)
    destf = A.f32(4)
    ss4 = [A.f32(2) for _ in range(2)]
    rstd4 = [A.f32(2) for _ in range(2)]

    t_c4 = S.tok("c4")
    t_Wo = S.tok("Wo")
    for c in range(KC):
        S.dma("pool", lambda e, c=c: e.dma_start(out=Wo3[:, c, :], in_=w_out[c * 128:(c + 1) * 128, :]), writes=[t_Wo])
    S.dma("sp", lambda e: e.dma_start(out=wr3, in_=w_router.rearrange("(kc p) c -> p kc c", p=128)), writes=[t_c4])
    S.dma("sp", lambda e: e.dma_start(out=br_b, in_=b_router.partition_broadcast(128)), writes=[t_c4])
    S.dma("sp", lambda e: e.dma_start(out=fnw_b, in_=fnw.partition_broadcast(128)), writes=[t_c4])
    S.dma("sp", lambda e: e.dma_start(out=cb[0], in_=ebase_d), writes=[t_c4])
    t_xt4 = [S.tok("xt")] * 2
    t_h = S.toks_n(2, "h")
    t_hnf = t_h
    t_hnb = [S.tok("hnb")] * 2
    t_ss4 = S.toks_n(2, "ss4")
    t_hnT = S.tok("hnT")
    t_r = S.tok("route")
    t_cb = S.toks_n(2, "cb")
    t_ga = S.tok("gate_all")
    t_di = S.tok("dest_i")
    t_hbuf = S.tok("hbuf")
    t_out = S.tok("out")
    t_GdT = S.tok("GdT")
    gate3 = r3(gate_all, 4)
    desti3 = r3(dest_i, 4)
    for j in range(NQ):
        b = j % 2
        S.dma("sp", lambda e, b=b, j=j: e.dma_start(out=xt4[b], in_=xkv[(2 * j + 1) * 128:(2 * j + 2) * 128, :]),
              writes=[t_xt4[b]])
        for n in range(NB):
            for kc in range(KC):
                S.op("pe", lambda e, kc=kc, n=n, j=j: e.matmul(
                    bank(n, BW), yT3[:, kc, j * 128:(j + 1) * 128], Wo3[:, kc, n * BW:(n + 1) * BW],
                    start=(kc == 0), stop=(kc == KC - 1)),
                    reads=[t_zT[j], t_Wo], writes=[psk[n]], signal=(kc == KC - 1))
            S.op("dve", lambda e, b=b, n=n: e.tensor_tensor(
                out=hh_[b][:, n * BW:(n + 1) * BW], in0=bank(n, BW), in1=xt4[b][:, n * BW:(n + 1) * BW], op=ALU.add),
                reads=[psk[n], t_xt4[b]], writes=[t_h[b]])
        S.dma("sp", lambda e, b=b, j=j: e.dma_start(out=out[j * 128:(j + 1) * 128, :], in_=hh_[b]),
              reads=[t_h[b]], writes=[t_out])
        rms_rstd(hh_[b], ss4[b], rstd4[b], D, [t_h[b]], t_ss4[b], hnb[b], t_hnb[b])
        S.op("dve", lambda e, b=b: e.scalar_tensor_tensor(out=hnf[b], in0=hh_[b], scalar=rstd4[b][:, 0:1],
                                                         in1=fnw_b, op0=ALU.mult, op1=ALU.mult),
             reads=[t_h[b], t_ss4[b], t_c4], writes=[t_hnf[b]])
        S.op("act", lambda e, b=b: e.activation(out=hnb[b], in_=hnf[b], func=AF.Copy),
             reads=[t_hnf[b]], writes=[t_hnb[b]])
        tb_banks = (4, 5, 6, 7)
        transposes_to(hnf[b], t_hnf[b], KC, lambda c, n: hnT3[:, c:c + n, :], t_hnT, tb_banks, False, ident_f)
        LB = 4
        for kc in range(KC):
            S.op("pe", lambda e, kc=kc: e.matmul(bank(LB, E), hnT3[:, kc, :], wr3[:, kc, :],
                                                 start=(kc == 0), stop=(kc == KC - 1)),
                 reads=[t_hnT, t_c4], writes=[psk[LB]], signal=(kc == KC - 1))
        S.op("dve", lambda e: e.tensor_tensor(out=logit, in0=bank(LB, E), in1=br_b, op=ALU.add),
             reads=[psk[LB], t_c4], writes=[t_r])
        S.op("dve", lambda e: e.max(out=top8, in_=logit), reads=[t_r], writes=[t_r])
        S.op("dve", lambda e: e.tensor_scalar(out=mask, in0=logit, scalar1=top8[:, 3:4], scalar2=None,
                                              op0=ALU.is_ge), reads=[t_r], writes=[t_r])
        S.op("dve", lambda e: e.tensor_scalar(out=negm[:, 0:1], in0=top8[:, 0:1], scalar1=-1.0, scalar2=None,
                                              op0=ALU.mult), reads=[t_r], writes=[t_r])
        S.op("act", lambda e: e.activation(out=ex, in_=logit, func=AF.Exp, bias=negm[:, 0:1], scale=1.0),
             reads=[t_r], writes=[t_r])
        S.op("dve", lambda e: e.scalar_tensor_tensor(out=exm, in0=ex, scalar=1.0, in1=mask, op0=ALU.mult, op1=ALU.mult, accum_out=ssum[:, 0:1]),
             reads=[t_r], writes=[t_r])
        S.op("dve", lambda e: e.reciprocal(out=ssum[:, 0:1], in_=ssum[:, 0:1]), reads=[t_r], writes=[t_r])
        S.op("dve", lambda e, j=j: e.tensor_scalar(out=Gd3[:, j, :], in0=exm, scalar1=ssum[:, 0:1], scalar2=None,
                                                   op0=ALU.mult),
             reads=[t_r], writes=[t_r])
        PB = 5
        S.op("pe", lambda e: e.matmul(bank(PB, E), tri_strict_f, mask, start=True, stop=True),
             reads=[t_r], writes=[psk[PB]], signal=False)
        S.op("pe", lambda e: e.matmul(bank(PB, E, E), ones_f, mask, start=True, stop=True),
             reads=[t_r], writes=[psk[PB]])
        c0_, c1_ = cb[j % 2], cb[(j + 1) % 2]
        S.op("dve", lambda e, c0_=c0_: e.tensor_tensor(out=dest_e, in0=bank(PB, E), in1=c0_, op=ALU.add),
             reads=[psk[PB], t_cb[j % 2], t_c4], writes=[t_r])
        S.op("dve", lambda e, c0_=c0_, c1_=c1_: e.tensor_tensor(out=c1_, in0=bank(PB, E, E), in1=c0_, op=ALU.add),
             reads=[psk[PB], t_cb[j % 2], t_c4], writes=[t_cb[(j + 1) % 2]])
        for k in range(4):
            S.op("dve", lambda e, k=k: e.tensor_scalar(out=oh, in0=logit, scalar1=top8[:, k:k + 1], scalar2=None,
                                                       op0=ALU.is_equal), reads=[t_r], writes=[t_r])
            S.op("dve", lambda e, k=k: e.scalar_tensor_tensor(out=junkE, in0=oh, scalar=1.0, in1=dest_e, op0=ALU.mult, op1=ALU.mult, accum_out=destf[:, k:k + 1]),
                 reads=[t_r], writes=[t_r])
            S.op("dve", lambda e, k=k, j=j: e.scalar_tensor_tensor(out=junkE, in0=oh, scalar=1.0, in1=Gd3[:, j, :], op0=ALU.mult, op1=ALU.mult, accum_out=gate3[:, j, k:k + 1]),
                 reads=[t_r], writes=[t_r, t_ga])
        S.op("dve", lambda e, j=j: e.tensor_copy(out=desti3[:, j, :], in_=destf), reads=[t_r], writes=[t_di])
        for k in range(4):
            S.dma("pool", lambda e, b=b, j=j, k=k: e.indirect_dma_start(
                out=hbuf[:, :], out_offset=bass.IndirectOffsetOnAxis(ap=desti3[:, j, k:k + 1], axis=0),
                in_=hnb[b], in_offset=None), reads=[t_hnb[b], t_di], writes=[t_hbuf])
        if debug:
            S.dma("sp", lambda e, b=b, j=j: e.dma_start(out=h_dbg[j * 128:(j + 1) * 128, :], in_=hnf[b]),
                  reads=[t_hnf[b]])
    if debug:
        S.dma("sp", lambda e: e.dma_start(out=gate_dbg, in_=gate_all), reads=[t_ga])
        S.dma("sp", lambda e: e.dma_start(out=dest_dbg, in_=dest_i), reads=[t_di])
    S.barrier()
    psk = new_ps()
    A.release(m3)

    m5 = A.mark()
    NWB = 5
    wbk = [A.bf16(KC * BW) for _ in range(NWB)]
    xe = A.bf16(CT * D)
    xe3 = r3(xe, D)
    hTe = [A.bf16(KC * C)] * 2
    hbT = [A.bf16(KCE * C)] * 2
    SW = BW // 2
    NSTG = 4

    gsb = [A.f32(HPB * C) for _ in range(2)]
    gtmp = [A.f32(C) for _ in range(2)]
    stmp = [A.f32(C) for _ in range(2)]
    utmp = [A.f32(C) for _ in range(2)]
    ystage = [A.f32(BW) for _ in range(3)]
    bgu = A.f32(E * NGU)
    bgu3 = r3(bgu, NGU)
    t_bgu = S.tok("bgu")
    S.dma("sp", lambda e: e.dma_start(out=bgu, in_=bgu_t), writes=[t_bgu])
    t_wbk = S.toks_n(NWB, "wbk")
    t_xe = S.tok("xe")
    t_hTe = [S.tok("hTe")] * 2
    t_hbT = [S.tok("hbT")] * 2
    t_gsb = S.toks_n(2, "gsb")
    t_g = S.toks_n(2, "g")
    t_s = S.toks_n(2, "s")
    t_u = S.toks_n(2, "u")
    t_ys = S.toks_n(3, "ys")
    t_ybuf = S.tok("ybuf")
    c5 = {"w": 0, "gu": 0, "dn": 0, "t": 0, "ys": 0, "tr": 0}
    GU_BANKS = (0, 1, 2, 3)
    DN_BANKS = (4, 5)
    wsrc = []
    for ex2 in range(E):
        for bidx2 in range(DE // BW):
            for part2 in range(2):
                c0_ = part2 * DE + bidx2 * BW
                wsrc.append((w_gu, wgu_bf, ex2, c0_))
        for n2 in range(NB):
            wsrc.append((w_dn, wdn_bf, ex2, n2 * BW))
    wst = {"issued": 0, "half": 0}

    def ensure_weights(upto):
        while wst["issued"] <= min(upto, len(wsrc) - 1):
            i_ = wst["issued"]
            wst["issued"] += 1
            wsrc_f, wsrc_b, ex2, c0_ = wsrc[i_]
            wi2 = i_ % NWB
            if ex2 >= E - NP:
                S.dma("sp", lambda e, wi2=wi2, ex2=ex2, c0_=c0_, wsrc_b=wsrc_b: e.dma_start(
                    out=r3(wbk[wi2], BW),
                    in_=wsrc_b[ex2 - (E - NP), :, c0_:c0_ + BW].rearrange("(kc p) c -> p kc c", p=128)),
                    writes=[t_wbk[wi2]])
                continue
            S.dma("pool", lambda e, wi2=wi2, ex2=ex2, c0_=c0_, wsrc_f=wsrc_f: e.dma_start(
                out=r3(wbk[wi2], BW),
                in_=wsrc_f[ex2, :, c0_:c0_ + BW].rearrange("(kc p) c -> p kc c", p=128)),
                writes=[t_wbk[wi2]])

    widx = [0]
    for ex_ in range(E):
        eb = ex_ % 2
        nfull = C // 128
        if nfull:
            S.dma("sp", lambda e, ex_=ex_: e.dma_start(
                out=xe3[:, 0:nfull, :],
                in_=hbuf[ex_ * C:ex_ * C + nfull * 128, :].rearrange("(s p) d -> p s d", p=128)), writes=[t_xe])
        if C % 128:
            S.dma("sp", lambda e, ex_=ex_: e.dma_start(
                out=xe3[0:C % 128, nfull, :], in_=hbuf[ex_ * C + nfull * 128:(ex_ + 1) * C, :]), writes=[t_xe])
        he3 = r3(hTe[eb], C)
        for s_ in range(CT):
            np_ = min(128, C - s_ * 128)
            transposes_to(xe3[:, s_, :], t_xe, KC,
                          lambda c, n, s_=s_, he3=he3, np_=np_: he3[:, c:c + n, s_ * 128:s_ * 128 + np_],
                          t_hTe[eb], (6, 7), True, ident_b, npart=np_)
        hb3 = r3(hbT[eb], C)
        nblk = DE // BW
        for bidx in range(nblk):
            gsi = c5["t"] % 2
            c5["t"] += 1
            gs3 = r3(gsb[gsi], C)
            for part in range(2):
                wi = widx[0] % NWB
                ensure_weights(widx[0] + NWB - 1)
                widx[0] += 1
                for m in range(HPB):
                    gb_ = GU_BANKS[c5["gu"] % 4]
                    c5["gu"] += 1
                    for kc in range(KC):
                        S.op("pe", lambda e, kc=kc, wi=wi, m=m, gb_=gb_, he3=he3: e.matmul(
                            bank(gb_, C), r3(wbk[wi], BW)[:, kc, m * 128:(m + 1) * 128], he3[:, kc, :],
                            start=(kc == 0), stop=(kc == KC - 1)),
                            reads=[t_wbk[wi], t_hTe[eb]], writes=[psk[gb_]], signal=(kc == KC - 1))
                    chunk = bidx * HPB + m
                    ti = (c5["gu"]) % 2
                    if part == 0:
                        bcol = bgu3[:, ex_, chunk:chunk + 1]
                        S.op("dve", lambda e, gb_=gb_, ti=ti, bcol=bcol: e.tensor_scalar(
                            out=gtmp[ti], in0=bank(gb_, C), scalar1=bcol, scalar2=SWIGLU_LIMIT, op0=ALU.add,
                            op1=ALU.min), reads=[psk[gb_], t_bgu], writes=[t_g[ti]])
                        S.op("act", lambda e, ti=ti: e.activation(out=stmp[ti], in_=gtmp[ti], func=AF.Sigmoid,
                                                                  scale=SWIGLU_ALPHA),
                             reads=[t_g[ti]], writes=[t_s[ti]])
                        S.op("dve", lambda e, ti=ti, m=m, gs3=gs3: e.tensor_tensor(
                            out=gs3[:, m, :], in0=gtmp[ti], in1=stmp[ti], op=ALU.mult),
                            reads=[t_g[ti], t_s[ti]], writes=[t_gsb[gsi]])
                    else:
                        bcol = bgu3[:, ex_, KCE + chunk:KCE + chunk + 1]
                        S.op("dve", lambda e, gb_=gb_, ti=ti, bcol=bcol: e.tensor_scalar(
                            out=utmp[ti], in0=bank(gb_, C), scalar1=bcol, scalar2=SWIGLU_LIMIT, op0=ALU.add,
                            op1=ALU.min), reads=[psk[gb_], t_bgu], writes=[t_u[ti]])
                        S.op("dve", lambda e, ti=ti: e.tensor_scalar(
                            out=utmp[ti], in0=utmp[ti], scalar1=-SWIGLU_LIMIT, scalar2=1.0, op0=ALU.max,
                            op1=ALU.add), reads=[t_u[ti]], writes=[t_u[ti]])
                        S.op("dve", lambda e, ti=ti, m=m, gs3=gs3, chunk=chunk, hb3=hb3: e.tensor_tensor(
                            out=hb3[:, chunk, :], in0=utmp[ti], in1=gs3[:, m, :], op=ALU.mult),
                            reads=[t_u[ti], t_gsb[gsi]], writes=[t_hbT[eb]])
        for n in range(NB):
            wi = widx[0] % NWB
            ensure_weights(widx[0] + NWB - 1)
            widx[0] += 1
            for s_ in range(CT):
                db = DN_BANKS[c5["dn"] % 2]
                c5["dn"] += 1
                np_ = min(128, C - s_ * 128)
                for kc in range(KCE):
                    S.op("pe", lambda e, kc=kc, wi=wi, s_=s_, db=db, hb3=hb3, np_=np_: e.matmul(
                        bank(db, BW)[0:np_, :], hb3[:, kc, s_ * 128:s_ * 128 + np_], r3(wbk[wi], BW)[:, kc, :],
                        start=(kc == 0), stop=(kc == KCE - 1)),
                        reads=[t_wbk[wi], t_hbT[eb]], writes=[psk[db]], signal=(kc == KCE - 1))
                yi = c5["ys"] % 3
                c5["ys"] += 1
                S.op("act", lambda e, yi=yi, db=db, np_=np_: e.activation(
                    out=ystage[yi][0:np_, :], in_=bank(db, BW)[0:np_, :], func=AF.Copy),
                     reads=[psk[db]], writes=[t_ys[yi]])
                S.dma("act", lambda e, yi=yi, ex_=ex_, s_=s_, n=n, np_=np_: e.dma_start(
                    out=ybuf[ex_ * C + s_ * 128:ex_ * C + s_ * 128 + np_, n * BW:(n + 1) * BW],
                    in_=ystage[yi][0:np_, :]),
                    reads=[t_ys[yi]], writes=[t_ybuf])
    S.barrier()
    psk = new_ps()
    A.release(m5)

    hp = [A.f32(D) for _ in range(2)]
    yk = [[A.f32(D) for _ in range(4)] for _ in range(2)]
    bdn = A.f32(D)
    GdT = A.f32(128)
    t_bdn = S.tok("bdn")
    t_GdT = S.tok("GdT")
    S.dma("sp", lambda e: e.dma_start(out=bdn[0:E, :], in_=b_dn), writes=[t_bdn])
    t_hp = S.toks_n(2, "hp")
    t_yk = [S.toks_n(4, "yk%d" % i) for i in range(2)]
    t_out = S.tok("out")
    for j in range(NQ):
        b = j % 2
        S.dma("sp", lambda e, b=b, j=j: e.dma_start(out=hp[b], in_=out[j * 128:(j + 1) * 128, :]), writes=[t_hp[b]])
        GB = 6
        S.op("pe", lambda e, j=j: e.transpose(bank(GB, 128)[0:E, :], Gd3[:, j, :], ident_f), writes=[psk[GB]])
        S.op("act", lambda e: e.activation(out=GdT[0:E, :], in_=bank(GB, 128)[0:E, :], func=AF.Copy),
             reads=[psk[GB]], writes=[t_GdT])
        for n in range(NB):
            S.op("pe", lambda e, n=n: e.matmul(bank(n, BW), GdT[0:E, :], bdn[0:E, n * BW:(n + 1) * BW],
                                               start=True, stop=True),
                 reads=[t_GdT, t_bdn], writes=[psk[n]])
            S.op("dve", lambda e, n=n, b=b: e.tensor_tensor(
                out=hp[b][:, n * BW:(n + 1) * BW], in0=bank(n, BW), in1=hp[b][:, n * BW:(n + 1) * BW], op=ALU.add),
                reads=[psk[n], t_hp[b]], writes=[t_hp[b]])
        for k in range(4):
            S.dma("pool", lambda e, b=b, j=j, k=k: e.indirect_dma_start(
                out=yk[b][k], out_offset=None, in_=ybuf[:, :],
                in_offset=bass.IndirectOffsetOnAxis(ap=desti3[:, j, k:k + 1], axis=0)), writes=[t_yk[b][k]])
            S.op("dve", lambda e, b=b, j=j, k=k: e.scalar_tensor_tensor(
                out=hp[b], in0=yk[b][k], scalar=gate3[:, j, k:k + 1], in1=hp[b], op0=ALU.mult, op1=ALU.add),
                reads=[t_yk[b][k], t_hp[b]], writes=[t_hp[b]])
        S.dma("sp", lambda e, b=b, j=j: e.dma_start(out=out[j * 128:(j + 1) * 128, :], in_=hp[b]),
              reads=[t_hp[b]], writes=[t_out])
    S.barrier()

    blk = st.enter_context(nc.Block())

    @blk.tensor
    def _(e):
        for f in S.ops["pe"]:
            f(e)

    @blk.scalar
    def _(e):
        for f in S.ops["act"]:
            f(e)

    @blk.vector
    def _(e):
        for f in S.ops["dve"]:
            f(e)

    @blk.gpsimd
    def _(e):
        for f in S.ops["pool"]:
            f(e)

    @blk.sync
    def _(e):
        for f in S.ops["sp"]:
            f(e)

    st.close()
    return nc


def make_consts(cfg):
    t = np.arange(128)
    ident = np.eye(128, dtype=np.float32)
    tri_incl = (t[:, None] <= t[None, :]).astype(np.float32)
    tri_strict = (t[:, None] < t[None, :]).astype(np.float32)
    ones = np.ones((128, 128), np.float32)
    cst = np.concatenate([ident, tri_incl, tri_strict, ones], axis=1)
    ebase = np.tile((np.arange(cfg.E) * cfg.C).astype(np.float32)[None, :], (128, 1))
    return cst, ebase


def make_btab(rel_bias, cfg):
    HA = cfg.HA
    kk = np.arange(128)[:, None, None]
    r = np.arange(5)[None, :, None]
    qi = np.arange(128)[None, None, :]
    dist = (4 - r) * 128 + qi - kk
    qo = qi % 64
    ok = (dist >= -(63 - qo)) & (dist <= qo + 512)
    idx = np.clip(dist, -63, 256) + 63
    tab = rel_bias[:, idx]
    tab = np.where(ok[None], tab, np.float32(NEG)).astype(np.float32)
    return np.ascontiguousarray(tab.transpose(1, 0, 2, 3).reshape(128, HA, 640))


def prepare(cfg, inp):
    D, NS, E = cfg.D, cfg.NS, cfg.E
    x = np.asarray(inp["x"], np.float32)
    B = x.shape[0]
    cst, ebase = make_consts(cfg)
    btab = make_btab(np.asarray(inp["rel_bias"], np.float32), cfg)
    qkn = np.concatenate([np.tile(np.asarray(inp[k], np.float32), cfg.HPB)
                          for k in ("qn_a", "kn_a", "qn_b", "kn_b")])
    NGU = 2 * D // 128
    bgu_t = np.ascontiguousarray(
        np.asarray(inp["b_gate_up"], np.float32).reshape(E, NGU, 128).transpose(2, 0, 1).reshape(128, E * NGU))
    shared = {
        "w_in": np.asarray(inp["w_in"], np.float32),
        "attn_norm_w": np.asarray(inp["attn_norm_w"], np.float32),
        "ffn_norm_w": np.asarray(inp["ffn_norm_w"], np.float32),
        "qkn": qkn,
        "b_forget": np.asarray(inp["b_forget"], np.float32),
        "btab": btab,
        "w_branch_a": np.asarray(inp["w_branch_a"], np.float32),
        "w_branch_b": np.asarray(inp["w_branch_b"], np.float32),
        "w_out": np.asarray(inp["w_out"], np.float32),
        "w_router": np.asarray(inp["w_router"], np.float32),
        "b_router": np.asarray(inp["b_router"], np.float32),
        "w_gate_up": np.asarray(inp["w_gate_up"], np.float32),
        "bgu_t": bgu_t,
        "w_down": np.asarray(inp["w_down"], np.float32),
        "b_down": np.asarray(inp["b_down"], np.float32),
        "cst": cst,
        "ebase": ebase,
    }
    in_maps = []
    for c in range(2 * B):
        b, p = c // 2, c % 2
        if p == 0:
            xk = np.concatenate([np.zeros((128, D), np.float32), x[b, :(NS - 1) * 128]], axis=0)
            valid = np.ones((128, NS), np.float32)
            valid[:, 0] = 0.0
        else:
            xk = x[b, :NS * 128]
            valid = np.ones((128, NS), np.float32)
        m = dict(shared)
        m["xkv"] = np.ascontiguousarray(xk)
        m["valid"] = valid
        in_maps.append(m)
    return in_maps


def assemble(cfg, results, B):
    D, NS, NQ = cfg.D, cfg.NS, cfg.NQ
    y = np.zeros((B, NS * 128, D), np.float32)
    for c in range(2 * B):
        b, p = c // 2, c % 2
        o = np.asarray(results[c]["out"]).reshape(NQ, 128, D)
        yv = y[b].reshape(NS, 128, D)
        for j in range(NQ):
            yv[2 * j + p] = o[j]
    return y


_CACHE = {}


def kernel(**inputs):
    cfg = Cfg()
    if "nc" not in _CACHE:
        _CACHE["nc"] = build(cfg)
    nc = _CACHE["nc"]
    in_maps = prepare(cfg, inputs)
    res = run_bass_kernel_spmd(nc, in_maps, core_ids=list(range(cfg.n_cores)))
    return assemble(cfg, res.results, 4)
```
